# Optimizing a Trainium2 kernel written in Bass

```python
import math
import jax, jax.numpy as jnp
from jax import lax
import numpy as np

D_MODEL = 1024
BATCH = 32
SEQ = 2048
DEPTH = 1

GRID_W = 64
CTX_LEN = 256
MIX_W = D_MODEL
S5_W = MIX_W // 2
S5_H = 16
S5_G = S5_W // S5_H
S5_P = 64
HY_W = MIX_W - S5_W
HY_ORDER = 2
HY_SHORT = 3
HF_BANDS = 16
HF_EMB = 2 * HF_BANDS + 1
HF_HID = 64
HF_INNER = 2
HF_DECAY_SLOW = -math.log(1e-2) / 1.5
HF_DECAY_FAST = -math.log(1e-2) / 0.3
PROJ_W = S5_W + (HY_ORDER + 1) * HY_W
PEER_HEADS = 8
PEER_NKEYS = 128
PEER_EXPERTS = PEER_NKEYS * PEER_NKEYS
PEER_DK = 256
PEER_DK_HALF = PEER_DK // 2
PEER_TOPK = 16
PEER_CHUNK = 128
N_MOD = 6
EPS = 1e-6

kernel_name = "hybrid_s5_hyena_peer_dit_block"


def rmsnorm(x, g):
    xf = x.astype(jnp.float32)
    y = xf * lax.rsqrt(jnp.mean(xf * xf, axis=-1, keepdims=True) + EPS) * g.astype(jnp.float32)
    return y.astype(x.dtype)


def modulate(h, shift, scale):
    return h * (1 + scale) + shift


def short_conv(x, w, b):
    pad = HY_SHORT // 2
    W = x.shape[1]
    xp = jnp.pad(x, ((0, 0), (pad, pad), (0, 0)))
    y = b
    for j in range(HY_SHORT):
        y = y + w[j] * xp[:, j:j + W]
    return y


def s5_discretise(a_re, a_im, log_step, b_re, b_im):
    lam = lax.complex(a_re.astype(jnp.float32), a_im.astype(jnp.float32))
    step = jnp.exp(log_step.astype(jnp.float32))[:, None]
    lam_bar = jnp.exp(lam * step)
    b = lax.complex(b_re.astype(jnp.float32), b_im.astype(jnp.float32))
    b_bar = ((lam_bar - 1.0) / lam)[..., None] * b
    return lam_bar, b_bar


def _linear_combine(e1, e2):
    a1, b1 = e1
    a2, b2 = e2
    return a1 * a2, a2 * b1 + b2


def s5_states(u, lam_bar, b_bar, h0, reverse):
    bu = jnp.einsum('nlgh,gph->nlgp', u.astype(jnp.float32).astype(jnp.complex64), b_bar)
    if reverse:
        bu = jnp.flip(bu, axis=1)
    if h0 is not None:
        bu = bu.at[:, 0].add(lam_bar * h0)
    a = jnp.broadcast_to(lam_bar, (1, bu.shape[1]) + lam_bar.shape)
    _, st = lax.associative_scan(_linear_combine, (a, bu), axis=1)
    if reverse:
        st = jnp.flip(st, axis=1)
    return st


def s5_readout(st, c_re, c_im):
    cc = lax.complex(c_re.astype(jnp.float32), c_im.astype(jnp.float32))
    return jnp.real(jnp.einsum('nlgp,ghp->nlgh', st, cc))


def s5_glu(y, w, b):
    gy = jax.nn.gelu(y)
    return gy * jax.nn.sigmoid(gy @ w.astype(jnp.float32) + b.astype(jnp.float32))


def hyena_filters(L, w1, b1, wh, bh, freq, wout, decay):
    f32 = jnp.float32
    pos = jnp.arange(L, dtype=f32)
    t = pos / max(L - 1, 1)
    bands = jnp.linspace(1e-4, HF_BANDS - 1, HF_BANDS, dtype=f32)
    ang = (2.0 * math.pi / L) * pos[:, None] * bands[None, :]
    feats = jnp.concatenate([t[:, None], jnp.cos(ang), -jnp.sin(ang)], axis=-1)
    fr = freq.astype(f32)
    hid = jnp.sin(fr * (feats @ w1.astype(f32) + b1.astype(f32)))
    for i in range(HF_INNER):
        hid = jnp.sin(fr * (hid @ wh[i].astype(f32) + bh[i].astype(f32)))
    h = (hid @ wout.astype(f32)) * jnp.exp(-t[:, None] * jnp.abs(decay.astype(f32)))
    h = h.reshape(L, HY_ORDER, 2, HY_W)
    return h * lax.rsqrt(jnp.sum(h * h, axis=(0, 2), keepdims=True) + EPS)


def bidir_long_conv(z, h_fwd, h_bwd):
    L, C = h_fwd.shape
    k = jnp.concatenate([h_fwd, jnp.zeros((1, C), jnp.float32), h_bwd[:0:-1]], axis=0)
    zf = jnp.fft.rfft(z.astype(jnp.float32), n=2 * L, axis=1)
    kf = jnp.fft.rfft(k, axis=0)
    return jnp.fft.irfft(zf * kf[None], n=2 * L, axis=1)[:, :L]


def hyena_mixer(p, conv_w, conv_b, filt, d_hy, n_rows):
    N, L, C = p.shape
    q = short_conv(p.reshape(N * n_rows, L // n_rows, C), conv_w, conv_b)
    q = q.reshape(N, L, C).astype(jnp.float32)
    v, x1, x2 = jnp.split(q, HY_ORDER + 1, axis=-1)
    z = v
    for o, gate in enumerate((x1, x2)):
        z = gate * (bidir_long_conv(z, filt[:, o, 0], filt[:, o, 1]) + d_hy[o].astype(jnp.float32) * z)
    return z


def merge_head_groups(y_s5, z_hy, w_glu, b_glu, g_s5, g_hy, w_out, dtype):
    s5_out = s5_glu(y_s5, w_glu, b_glu)
    cat = jnp.concatenate([rmsnorm(s5_out, g_s5), rmsnorm(z_hy, g_hy)], axis=-1)
    return cat.astype(dtype) @ w_out


def peer(h, wq, k1, k2, u_tab, v_tab):
    T, D = h.shape
    f32 = jnp.float32
    k1f = k1.astype(f32)
    k2f = k2.astype(f32)

    def block(hb):
        q = (hb @ wq).astype(f32).reshape(hb.shape[0], PEER_HEADS, 2, PEER_DK_HALF)
        s1 = jnp.einsum('chd,hnd->chn', q[:, :, 0], k1f)
        s2 = jnp.einsum('chd,hnd->chn', q[:, :, 1], k2f)
        v1, i1 = lax.top_k(s1, PEER_TOPK)
        v2, i2 = lax.top_k(s2, PEER_TOPK)
        cand = (v1[..., :, None] + v2[..., None, :]).reshape(v1.shape[:-1] + (PEER_TOPK * PEER_TOPK,))
        sc, ci = lax.top_k(cand, PEER_TOPK)
        e = (jnp.take_along_axis(i1, ci // PEER_TOPK, axis=-1) * PEER_NKEYS
             + jnp.take_along_axis(i2, ci % PEER_TOPK, axis=-1))
        g = jax.nn.softmax(sc, axis=-1)
        act = jax.nn.gelu(jnp.einsum('chkd,cd->chk', jnp.take(u_tab, e, axis=0), hb).astype(f32))
        return jnp.einsum('chk,chkd->cd', (g * act).astype(hb.dtype), jnp.take(v_tab, e, axis=0))

    out = lax.map(block, h.reshape(T // PEER_CHUNK, PEER_CHUNK, D))
    return out.reshape(T, D)


def setup_inputs(seed: int = 0) -> dict:
    key = jax.random.key(seed)
    ks = iter(jax.random.split(key, 64))
    f32 = jnp.float32

    def nrm(shape, s):
        return s * jax.random.normal(next(ks), shape, f32)

    n_idx = jnp.arange(S5_P, dtype=f32)
    return {
        "x": nrm((BATCH, SEQ, D_MODEL), 1.0),
        "c": nrm((BATCH, D_MODEL), 1.0),
        "ctx": nrm((BATCH, CTX_LEN, D_MODEL), 1.0),
        "c_ctx": nrm((D_MODEL,), 1.0),
        "w_ada": nrm((DEPTH, D_MODEL, N_MOD * D_MODEL), 0.5 * D_MODEL ** -0.5),
        "b_ada": nrm((DEPTH, N_MOD * D_MODEL), 0.02),
        "g_norm1": 1.0 + nrm((DEPTH, D_MODEL), 0.02),
        "g_norm2": 1.0 + nrm((DEPTH, D_MODEL), 0.02),
        "w_in": nrm((DEPTH, D_MODEL, PROJ_W), D_MODEL ** -0.5),
        "b_in": nrm((DEPTH, PROJ_W), 0.02),
        "s5_a_re": -0.5 + nrm((DEPTH, 2, S5_G, S5_P), 0.01),
        "s5_a_im": math.pi * n_idx + nrm((DEPTH, 2, S5_G, S5_P), 0.01),
        "s5_log_step": jax.random.uniform(next(ks), (DEPTH, 2, S5_G), f32, math.log(1e-3), math.log(1e-1)),
        "s5_b_re": nrm((DEPTH, 2, S5_G, S5_P, S5_H), (2 * S5_H) ** -0.5),
        "s5_b_im": nrm((DEPTH, 2, S5_G, S5_P, S5_H), (2 * S5_H) ** -0.5),
        "s5_c_re": nrm((DEPTH, 2, S5_G, S5_H, S5_P), 0.5),
        "s5_c_im": nrm((DEPTH, 2, S5_G, S5_H, S5_P), 0.5),
        "s5_d": nrm((DEPTH, S5_W), 1.0),
        "w_glu": nrm((DEPTH, S5_W, S5_W), S5_W ** -0.5),
        "b_glu": nrm((DEPTH, S5_W), 0.02),
        "hy_conv_w": nrm((DEPTH, HY_SHORT, (HY_ORDER + 1) * HY_W), HY_SHORT ** -0.5),
        "hy_conv_b": nrm((DEPTH, (HY_ORDER + 1) * HY_W), 0.02),
        "hf_w1": nrm((DEPTH, HF_EMB, HF_HID), HF_EMB ** -0.5),
        "hf_b1": nrm((DEPTH, HF_HID), 0.02),
        "hf_wh": nrm((DEPTH, HF_INNER, HF_HID, HF_HID), HF_HID ** -0.5),
        "hf_bh": nrm((DEPTH, HF_INNER, HF_HID), 0.02),
        "hf_freq": 1.0 + nrm((DEPTH, HF_HID), 0.01),
        "hf_wout": nrm((DEPTH, HF_HID, HY_ORDER * 2 * HY_W), HF_HID ** -0.5),
        "hf_decay": jnp.tile(jnp.linspace(HF_DECAY_SLOW, HF_DECAY_FAST, HY_W, dtype=f32), HY_ORDER * 2)[None]
                    + nrm((DEPTH, HY_ORDER * 2 * HY_W), 0.1),
        "hy_d": nrm((DEPTH, HY_ORDER, HY_W), 0.1),
        "g_out_s5": 1.0 + nrm((DEPTH, S5_W), 0.02),
        "g_out_hy": 1.0 + nrm((DEPTH, HY_W), 0.02),
        "w_out": nrm((DEPTH, MIX_W, D_MODEL), MIX_W ** -0.5),
        "peer_wq": nrm((DEPTH, D_MODEL, PEER_HEADS * PEER_DK), D_MODEL ** -0.5),
        "peer_k1": nrm((DEPTH, PEER_HEADS, PEER_NKEYS, PEER_DK_HALF), PEER_DK_HALF ** -0.5),
        "peer_k2": nrm((DEPTH, PEER_HEADS, PEER_NKEYS, PEER_DK_HALF), PEER_DK_HALF ** -0.5),
        "peer_u": nrm((DEPTH, PEER_EXPERTS, D_MODEL), D_MODEL ** -0.5),
        "peer_v": nrm((DEPTH, PEER_EXPERTS, D_MODEL), 0.5),
        "g_final": 1.0 + nrm((D_MODEL,), 0.02),
    }


def reference(x, c, ctx, c_ctx, w_ada, b_ada, g_norm1, g_norm2, w_in, b_in,
              s5_a_re, s5_a_im, s5_log_step, s5_b_re, s5_b_im, s5_c_re, s5_c_im, s5_d, w_glu, b_glu,
              hy_conv_w, hy_conv_b, hf_w1, hf_b1, hf_wh, hf_bh, hf_freq, hf_wout, hf_decay, hy_d,
              g_out_s5, g_out_hy, w_out, peer_wq, peer_k1, peer_k2, peer_u, peer_v, g_final):
    B, L, D = x.shape
    Lc = ctx.shape[1]
    rows = L // GRID_W
    dt = x.dtype
    for l in range(DEPTH):
        last = l == DEPTH - 1
        mod_x = (jax.nn.silu(c) @ w_ada[l] + b_ada[l])[:, None, :]
        mod_c = (jax.nn.silu(c_ctx) @ w_ada[l] + b_ada[l])[None, None, :]
        shx1, scx1, gx1, shx2, scx2, gx2 = jnp.split(mod_x, N_MOD, axis=-1)
        shc1, scc1, gc1, shc2, scc2, gc2 = jnp.split(mod_c, N_MOD, axis=-1)

        px = modulate(rmsnorm(x, g_norm1[l]), shx1, scx1) @ w_in[l] + b_in[l]
        pc = modulate(rmsnorm(ctx, g_norm1[l]), shc1, scc1) @ w_in[l] + b_in[l]

        d5 = s5_d[l].astype(jnp.float32).reshape(S5_G, S5_H)
        ux = px[..., :S5_W].astype(jnp.float32).reshape(B, L, S5_G, S5_H)
        uc = pc[..., :S5_W].astype(jnp.float32).reshape(B, Lc, S5_G, S5_H)
        y_x = d5 * ux
        y_c = None if last else d5 * uc
        for dr in range(2):
            lam_bar, b_bar = s5_discretise(s5_a_re[l, dr], s5_a_im[l, dr], s5_log_step[l, dr],
                                           s5_b_re[l, dr], s5_b_im[l, dr])
            st_c = s5_states(uc, lam_bar, b_bar, None, dr == 1)
            h0 = st_c[:, -1] if dr == 0 else st_c[:, 0]
            st_x = s5_states(ux, lam_bar, b_bar, h0, dr == 1)
            y_x = y_x + s5_readout(st_x, s5_c_re[l, dr], s5_c_im[l, dr])
            if y_c is not None:
                y_c = y_c + s5_readout(st_c, s5_c_re[l, dr], s5_c_im[l, dr])

        filt_x = hyena_filters(L, hf_w1[l], hf_b1[l], hf_wh[l], hf_bh[l], hf_freq[l], hf_wout[l], hf_decay[l])
        z_x = hyena_mixer(px[..., S5_W:], hy_conv_w[l], hy_conv_b[l], filt_x, hy_d[l], rows)
        mix_x = merge_head_groups(y_x.reshape(B, L, S5_W), z_x, w_glu[l], b_glu[l],
                                  g_out_s5[l], g_out_hy[l], w_out[l], dt)

        if not last:
            filt_c = hyena_filters(Lc, hf_w1[l], hf_b1[l], hf_wh[l], hf_bh[l], hf_freq[l], hf_wout[l], hf_decay[l])
            z_c = hyena_mixer(pc[..., S5_W:], hy_conv_w[l], hy_conv_b[l], filt_c, hy_d[l], 1)
            mix_c = merge_head_groups(y_c.reshape(B, Lc, S5_W), z_c, w_glu[l], b_glu[l],
                                      g_out_s5[l], g_out_hy[l], w_out[l], dt)
            ctx = ctx + gc1 * mix_c
            hc2 = modulate(rmsnorm(ctx, g_norm2[l]), shc2, scc2)
            ctx = ctx + gc2 * peer(hc2.reshape(B * Lc, D), peer_wq[l], peer_k1[l], peer_k2[l],
                                   peer_u[l], peer_v[l]).reshape(B, Lc, D)

        x = x + gx1 * mix_x
        hx2 = modulate(rmsnorm(x, g_norm2[l]), shx2, scx2)
        x = x + gx2 * peer(hx2.reshape(B * L, D), peer_wq[l], peer_k1[l], peer_k2[l],
                           peer_u[l], peer_v[l]).reshape(B, L, D)
    return rmsnorm(x, g_final)
```

```python
import contextlib
import math
import types
import numpy as np
import ml_dtypes
import concourse.bass as bass
import concourse.mybir as mybir
from concourse.bass_utils import run_bass_kernel_spmd

F32 = mybir.dt.float32
BF16 = mybir.dt.bfloat16
I32 = mybir.dt.int32
ALU = mybir.AluOpType
AF = mybir.ActivationFunctionType
AX = mybir.AxisListType

NSLOT = 12
NCORES = 8
NS = 4
L = 2048
D = 1024
LC = 256
EPS = 1e-6
NFFT = 4096


def _freeze(fn):
    if fn.__closure__ is None:
        return fn
    cells = []
    for c in fn.__closure__:
        try:
            cells.append(types.CellType(c.cell_contents))
        except ValueError:
            cells.append(c)
    return types.FunctionType(fn.__code__, fn.__globals__, fn.__name__, fn.__defaults__, tuple(cells))


class Prog:
    DMAQ = ("sp", "act", "pool")

    def __init__(self, nc):
        self.nc = nc
        self.streams = {k: [] for k in ("pe", "act", "dve", "pool", "sp")}
        self.cnt = {}
        self.waited = {k: {} for k in self.streams}
        self.lastw = {}
        self.reads = {}
        self.slot_next = {q: 0 for q in self.DMAQ}
        self.ninst = 0
        self.out_deps = []

    def _key(self, x):
        if isinstance(x, (str, tuple)):
            return x
        t = getattr(x, "tensor", x)
        return getattr(t, "name", None) or id(t)

    def _deps(self, reads, writes):
        deps = {}

        def add(s, c):
            if c > deps.get(s, 0):
                deps[s] = c
        for k in reads:
            lw = self.lastw.get(k)
            if lw:
                add(*lw)
        for k in writes:
            lw = self.lastw.get(k)
            if lw:
                add(*lw)
            for s, c in self.reads.get(k, {}).items():
                add(s, c)
        return deps

    def _emit_waits(self, stream, deps):
        w = self.waited[stream]
        for s, c in deps.items():
            if s == stream and stream == "pe":
                continue
            if c > w.get(s, 0):
                w[s] = c
                self.streams[stream].append(("wait", s, c))

    def _commit(self, sem, cnt, reads, writes):
        for k in writes:
            self.lastw[k] = (sem, cnt)
            self.reads[k] = {}
        for k in reads:
            self.reads.setdefault(k, {})
            if self.reads[k].get(sem, 0) < cnt:
                self.reads[k][sem] = cnt

    def op(self, stream, fn, reads=(), writes=()):
        reads = [self._key(r) for r in reads]
        writes = [self._key(r) for r in writes]
        deps = self._deps(reads, writes)
        self._emit_waits(stream, deps)
        c = self.cnt.get(stream, 0) + 1
        self.cnt[stream] = c
        self.streams[stream].append(("op", _freeze(fn), stream, 1))
        self._commit(stream, c, reads, writes)
        self.ninst += 1

    def dma(self, out, in_, reads=None, writes=None, q="sp", is_output=False, **kw):
        reads = [self._key(r) for r in (reads if reads is not None else [in_])]
        writes = [self._key(r) for r in (writes if writes is not None else [out])]
        slot = self.slot_next[q]
        self.slot_next[q] = (slot + 1) % NSLOT
        sem = ("dma", q, slot)
        deps = self._deps(reads, writes)
        prev = self.cnt.get(sem, 0)
        if prev:
            deps[sem] = max(deps.get(sem, 0), prev)
        self._emit_waits(q, deps)
        c = prev + 16
        self.cnt[sem] = c
        self.streams[q].append(("dma", out, in_, kw, sem))
        self._commit(sem, c, reads, writes)
        if is_output:
            self.out_deps.append((sem, c))
        self.ninst += 1

    def barrier(self):
        allc = dict(self.cnt)
        for st in self.streams:
            self._emit_waits(st, allc)

    def emit(self):
        nc = self.nc
        fin = {}
        for s, c in self.out_deps:
            fin[s] = max(fin.get(s, 0), c)
        self._emit_waits("sp", fin)
        semkeys = list(self.cnt.keys())
        with contextlib.ExitStack() as es:
            sems = {}
            for i, k in enumerate(semkeys):
                sems[k] = es.enter_context(nc.semaphore("s%d" % i))
            block = es.enter_context(nc.Block())
            engs = {"pe": block.tensor, "act": block.scalar, "dve": block.vector,
                    "pool": block.gpsimd, "sp": block.sync}

            def make(stream):
                items = self.streams[stream]

                def body(eng):
                    for it in items:
                        if it[0] == "wait":
                            eng.wait_ge(sems[it[1]], it[2])
                        elif it[0] == "op":
                            it[1](eng).then_inc(sems[it[2]], 1)
                        else:
                            _, out, in_, kw, sem = it
                            eng.dma_start(out=out, in_=in_, **kw).then_inc(sems[sem], 16)
                return body
            for st in ("sp", "act", "pool", "dve", "pe"):
                if self.streams[st]:
                    engs[st](make(st))


def _constants():
    c = {}
    c["ident"] = np.eye(128, dtype=np.float32).astype(ml_dtypes.bfloat16)
    c["identf"] = np.eye(128, dtype=np.float32)
    sm = np.zeros((128, 128), np.float32)
    sp = np.zeros((128, 128), np.float32)
    for t in range(128):
        if t % 64 != 0:
            sm[t - 1, t] = 1
        if t % 64 != 63:
            sp[t + 1, t] = 1
    c["shm"] = sm.astype(ml_dtypes.bfloat16)
    c["shp"] = sp.astype(ml_dtypes.bfloat16)
    t = np.arange(L, dtype=np.float64) + 0.5
    ang = 2 * np.pi * np.outer(t, t) / NFFT
    c["ctab"] = np.cos(ang).astype(np.float32).astype(ml_dtypes.bfloat16)
    c["stab"] = np.sin(ang).astype(np.float32).astype(ml_dtypes.bfloat16)
    phi = np.pi * (np.arange(L, dtype=np.float64) + 0.5) / NFFT
    c["cphi"] = np.cos(phi).astype(np.float32).reshape(16, 128).T.copy()
    c["sphi"] = np.sin(phi).astype(np.float32).reshape(16, 128).T.copy()
    pos = np.arange(L, dtype=np.float32)
    tt = pos / np.float32(L - 1)
    bands = np.linspace(1e-4, 15, 16, dtype=np.float32)
    a = (np.float32(2.0 * math.pi / L) * pos[:, None]) * bands[None, :]
    feats = np.concatenate([tt[:, None], np.cos(a), -np.sin(a)], axis=-1).astype(np.float32)
    c["featsT"] = feats.T.copy()
    c["tneg"] = (-tt).reshape(16, 128).T.copy()
    j = np.repeat(np.arange(8), 16).astype(np.float32)
    ex = np.stack([7 - j, j, j + 1, 8 - j, j - 7, -j])
    c["s5exp"] = np.broadcast_to(ex[None], (64, 6, 128)).astype(np.float32).copy()
    jj = np.repeat(np.arange(8), 16)
    c["mf"] = (jj[None, :] >= jj[:, None]).astype(np.float32)
    c["mb"] = (jj[:, None] >= jj[None, :]).astype(np.float32)
    m0 = np.ones((128, 1), np.float32)
    m0[0, 0] = 0.0
    c["m0"] = m0
    c["onesf"] = np.ones((128, 128), np.float32)
    c["mix"] = np.broadcast_to(np.arange(256, dtype=np.float32)[None], (64, 256)).copy()
    return c


CONST = None


def build_program(upto=99, debug=False, lite=False):
    nc = bass.Bass("TRN2", target_bir_lowering=False)
    P = Prog(nc)
    dbg = {}

    def din(name, shape, dt=F32):
        return nc.dram_tensor(name, list(shape), dt, kind="ExternalInput").ap()

    def dscr(name, shape, dt=F32):
        return nc.dram_tensor(name, list(shape), dt).ap()

    def dout(name, shape, dt=F32):
        return nc.dram_tensor(name, list(shape), dt, kind="ExternalOutput").ap()

    x_d = din("x", [NS * L, D])
    ctx_d = din("ctx", [NS * LC, D])
    cT_d = din("cT", [128, 8, NS + 1])
    w_ada = din("w_ada", [D, 6 * D])
    b_ada = din("b_ada", [6 * D])
    g1_d = din("g_norm1", [D])
    g2_d = din("g_norm2", [D])
    w_in_d = din("w_in", [D, 2048])
    b_in_d = din("b_in", [2048])
    s5p_d = din("s5p", [64, 3, 64])
    s5b_d = din("s5b", [64, 2, 64, 16])
    s5c_d = din("s5c", [64, 2, 64, 16])
    s5d_d = din("s5d", [128, 32])
    w_glu_d = din("w_glu", [512, 512])
    b_glu_d = din("b_glu", [512])
    cw_d = din("hy_conv_w", [3, 1536])
    cb_d = din("hy_conv_b", [1536])
    hf_w1 = din("hf_w1", [33, 64])
    hf_b1 = din("hf_b1", [64, 1])
    hf_wh = din("hf_wh", [2, 64, 64])
    hf_bh = din("hf_bh", [64, 2])
    hf_fr = din("hf_freq", [64, 1])
    hf_wout = din("hf_wout", [64, 2048])
    hf_dec = din("hf_decay", [2048])
    hyd_d = din("hy_d", [2, 512])
    gs5_d = din("g_out_s5", [512])
    ghy_d = din("g_out_hy", [512])
    w_out_d = din("w_out", [D, D])
    wq_d = din("peer_wq", [D, 2048])
    kT_d = din("peer_kT", [128, 16, 128])
    uT_d = din("peer_uT", [D, 16384 if not lite else 128])
    v_d = din("peer_v", [16384 if not lite else 128, D])
    gf_d = din("g_final", [D])
    cst = {k: din("k_" + k, list(v.shape), BF16 if v.dtype == ml_dtypes.bfloat16 else F32)
           for k, v in CONST.items()}
    out_d = dout("out", [NS * L, D])

    modx_d = dscr("modx_s", [NS + 1, 128, 6 * D])
    q_s = dscr("q_s", [NS * L, 1536])
    y_s = dscr("y_s", [NS * L, 512])
    z_s = dscr("z_s", [NS * L, 512])
    kspec_s = dscr("kspec_s", [2, 2, 16, 128, 512])
    uTb_s = dscr("uTb_s", [32, 128, 8, 512], BF16)
    vb_s = dscr("vb_s", [32, 128, 4, 1024], BF16)

    if debug:
        dbg["modx"] = dout("dbg_modx", [NS + 1, 6 * D])

    top = contextlib.ExitStack()
    uid = [0]

    def SB(es, shape, dt=F32, name=None):
        uid[0] += 1
        return es.enter_context(nc.sbuf_tensor((name or "t") + str(uid[0]), list(shape), dt))

    def PS(es, shape, dt=F32, name=None):
        uid[0] += 1
        return es.enter_context(nc.psum_tensor((name or "p") + str(uid[0]), list(shape), dt))

    def mm(ps_ap, pairs, reads, writes):
        n = len(pairs)
        for i, (l, r) in enumerate(pairs):
            P.op("pe", lambda e, l=l, r=r, i=i: e.matmul(ps_ap, lhsT=l, rhs=r, start=(i == 0), stop=(i == n - 1)),
                 reads=reads, writes=writes)

    def V(eng, fn, reads, writes):
        P.op(eng, fn, reads=reads, writes=writes)

    ident = SB(top, [128, 128], BF16, "ident")
    identf = SB(top, [128, 128], F32, "identf")
    mhalf = SB(top, [128, 1], F32, "mhalf")
    P.dma(ident[:], cst["ident"][:, :])
    P.dma(identf[:], cst["identf"][:, :])
    V("pool", lambda e: e.memset(mhalf[:], -0.5), [], [mhalf])

    def rsqrt_mean(es, ssum, n, out_rstd):
        V("dve", lambda e: e.tensor_scalar(out=out_rstd[:], in0=ssum[:], scalar1=1.0 / n, scalar2=EPS,
                                           op0=ALU.mult, op1=ALU.add), [ssum], [out_rstd])
        V("pool", lambda e: e.tensor_tensor(out=out_rstd[:], in0=out_rstd[:], in1=mhalf[:], op=ALU.pow),
          [out_rstd, mhalf], [out_rstd])

    with contextlib.ExitStack() as es:
        cT = SB(es, [128, 8, NS + 1])
        sil = SB(es, [128, 8, NS + 1])
        rep = SB(es, [128, NS + 1, 8, 128])
        bada = SB(es, [128, 6 * D])
        P.dma(cT[:], cT_d[:, :, :])
        P.dma(bada[:], b_ada.partition_broadcast(128))
        V("act", lambda e: e.activation(out=sil[:], in_=cT[:], func=AF.Silu), [cT], [sil])
        for n in range(NS + 1):
            V("dve", lambda e, n=n: e.tensor_copy(out=rep[:, n], in_=sil[:, :, n:n + 1].to_broadcast([128, 8, 128])),
              [sil], [rep])
        wts = [SB(es, [128, 8, 512]) for _ in range(2)]
        mps = [PS(es, [128, 512]) for _ in range(2)]
        mo = [SB(es, [128, 512]) for _ in range(2)]
        it = 0
        for nt in range(12):
            wt = wts[nt % 2]
            P.dma(wt[:], w_ada[:, nt * 512:(nt + 1) * 512].rearrange("(k p) n -> p k n", p=128))
            for n in range(NS + 1):
                ps = mps[it % 2]
                o = mo[it % 2]
                it += 1
                mm(ps[:], [(rep[:, n, k, :], wt[:, k, :]) for k in range(8)], [rep, wt], [ps])
                V("dve", lambda e, ps=ps, o=o, nt=nt: e.tensor_tensor(out=o[:], in0=ps[:], in1=bada[:, nt * 512:(nt + 1) * 512],
                                                                      op=ALU.add), [ps, bada], [o])
                P.dma(modx_d[n, :, nt * 512:(nt + 1) * 512], o[:], q="act")
                if debug:
                    P.dma(dbg["modx"][n:n + 1, nt * 512:(nt + 1) * 512], o[0:1, :], q="act", is_output=True)
    P.barrier()
    if upto <= 0:
        P.emit()
        top.close()
        return nc


    KMASK = 1.0e5
    NEXP = 16384 if not lite else 128
    NCH = NEXP // 512 if not lite else 0
    TS = 256
    NT = TS // 128
    wq_b = dscr("wq_b", [128, 8, 2048], BF16)
    wout_b = dscr("wout_b", [128, 8, 1024], BF16)
    wglu_b = dscr("wglu_b", [128, 4, 512], BF16)
    kT_b = dscr("kT_b", [128, 16, 128], BF16)
    if debug:
        dbg["x1"] = dout("dbg_x1", [NS * L, D])
        dbg["hx2"] = dout("dbg_hx2", [NS * L, D], BF16)
        dbg["pe"] = dout("dbg_pe", [NS * L, D])
    jobs = []
    for k in range(8):
        jobs.append((wq_b[:, k, :], wq_d[k * 128:(k + 1) * 128, :], 2048))
    for k in range(8):
        jobs.append((wout_b[:, k, :], w_out_d[k * 128:(k + 1) * 128, :], 1024))
    for k in range(4):
        jobs.append((wglu_b[:, k, :], w_glu_d[k * 128:(k + 1) * 128, :], 512))
    jobs.append((kT_b[:].rearrange("p c n -> p (c n)"), kT_d[:].rearrange("p c n -> p (c n)"), 2048))
    for ec in range(NCH):
        jobs.append((uTb_s[ec].rearrange("p k e -> p (k e)"), uT_d[:, ec * 512:(ec + 1) * 512].rearrange("(k p) e -> p k e", p=128), 4096))
        jobs.append((vb_s[ec].rearrange("p b d -> p (b d)"), v_d[ec * 512:(ec + 1) * 512, :].rearrange("(b p) d -> p b d", p=128), 4096))
    job_i = [0]

    def emit_job(stg, stb):
        i = job_i[0]
        if i >= len(jobs):
            return
        job_i[0] += 1
        dst, srcap, nel = jobs[i]
        a = stg[i % 2]
        b = stb[i % 2]
        if len(srcap.shape) == 3:
            P.dma(a[:, 0:nel].rearrange("p (k e) -> p k e", k=srcap.shape[1]), srcap, q="sp")
        else:
            P.dma(a[:, 0:nel], srcap, q="sp")
        V("act", lambda e: e.copy(out=b[:, 0:nel], in_=a[:, 0:nel]), [a], [b])
        P.dma(dst, b[:, 0:nel], q="sp")

    U_s = dscr("U_s", [32, 128, NS * 256 + 128], BF16)
    if debug:
        dbg["U"] = dout("dbg_U", [32, 128, NS * 256 + 128], BF16)
        dbg["q"] = dout("dbg_q", [NS * L, 1536])
    with contextlib.ExitStack() as es:
        w_in = SB(es, [128, 8, 2048], BF16)
        stage = [SB(es, [128, 2048]) for _ in range(2)]
        for k in range(8):
            st = stage[k % 2]
            P.dma(st[:], w_in_d[k * 128:(k + 1) * 128, :])
            V("dve", lambda e, st=st, k=k: e.tensor_copy(out=w_in[:, k, :], in_=st[:]), [st], [w_in])
        b_in = SB(es, [128, 2048])
        g1 = SB(es, [128, D])
        cw = SB(es, [128, 3, 1536])
        cb = SB(es, [128, 1536])
        shm = SB(es, [128, 128], BF16)
        shp = SB(es, [128, 128], BF16)
        P.dma(b_in[:], b_in_d.partition_broadcast(128))
        P.dma(g1[:], g1_d.partition_broadcast(128))
        for j in range(3):
            P.dma(cw[:, j, :], cw_d[j].partition_broadcast(128))
        P.dma(cb[:], cb_d.partition_broadcast(128))
        P.dma(shm[:], cst["shm"][:, :])
        P.dma(shp[:], cst["shp"][:, :])
        hT = SB(es, [128, 8, 1024], BF16)
        um2 = SB(es, [128, 32, 8, 16], BF16)
        ug = [SB(es, [128, 8, 128], BF16) for _ in range(2)]
        xt = [SB(es, [128, D]) for _ in range(2)]
        tmp = SB(es, [128, D])
        hb = [SB(es, [128, D], BF16) for _ in range(2)]
        g1p = SB(es, [128, D])
        sh1 = SB(es, [128, D])
        ssum = [SB(es, [128, 1]) for _ in range(2)]
        rstd = [SB(es, [128, 1]) for _ in range(2)]
        junk = SB(es, [128, D], BF16)
        pbf = SB(es, [128, 1536], BF16)
        t1 = SB(es, [128, 1536])
        t2 = SB(es, [128, 1536])
        pA = PS(es, [128, 1536])
        pB = PS(es, [128, 1536])
        trp = [PS(es, [128, 8, 128], BF16) for _ in range(1)]
        ps5 = PS(es, [128, 512])

        cstg = [SB(es, [128, 4096]) for _ in range(2)]
        cstb = [SB(es, [128, 4096], BF16) for _ in range(2)]
        blocks = [("x", n, half) for n in range(NS) for half in range(2)] + [("c", NS, 0)]
        cur_mod = None
        for bi, (kind, n, half) in enumerate(blocks):
            if cur_mod != n:
                cur_mod = n
                P.dma(sh1[:], modx_d[n, :, 0:D])
                P.dma(g1p[:], modx_d[n, :, D:2 * D])
                V("dve", lambda e: e.scalar_tensor_tensor(out=g1p[:], in0=g1p[:], scalar=1.0, in1=g1[:],
                                                          op0=ALU.add, op1=ALU.mult), [g1p, g1], [g1p])
            src = x_d if kind == "x" else ctx_d
            row0 = (n * L + half * 1024) if kind == "x" else 0
            for ti in range(8):
                X = xt[ti % 2]
                H = hb[ti % 2]
                ss = ssum[ti % 2]
                rs = rstd[ti % 2]
                P.dma(X[:], src[row0 + ti * 128: row0 + (ti + 1) * 128, :], q="sp" if ti % 2 else "act")
                V("act", lambda e, X=X, ss=ss: e.activation(out=junk[:], in_=X[:], func=AF.Square, accum_out=ss[:]),
                  [X], [junk, ss])
                rsqrt_mean(es, ss, D, rs)
                V("dve", lambda e, X=X, rs=rs: e.scalar_tensor_tensor(out=tmp[:], in0=X[:], scalar=rs[:, 0:1], in1=g1p[:],
                                                                      op0=ALU.mult, op1=ALU.mult), [X, rs, g1p], [tmp])
                V("pool", lambda e, H=H: e.tensor_tensor(out=H[:], in0=tmp[:], in1=sh1[:], op=ALU.add), [tmp, sh1], [H])
                tp = trp[0]
                for k in range(8):
                    P.op("pe", lambda e, k=k, H=H, tp=tp: e.transpose(out=tp[:, k, :], in_=H[:, k * 128:(k + 1) * 128], identity=ident[:]),
                         reads=[H, ident], writes=[tp])
                V("act", lambda e, tp=tp, ti=ti: e.copy(out=hT[:, :, ti * 128:(ti + 1) * 128], in_=tp[:]), [tp], [hT])
                emit_job(cstg, cstb)
            if kind == "x":
                for ti in range(8):
                    for nt in range(3):
                        mm(pA[:, nt * 512:(nt + 1) * 512],
                           [(hT[:, k, ti * 128:(ti + 1) * 128], w_in[:, k, 512 + nt * 512: 512 + (nt + 1) * 512]) for k in range(8)],
                           [hT, w_in], [pA])
                    V("dve", lambda e: e.tensor_tensor(out=pbf[:], in0=pA[:], in1=b_in[:, 512:2048], op=ALU.add), [pA, b_in], [pbf])
                    for nt in range(3):
                        mm(pB[:, nt * 512:(nt + 1) * 512], [(shm[:], pbf[:, nt * 512:(nt + 1) * 512])], [shm, pbf], [pB])
                    for nt in range(3):
                        mm(pA[:, nt * 512:(nt + 1) * 512], [(shp[:], pbf[:, nt * 512:(nt + 1) * 512])], [shp, pbf], [pA])
                    V("pool", lambda e: e.tensor_tensor(out=t1[:], in0=pbf[:], in1=cw[:, 1, :], op=ALU.mult), [pbf, cw], [t1])
                    V("pool", lambda e: e.tensor_tensor(out=t1[:], in0=t1[:], in1=cb[:], op=ALU.add), [t1, cb], [t1])
                    V("dve", lambda e: e.tensor_tensor(out=t2[:], in0=pB[:], in1=cw[:, 0, :], op=ALU.mult), [pB, cw], [t2])
                    V("pool", lambda e: e.tensor_tensor(out=t1[:], in0=t1[:], in1=t2[:], op=ALU.add), [t1, t2], [t1])
                    V("dve", lambda e: e.tensor_tensor(out=t2[:], in0=pA[:], in1=cw[:, 2, :], op=ALU.mult), [pA, cw], [t2])
                    V("pool", lambda e: e.tensor_tensor(out=t1[:], in0=t1[:], in1=t2[:], op=ALU.add), [t1, t2], [t1])
                    r0 = row0 + ti * 128
                    P.dma(q_s[r0:r0 + 128, :], t1[:], q="act")
                    if debug:
                        P.dma(dbg["q"][r0:r0 + 128, :], t1[:], q="act", is_output=True)
            for j in range(8):
                mm(ps5[:], [(hT[:, k, j::8], w_in[:, k, 0:512]) for k in range(8)], [hT, w_in], [ps5])
                V("dve", lambda e, j=j: e.tensor_tensor(out=um2[:, :, j, :], in0=ps5[:].rearrange("p (g h) -> p g h", g=32),
                                                        in1=b_in[:, 0:512].rearrange("p (g h) -> p g h", g=32), op=ALU.add),
                  [ps5, b_in], [um2])
            col0 = (n * 256 + half * 128) if kind == "x" else NS * 256
            for gb in range(4):
                tp = trp[0]
                ugt = ug[gb % 2]
                for gi in range(8):
                    g = gb * 8 + gi
                    P.op("pe", lambda e, g=g, gi=gi, tp=tp: e.transpose(out=tp[:, gi, :], in_=um2[:, g].rearrange("p j h -> p (j h)"),
                                                                        identity=ident[:]), reads=[um2, ident], writes=[tp])
                V("act", lambda e, tp=tp, ugt=ugt: e.copy(out=ugt[:], in_=tp[:]), [tp], [ugt])
                P.dma(U_s[gb * 8:(gb + 1) * 8, :, col0:col0 + 128].rearrange("g p c -> p g c"), ugt[:], q="sp")
                if debug:
                    P.dma(dbg["U"][gb * 8:(gb + 1) * 8, :, col0:col0 + 128].rearrange("g p c -> p g c"), ugt[:], q="sp", is_output=True)
        while job_i[0] < len(jobs):
            emit_job(cstg, cstb)
    P.barrier()
    if upto <= 1:
        P.emit()
        top.close()
        return nc


    TWO_PI = 2.0 * math.pi
    PI_LO = 3.1415925

    def trig(ang, n_free_shape, scratch, out_sin=None, out_cos=None):
        ki, kf, r = scratch
        for out, off in ((out_sin, 0.0), (out_cos, math.pi / 2)):
            if out is None:
                continue
            V("dve", lambda e, off=off: e.tensor_scalar(out=kf, in0=ang, scalar1=off, scalar2=1.0 / TWO_PI, op0=ALU.add, op1=ALU.mult),
              [ang], [kf])
            V("dve", lambda e: e.tensor_copy(out=ki, in_=kf), [kf], [ki])
            V("dve", lambda e: e.tensor_copy(out=kf, in_=ki), [ki], [kf])
            V("dve", lambda e: e.scalar_tensor_tensor(out=r, in0=kf, scalar=-TWO_PI, in1=ang, op0=ALU.mult, op1=ALU.add), [kf, ang], [r])
            V("dve", lambda e, off=off: e.tensor_scalar(out=r, in0=r, scalar1=off, scalar2=PI_LO, op0=ALU.add, op1=ALU.min), [r], [r])
            V("dve", lambda e: e.tensor_scalar(out=r, in0=r, scalar1=-PI_LO, scalar2=None, op0=ALU.max), [r], [r])
            V("act", lambda e, out=out: e.activation(out=out, in_=r, func=AF.Sin), [r], [out])

    if debug:
        dbg["y"] = dout("dbg_y", [NS * L, 512])
        dbg["h0"] = dout("dbg_h0", [64, 2, 64, NS])
    s5es = contextlib.ExitStack()
    R8 = SB(s5es, [128, 64, 2, 64], BF16)
    O8 = SB(s5es, [64, 64, 2, 128], BF16)
    T8 = SB(s5es, [128, 32, 128], BF16)
    th8 = SB(s5es, [64, 64])
    r8 = SB(s5es, [64, 64])
    with contextlib.ExitStack() as es:
        s5p = SB(es, [64, 3, 64])
        s5b = SB(es, [64, 2, 64, 16])
        s5c = SB(es, [64, 2, 64, 16])
        s5d = SB(es, [128, 32])
        sexp = SB(es, [64, 6, 128])
        mf = SB(es, [128, 128])
        mb = SB(es, [128, 128])
        P.dma(s5p[:], s5p_d[:, :, :])
        P.dma(s5b[:], s5b_d[:, :, :, :])
        P.dma(s5c[:], s5c_d[:, :, :, :])
        P.dma(s5d[:], s5d_d[:, :])
        P.dma(sexp[:], cst["s5exp"][:, :, :])
        P.dma(mf[:], cst["mf"][:, :])
        P.dma(mb[:], cst["mb"][:, :])
        step = SB(es, [64, 64])
        rho = SB(es, [64, 64])
        th = SB(es, [64, 64])
        ebase = SB(es, [64, 64])
        V("pool", lambda e: e.memset(ebase[:], math.e), [], [ebase])
        V("pool", lambda e: e.tensor_tensor(out=step[:], in0=ebase[:], in1=s5p[:, 2, :], op=ALU.pow), [ebase, s5p], [step])
        V("dve", lambda e: e.tensor_tensor(out=rho[:], in0=s5p[:, 0, :], in1=step[:], op=ALU.mult), [s5p, step], [rho])
        V("dve", lambda e: e.tensor_tensor(out=th[:], in0=s5p[:, 1, :], in1=step[:], op=ALU.mult), [s5p, step], [th])
        ki_s = SB(es, [64, 1024], I32)
        kf_s = SB(es, [64, 1024])
        r_s = SB(es, [64, 1024])
        sn = SB(es, [64, 1024])
        cs = SB(es, [64, 1024])
        mg = SB(es, [64, 1024])
        ang = SB(es, [64, 1024])
        V("dve", lambda e: e.tensor_copy(out=ang[:, 0:64], in_=th[:]), [th], [ang])
        trig(ang[:, 0:64], None, (ki_s[:, 0:64], kf_s[:, 0:64], r_s[:, 0:64]), sn[:, 0:64], cs[:, 0:64])
        V("act", lambda e: e.activation(out=mg[:, 0:64], in_=rho[:], func=AF.Exp), [rho], [mg])
        l1re = SB(es, [64, 64])
        l1im = SB(es, [64, 64])
        V("dve", lambda e: e.tensor_tensor(out=l1re[:], in0=mg[:, 0:64], in1=cs[:, 0:64], op=ALU.mult), [mg, cs], [l1re])
        V("dve", lambda e: e.tensor_tensor(out=l1im[:], in0=mg[:, 0:64], in1=sn[:, 0:64], op=ALU.mult), [mg, sn], [l1im])
        V("dve", lambda e: e.tensor_scalar(out=l1re[:], in0=l1re[:], scalar1=-1.0, scalar2=None, op0=ALU.add), [l1re], [l1re])
        den = SB(es, [64, 64])
        t64 = SB(es, [64, 64])
        cre = SB(es, [64, 64])
        cim = SB(es, [64, 64])
        are = s5p[:, 0, :]
        aim = s5p[:, 1, :]
        V("dve", lambda e: e.tensor_tensor(out=den[:], in0=are, in1=are, op=ALU.mult), [s5p], [den])
        V("dve", lambda e: e.tensor_tensor(out=t64[:], in0=aim, in1=aim, op=ALU.mult), [s5p], [t64])
        V("dve", lambda e: e.tensor_tensor(out=den[:], in0=den[:], in1=t64[:], op=ALU.add), [den, t64], [den])
        V("dve", lambda e: e.reciprocal(out=den[:], in_=den[:]), [den], [den])
        V("dve", lambda e: e.tensor_tensor(out=cre[:], in0=l1re[:], in1=are, op=ALU.mult), [l1re, s5p], [cre])
        V("dve", lambda e: e.tensor_tensor(out=t64[:], in0=l1im[:], in1=aim, op=ALU.mult), [l1im, s5p], [t64])
        V("dve", lambda e: e.tensor_tensor(out=cre[:], in0=cre[:], in1=t64[:], op=ALU.add), [cre, t64], [cre])
        V("dve", lambda e: e.tensor_tensor(out=cre[:], in0=cre[:], in1=den[:], op=ALU.mult), [cre, den], [cre])
        V("dve", lambda e: e.tensor_tensor(out=cim[:], in0=l1im[:], in1=are, op=ALU.mult), [l1im, s5p], [cim])
        V("dve", lambda e: e.tensor_tensor(out=t64[:], in0=l1re[:], in1=aim, op=ALU.mult), [l1re, s5p], [t64])
        V("dve", lambda e: e.tensor_tensor(out=cim[:], in0=cim[:], in1=t64[:], op=ALU.subtract), [cim, t64], [cim])
        V("dve", lambda e: e.tensor_tensor(out=cim[:], in0=cim[:], in1=den[:], op=ALU.mult), [cim, den], [cim])
        bbre = SB(es, [64, 64, 16])
        bbim = SB(es, [64, 64, 16])
        tb = SB(es, [64, 64, 16])
        creb = cre[:].unsqueeze(2).to_broadcast([64, 64, 16])
        cimb = cim[:].unsqueeze(2).to_broadcast([64, 64, 16])
        V("dve", lambda e: e.tensor_tensor(out=bbre[:], in0=s5b[:, 0], in1=creb, op=ALU.mult), [s5b, cre], [bbre])
        V("dve", lambda e: e.tensor_tensor(out=tb[:], in0=s5b[:, 1], in1=cimb, op=ALU.mult), [s5b, cim], [tb])
        V("dve", lambda e: e.tensor_tensor(out=bbre[:], in0=bbre[:], in1=tb[:], op=ALU.subtract), [bbre, tb], [bbre])
        V("dve", lambda e: e.tensor_tensor(out=bbim[:], in0=s5b[:, 1], in1=creb, op=ALU.mult), [s5b, cre], [bbim])
        V("dve", lambda e: e.tensor_tensor(out=tb[:], in0=s5b[:, 0], in1=cimb, op=ALU.mult), [s5b, cim], [tb])
        V("dve", lambda e: e.tensor_tensor(out=bbim[:], in0=bbim[:], in1=tb[:], op=ALU.add), [bbim, tb], [bbim])
        V("dve", lambda e: e.tensor_scalar(out=ang[:, 0:64], in0=th[:], scalar1=8.0, scalar2=None, op0=ALU.mult), [th], [ang])
        V("dve", lambda e: e.tensor_scalar(out=kf_s[:, 0:64], in0=ang[:, 0:64], scalar1=1.0 / TWO_PI, scalar2=None, op0=ALU.mult), [ang], [kf_s])
        V("dve", lambda e: e.tensor_copy(out=ki_s[:, 0:64], in_=kf_s[:, 0:64]), [kf_s], [ki_s])
        V("dve", lambda e: e.tensor_copy(out=kf_s[:, 0:64], in_=ki_s[:, 0:64]), [ki_s], [kf_s])
        V("dve", lambda e: e.scalar_tensor_tensor(out=th8[:], in0=kf_s[:, 0:64], scalar=-TWO_PI, in1=ang[:, 0:64], op0=ALU.mult, op1=ALU.add),
          [kf_s, ang], [th8])
        V("act", lambda e: e.activation(out=r8[:], in_=rho[:], func=AF.Exp, scale=8.0), [rho], [r8])
        Lre = SB(es, [64, 3, 8, 128])
        Lim = SB(es, [64, 3, 8, 128])
        RT = SB(es, [64, 2, 8, 128])
        OP = SB(es, [64, 2, 8, 128])
        OO = SB(es, [64, 2, 8, 128])
        tw = SB(es, [64, 8, 128])
        pT = [PS(es, [128, 128]) for _ in range(2)]
        pR = PS(es, [128, 4, 64])
        tt8 = SB(es, [128, 128])
        tt8b = SB(es, [128, 128])
        for d in range(2):
            for gb in range(4):
                dg0 = d * 32 + gb * 8
                for kk, kind in enumerate((d, 2 + d, 4 + d)):
                    a3 = ang[:].rearrange("p (a c) -> p a c", a=8)
                    exb = sexp[:, kind, :].unsqueeze(1).to_broadcast([64, 8, 128])
                    V("dve", lambda e, exb=exb, dg0=dg0: e.tensor_tensor(out=a3, in0=exb, in1=th[:, dg0:dg0 + 8].unsqueeze(2).to_broadcast([64, 8, 128]),
                                                                         op=ALU.mult), [sexp, th], [ang])
                    trig(ang[:], None, (ki_s[:], kf_s[:], r_s[:]), sn[:], cs[:])
                    m3 = mg[:].rearrange("p (a c) -> p a c", a=8)
                    V("dve", lambda e, exb=exb, dg0=dg0: e.tensor_tensor(out=m3, in0=exb, in1=rho[:, dg0:dg0 + 8].unsqueeze(2).to_broadcast([64, 8, 128]),
                                                                         op=ALU.mult), [sexp, rho], [mg])
                    V("act", lambda e: e.activation(out=mg[:], in_=mg[:], func=AF.Exp), [mg], [mg])
                    V("dve", lambda e, kk=kk: e.tensor_tensor(out=Lre[:, kk].rearrange("p a c -> p (a c)"), in0=mg[:], in1=cs[:], op=ALU.mult), [mg, cs], [Lre])
                    V("dve", lambda e, kk=kk: e.tensor_tensor(out=Lim[:, kk].rearrange("p a c -> p (a c)"), in0=mg[:], in1=sn[:], op=ALU.mult), [mg, sn], [Lim])
                def cmul(out_re, out_im, kind, vre, vim, neg_im):
                    lre = Lre[:, kind].rearrange("p a (j h) -> p a j h", j=8)
                    lim = Lim[:, kind].rearrange("p a (j h) -> p a j h", j=8)
                    vr = vre.unsqueeze(2).to_broadcast([64, 8, 8, 16])
                    vi = vim.unsqueeze(2).to_broadcast([64, 8, 8, 16])
                    o_re = out_re.rearrange("p a (j h) -> p a j h", j=8)
                    o_im = out_im.rearrange("p a (j h) -> p a j h", j=8)
                    t4 = tw[:].rearrange("p a (j h) -> p a j h", j=8)
                    V("dve", lambda e: e.tensor_tensor(out=o_re, in0=lre, in1=vr, op=ALU.mult), [Lre, bbre, s5c], [out_re])
                    V("dve", lambda e: e.tensor_tensor(out=t4, in0=lim, in1=vi, op=ALU.mult), [Lim, bbim, s5c], [tw])
                    V("dve", lambda e: e.tensor_tensor(out=out_re, in0=out_re, in1=tw[:], op=ALU.subtract), [out_re, tw], [out_re])
                    V("dve", lambda e: e.tensor_tensor(out=o_im, in0=lre, in1=vi, op=ALU.mult), [Lre, bbim, s5c], [out_im])
                    V("dve", lambda e: e.tensor_tensor(out=t4, in0=lim, in1=vr, op=ALU.mult), [Lim, bbre, s5c], [tw])
                    if neg_im:
                        V("dve", lambda e: e.scalar_tensor_tensor(out=out_im.rearrange("p a c -> p (a c)"), in0=out_im.rearrange("p a c -> p (a c)"), scalar=-1.0,
                                                                  in1=tw[:].rearrange("p a c -> p (a c)"), op0=ALU.mult, op1=ALU.subtract), [out_im, tw], [out_im])
                    else:
                        V("dve", lambda e: e.tensor_tensor(out=out_im, in0=out_im, in1=tw[:], op=ALU.add), [out_im, tw], [out_im])
                cmul(RT[:, 0], RT[:, 1], 0, bbre[:, dg0:dg0 + 8, :], bbim[:, dg0:dg0 + 8, :], False)
                cmul(OO[:, 0], OO[:, 1], 1, s5c[:, 0, dg0:dg0 + 8, :], s5c[:, 1, dg0:dg0 + 8, :], True)
                cmul(OP[:, 0], OP[:, 1], 2, s5c[:, 0, dg0:dg0 + 8, :], s5c[:, 1, dg0:dg0 + 8, :], True)
                for i in range(8):
                    dg = dg0 + i
                    g = gb * 8 + i
                    V("act", lambda e, dg=dg, i=i: e.copy(out=O8[:, dg], in_=OO[:, :, i, :]), [OO], [O8])
                    for c2 in range(2):
                        P.op("pe", lambda e, c2=c2, i=i: e.transpose(out=pR[:, c2, :], in_=RT[:, c2, i, :], identity=identf[0:64, 0:64]),
                             reads=[RT, identf], writes=[pR])
                    V("act", lambda e, dg=dg: e.copy(out=R8[:, dg], in_=pR[:, 0:2, :]), [pR], [R8])
                    pt = pT[i % 2]
                    mm(pt[:], [(RT[:, 0, i, :], OP[:, 0, i, :]), (RT[:, 1, i, :], OP[:, 1, i, :])], [RT, OP], [pt])
                    if d == 0:
                        V("dve", lambda e, pt=pt: e.tensor_tensor(out=tt8[:], in0=pt[:], in1=mf[:], op=ALU.mult), [pt, mf], [tt8])
                        V("dve", lambda e, g=g: e.scalar_tensor_tensor(out=tt8[:], in0=identf[:], scalar=s5d[:, g:g + 1], in1=tt8[:],
                                                                       op0=ALU.mult, op1=ALU.add), [identf, s5d, tt8], [tt8])
                        V("pool", lambda e, g=g: e.tensor_copy(out=T8[:, g, :], in_=tt8[:]), [tt8], [T8])
                    else:
                        V("dve", lambda e, pt=pt: e.tensor_tensor(out=tt8b[:], in0=pt[:], in1=mb[:], op=ALU.mult), [pt, mb], [tt8b])
                        V("pool", lambda e, g=g: e.tensor_tensor(out=T8[:, g, :], in0=T8[:, g, :], in1=tt8b[:], op=ALU.add), [tt8b, T8], [T8])
    P.barrier()
    if upto <= 2:
        if debug:
            dbg["T8"] = dout("dbg_T8", [128, 32, 128], BF16)
            dbg["O8"] = dout("dbg_O8", [64, 64, 2, 128], BF16)
            dbg["R8"] = dout("dbg_R8", [128, 64, 2, 64], BF16)
            P.dma(dbg["T8"][:, :, :], T8[:], is_output=True)
            P.dma(dbg["O8"][:, :, :, :], O8[:], is_output=True)
            P.dma(dbg["R8"][:, :, :, :], R8[:], is_output=True)
        P.emit()
        s5es.close()
        top.close()
        return nc


    with contextlib.ExitStack() as es:
        mix = SB(es, [64, 256])
        P.dma(mix[:], cst["mix"][:, :])
        Ugs = [SB(es, [128, NS * 256 + 128], BF16) for _ in range(2)]
        cosr = [SB(es, [64, 256]) for _ in range(2)]
        sinr = [SB(es, [64, 256]) for _ in range(2)]
        angr = SB(es, [64, 256])
        kir = SB(es, [64, 256], I32)
        kfr = SB(es, [64, 256])
        rr = SB(es, [64, 256])
        cc = [SB(es, [64, NS, 255]) for _ in range(2)]
        ta = SB(es, [64, NS, 256])
        tb2 = SB(es, [64, NS, 256])
        W = [SB(es, [64, NS, 256]) for _ in range(2)]
        Wc = [SB(es, [64, NS, 32]) for _ in range(2)]
        h0 = SB(es, [64, 2, 2, NS])
        t4 = SB(es, [64, NS])
        Sin = SB(es, [64, 2, 2, NS * 256], BF16)
        Ysb = SB(es, [128, NS * 256])
        Yout = SB(es, [128, 8, 8, 128])
        psS = [PS(es, [64, NS * 256]) for _ in range(2)]
        psY = PS(es, [128, NS * 256])
        pTr = PS(es, [128, 4, 128])
        psC = PS(es, [64, 2, 128])

        def rot(out_re, out_im, s_re, s_im, cs_, sn_, conj, eng2="pool"):
            V("dve", lambda e: e.tensor_tensor(out=ta_v(out_re), in0=s_re, in1=cs_, op=ALU.mult), [psS[0], psC, W[0], Wc[0], cosr[0], cosr[1]], [ta])
            V("dve", lambda e: e.tensor_tensor(out=tb_v(out_re), in0=s_im, in1=sn_, op=ALU.mult), [psS[1], psC, W[1], Wc[1], sinr[0], sinr[1]], [tb2])
            V(eng2, lambda e: e.tensor_tensor(out=out_re, in0=ta_v(out_re), in1=tb_v(out_re), op=(ALU.add if conj else ALU.subtract)),
              [ta, tb2], [cc[0], Sin])
            V("dve", lambda e: e.tensor_tensor(out=ta_v(out_re), in0=s_im, in1=cs_, op=ALU.mult), [psS[1], psC, W[1], Wc[1], cosr[0], cosr[1]], [ta])
            V("dve", lambda e: e.tensor_tensor(out=tb_v(out_re), in0=s_re, in1=sn_, op=ALU.mult), [psS[0], psC, W[0], Wc[0], sinr[0], sinr[1]], [tb2])
            V(eng2, lambda e: e.tensor_tensor(out=out_im, in0=ta_v(out_re), in1=tb_v(out_re), op=(ALU.subtract if conj else ALU.add)),
              [ta, tb2], [cc[1], Sin])

        def ta_v(like):
            return ta[:, :, 0:like.shape[2]]

        def tb_v(like):
            return tb2[:, :, 0:like.shape[2]]

        for g in range(32):
            Ug = Ugs[g % 2]
            P.dma(Ug[:], U_s[g], q="act" if g % 2 else "sp")
            for d in range(2):
                dg = d * 32 + g
                V("dve", lambda e, dg=dg: e.tensor_scalar(out=angr[:], in0=mix[:], scalar1=th8[:, dg:dg + 1], scalar2=None, op0=ALU.mult),
                  [mix, th8], [angr])
                trig(angr[:], None, (kir[:], kfr[:], rr[:]), sinr[d][:], cosr[d][:])
            for d in range(2):
                dg = d * 32 + g
                for c2 in range(2):
                    mm(psC[:, c2, :], [(R8[:, dg, c2, :], Ug[:, NS * 256:NS * 256 + 128])], [R8, Ug], [psC])
                sre = psC[:, 0, :].rearrange("p (n m) -> p n m", n=NS)
                sim = psC[:, 1, :].rearrange("p (n m) -> p n m", n=NS)
                if d == 1:
                    sre = sre[:, :, ::-1]
                    sim = sim[:, :, ::-1]
                csb = cosr[d][:, 1:33].unsqueeze(1).to_broadcast([64, NS, 32])
                snb = sinr[d][:, 1:33].unsqueeze(1).to_broadcast([64, NS, 32])
                rot(cc[0][:, :, 0:32], cc[1][:, :, 0:32], sre, sim, csb, snb, True)
                for c2 in range(2):
                    for n in range(NS):
                        V("dve", lambda e, c2=c2, n=n, dg=dg: e.tensor_tensor_scan(out=Wc[c2][:, n, :], data0=r8[:, dg:dg + 1].to_broadcast([64, 32]),
                                                                                  data1=cc[c2][:, n, 0:32], initial=0.0, op0=ALU.mult, op1=ALU.add),
                          [r8, cc[c2]], [Wc[c2]])
                c32 = cosr[d][:, 32:33]
                s32 = sinr[d][:, 32:33]
                V("dve", lambda e, s32=s32: e.tensor_scalar(out=t4[:], in0=Wc[1][:, :, 31], scalar1=s32, scalar2=None, op0=ALU.mult), [Wc[1], sinr[d]], [t4])
                V("dve", lambda e, c32=c32, d=d: e.scalar_tensor_tensor(out=h0[:, d, 0, :], in0=Wc[0][:, :, 31], scalar=c32, in1=t4[:], op0=ALU.mult, op1=ALU.subtract),
                  [Wc[0], cosr[d], t4], [h0])
                V("dve", lambda e, c32=c32: e.tensor_scalar(out=t4[:], in0=Wc[1][:, :, 31], scalar1=c32, scalar2=None, op0=ALU.mult), [Wc[1], cosr[d]], [t4])
                V("dve", lambda e, s32=s32, d=d: e.scalar_tensor_tensor(out=h0[:, d, 1, :], in0=Wc[0][:, :, 31], scalar=s32, in1=t4[:], op0=ALU.mult, op1=ALU.add),
                  [Wc[0], sinr[d], t4], [h0])
                if debug:
                    P.dma(dbg["h0"][:, d, g, :], h0[:, d, 0, :], is_output=True)
                    if g == 0 and d == 0:
                        dbg["m"] = dout("dbg_m", [64, 8, 256])
                        dm = SB(es, [64, 8, 256])
                        V("dve", lambda e: e.memset(dm[:], 0.0), [], [dm])
                        V("dve", lambda e: e.tensor_copy(out=dm[:, 0, :], in_=psC[:].rearrange("p a b -> p (a b)")), [psC], [dm])
                        V("dve", lambda e: e.tensor_copy(out=dm[:, 1, 0:128].rearrange("p (n m) -> p n m", n=NS), in_=cc[0][:, :, 0:32]), [cc[0]], [dm])
                        V("dve", lambda e: e.tensor_copy(out=dm[:, 2, 0:128].rearrange("p (n m) -> p n m", n=NS), in_=Wc[0][:]), [Wc[0]], [dm])
                        V("dve", lambda e: e.tensor_copy(out=dm[:, 3, :], in_=cosr[0][:]), [cosr[0]], [dm])
                        V("dve", lambda e: e.tensor_copy(out=dm[:, 4, :], in_=sinr[0][:]), [sinr[0]], [dm])
                        V("dve", lambda e: e.tensor_copy(out=dm[:, 5, 0:64], in_=r8[:]), [r8], [dm])
                        V("dve", lambda e: e.tensor_copy(out=dm[:, 6, 0:64], in_=th8[:]), [th8], [dm])
                        V("dve", lambda e: e.tensor_copy(out=dm[:, 7, 0:128].rearrange("p (n m) -> p n m", n=NS), in_=cc[1][:, :, 0:32]), [cc[1]], [dm])
                        P.dma(dbg["m"][:, :, :], dm[:], is_output=True)
            for d in range(2):
                dg = d * 32 + g
                for c2 in range(2):
                    for hf in range(2):
                        mm(psS[c2][:, hf * 512:(hf + 1) * 512], [(R8[:, dg, c2, :], Ug[:, hf * 512:(hf + 1) * 512])], [R8, Ug], [psS[c2]])
                sre = psS[0][:].rearrange("p (n m) -> p n m", n=NS)
                sim = psS[1][:].rearrange("p (n m) -> p n m", n=NS)
                if d == 0:
                    sre = sre[:, :, 0:255]
                    sim = sim[:, :, 0:255]
                else:
                    sre = sre[:, :, 255:0:-1]
                    sim = sim[:, :, 255:0:-1]
                csb = cosr[d][:, 1:256].unsqueeze(1).to_broadcast([64, NS, 255])
                snb = sinr[d][:, 1:256].unsqueeze(1).to_broadcast([64, NS, 255])
                rot(cc[0][:], cc[1][:], sre, sim, csb, snb, True)
                for c2 in range(2):
                    V("pool", lambda e, c2=c2, d=d: e.tensor_copy(out=W[c2][:, :, 0:1], in_=h0[:, d, c2, :].unsqueeze(2)), [h0], [W[c2]])
                    for n in range(NS):
                        V("dve", lambda e, c2=c2, n=n, dg=dg, d=d: e.tensor_tensor_scan(out=W[c2][:, n, 1:256], data0=r8[:, dg:dg + 1].to_broadcast([64, 255]),
                                                                                       data1=cc[c2][:, n, :], initial=h0[:, d, c2, n:n + 1], op0=ALU.mult, op1=ALU.add),
                          [r8, cc[c2], h0], [W[c2]])
                csb = cosr[d][:].unsqueeze(1).to_broadcast([64, NS, 256])
                snb = sinr[d][:].unsqueeze(1).to_broadcast([64, NS, 256])
                o_re = Sin[:, d, 0, :].rearrange("p (n m) -> p n m", n=NS)
                o_im = Sin[:, d, 1, :].rearrange("p (n m) -> p n m", n=NS)
                if d == 1:
                    o_re = o_re[:, :, ::-1]
                    o_im = o_im[:, :, ::-1]
                rot(o_re, o_im, W[0][:], W[1][:], csb, snb, False)
            for hf in range(2):
                cs_ = slice(hf * 512, (hf + 1) * 512)
                mm(psY[:, cs_], [(T8[:, g, :], Ug[:, cs_]), (O8[:, g, 0, :], Sin[:, 0, 0, cs_]), (O8[:, g, 1, :], Sin[:, 0, 1, cs_]),
                                 (O8[:, 32 + g, 0, :], Sin[:, 1, 0, cs_]), (O8[:, 32 + g, 1, :], Sin[:, 1, 1, cs_])],
                   [T8, Ug, O8, Sin], [psY])
            V("act", lambda e: e.copy(out=Ysb[:], in_=psY[:]), [psY], [Ysb])
            gi = g % 8
            for q4 in range(2):
                for b4 in range(4):
                    blk = q4 * 4 + b4
                    P.op("pe", lambda e, blk=blk, b4=b4: e.transpose(out=pTr[:, b4, :], in_=Ysb[:, blk * 128:(blk + 1) * 128], identity=identf[:]),
                         reads=[Ysb, identf], writes=[pTr])
                V("act", lambda e, q4=q4, gi=gi: e.copy(out=Yout[:, q4 * 4:(q4 + 1) * 4, :, gi * 16:(gi + 1) * 16],
                                                        in_=pTr[:].rearrange("p b (j h) -> p b j h", j=8)), [pTr], [Yout])
            if gi == 7:
                gb = g // 8
                for blk in range(8):
                    dst = y_s[blk * 1024:(blk + 1) * 1024, gb * 128:(gb + 1) * 128].rearrange("(m j) c -> m j c", j=8)
                    P.dma(dst, Yout[:, blk], q="sp" if blk % 2 else "act")
                    if debug:
                        P.dma(dbg["y"][blk * 1024:(blk + 1) * 1024, gb * 128:(gb + 1) * 128].rearrange("(m j) c -> m j c", j=8), Yout[:, blk], is_output=True)
    s5es.close()
    P.barrier()
    if upto <= 3:
        P.emit()
        top.close()
        return nc


    h_s = dscr("h_s", [L, 2048])
    if debug:
        dbg["filt"] = dout("dbg_filt", [L, 2048])
        dbg["z"] = dout("dbg_z", [NS * L, 512])
    hyes = contextlib.ExitStack()
    rn = SB(hyes, [128, 2, 512])
    with contextlib.ExitStack() as es:
        fT = SB(es, [33, 2048])
        w1 = SB(es, [33, 64])
        b1 = SB(es, [64, 1])
        wh = SB(es, [64, 2, 64])
        bh = SB(es, [64, 2])
        fr = SB(es, [64, 1])
        wout = SB(es, [64, 2048])
        absd = SB(es, [128, 2048])
        tneg = SB(es, [128, 16])
        onesf = SB(es, [128, 128])
        P.dma(fT[:], cst["featsT"][:, :])
        P.dma(w1[:], hf_w1[:, :])
        P.dma(b1[:], hf_b1[:, :])
        P.dma(wh[:], hf_wh.rearrange("i a b -> a i b"))
        P.dma(bh[:], hf_bh[:, :])
        P.dma(fr[:], hf_fr[:, :])
        P.dma(wout[:], hf_wout[:, :])
        P.dma(absd[:], hf_dec.partition_broadcast(128))
        P.dma(tneg[:], cst["tneg"][:, :])
        P.dma(onesf[:], cst["onesf"][:, :])
        V("act", lambda e: e.activation(out=absd[:], in_=absd[:], func=AF.Abs), [absd], [absd])
        hid = [SB(es, [64, 2048]) for _ in range(2)]
        pre = SB(es, [64, 2048])
        kiH = SB(es, [64, 2048], I32)
        kfH = SB(es, [64, 2048])
        rH = SB(es, [64, 2048])
        psA = PS(es, [128, 2048])
        psB = PS(es, [128, 2048])
        for layer in range(3):
            for nt in range(4):
                cs_ = slice(nt * 512, (nt + 1) * 512)
                if layer == 0:
                    mm(psA[0:64, cs_], [(w1[:], fT[:, cs_])], [w1, fT], [psA])
                else:
                    mm(psA[0:64, cs_], [(wh[:, layer - 1, :], hid[(layer - 1) % 2][:, cs_])], [wh, hid[(layer - 1) % 2]], [psA])
            bias = b1[:, 0:1] if layer == 0 else bh[:, layer - 1:layer]
            V("dve", lambda e, bias=bias: e.tensor_scalar(out=pre[:], in0=psA[0:64, :], scalar1=bias, scalar2=fr[:, 0:1], op0=ALU.add, op1=ALU.mult),
              [psA, b1, bh, fr], [pre])
            trig(pre[:], None, (kiH[:], kfH[:], rH[:]), hid[layer % 2][:], None)
        hidF = hid[0]
        dec = SB(es, [128, 2048])
        hraw = [SB(es, [128, 2048]) for _ in range(2)]
        hsq = SB(es, [128, 2048])
        for tc in range(16):
            hr = hraw[tc % 2]
            for nt in range(4):
                cs_ = slice(nt * 512, (nt + 1) * 512)
                mm(psA[:, cs_], [(hidF[:, tc * 128:(tc + 1) * 128], wout[:, cs_])], [hidF, wout], [psA])
            V("act", lambda e, tc=tc: e.activation(out=dec[:], in_=absd[:], func=AF.Exp, scale=tneg[:, tc:tc + 1]), [absd, tneg], [dec])
            V("dve", lambda e, hr=hr: e.tensor_tensor(out=hr[:], in0=psA[:], in1=dec[:], op=ALU.mult), [psA, dec], [hr])
            V("pool", lambda e, hr=hr: e.tensor_tensor(out=hsq[:], in0=hr[:], in1=hr[:], op=ALU.mult), [hr], [hsq])
            for nt in range(4):
                cs_ = slice(nt * 512, (nt + 1) * 512)
                P.op("pe", lambda e, cs_=cs_, tc=tc: e.matmul(psB[:, cs_], lhsT=onesf[:], rhs=hsq[:, cs_], start=(tc == 0), stop=(tc == 15)),
                     reads=[onesf, hsq], writes=[psB])
            P.dma(h_s[tc * 128:(tc + 1) * 128, :], hr[:], q="act")
        ssb = SB(es, [128, 2048])
        V("act", lambda e: e.copy(out=ssb[:], in_=psB[:]), [psB], [ssb])
        s4 = ssb[:].rearrange("p (o d c) -> p o d c", o=2, d=2)
        V("dve", lambda e: e.tensor_tensor(out=rn[:], in0=s4[:, :, 0, :], in1=s4[:, :, 1, :], op=ALU.add), [ssb], [rn])
        V("dve", lambda e: e.tensor_scalar(out=rn[:], in0=rn[:], scalar1=EPS, scalar2=None, op0=ALU.add), [rn], [rn])
        mh = SB(es, [128, 1024])
        V("pool", lambda e: e.memset(mh[:], -0.5), [], [mh])
        V("pool", lambda e: e.tensor_tensor(out=rn[:].rearrange("p o c -> p (o c)"), in0=rn[:].rearrange("p o c -> p (o c)"), in1=mh[:], op=ALU.pow), [rn, mh], [rn])
    P.barrier()
    Ct = SB(hyes, [128, 16, 2048], BF16)
    St = SB(hyes, [128, 16, 2048], BF16)
    P.dma(Ct[:], cst["ctab"].rearrange("(tc p) f -> p tc f", p=128))
    P.dma(St[:], cst["stab"].rearrange("(tc p) f -> p tc f", p=128), q="act")
    with contextlib.ExitStack() as es:
        hraw = [SB(es, [128, 2048]) for _ in range(2)]
        psA = PS(es, [128, 2048])
        m0 = SB(es, [128, 1])
        cphi = SB(es, [128, 16])
        sphi = SB(es, [128, 16])
        P.dma(m0[:], cst["m0"][:, :])
        P.dma(cphi[:], cst["cphi"][:, :])
        P.dma(sphi[:], cst["sphi"][:, :])
        PM = SB(es, [128, 16, 2, 512], BF16)
        kre = [SB(es, [128, 512]) for _ in range(2)]
        kim = [SB(es, [128, 512]) for _ in range(2)]
        for o in range(2):
            for tc in range(16):
                hr = hraw[tc % 2]
                P.dma(hr[:], h_s[tc * 128:(tc + 1) * 128, :])
                h4 = hr[:].rearrange("p (o d c) -> p o d c", o=2, d=2)
                V("dve", lambda e, h4=h4, o=o, hr=hr: e.tensor_tensor(out=h4[:, o], in0=h4[:, o], in1=rn[:, o:o + 1, :].to_broadcast([128, 2, 512]), op=ALU.mult),
                  [hr, rn], [hr])
                if debug:
                    P.dma(dbg["filt"][tc * 128:(tc + 1) * 128, o * 1024:(o + 1) * 1024], hr[:, o * 1024:(o + 1) * 1024], is_output=True)
                if tc == 0:
                    V("dve", lambda e, h4=h4, o=o, hr=hr: e.tensor_scalar(out=h4[:, o, 1, :], in0=h4[:, o, 1, :], scalar1=m0[:, 0:1], scalar2=None, op0=ALU.mult),
                      [hr, m0], [hr])
                V("dve", lambda e, h4=h4, o=o, tc=tc, hr=hr: e.tensor_tensor(out=PM[:, tc, 0, :], in0=h4[:, o, 0, :], in1=h4[:, o, 1, :], op=ALU.add), [hr], [PM])
                V("pool", lambda e, h4=h4, o=o, tc=tc, hr=hr: e.tensor_tensor(out=PM[:, tc, 1, :], in0=h4[:, o, 0, :], in1=h4[:, o, 1, :], op=ALU.subtract), [hr], [PM])
            for fc in range(16):
                fs = slice(fc * 128, (fc + 1) * 128)
                mm(psA[:, 0:512], [(Ct[:, tc, fs], PM[:, tc, 0, :]) for tc in range(16)], [Ct, PM], [psA])
                mm(psA[:, 512:1024], [(St[:, tc, fs], PM[:, tc, 0, :]) for tc in range(16)], [St, PM], [psA])
                mm(psA[:, 1024:1536], [(Ct[:, tc, fs], PM[:, tc, 1, :]) for tc in range(16)], [Ct, PM], [psA])
                mm(psA[:, 1536:2048], [(St[:, tc, fs], PM[:, tc, 1, :]) for tc in range(16)], [St, PM], [psA])
                kr = kre[fc % 2]
                kq = kim[fc % 2]
                V("dve", lambda e, kr=kr, fc=fc: e.tensor_scalar(out=kr[:], in0=psA[:, 0:512], scalar1=cphi[:, fc:fc + 1], scalar2=None, op0=ALU.mult), [psA, cphi], [kr])
                V("dve", lambda e, kr=kr, fc=fc: e.scalar_tensor_tensor(out=kr[:], in0=psA[:, 512:1024], scalar=sphi[:, fc:fc + 1], in1=kr[:], op0=ALU.mult, op1=ALU.add),
                  [psA, sphi, kr], [kr])
                V("dve", lambda e, kq=kq, fc=fc: e.tensor_scalar(out=kq[:], in0=psA[:, 1536:2048], scalar1=cphi[:, fc:fc + 1], scalar2=None, op0=ALU.mult), [psA, cphi], [kq])
                V("dve", lambda e, kq=kq, fc=fc: e.scalar_tensor_tensor(out=kq[:], in0=psA[:, 1024:1536], scalar=sphi[:, fc:fc + 1], in1=kq[:], op0=ALU.mult, op1=ALU.subtract),
                  [psA, sphi, kq], [kq])
                P.dma(kspec_s[o, 0, fc], kr[:], q="act")
                P.dma(kspec_s[o, 1, fc], kq[:], q="act")
    P.barrier()
    if upto <= 4:
        P.emit()
        hyes.close()
        top.close()
        return nc


    with contextlib.ExitStack() as es:
        hyd = SB(es, [128, 2, 512])
        P.dma(hyd[:].rearrange("p o c -> p (o c)"), hyd_d.rearrange("o c -> (o c)").partition_broadcast(128))
        zin = SB(es, [128, 16, 512], BF16)
        YY = SB(es, [128, 16, 2, 512], BF16)
        kr2 = [SB(es, [128, 2, 512]) for _ in range(2)]
        gt = [SB(es, [128, 512]) for _ in range(2)]
        vst = gt
        u1 = SB(es, [128, 512])
        u2 = SB(es, [128, 512])
        u3 = SB(es, [128, 512])
        ot = [u1, u2]
        psX = [PS(es, [128, 2, 512]) for _ in range(2)]
        psO = [PS(es, [128, 512]) for _ in range(2)]
        for n in range(NS):
            for tc in range(16):
                vt = vst[tc % 2]
                r0 = n * L + tc * 128
                P.dma(vt[:], q_s[r0:r0 + 128, 0:512], q="act" if tc % 2 else "sp")
                V("act" if tc % 2 else "pool", lambda e, vt=vt, tc=tc: (e.copy if hasattr(e, "copy") else e.tensor_copy)(out=zin[:, tc, :], in_=vt[:]), [vt], [zin])
            for o in range(2):
                for fc in range(16):
                    fs = slice(fc * 128, (fc + 1) * 128)
                    px_ = psX[fc % 2]
                    kk = kr2[fc % 2]
                    P.dma(kk[:, 0, :], kspec_s[o, 0, fc], q="sp")
                    P.dma(kk[:, 1, :], kspec_s[o, 1, fc], q="act")
                    mm(px_[:, 0, :], [(Ct[:, tc, fs], zin[:, tc, :]) for tc in range(16)], [Ct, zin], [px_])
                    mm(px_[:, 1, :], [(St[:, tc, fs], zin[:, tc, :]) for tc in range(16)], [St, zin], [px_])
                    V("dve", lambda e, px_=px_, kk=kk: e.tensor_tensor(out=u1[:], in0=px_[:, 0, :], in1=kk[:, 0, :], op=ALU.mult), [px_, kk], [u1])
                    V("dve", lambda e, px_=px_, kk=kk: e.tensor_tensor(out=u2[:], in0=px_[:, 1, :], in1=kk[:, 1, :], op=ALU.mult), [px_, kk], [u2])
                    V("pool", lambda e, fc=fc: e.tensor_tensor(out=YY[:, fc, 0, :], in0=u1[:], in1=u2[:], op=ALU.add), [u1, u2], [YY])
                    V("dve", lambda e, px_=px_, kk=kk: e.tensor_tensor(out=u1[:], in0=px_[:, 1, :], in1=kk[:, 0, :], op=ALU.mult), [px_, kk], [u1])
                    V("dve", lambda e, px_=px_, kk=kk: e.tensor_tensor(out=u2[:], in0=px_[:, 0, :], in1=kk[:, 1, :], op=ALU.mult), [px_, kk], [u2])
                    V("pool", lambda e, fc=fc: e.tensor_tensor(out=YY[:, fc, 1, :], in0=u1[:], in1=u2[:], op=ALU.subtract), [u1, u2], [YY])
                for tc in range(16):
                    ts_ = slice(tc * 128, (tc + 1) * 128)
                    po = psO[tc % 2]
                    g_ = gt[tc % 2]
                    r0 = n * L + tc * 128
                    P.dma(g_[:], q_s[r0:r0 + 128, 512 * (o + 1):512 * (o + 2)], q="sp")
                    pairs = []
                    for fc in range(16):
                        pairs.append((Ct[:, fc, ts_], YY[:, fc, 0, :]))
                        pairs.append((St[:, fc, ts_], YY[:, fc, 1, :]))
                    mm(po[:], pairs, [Ct, St, YY], [po])
                    V("pool", lambda e, tc=tc, o=o: e.tensor_tensor(out=u3[:], in0=zin[:, tc, :], in1=hyd[:, o, :], op=ALU.mult), [zin, hyd], [u3])
                    V("dve", lambda e, po=po: e.scalar_tensor_tensor(out=u3[:], in0=po[:], scalar=2.0 / NFFT, in1=u3[:], op0=ALU.mult, op1=ALU.add), [po, u3], [u3])
                    if o == 0:
                        V("dve", lambda e, tc=tc, g_=g_: e.tensor_tensor(out=zin[:, tc, :], in0=u3[:], in1=g_[:], op=ALU.mult), [u3, g_], [zin])
                    else:
                        o_ = ot[tc % 2]
                        V("dve", lambda e, o_=o_, g_=g_: e.tensor_tensor(out=o_[:], in0=u3[:], in1=g_[:], op=ALU.mult), [u3, g_], [o_])
                        P.dma(z_s[r0:r0 + 128, :], o_[:], q="act")
                        if debug:
                            P.dma(dbg["z"][r0:r0 + 128, :], o_[:], q="act", is_output=True)
    hyes.close()
    P.barrier()
    if upto <= 5:
        P.emit()
        top.close()
        return nc


    NTG = 2
    TSG = NTG * 128
    x1_s = dscr("x1_s", [NS * L, D])
    hx2T_s = dscr("hx2T_s", [NS * L // TS, 128, 8, TS], BF16)
    sc_s = dscr("sc_s", [NS * L // 128, 128, 16 * 128])
    thr_s = dscr("thr_s", [NS * L // 128, 128, 16])
    with contextlib.ExitStack() as es:
        bglu = SB(es, [128, 512])
        gs5 = SB(es, [128, 512])
        ghy = SB(es, [128, 512])
        g2 = SB(es, [128, D])
        gx1 = SB(es, [128, D])
        g2p = SB(es, [128, D])
        sh2 = SB(es, [128, D])
        P.dma(bglu[:], b_glu_d.partition_broadcast(128))
        P.dma(gs5[:], gs5_d.partition_broadcast(128))
        P.dma(ghy[:], ghy_d.partition_broadcast(128))
        P.dma(g2[:], g2_d.partition_broadcast(128))
        hx2T = SB(es, [128, 8, TSG], BF16)
        x1t = [SB(es, [128, D]) for _ in range(NTG)]
        scs = [SB(es, [128, 16, 128]) for _ in range(NTG)]
        c16s = [SB(es, [128, 8, 16]) for _ in range(NTG)]
        thrs = [SB(es, [128, 16]) for _ in range(NTG)]
        nbs = [t_[:, 0:8] for t_ in thrs]
        ntaus = [t_[:, 8:16] for t_ in thrs]
        woutb = SB(es, [128, 8, 1024], BF16)
        wglub = SB(es, [128, 4, 512], BF16)
        wqb = SB(es, [128, 8, 2048], BF16)
        kTb = SB(es, [128, 16, 128], BF16)
        P.dma(woutb[:], wout_b[:, :, :], q="sp")
        P.dma(wglub[:], wglu_b[:, :, :], q="act")
        P.dma(wqb[:], wq_b[:, :, :], q="sp")
        P.dma(kTb[:], kT_b[:, :, :], q="act")
        qT = SB(es, [128, 16, TSG], BF16)
        yts = [SB(es, [128, 512]) for _ in range(NTG)]
        zts = [SB(es, [128, 512]) for _ in range(NTG)]
        xt4s = [SB(es, [128, D]) for _ in range(NTG)]
        gys = [SB(es, [128, 512]) for _ in range(NTG)]
        gybs = [SB(es, [128, 512], BF16) for _ in range(NTG)]
        gyTs = [SB(es, [128, 4, 128], BF16) for _ in range(NTG)]
        gpres = [SB(es, [128, 512]) for _ in range(NTG)]
        s5os = [SB(es, [128, 512]) for _ in range(NTG)]
        catbs = [SB(es, [128, D], BF16) for _ in range(NTG)]
        catTs = [SB(es, [128, 8, 128], BF16) for _ in range(NTG)]
        tms = [SB(es, [128, D]) for _ in range(NTG)]
        hb4s = [SB(es, [128, D], BF16) for _ in range(NTG)]
        jks = [SB(es, [128, D], BF16) for _ in range(NTG)]
        sss = [[SB(es, [128, 1]) for _ in range(6)] for _ in range(NTG)]
        wks = [SB(es, [128, 256]) for _ in range(NTG)]
        m16s = [SB(es, [128, 16, 16]) for _ in range(NTG)]
        cands = [SB(es, [128, 8, 256]) for _ in range(NTG)]
        ews = [SB(es, [128, 8, 16]) for _ in range(NTG)]
        zss = [SB(es, [128, 8]) for _ in range(NTG)]
        ptrs = [PS(es, [128, 8, 128], BF16) for _ in range(2)]
        pAB = [PS(es, [128, 512]) for _ in range(2)]
        pMs = [PS(es, [128, D]) for _ in range(2)]
        for g_i in range(NS * L // TSG):
            n = (g_i * TSG) // L
            row0 = g_i * TSG
            if (g_i * TSG) % L == 0:
                P.dma(gx1[:], modx_d[n, :, 2 * D:3 * D])
                P.dma(sh2[:], modx_d[n, :, 3 * D:4 * D])
                P.dma(g2p[:], modx_d[n, :, 4 * D:5 * D])
                V("dve", lambda e: e.scalar_tensor_tensor(out=g2p[:], in0=g2p[:], scalar=1.0, in1=g2[:], op0=ALU.add, op1=ALU.mult), [g2p, g2], [g2p])
            TI = list(range(NTG))

            def ctx(ti):
                return dict(r0=row0 + ti * 128, x1=x1t[ti], yt=yts[ti], zt=zts[ti], xt4=xt4s[ti], gy=gys[ti], gyb=gybs[ti], gyT=gyTs[ti], gpre=gpres[ti],
                            s5o=s5os[ti], catb=catbs[ti], catT=catTs[ti], tm=tms[ti], hb4=hb4s[ti], jk=jks[ti], ss=sss[ti], ptr=ptrs[ti % 2], pG=pAB[ti % 2], pM=pMs[ti % 2])

            for ti in TI:
                c_ = ctx(ti); r0, yt, zt, xt4, gy, gyb = c_["r0"], c_["yt"], c_["zt"], c_["xt4"], c_["gy"], c_["gyb"]
                P.dma(yt[:], y_s[r0:r0 + 128, :], q="sp")
                P.dma(zt[:], z_s[r0:r0 + 128, :], q="act")
                P.dma(xt4[:], x_d[r0:r0 + 128, :], q="sp")
                V("act", lambda e: e.activation(out=gy[:], in_=yt[:], func=AF.Gelu_apprx_tanh), [yt], [gy])
                V("pool", lambda e: e.tensor_copy(out=gyb[:], in_=gy[:]), [gy], [gyb])
            for ti in TI:
                c_ = ctx(ti); gyb, ptr, gyT, pG = c_["gyb"], c_["ptr"], c_["gyT"], c_["pG"]
                for k in range(4):
                    P.op("pe", lambda e, k=k: e.transpose(out=ptr[:, k, :], in_=gyb[:, k * 128:(k + 1) * 128], identity=ident[:]), reads=[gyb, ident], writes=[ptr])
                V("act", lambda e: e.copy(out=gyT[:], in_=ptr[:, 0:4, :]), [ptr], [gyT])
            for ti in TI:
                c_ = ctx(ti); gyT, pG = c_["gyT"], c_["pG"]
                mm(pG[:], [(gyT[:, k, :], wglub[:, k, :]) for k in range(4)], [gyT, wglub], [pG])
            for ti in TI:
                c_ = ctx(ti); pG, gpre = c_["pG"], c_["gpre"]
                V("dve", lambda e: e.tensor_tensor(out=gpre[:], in0=pG[:], in1=bglu[:], op=ALU.add), [pG, bglu], [gpre])
                V("act", lambda e: e.activation(out=gpre[:], in_=gpre[:], func=AF.Sigmoid), [gpre], [gpre])
            for ti in TI:
                c_ = ctx(ti); gy, gpre, s5o, jk, zt = c_["gy"], c_["gpre"], c_["s5o"], c_["jk"], c_["zt"]
                ss1, ss2, ss3, rs1, rs2, rs3 = c_["ss"]
                V("dve", lambda e: e.tensor_tensor(out=s5o[:], in0=gy[:], in1=gpre[:], op=ALU.mult), [gy, gpre], [s5o])
                V("act", lambda e: e.activation(out=jk[:, 0:512], in_=s5o[:], func=AF.Square, accum_out=ss1[:]), [s5o], [jk, ss1])
                V("act", lambda e: e.activation(out=jk[:, 512:1024], in_=zt[:], func=AF.Square, accum_out=ss2[:]), [zt], [jk, ss2])
            for ti in TI:
                c_ = ctx(ti)
                ss1, ss2, ss3, rs1, rs2, rs3 = c_["ss"]
                rsqrt_mean(es, ss1, 512, rs1)
                rsqrt_mean(es, ss2, 512, rs2)
            for ti in TI:
                c_ = ctx(ti); s5o, zt, catb = c_["s5o"], c_["zt"], c_["catb"]
                ss1, ss2, ss3, rs1, rs2, rs3 = c_["ss"]
                V("dve", lambda e: e.scalar_tensor_tensor(out=catb[:, 0:512], in0=s5o[:], scalar=rs1[:, 0:1], in1=gs5[:], op0=ALU.mult, op1=ALU.mult),
                  [s5o, rs1, gs5], [catb])
                V("dve", lambda e: e.scalar_tensor_tensor(out=catb[:, 512:1024], in0=zt[:], scalar=rs2[:, 0:1], in1=ghy[:], op0=ALU.mult, op1=ALU.mult),
                  [zt, rs2, ghy], [catb])
            for ti in TI:
                c_ = ctx(ti); catb, ptr, catT = c_["catb"], c_["ptr"], c_["catT"]
                for k in range(8):
                    P.op("pe", lambda e, k=k: e.transpose(out=ptr[:, k, :], in_=catb[:, k * 128:(k + 1) * 128], identity=ident[:]), reads=[catb, ident], writes=[ptr])
                V("act", lambda e: e.copy(out=catT[:], in_=ptr[:]), [ptr], [catT])
            for ti in TI:
                c_ = ctx(ti); catT, pM = c_["catT"], c_["pM"]
                for hf in range(2):
                    mm(pM[:, hf * 512:(hf + 1) * 512], [(catT[:, k, :], woutb[:, k, hf * 512:(hf + 1) * 512]) for k in range(8)], [catT, woutb], [pM])
            for ti in TI:
                c_ = ctx(ti); pM, tm, x1, xt4, jk, r0 = c_["pM"], c_["tm"], c_["x1"], c_["xt4"], c_["jk"], c_["r0"]
                ss1, ss2, ss3, rs1, rs2, rs3 = c_["ss"]
                V("dve", lambda e: e.tensor_tensor(out=tm[:], in0=pM[:], in1=gx1[:], op=ALU.mult), [pM, gx1], [tm])
                V("pool", lambda e: e.tensor_tensor(out=x1[:], in0=tm[:], in1=xt4[:], op=ALU.add), [tm, xt4], [x1])
                if debug:
                    P.dma(dbg["x1"][r0:r0 + 128, :], x1[:], is_output=True)
                V("act", lambda e: e.activation(out=jk[:], in_=x1[:], func=AF.Square, accum_out=ss3[:]), [x1], [jk, ss3])
            for ti in TI:
                c_ = ctx(ti)
                ss1, ss2, ss3, rs1, rs2, rs3 = c_["ss"]
                rsqrt_mean(es, ss3, D, rs3)
            for ti in TI:
                c_ = ctx(ti); tm, x1, hb4, r0 = c_["tm"], c_["x1"], c_["hb4"], c_["r0"]
                ss1, ss2, ss3, rs1, rs2, rs3 = c_["ss"]
                V("dve", lambda e: e.scalar_tensor_tensor(out=tm[:], in0=x1[:], scalar=rs3[:, 0:1], in1=g2p[:], op0=ALU.mult, op1=ALU.mult),
                  [x1, rs3, g2p], [tm])
                V("pool", lambda e: e.tensor_tensor(out=hb4[:], in0=tm[:], in1=sh2[:], op=ALU.add), [tm, sh2], [hb4])
                if debug:
                    P.dma(dbg["hx2"][r0:r0 + 128, :], hb4[:], is_output=True)
            for ti in TI:
                c_ = ctx(ti); hb4, ptr = c_["hb4"], c_["ptr"]
                for k in range(8):
                    P.op("pe", lambda e, k=k: e.transpose(out=ptr[:, k, :], in_=hb4[:, k * 128:(k + 1) * 128], identity=ident[:]), reads=[hb4, ident], writes=[ptr])
                V("act", lambda e: e.copy(out=hx2T[:, :, ti * 128:(ti + 1) * 128], in_=ptr[:]), [ptr], [hx2T])
            for c in range(16):
                pp = pAB[c % 2]
                mm(pp[:, 0:TSG], [(wqb[:, k, c * 128:(c + 1) * 128], hx2T[:, k, :]) for k in range(8)], [wqb, hx2T], [pp])
                if c % 2:
                    V("act", lambda e: e.copy(out=qT[:, c, :], in_=pp[:, 0:TSG]), [pp], [qT])
                else:
                    V("dve", lambda e: e.tensor_copy(out=qT[:, c, :], in_=pp[:, 0:TSG]), [pp], [qT])
            if not lite:
                for hh in range(4):
                    for ti in TI:
                        sc = scs[ti]
                        ps_ = pAB[ti % 2]
                        for c4 in range(4):
                            c = hh * 4 + c4
                            mm(ps_[:, c4 * 128:(c4 + 1) * 128], [(qT[:, c, ti * 128:(ti + 1) * 128], kTb[:, c, :])], [qT, kTb], [ps_])
                        V("act", lambda e: e.copy(out=sc[:, hh * 4:(hh + 1) * 4, :], in_=ps_[:].rearrange("p (a b) -> p a b", a=4)), [ps_], [sc])
                for c in range(16):
                    for ti in TI:
                        sc, m16 = scs[ti], m16s[ti]
                        V("dve", lambda e: e.max(out=m16[:, c, 0:8], in_=sc[:, c, :]), [sc], [m16])
                    for ti in TI:
                        sc, m16, wk = scs[ti], m16s[ti], wks[ti]
                        V("dve", lambda e: e.match_replace(out=wk[:, 0:128], in_to_replace=m16[:, c, 0:8], in_values=sc[:, c, :], imm_value=-1e30), [sc, m16], [wk])
                    for ti in TI:
                        m16, wk = m16s[ti], wks[ti]
                        V("dve", lambda e: e.max(out=m16[:, c, 8:16], in_=wk[:, 0:128]), [wk], [m16])
                for ti in TI:
                    m16, cand = m16s[ti], cands[ti]
                    m4 = m16[:].rearrange("p (h two) k -> p h two k", two=2)
                    V("dve", lambda e: e.tensor_tensor(out=cand[:].rearrange("p h (a b) -> p h a b", a=16),
                                                       in0=m4[:, :, 0, :].unsqueeze(3).to_broadcast([128, 8, 16, 16]),
                                                       in1=m4[:, :, 1, :].unsqueeze(2).to_broadcast([128, 8, 16, 16]), op=ALU.add), [m16], [cand])
                for h in range(8):
                    for ti in TI:
                        cand, c16 = cands[ti], c16s[ti]
                        V("dve", lambda e: e.max(out=c16[:, h, 0:8], in_=cand[:, h, :]), [cand], [c16])
                    for ti in TI:
                        cand, c16, wk = cands[ti], c16s[ti], wks[ti]
                        V("dve", lambda e: e.match_replace(out=wk[:], in_to_replace=c16[:, h, 0:8], in_values=cand[:, h, :], imm_value=-1e30), [cand, c16], [wk])
                    for ti in TI:
                        c16, wk = c16s[ti], wks[ti]
                        V("dve", lambda e: e.max(out=c16[:, h, 8:16], in_=wk[:]), [wk], [c16])
                for ti in TI:
                    c16, ew, zs, nb = c16s[ti], ews[ti], zss[ti], nbs[ti]
                    V("dve", lambda e: e.tensor_tensor(out=ew[:], in0=c16[:], in1=c16[:, :, 0:1].to_broadcast([128, 8, 16]), op=ALU.subtract), [c16], [ew])
                    V("act", lambda e: e.activation(out=ew[:], in_=ew[:], func=AF.Exp), [ew], [ew])
                for ti in TI:
                    c16, ew, zs, nb = c16s[ti], ews[ti], zss[ti], nbs[ti]
                    V("dve", lambda e: e.tensor_reduce(out=zs[:], in_=ew[:], axis=AX.X, op=ALU.add), [ew], [zs])
                    V("act", lambda e: e.activation(out=zs[:], in_=zs[:], func=AF.Ln), [zs], [zs])
                for ti in TI:
                    c16, ew, zs, nb = c16s[ti], ews[ti], zss[ti], nbs[ti]
                    V("dve", lambda e: e.tensor_tensor(out=nb[:], in0=zs[:], in1=c16[:, :, 0], op=ALU.add), [zs, c16], [nb])
                    ntau_ = ntaus[ti]
                    V("dve", lambda e: e.tensor_scalar(out=ntau_[:], in0=c16[:, :, 15], scalar1=-1e-5, scalar2=None, op0=ALU.add), [c16], [ntau_])
                    V("dve", lambda e: e.tensor_tensor(out=nb[:], in0=ntau_[:], in1=nb[:], op=ALU.subtract), [nb, ntau_], [nb])
                    sc = scs[ti]
                    for h in range(8):
                        V("dve", lambda e: e.tensor_scalar(out=sc[:, 2 * h, :], in0=sc[:, 2 * h, :], scalar1=ntau_[:, h:h + 1], scalar2=None, op0=ALU.subtract),
                          [sc, ntau_], [sc])
            for ti in TI:
                r0 = row0 + ti * 128
                P.dma(x1_s[r0:r0 + 128, :], x1t[ti][:], q="sp")
                if not lite:
                    P.dma(sc_s[r0 // 128], scs[ti][:].rearrange("p a b -> p (a b)"), q="act")
                    P.dma(thr_s[r0 // 128], thrs[ti][:], q="sp")
            for hf2 in range(TSG // TS):
                P.dma(hx2T_s[row0 // TS + hf2], hx2T[:, :, hf2 * TS:(hf2 + 1) * TS], q="act")
    P.barrier()
    if not lite:
      with contextlib.ExitStack() as es:
        gx2 = SB(es, [128, D])
        gfin = SB(es, [128, D])
        P.dma(gfin[:], gf_d.partition_broadcast(128))
        hx2Tb = [SB(es, [128, 8, TS], BF16) for _ in range(2)]
        scsb = [[SB(es, [128, 16, 128]) for _ in range(NT)] for _ in range(2)]
        thrb = [[SB(es, [128, 16]) for _ in range(NT)] for _ in range(2)]
        x1t = [SB(es, [128, D]) for _ in range(NT)]
        NB_G = 4
        Sb = [SB(es, [128, 16, 128]) for _ in range(NB_G)]
        Eb = [SB(es, [128, 16, 128], BF16) for _ in range(NB_G)]
        Mb = [SB(es, [128, 16, 128], BF16) for _ in range(NB_G)]
        Gh = Eb
        Gc = [[SB(es, [128, 2048], BF16) for _ in range(2)] for _ in range(NT)]
        uTc = [SB(es, [128, 8, 512], BF16) for _ in range(2)]
        vc = [SB(es, [128, 4, 1024], BF16) for _ in range(3)]
        actb = [SB(es, [128, 512], BF16) for _ in range(2)]
        wab = [SB(es, [128, 512], BF16) for _ in range(2)]
        waT = [SB(es, [128, 4, 128], BF16) for _ in range(2)]
        tm2 = SB(es, [128, D])
        x2 = SB(es, [128, D])
        jk2 = SB(es, [128, D], BF16)
        ss4 = SB(es, [128, 1])
        rs4 = SB(es, [128, 1])
        ob = [SB(es, [128, D]) for _ in range(1)]
        pO = [PS(es, [128, D]) for _ in range(NT)]
        pA = [PS(es, [128, 512]) for _ in range(2)]
        pW = [PS(es, [128, 4, 128], BF16) for _ in range(2)]
        cnt = {"g": 0, "d": 0}
        for st_i in range(NS * L // TS):
            n = (st_i * TS) // L
            row0 = st_i * TS
            par = st_i % 2
            if (st_i * TS) % L == 0:
                P.dma(gx2[:], modx_d[n, :, 5 * D:6 * D])
            def srcs_of(p_):
                return (scsb[p_], [t_[:, 0:8] for t_ in thrb[p_]], [t_[:, 8:16] for t_ in thrb[p_]])

            def preload(si):
                p_ = si % 2
                P.dma(hx2Tb[p_][:], hx2T_s[si], q="sp")
                for ti in range(NT):
                    r0_ = si * TS + ti * 128
                    P.dma(scsb[p_][ti][:].rearrange("p a b -> p (a b)"), sc_s[r0_ // 128], q="act" if ti else "sp")
                    P.dma(thrb[p_][ti][:], thr_s[r0_ // 128], q="sp")

            if st_i == 0:
                preload(0)
            if st_i + 1 < NS * L // TS:
                preload(st_i + 1)
            hx2T = hx2Tb[par]
            for ti in range(NT):
                r0 = row0 + ti * 128
                P.dma(x1t[ti][:], x1_s[r0:r0 + 128, :], q="act")
            cnt_unused = None

            def g_pair(ic, ulist, srcs):
                scs_, nbs_, ntaus_ = srcs
                st = []
                for (ti, h) in ulist:
                    i3 = cnt["g"] % NB_G
                    cnt["g"] += 1
                    st.append((ti, h, Sb[i3], Eb[i3], Mb[i3], Gh[i3]))
                for (ti, h, S_, E_, M_, G_) in st:
                    sc = scs_[ti]
                    V("pool", lambda e: e.tensor_tensor(out=S_[:], in0=sc[:, 2 * h, ic * 16:(ic + 1) * 16].unsqueeze(2).to_broadcast([128, 16, 128]),
                                                        in1=sc[:, 2 * h + 1, :].unsqueeze(1).to_broadcast([128, 16, 128]), op=ALU.add), [sc], [S_])
                for (ti, h, S_, E_, M_, G_) in st:
                    if ti == 1 and h % 2 == 1:
                        V("dve", lambda e: e.scalar_tensor_tensor(out=S_[:].rearrange("p a b -> p (a b)"), in0=S_[:].rearrange("p a b -> p (a b)"), scalar=KMASK,
                                                                  in1=S_[:].rearrange("p a b -> p (a b)"), op0=ALU.mult, op1=ALU.min), [S_], [S_])
                    else:
                        V("act", lambda e: e.activation(out=S_[:], in_=S_[:], func=AF.Prelu, alpha=KMASK), [S_], [S_])
                for (ti, h, S_, E_, M_, G_) in st:
                    nb = nbs_[ti]
                    gdst = Gc[ti][ic % 2]
                    if h == 0:
                        V("act", lambda e: e.activation(out=gdst[:], in_=S_[:].rearrange("p a b -> p (a b)"), func=AF.Exp, bias=nb[:, h:h + 1]), [S_, nb], [gdst])
                    else:
                        V("act", lambda e: e.activation(out=E_[:], in_=S_[:], func=AF.Exp, bias=nb[:, h:h + 1]), [S_, nb], [E_])
                for (ti, h, S_, E_, M_, G_) in st:
                    gdst = Gc[ti][ic % 2]
                    if h != 0:
                        V("dve", lambda e: e.tensor_tensor(out=gdst[:], in0=gdst[:], in1=E_[:].rearrange("p a b -> p (a b)"), op=ALU.add), [E_, gdst], [gdst])

            def item_ctx(j):
                ic, r = divmod(j, 2 * 4)
                e4, ti = divmod(r, NT)
                ec = ic * 4 + e4
                return dict(ic=ic, e4=e4, ti=ti, ec=ec, u_=uTc[ec % 2], v_=vc[ec % 3], pa=pA[j % 2], a_=actb[j % 2], w_=wab[j % 2], wT=waT[j % 2], pw=pW[j % 2],
                            gsrc=Gc[ti][ic % 2])

            def D1(j):
                c_ = item_ctx(j); ec, ti, u_, v_, pa = c_["ec"], c_["ti"], c_["u_"], c_["v_"], c_["pa"]
                if ti == 0:
                    P.dma(u_[:], uTb_s[ec], q="sp")
                    P.dma(v_[:], vb_s[ec], q="act")
                mm(pa[:], [(hx2T[:, k, ti * 128:(ti + 1) * 128], u_[:, k, :]) for k in range(8)], [hx2T, u_], [pa])

            def D2(j):
                c_ = item_ctx(j); pa, a_ = c_["pa"], c_["a_"]
                V("act", lambda e: e.activation(out=a_[:], in_=pa[:], func=AF.Gelu_apprx_tanh), [pa], [a_])

            def D3(j):
                c_ = item_ctx(j); a_, w_, gsrc, e4 = c_["a_"], c_["w_"], c_["gsrc"], c_["e4"]
                V("dve", lambda e: e.tensor_tensor(out=w_[:], in0=a_[:], in1=gsrc[:, e4 * 512:(e4 + 1) * 512], op=ALU.mult), [a_, gsrc], [w_])

            def D4(j):
                c_ = item_ctx(j); w_, pw = c_["w_"], c_["pw"]
                for eb in range(4):
                    P.op("pe", lambda e, eb=eb: e.transpose(out=pw[:, eb, :], in_=w_[:, eb * 128:(eb + 1) * 128], identity=ident[:]), reads=[w_, ident], writes=[pw])

            def D5(j):
                c_ = item_ctx(j); pw, wT = c_["pw"], c_["wT"]
                V("act", lambda e: e.copy(out=wT[:], in_=pw[:]), [pw], [wT])

            def D6(j):
                c_ = item_ctx(j); ec, ti, wT, v_ = c_["ec"], c_["ti"], c_["wT"], c_["v_"]
                for hf in range(2):
                    for eb in range(4):
                        first = (ec == 0 and eb == 0)
                        last = (ec == NCH - 1 and eb == 3)
                        P.op("pe", lambda e, hf=hf, eb=eb, first=first, last=last:
                             e.matmul(pO[ti][:, hf * 512:(hf + 1) * 512], lhsT=wT[:, eb, :], rhs=v_[:, eb, hf * 512:(hf + 1) * 512], start=first, stop=last),
                             reads=[wT, v_], writes=[pO[ti]])

            units = [(ti, h) for h in range(8) for ti in range(NT)]
            NJ = 64
            upi = 2
            if st_i == 0:
                for j in range(8):
                    g_pair(0, units[j * upi:(j + 1) * upi], srcs_of(0))
            def ok(j):
                return 0 <= j < NJ

            for s in range(NJ + 3):
                if ok(s - 2):
                    D4(s - 2)
                if s % 2 == 0 and ok(s - 1):
                    D2(s - 1)
                    D3(s - 1)
                if s < NJ:
                    ic, r = divmod(s, 8)
                    if ic + 1 < 8:
                        g_pair(ic + 1, units[r * upi:(r + 1) * upi], srcs_of(par))
                    elif st_i + 1 < NS * L // TS:
                        g_pair(0, units[r * upi:(r + 1) * upi], srcs_of(1 - par))
                if s % 2 == 1 and ok(s - 1):
                    D2(s - 1)
                    D3(s - 1)
                if ok(s - 2):
                    D5(s - 2)
                if ok(s - 3):
                    D6(s - 3)
                if s < NJ:
                    D1(s)
            for ti in range(NT):
                r0 = row0 + ti * 128
                x1 = x1t[ti]
                o_ = ob[0]
                if debug:
                    V("act", lambda e: e.copy(out=tm2[:], in_=pO[ti][:]), [pO[ti]], [tm2])
                    P.dma(dbg["pe"][r0:r0 + 128, :], tm2[:], is_output=True)
                V("dve", lambda e: e.tensor_tensor(out=tm2[:], in0=pO[ti][:], in1=gx2[:], op=ALU.mult), [pO[ti], gx2], [tm2])
                V("pool", lambda e: e.tensor_tensor(out=x2[:], in0=tm2[:], in1=x1[:], op=ALU.add), [tm2, x1], [x2])
                V("act", lambda e: e.activation(out=jk2[:], in_=x2[:], func=AF.Square, accum_out=ss4[:]), [x2], [jk2, ss4])
                rsqrt_mean(es, ss4, D, rs4)
                V("dve", lambda e: e.scalar_tensor_tensor(out=o_[:], in0=x2[:], scalar=rs4[:, 0:1], in1=gfin[:], op0=ALU.mult, op1=ALU.mult),
                  [x2, rs4, gfin], [o_])
                P.dma(out_d[r0:r0 + 128, :], o_[:], q="sp", is_output=True)

    P.emit()
    top.close()
    return nc


def _prep_inputs(inp):
    global CONST
    if CONST is None:
        CONST = _constants()
    f = lambda a: np.ascontiguousarray(np.asarray(a, dtype=np.float32))
    shared = {}
    shared["w_ada"] = f(inp["w_ada"][0])
    shared["b_ada"] = f(inp["b_ada"][0])
    shared["g_norm1"] = f(inp["g_norm1"][0])
    shared["g_norm2"] = f(inp["g_norm2"][0])
    shared["w_in"] = f(inp["w_in"][0])
    shared["b_in"] = f(inp["b_in"][0])
    a_re = np.asarray(inp["s5_a_re"][0]).reshape(64, 64).T
    a_im = np.asarray(inp["s5_a_im"][0]).reshape(64, 64).T
    ls = np.broadcast_to(np.asarray(inp["s5_log_step"][0]).reshape(1, 64), (64, 64))
    shared["s5p"] = f(np.stack([a_re, a_im, ls], axis=1))
    bre = np.asarray(inp["s5_b_re"][0]).reshape(64, 64, 16).transpose(1, 0, 2)
    bim = np.asarray(inp["s5_b_im"][0]).reshape(64, 64, 16).transpose(1, 0, 2)
    shared["s5b"] = f(np.stack([bre, bim], axis=1))
    cre = np.asarray(inp["s5_c_re"][0]).reshape(64, 16, 64).transpose(2, 0, 1)
    cim = np.asarray(inp["s5_c_im"][0]).reshape(64, 16, 64).transpose(2, 0, 1)
    shared["s5c"] = f(np.stack([cre, cim], axis=1))
    d5 = np.asarray(inp["s5_d"][0]).reshape(32, 16)
    shared["s5d"] = f(np.broadcast_to(d5.T[None], (8, 16, 32)).reshape(128, 32))
    shared["w_glu"] = f(inp["w_glu"][0])
    shared["b_glu"] = f(inp["b_glu"][0])
    shared["hy_conv_w"] = f(inp["hy_conv_w"][0])
    shared["hy_conv_b"] = f(inp["hy_conv_b"][0])
    shared["hf_w1"] = f(inp["hf_w1"][0])
    shared["hf_b1"] = f(np.asarray(inp["hf_b1"][0]).reshape(64, 1))
    shared["hf_wh"] = f(inp["hf_wh"][0])
    shared["hf_bh"] = f(np.asarray(inp["hf_bh"][0]).T)
    shared["hf_freq"] = f(np.asarray(inp["hf_freq"][0]).reshape(64, 1))
    shared["hf_wout"] = f(inp["hf_wout"][0])
    shared["hf_decay"] = f(inp["hf_decay"][0])
    shared["hy_d"] = f(inp["hy_d"][0])
    shared["g_out_s5"] = f(inp["g_out_s5"][0])
    shared["g_out_hy"] = f(inp["g_out_hy"][0])
    shared["w_out"] = f(inp["w_out"][0])
    shared["peer_wq"] = f(inp["peer_wq"][0])
    k1 = np.asarray(inp["peer_k1"][0])
    k2 = np.asarray(inp["peer_k2"][0])
    kk = np.stack([k1, k2], axis=1).reshape(16, 128, 128)
    shared["peer_kT"] = f(kk.transpose(2, 0, 1))
    shared["peer_uT"] = f(np.asarray(inp["peer_u"][0]).T)
    shared["peer_v"] = f(inp["peer_v"][0])
    shared["g_final"] = f(inp["g_final"])
    for k, v in CONST.items():
        shared["k_" + k] = v
    maps = []
    x = np.asarray(inp["x"])
    ctx = np.asarray(inp["ctx"])
    c = np.asarray(inp["c"])
    cc = np.asarray(inp["c_ctx"])
    for i in range(NCORES):
        m = dict(shared)
        m["x"] = f(x[i * NS:(i + 1) * NS].reshape(NS * L, D))
        m["ctx"] = f(ctx[i * NS:(i + 1) * NS].reshape(NS * LC, D))
        c5 = np.concatenate([c[i * NS:(i + 1) * NS], cc[None]], axis=0)
        m["cT"] = f(c5.reshape(NS + 1, 8, 128).transpose(2, 1, 0))
        maps.append(m)
    return maps


def kernel(**inputs):
    maps = _prep_inputs(inputs)
    nc = build_program()
    res = run_bass_kernel_spmd(nc, maps, core_ids=list(range(NCORES)))
    out = np.concatenate([np.asarray(r["out"]).reshape(NS, L, D) for r in res.results], axis=0)
    return out.astype(np.float32)
```

```python
import contextlib
import math
import types
import numpy as np
import ml_dtypes
import concourse.bass as bass
import concourse.mybir as mybir
from concourse.bass_utils import run_bass_kernel_spmd

F32 = mybir.dt.float32
BF16 = mybir.dt.bfloat16
I32 = mybir.dt.int32
ALU = mybir.AluOpType
AF = mybir.ActivationFunctionType
AX = mybir.AxisListType

NSLOT = 12
NCORES = 8
NS = 4
L = 2048
D = 1024
LC = 256
EPS = 1e-6
NFFT = 4096


def _freeze(fn):
    if fn.__closure__ is None:
        return fn
    cells = []
    for c in fn.__closure__:
        try:
            cells.append(types.CellType(c.cell_contents))
        except ValueError:
            cells.append(c)
    return types.FunctionType(fn.__code__, fn.__globals__, fn.__name__, fn.__defaults__, tuple(cells))


class Prog:
    DMAQ = ("sp", "act", "pool")

    def __init__(self, nc):
        self.nc = nc
        self.streams = {k: [] for k in ("pe", "act", "dve", "pool", "sp")}
        self.cnt = {}
        self.waited = {k: {} for k in self.streams}
        self.lastw = {}
        self.reads = {}
        self.slot_next = {q: 0 for q in self.DMAQ}
        self.ninst = 0
        self.out_deps = []

    def _key(self, x):
        if isinstance(x, (str, tuple)):
            return x
        t = getattr(x, "tensor", x)
        return getattr(t, "name", None) or id(t)

    def _deps(self, reads, writes):
        deps = {}

        def add(s, c):
            if c > deps.get(s, 0):
                deps[s] = c
        for k in reads:
            lw = self.lastw.get(k)
            if lw:
                add(*lw)
        for k in writes:
            lw = self.lastw.get(k)
            if lw:
                add(*lw)
            for s, c in self.reads.get(k, {}).items():
                add(s, c)
        return deps

    def _emit_waits(self, stream, deps):
        w = self.waited[stream]
        for s, c in deps.items():
            if s == stream and stream == "pe":
                continue
            if c > w.get(s, 0):
                w[s] = c
                self.streams[stream].append(("wait", s, c))

    def _commit(self, sem, cnt, reads, writes):
        for k in writes:
            self.lastw[k] = (sem, cnt)
            self.reads[k] = {}
        for k in reads:
            self.reads.setdefault(k, {})
            if self.reads[k].get(sem, 0) < cnt:
                self.reads[k][sem] = cnt

    def op(self, stream, fn, reads=(), writes=()):
        reads = [self._key(r) for r in reads]
        writes = [self._key(r) for r in writes]
        deps = self._deps(reads, writes)
        self._emit_waits(stream, deps)
        c = self.cnt.get(stream, 0) + 1
        self.cnt[stream] = c
        self.streams[stream].append(("op", _freeze(fn), stream, 1))
        self._commit(stream, c, reads, writes)
        self.ninst += 1

    def dma(self, out, in_, reads=None, writes=None, q="sp", is_output=False, **kw):
        reads = [self._key(r) for r in (reads if reads is not None else [in_])]
        writes = [self._key(r) for r in (writes if writes is not None else [out])]
        slot = self.slot_next[q]
        self.slot_next[q] = (slot + 1) % NSLOT
        sem = ("dma", q, slot)
        deps = self._deps(reads, writes)
        prev = self.cnt.get(sem, 0)
        if prev:
            deps[sem] = max(deps.get(sem, 0), prev)
        self._emit_waits(q, deps)
        c = prev + 16
        self.cnt[sem] = c
        self.streams[q].append(("dma", out, in_, kw, sem))
        self._commit(sem, c, reads, writes)
        if is_output:
            self.out_deps.append((sem, c))
        self.ninst += 1

    def barrier(self):
        allc = dict(self.cnt)
        for st in self.streams:
            self._emit_waits(st, allc)

    def emit(self):
        nc = self.nc
        fin = {}
        for s, c in self.out_deps:
            fin[s] = max(fin.get(s, 0), c)
        self._emit_waits("sp", fin)
        semkeys = list(self.cnt.keys())
        with contextlib.ExitStack() as es:
            sems = {}
            for i, k in enumerate(semkeys):
                sems[k] = es.enter_context(nc.semaphore("s%d" % i))
            block = es.enter_context(nc.Block())
            engs = {"pe": block.tensor, "act": block.scalar, "dve": block.vector,
                    "pool": block.gpsimd, "sp": block.sync}

            def make(stream):
                items = self.streams[stream]

                def body(eng):
                    for it in items:
                        if it[0] == "wait":
                            eng.wait_ge(sems[it[1]], it[2])
                        elif it[0] == "op":
                            it[1](eng).then_inc(sems[it[2]], 1)
                        else:
                            _, out, in_, kw, sem = it
                            eng.dma_start(out=out, in_=in_, **kw).then_inc(sems[sem], 16)
                return body
            for st in ("sp", "act", "pool", "dve", "pe"):
                if self.streams[st]:
                    engs[st](make(st))


def _constants():
    c = {}
    c["ident"] = np.eye(128, dtype=np.float32).astype(ml_dtypes.bfloat16)
    c["identf"] = np.eye(128, dtype=np.float32)
    sm = np.zeros((128, 128), np.float32)
    sp = np.zeros((128, 128), np.float32)
    for t in range(128):
        if t % 64 != 0:
            sm[t - 1, t] = 1
        if t % 64 != 63:
            sp[t + 1, t] = 1
    c["shm"] = sm.astype(ml_dtypes.bfloat16)
    c["shp"] = sp.astype(ml_dtypes.bfloat16)
    t = np.arange(L, dtype=np.float64) + 0.5
    ang = 2 * np.pi * np.outer(t, t) / NFFT
    c["ctab"] = np.cos(ang).astype(np.float32).astype(ml_dtypes.bfloat16)
    c["stab"] = np.sin(ang).astype(np.float32).astype(ml_dtypes.bfloat16)
    phi = np.pi * (np.arange(L, dtype=np.float64) + 0.5) / NFFT
    c["cphi"] = np.cos(phi).astype(np.float32).reshape(16, 128).T.copy()
    c["sphi"] = np.sin(phi).astype(np.float32).reshape(16, 128).T.copy()
    pos = np.arange(L, dtype=np.float32)
    tt = pos / np.float32(L - 1)
    bands = np.linspace(1e-4, 15, 16, dtype=np.float32)
    a = (np.float32(2.0 * math.pi / L) * pos[:, None]) * bands[None, :]
    feats = np.concatenate([tt[:, None], np.cos(a), -np.sin(a)], axis=-1).astype(np.float32)
    c["featsT"] = feats.T.copy()
    c["tneg"] = (-tt).reshape(16, 128).T.copy()
    j = np.repeat(np.arange(8), 16).astype(np.float32)
    ex = np.stack([7 - j, j, j + 1, 8 - j, j - 7, -j])
    c["s5exp"] = np.broadcast_to(ex[None], (64, 6, 128)).astype(np.float32).copy()
    jj = np.repeat(np.arange(8), 16)
    c["mf"] = (jj[None, :] >= jj[:, None]).astype(np.float32)
    c["mb"] = (jj[:, None] >= jj[None, :]).astype(np.float32)
    m0 = np.ones((128, 1), np.float32)
    m0[0, 0] = 0.0
    c["m0"] = m0
    c["onesf"] = np.ones((128, 128), np.float32)
    c["mix"] = np.broadcast_to(np.arange(256, dtype=np.float32)[None], (64, 256)).copy()
    return c


CONST = None


def build_program(upto=99, debug=False, lite=False):
    nc = bass.Bass("TRN2", target_bir_lowering=False)
    P = Prog(nc)
    dbg = {}

    def din(name, shape, dt=F32):
        return nc.dram_tensor(name, list(shape), dt, kind="ExternalInput").ap()

    def dscr(name, shape, dt=F32):
        return nc.dram_tensor(name, list(shape), dt).ap()

    def dout(name, shape, dt=F32):
        return nc.dram_tensor(name, list(shape), dt, kind="ExternalOutput").ap()

    x_d = din("x", [NS * L, D])
    ctx_d = din("ctx", [NS * LC, D])
    cT_d = din("cT", [128, 8, NS + 1])
    w_ada = din("w_ada", [D, 6 * D])
    b_ada = din("b_ada", [6 * D])
    g1_d = din("g_norm1", [D])
    g2_d = din("g_norm2", [D])
    w_in_d = din("w_in", [D, 2048])
    b_in_d = din("b_in", [2048])
    s5p_d = din("s5p", [64, 3, 64])
    s5b_d = din("s5b", [64, 2, 64, 16])
    s5c_d = din("s5c", [64, 2, 64, 16])
    s5d_d = din("s5d", [128, 32])
    w_glu_d = din("w_glu", [512, 512])
    b_glu_d = din("b_glu", [512])
    cw_d = din("hy_conv_w", [3, 1536])
    cb_d = din("hy_conv_b", [1536])
    hf_w1 = din("hf_w1", [33, 64])
    hf_b1 = din("hf_b1", [64, 1])
    hf_wh = din("hf_wh", [2, 64, 64])
    hf_bh = din("hf_bh", [64, 2])
    hf_fr = din("hf_freq", [64, 1])
    hf_wout = din("hf_wout", [64, 2048])
    hf_dec = din("hf_decay", [2048])
    hyd_d = din("hy_d", [2, 512])
    gs5_d = din("g_out_s5", [512])
    ghy_d = din("g_out_hy", [512])
    w_out_d = din("w_out", [D, D])
    wq_d = din("peer_wq", [D, 2048])
    kT_d = din("peer_kT", [128, 16, 128])
    uT_d = din("peer_uT", [D, 16384 if not lite else 128])
    v_d = din("peer_v", [16384 if not lite else 128, D])
    gf_d = din("g_final", [D])
    cst = {k: din("k_" + k, list(v.shape), BF16 if v.dtype == ml_dtypes.bfloat16 else F32)
           for k, v in CONST.items()}
    out_d = dout("out", [NS * L, D])

    modx_d = dscr("modx_s", [NS + 1, 128, 6 * D])
    q_s = dscr("q_s", [NS * L, 1536])
    y_s = dscr("y_s", [NS * L, 512])
    z_s = dscr("z_s", [NS * L, 512])
    kspec_s = dscr("kspec_s", [2, 2, 16, 128, 512])
    uTb_s = dscr("uTb_s", [32, 128, 8, 512], BF16)
    vb_s = dscr("vb_s", [32, 128, 4, 1024], BF16)

    if debug:
        dbg["modx"] = dout("dbg_modx", [NS + 1, 6 * D])

    top = contextlib.ExitStack()
    uid = [0]

    def SB(es, shape, dt=F32, name=None):
        uid[0] += 1
        return es.enter_context(nc.sbuf_tensor((name or "t") + str(uid[0]), list(shape), dt))

    def PS(es, shape, dt=F32, name=None):
        uid[0] += 1
        return es.enter_context(nc.psum_tensor((name or "p") + str(uid[0]), list(shape), dt))

    def mm(ps_ap, pairs, reads, writes):
        n = len(pairs)
        for i, (l, r) in enumerate(pairs):
            P.op("pe", lambda e, l=l, r=r, i=i: e.matmul(ps_ap, lhsT=l, rhs=r, start=(i == 0), stop=(i == n - 1)),
                 reads=reads, writes=writes)

    def V(eng, fn, reads, writes):
        P.op(eng, fn, reads=reads, writes=writes)

    ident = SB(top, [128, 128], BF16, "ident")
    identf = SB(top, [128, 128], F32, "identf")
    mhalf = SB(top, [128, 1], F32, "mhalf")
    P.dma(ident[:], cst["ident"][:, :])
    P.dma(identf[:], cst["identf"][:, :])
    V("pool", lambda e: e.memset(mhalf[:], -0.5), [], [mhalf])

    def rsqrt_mean(es, ssum, n, out_rstd):
        V("dve", lambda e: e.tensor_scalar(out=out_rstd[:], in0=ssum[:], scalar1=1.0 / n, scalar2=EPS,
                                           op0=ALU.mult, op1=ALU.add), [ssum], [out_rstd])
        V("pool", lambda e: e.tensor_tensor(out=out_rstd[:], in0=out_rstd[:], in1=mhalf[:], op=ALU.pow),
          [out_rstd, mhalf], [out_rstd])

    with contextlib.ExitStack() as es:
        cT = SB(es, [128, 8, NS + 1])
        sil = SB(es, [128, 8, NS + 1])
        rep = SB(es, [128, NS + 1, 8, 128])
        bada = SB(es, [128, 6 * D])
        P.dma(cT[:], cT_d[:, :, :])
        P.dma(bada[:], b_ada.partition_broadcast(128))
        V("act", lambda e: e.activation(out=sil[:], in_=cT[:], func=AF.Silu), [cT], [sil])
        for n in range(NS + 1):
            V("dve", lambda e, n=n: e.tensor_copy(out=rep[:, n], in_=sil[:, :, n:n + 1].to_broadcast([128, 8, 128])),
              [sil], [rep])
        wts = [SB(es, [128, 8, 512]) for _ in range(2)]
        mps = [PS(es, [128, 512]) for _ in range(2)]
        mo = [SB(es, [128, 512]) for _ in range(2)]
        it = 0
        for nt in range(12):
            wt = wts[nt % 2]
            P.dma(wt[:], w_ada[:, nt * 512:(nt + 1) * 512].rearrange("(k p) n -> p k n", p=128))
            for n in range(NS + 1):
                ps = mps[it % 2]
                o = mo[it % 2]
                it += 1
                mm(ps[:], [(rep[:, n, k, :], wt[:, k, :]) for k in range(8)], [rep, wt], [ps])
                V("dve", lambda e, ps=ps, o=o, nt=nt: e.tensor_tensor(out=o[:], in0=ps[:], in1=bada[:, nt * 512:(nt + 1) * 512],
                                                                      op=ALU.add), [ps, bada], [o])
                P.dma(modx_d[n, :, nt * 512:(nt + 1) * 512], o[:], q="act")
                if debug:
                    P.dma(dbg["modx"][n:n + 1, nt * 512:(nt + 1) * 512], o[0:1, :], q="act", is_output=True)
    P.barrier()
    if upto <= 0:
        P.emit()
        top.close()
        return nc


    KMASK = 1.0e5
    NEXP = 16384 if not lite else 128
    NCH = NEXP // 512 if not lite else 0
    TS = 256
    NT = TS // 128
    wq_b = dscr("wq_b", [128, 8, 2048], BF16)
    wout_b = dscr("wout_b", [128, 8, 1024], BF16)
    wglu_b = dscr("wglu_b", [128, 4, 512], BF16)
    kT_b = dscr("kT_b", [128, 16, 128], BF16)
    if debug:
        dbg["x1"] = dout("dbg_x1", [NS * L, D])
        dbg["hx2"] = dout("dbg_hx2", [NS * L, D], BF16)
        dbg["pe"] = dout("dbg_pe", [NS * L, D])
    jobs = []
    for k in range(8):
        jobs.append((wq_b[:, k, :], wq_d[k * 128:(k + 1) * 128, :], 2048))
    for k in range(8):
        jobs.append((wout_b[:, k, :], w_out_d[k * 128:(k + 1) * 128, :], 1024))
    for k in range(4):
        jobs.append((wglu_b[:, k, :], w_glu_d[k * 128:(k + 1) * 128, :], 512))
    jobs.append((kT_b[:].rearrange("p c n -> p (c n)"), kT_d[:].rearrange("p c n -> p (c n)"), 2048))
    for ec in range(NCH):
        jobs.append((uTb_s[ec].rearrange("p k e -> p (k e)"), uT_d[:, ec * 512:(ec + 1) * 512].rearrange("(k p) e -> p k e", p=128), 4096))
        jobs.append((vb_s[ec].rearrange("p b d -> p (b d)"), v_d[ec * 512:(ec + 1) * 512, :].rearrange("(b p) d -> p b d", p=128), 4096))
    job_i = [0]

    def emit_job(stg, stb):
        i = job_i[0]
        if i >= len(jobs):
            return
        job_i[0] += 1
        dst, srcap, nel = jobs[i]
        a = stg[i % 2]
        b = stb[i % 2]
        if len(srcap.shape) == 3:
            P.dma(a[:, 0:nel].rearrange("p (k e) -> p k e", k=srcap.shape[1]), srcap, q="sp")
        else:
            P.dma(a[:, 0:nel], srcap, q="sp")
        V("act", lambda e: e.copy(out=b[:, 0:nel], in_=a[:, 0:nel]), [a], [b])
        P.dma(dst, b[:, 0:nel], q="sp")

    U_s = dscr("U_s", [32, 128, NS * 256 + 128], BF16)
    if debug:
        dbg["U"] = dout("dbg_U", [32, 128, NS * 256 + 128], BF16)
        dbg["q"] = dout("dbg_q", [NS * L, 1536])
    with contextlib.ExitStack() as es:
        w_in = SB(es, [128, 8, 2048], BF16)
        stage = [SB(es, [128, 2048]) for _ in range(2)]
        for k in range(8):
            st = stage[k % 2]
            P.dma(st[:], w_in_d[k * 128:(k + 1) * 128, :])
            V("dve", lambda e, st=st, k=k: e.tensor_copy(out=w_in[:, k, :], in_=st[:]), [st], [w_in])
        b_in = SB(es, [128, 2048])
        g1 = SB(es, [128, D])
        cw = SB(es, [128, 3, 1536])
        cb = SB(es, [128, 1536])
        shm = SB(es, [128, 128], BF16)
        shp = SB(es, [128, 128], BF16)
        P.dma(b_in[:], b_in_d.partition_broadcast(128))
        P.dma(g1[:], g1_d.partition_broadcast(128))
        for j in range(3):
            P.dma(cw[:, j, :], cw_d[j].partition_broadcast(128))
        P.dma(cb[:], cb_d.partition_broadcast(128))
        P.dma(shm[:], cst["shm"][:, :])
        P.dma(shp[:], cst["shp"][:, :])
        hT = SB(es, [128, 8, 1024], BF16)
        um2 = SB(es, [128, 32, 8, 16], BF16)
        ug = [SB(es, [128, 8, 128], BF16) for _ in range(2)]
        xt = [SB(es, [128, D]) for _ in range(2)]
        tmp = SB(es, [128, D])
        hb = [SB(es, [128, D], BF16) for _ in range(2)]
        g1p = SB(es, [128, D])
        sh1 = SB(es, [128, D])
        ssum = [SB(es, [128, 1]) for _ in range(2)]
        rstd = [SB(es, [128, 1]) for _ in range(2)]
        junk = SB(es, [128, D], BF16)
        pbf = SB(es, [128, 1536], BF16)
        t1 = SB(es, [128, 1536])
        t2 = SB(es, [128, 1536])
        pA = PS(es, [128, 1536])
        pB = PS(es, [128, 1536])
        trp = [PS(es, [128, 8, 128], BF16) for _ in range(1)]
        ps5 = PS(es, [128, 512])

        cstg = [SB(es, [128, 4096]) for _ in range(2)]
        cstb = [SB(es, [128, 4096], BF16) for _ in range(2)]
        blocks = [("x", n, half) for n in range(NS) for half in range(2)] + [("c", NS, 0)]
        cur_mod = None
        for bi, (kind, n, half) in enumerate(blocks):
            if cur_mod != n:
                cur_mod = n
                P.dma(sh1[:], modx_d[n, :, 0:D])
                P.dma(g1p[:], modx_d[n, :, D:2 * D])
                V("dve", lambda e: e.scalar_tensor_tensor(out=g1p[:], in0=g1p[:], scalar=1.0, in1=g1[:],
                                                          op0=ALU.add, op1=ALU.mult), [g1p, g1], [g1p])
            src = x_d if kind == "x" else ctx_d
            row0 = (n * L + half * 1024) if kind == "x" else 0
            for ti in range(8):
                X = xt[ti % 2]
                H = hb[ti % 2]
                ss = ssum[ti % 2]
                rs = rstd[ti % 2]
                P.dma(X[:], src[row0 + ti * 128: row0 + (ti + 1) * 128, :], q="sp" if ti % 2 else "act")
                V("act", lambda e, X=X, ss=ss: e.activation(out=junk[:], in_=X[:], func=AF.Square, accum_out=ss[:]),
                  [X], [junk, ss])
                rsqrt_mean(es, ss, D, rs)
                V("dve", lambda e, X=X, rs=rs: e.scalar_tensor_tensor(out=tmp[:], in0=X[:], scalar=rs[:, 0:1], in1=g1p[:],
                                                                      op0=ALU.mult, op1=ALU.mult), [X, rs, g1p], [tmp])
                V("pool", lambda e, H=H: e.tensor_tensor(out=H[:], in0=tmp[:], in1=sh1[:], op=ALU.add), [tmp, sh1], [H])
                tp = trp[0]
                for k in range(8):
                    P.op("pe", lambda e, k=k, H=H, tp=tp: e.transpose(out=tp[:, k, :], in_=H[:, k * 128:(k + 1) * 128], identity=ident[:]),
                         reads=[H, ident], writes=[tp])
                V("act", lambda e, tp=tp, ti=ti: e.copy(out=hT[:, :, ti * 128:(ti + 1) * 128], in_=tp[:]), [tp], [hT])
                emit_job(cstg, cstb)
            if kind == "x":
                for ti in range(8):
                    for nt in range(3):
                        mm(pA[:, nt * 512:(nt + 1) * 512],
                           [(hT[:, k, ti * 128:(ti + 1) * 128], w_in[:, k, 512 + nt * 512: 512 + (nt + 1) * 512]) for k in range(8)],
                           [hT, w_in], [pA])
                    V("dve", lambda e: e.tensor_tensor(out=pbf[:], in0=pA[:], in1=b_in[:, 512:2048], op=ALU.add), [pA, b_in], [pbf])
                    for nt in range(3):
                        mm(pB[:, nt * 512:(nt + 1) * 512], [(shm[:], pbf[:, nt * 512:(nt + 1) * 512])], [shm, pbf], [pB])
                    for nt in range(3):
                        mm(pA[:, nt * 512:(nt + 1) * 512], [(shp[:], pbf[:, nt * 512:(nt + 1) * 512])], [shp, pbf], [pA])
                    V("pool", lambda e: e.tensor_tensor(out=t1[:], in0=pbf[:], in1=cw[:, 1, :], op=ALU.mult), [pbf, cw], [t1])
                    V("pool", lambda e: e.tensor_tensor(out=t1[:], in0=t1[:], in1=cb[:], op=ALU.add), [t1, cb], [t1])
                    V("dve", lambda e: e.tensor_tensor(out=t2[:], in0=pB[:], in1=cw[:, 0, :], op=ALU.mult), [pB, cw], [t2])
                    V("pool", lambda e: e.tensor_tensor(out=t1[:], in0=t1[:], in1=t2[:], op=ALU.add), [t1, t2], [t1])
                    V("dve", lambda e: e.tensor_tensor(out=t2[:], in0=pA[:], in1=cw[:, 2, :], op=ALU.mult), [pA, cw], [t2])
                    V("pool", lambda e: e.tensor_tensor(out=t1[:], in0=t1[:], in1=t2[:], op=ALU.add), [t1, t2], [t1])
                    r0 = row0 + ti * 128
                    P.dma(q_s[r0:r0 + 128, :], t1[:], q="act")
                    if debug:
                        P.dma(dbg["q"][r0:r0 + 128, :], t1[:], q="act", is_output=True)
            for j in range(8):
                mm(ps5[:], [(hT[:, k, j::8], w_in[:, k, 0:512]) for k in range(8)], [hT, w_in], [ps5])
                V("dve", lambda e, j=j: e.tensor_tensor(out=um2[:, :, j, :], in0=ps5[:].rearrange("p (g h) -> p g h", g=32),
                                                        in1=b_in[:, 0:512].rearrange("p (g h) -> p g h", g=32), op=ALU.add),
                  [ps5, b_in], [um2])
            col0 = (n * 256 + half * 128) if kind == "x" else NS * 256
            for gb in range(4):
                tp = trp[0]
                ugt = ug[gb % 2]
                for gi in range(8):
                    g = gb * 8 + gi
                    P.op("pe", lambda e, g=g, gi=gi, tp=tp: e.transpose(out=tp[:, gi, :], in_=um2[:, g].rearrange("p j h -> p (j h)"),
                                                                        identity=ident[:]), reads=[um2, ident], writes=[tp])
                V("act", lambda e, tp=tp, ugt=ugt: e.copy(out=ugt[:], in_=tp[:]), [tp], [ugt])
                P.dma(U_s[gb * 8:(gb + 1) * 8, :, col0:col0 + 128].rearrange("g p c -> p g c"), ugt[:], q="sp")
                if debug:
                    P.dma(dbg["U"][gb * 8:(gb + 1) * 8, :, col0:col0 + 128].rearrange("g p c -> p g c"), ugt[:], q="sp", is_output=True)
        while job_i[0] < len(jobs):
            emit_job(cstg, cstb)
    P.barrier()
    if upto <= 1:
        P.emit()
        top.close()
        return nc


    TWO_PI = 2.0 * math.pi
    PI_LO = 3.1415925

    def trig(ang, n_free_shape, scratch, out_sin=None, out_cos=None):
        ki, kf, r = scratch
        for out, off in ((out_sin, 0.0), (out_cos, math.pi / 2)):
            if out is None:
                continue
            V("dve", lambda e, off=off: e.tensor_scalar(out=kf, in0=ang, scalar1=off, scalar2=1.0 / TWO_PI, op0=ALU.add, op1=ALU.mult),
              [ang], [kf])
            V("dve", lambda e: e.tensor_copy(out=ki, in_=kf), [kf], [ki])
            V("dve", lambda e: e.tensor_copy(out=kf, in_=ki), [ki], [kf])
            V("dve", lambda e: e.scalar_tensor_tensor(out=r, in0=kf, scalar=-TWO_PI, in1=ang, op0=ALU.mult, op1=ALU.add), [kf, ang], [r])
            V("dve", lambda e, off=off: e.tensor_scalar(out=r, in0=r, scalar1=off, scalar2=PI_LO, op0=ALU.add, op1=ALU.min), [r], [r])
            V("dve", lambda e: e.tensor_scalar(out=r, in0=r, scalar1=-PI_LO, scalar2=None, op0=ALU.max), [r], [r])
            V("act", lambda e, out=out: e.activation(out=out, in_=r, func=AF.Sin), [r], [out])

    if debug:
        dbg["y"] = dout("dbg_y", [NS * L, 512])
        dbg["h0"] = dout("dbg_h0", [64, 2, 64, NS])
    s5es = contextlib.ExitStack()
    R8 = SB(s5es, [128, 64, 2, 64], BF16)
    O8 = SB(s5es, [64, 64, 2, 128], BF16)
    T8 = SB(s5es, [128, 32, 128], BF16)
    th8 = SB(s5es, [64, 64])
    r8 = SB(s5es, [64, 64])
    with contextlib.ExitStack() as es:
        s5p = SB(es, [64, 3, 64])
        s5b = SB(es, [64, 2, 64, 16])
        s5c = SB(es, [64, 2, 64, 16])
        s5d = SB(es, [128, 32])
        sexp = SB(es, [64, 6, 128])
        mf = SB(es, [128, 128])
        mb = SB(es, [128, 128])
        P.dma(s5p[:], s5p_d[:, :, :])
        P.dma(s5b[:], s5b_d[:, :, :, :])
        P.dma(s5c[:], s5c_d[:, :, :, :])
        P.dma(s5d[:], s5d_d[:, :])
        P.dma(sexp[:], cst["s5exp"][:, :, :])
        P.dma(mf[:], cst["mf"][:, :])
        P.dma(mb[:], cst["mb"][:, :])
        step = SB(es, [64, 64])
        rho = SB(es, [64, 64])
        th = SB(es, [64, 64])
        ebase = SB(es, [64, 64])
        V("pool", lambda e: e.memset(ebase[:], math.e), [], [ebase])
        V("pool", lambda e: e.tensor_tensor(out=step[:], in0=ebase[:], in1=s5p[:, 2, :], op=ALU.pow), [ebase, s5p], [step])
        V("dve", lambda e: e.tensor_tensor(out=rho[:], in0=s5p[:, 0, :], in1=step[:], op=ALU.mult), [s5p, step], [rho])
        V("dve", lambda e: e.tensor_tensor(out=th[:], in0=s5p[:, 1, :], in1=step[:], op=ALU.mult), [s5p, step], [th])
        ki_s = SB(es, [64, 1024], I32)
        kf_s = SB(es, [64, 1024])
        r_s = SB(es, [64, 1024])
        sn = SB(es, [64, 1024])
        cs = SB(es, [64, 1024])
        mg = SB(es, [64, 1024])
        ang = SB(es, [64, 1024])
        V("dve", lambda e: e.tensor_copy(out=ang[:, 0:64], in_=th[:]), [th], [ang])
        trig(ang[:, 0:64], None, (ki_s[:, 0:64], kf_s[:, 0:64], r_s[:, 0:64]), sn[:, 0:64], cs[:, 0:64])
        V("act", lambda e: e.activation(out=mg[:, 0:64], in_=rho[:], func=AF.Exp), [rho], [mg])
        l1re = SB(es, [64, 64])
        l1im = SB(es, [64, 64])
        V("dve", lambda e: e.tensor_tensor(out=l1re[:], in0=mg[:, 0:64], in1=cs[:, 0:64], op=ALU.mult), [mg, cs], [l1re])
        V("dve", lambda e: e.tensor_tensor(out=l1im[:], in0=mg[:, 0:64], in1=sn[:, 0:64], op=ALU.mult), [mg, sn], [l1im])
        V("dve", lambda e: e.tensor_scalar(out=l1re[:], in0=l1re[:], scalar1=-1.0, scalar2=None, op0=ALU.add), [l1re], [l1re])
        den = SB(es, [64, 64])
        t64 = SB(es, [64, 64])
        cre = SB(es, [64, 64])
        cim = SB(es, [64, 64])
        are = s5p[:, 0, :]
        aim = s5p[:, 1, :]
        V("dve", lambda e: e.tensor_tensor(out=den[:], in0=are, in1=are, op=ALU.mult), [s5p], [den])
        V("dve", lambda e: e.tensor_tensor(out=t64[:], in0=aim, in1=aim, op=ALU.mult), [s5p], [t64])
        V("dve", lambda e: e.tensor_tensor(out=den[:], in0=den[:], in1=t64[:], op=ALU.add), [den, t64], [den])
        V("dve", lambda e: e.reciprocal(out=den[:], in_=den[:]), [den], [den])
        V("dve", lambda e: e.tensor_tensor(out=cre[:], in0=l1re[:], in1=are, op=ALU.mult), [l1re, s5p], [cre])
        V("dve", lambda e: e.tensor_tensor(out=t64[:], in0=l1im[:], in1=aim, op=ALU.mult), [l1im, s5p], [t64])
        V("dve", lambda e: e.tensor_tensor(out=cre[:], in0=cre[:], in1=t64[:], op=ALU.add), [cre, t64], [cre])
        V("dve", lambda e: e.tensor_tensor(out=cre[:], in0=cre[:], in1=den[:], op=ALU.mult), [cre, den], [cre])
        V("dve", lambda e: e.tensor_tensor(out=cim[:], in0=l1im[:], in1=are, op=ALU.mult), [l1im, s5p], [cim])
        V("dve", lambda e: e.tensor_tensor(out=t64[:], in0=l1re[:], in1=aim, op=ALU.mult), [l1re, s5p], [t64])
        V("dve", lambda e: e.tensor_tensor(out=cim[:], in0=cim[:], in1=t64[:], op=ALU.subtract), [cim, t64], [cim])
        V("dve", lambda e: e.tensor_tensor(out=cim[:], in0=cim[:], in1=den[:], op=ALU.mult), [cim, den], [cim])
        bbre = SB(es, [64, 64, 16])
        bbim = SB(es, [64, 64, 16])
        tb = SB(es, [64, 64, 16])
        creb = cre[:].unsqueeze(2).to_broadcast([64, 64, 16])
        cimb = cim[:].unsqueeze(2).to_broadcast([64, 64, 16])
        V("dve", lambda e: e.tensor_tensor(out=bbre[:], in0=s5b[:, 0], in1=creb, op=ALU.mult), [s5b, cre], [bbre])
        V("dve", lambda e: e.tensor_tensor(out=tb[:], in0=s5b[:, 1], in1=cimb, op=ALU.mult), [s5b, cim], [tb])
        V("dve", lambda e: e.tensor_tensor(out=bbre[:], in0=bbre[:], in1=tb[:], op=ALU.subtract), [bbre, tb], [bbre])
        V("dve", lambda e: e.tensor_tensor(out=bbim[:], in0=s5b[:, 1], in1=creb, op=ALU.mult), [s5b, cre], [bbim])
        V("dve", lambda e: e.tensor_tensor(out=tb[:], in0=s5b[:, 0], in1=cimb, op=ALU.mult), [s5b, cim], [tb])
        V("dve", lambda e: e.tensor_tensor(out=bbim[:], in0=bbim[:], in1=tb[:], op=ALU.add), [bbim, tb], [bbim])
        V("dve", lambda e: e.tensor_scalar(out=ang[:, 0:64], in0=th[:], scalar1=8.0, scalar2=None, op0=ALU.mult), [th], [ang])
        V("dve", lambda e: e.tensor_scalar(out=kf_s[:, 0:64], in0=ang[:, 0:64], scalar1=1.0 / TWO_PI, scalar2=None, op0=ALU.mult), [ang], [kf_s])
        V("dve", lambda e: e.tensor_copy(out=ki_s[:, 0:64], in_=kf_s[:, 0:64]), [kf_s], [ki_s])
        V("dve", lambda e: e.tensor_copy(out=kf_s[:, 0:64], in_=ki_s[:, 0:64]), [ki_s], [kf_s])
        V("dve", lambda e: e.scalar_tensor_tensor(out=th8[:], in0=kf_s[:, 0:64], scalar=-TWO_PI, in1=ang[:, 0:64], op0=ALU.mult, op1=ALU.add),
          [kf_s, ang], [th8])
        V("act", lambda e: e.activation(out=r8[:], in_=rho[:], func=AF.Exp, scale=8.0), [rho], [r8])
        Lre = SB(es, [64, 3, 8, 128])
        Lim = SB(es, [64, 3, 8, 128])
        RT = SB(es, [64, 2, 8, 128])
        OP = SB(es, [64, 2, 8, 128])
        OO = SB(es, [64, 2, 8, 128])
        tw = SB(es, [64, 8, 128])
        pT = [PS(es, [128, 128]) for _ in range(2)]
        pR = PS(es, [128, 4, 64])
        tt8 = SB(es, [128, 128])
        tt8b = SB(es, [128, 128])
        for d in range(2):
            for gb in range(4):
                dg0 = d * 32 + gb * 8
                for kk, kind in enumerate((d, 2 + d, 4 + d)):
                    a3 = ang[:].rearrange("p (a c) -> p a c", a=8)
                    exb = sexp[:, kind, :].unsqueeze(1).to_broadcast([64, 8, 128])
                    V("dve", lambda e, exb=exb, dg0=dg0: e.tensor_tensor(out=a3, in0=exb, in1=th[:, dg0:dg0 + 8].unsqueeze(2).to_broadcast([64, 8, 128]),
                                                                         op=ALU.mult), [sexp, th], [ang])
                    trig(ang[:], None, (ki_s[:], kf_s[:], r_s[:]), sn[:], cs[:])
                    m3 = mg[:].rearrange("p (a c) -> p a c", a=8)
                    V("dve", lambda e, exb=exb, dg0=dg0: e.tensor_tensor(out=m3, in0=exb, in1=rho[:, dg0:dg0 + 8].unsqueeze(2).to_broadcast([64, 8, 128]),
                                                                         op=ALU.mult), [sexp, rho], [mg])
                    V("act", lambda e: e.activation(out=mg[:], in_=mg[:], func=AF.Exp), [mg], [mg])
                    V("dve", lambda e, kk=kk: e.tensor_tensor(out=Lre[:, kk].rearrange("p a c -> p (a c)"), in0=mg[:], in1=cs[:], op=ALU.mult), [mg, cs], [Lre])
                    V("dve", lambda e, kk=kk: e.tensor_tensor(out=Lim[:, kk].rearrange("p a c -> p (a c)"), in0=mg[:], in1=sn[:], op=ALU.mult), [mg, sn], [Lim])
                def cmul(out_re, out_im, kind, vre, vim, neg_im):
                    lre = Lre[:, kind].rearrange("p a (j h) -> p a j h", j=8)
                    lim = Lim[:, kind].rearrange("p a (j h) -> p a j h", j=8)
                    vr = vre.unsqueeze(2).to_broadcast([64, 8, 8, 16])
                    vi = vim.unsqueeze(2).to_broadcast([64, 8, 8, 16])
                    o_re = out_re.rearrange("p a (j h) -> p a j h", j=8)
                    o_im = out_im.rearrange("p a (j h) -> p a j h", j=8)
                    t4 = tw[:].rearrange("p a (j h) -> p a j h", j=8)
                    V("dve", lambda e: e.tensor_tensor(out=o_re, in0=lre, in1=vr, op=ALU.mult), [Lre, bbre, s5c], [out_re])
                    V("dve", lambda e: e.tensor_tensor(out=t4, in0=lim, in1=vi, op=ALU.mult), [Lim, bbim, s5c], [tw])
                    V("dve", lambda e: e.tensor_tensor(out=out_re, in0=out_re, in1=tw[:], op=ALU.subtract), [out_re, tw], [out_re])
                    V("dve", lambda e: e.tensor_tensor(out=o_im, in0=lre, in1=vi, op=ALU.mult), [Lre, bbim, s5c], [out_im])
                    V("dve", lambda e: e.tensor_tensor(out=t4, in0=lim, in1=vr, op=ALU.mult), [Lim, bbre, s5c], [tw])
                    if neg_im:
                        V("dve", lambda e: e.scalar_tensor_tensor(out=out_im.rearrange("p a c -> p (a c)"), in0=out_im.rearrange("p a c -> p (a c)"), scalar=-1.0,
                                                                  in1=tw[:].rearrange("p a c -> p (a c)"), op0=ALU.mult, op1=ALU.subtract), [out_im, tw], [out_im])
                    else:
                        V("dve", lambda e: e.tensor_tensor(out=out_im, in0=out_im, in1=tw[:], op=ALU.add), [out_im, tw], [out_im])
                cmul(RT[:, 0], RT[:, 1], 0, bbre[:, dg0:dg0 + 8, :], bbim[:, dg0:dg0 + 8, :], False)
                cmul(OO[:, 0], OO[:, 1], 1, s5c[:, 0, dg0:dg0 + 8, :], s5c[:, 1, dg0:dg0 + 8, :], True)
                cmul(OP[:, 0], OP[:, 1], 2, s5c[:, 0, dg0:dg0 + 8, :], s5c[:, 1, dg0:dg0 + 8, :], True)
                for i in range(8):
                    dg = dg0 + i
                    g = gb * 8 + i
                    V("act", lambda e, dg=dg, i=i: e.copy(out=O8[:, dg], in_=OO[:, :, i, :]), [OO], [O8])
                    for c2 in range(2):
                        P.op("pe", lambda e, c2=c2, i=i: e.transpose(out=pR[:, c2, :], in_=RT[:, c2, i, :], identity=identf[0:64, 0:64]),
                             reads=[RT, identf], writes=[pR])
                    V("act", lambda e, dg=dg: e.copy(out=R8[:, dg], in_=pR[:, 0:2, :]), [pR], [R8])
                    pt = pT[i % 2]
                    mm(pt[:], [(RT[:, 0, i, :], OP[:, 0, i, :]), (RT[:, 1, i, :], OP[:, 1, i, :])], [RT, OP], [pt])
                    if d == 0:
                        V("dve", lambda e, pt=pt: e.tensor_tensor(out=tt8[:], in0=pt[:], in1=mf[:], op=ALU.mult), [pt, mf], [tt8])
                        V("dve", lambda e, g=g: e.scalar_tensor_tensor(out=tt8[:], in0=identf[:], scalar=s5d[:, g:g + 1], in1=tt8[:],
                                                                       op0=ALU.mult, op1=ALU.add), [identf, s5d, tt8], [tt8])
                        V("pool", lambda e, g=g: e.tensor_copy(out=T8[:, g, :], in_=tt8[:]), [tt8], [T8])
                    else:
                        V("dve", lambda e, pt=pt: e.tensor_tensor(out=tt8b[:], in0=pt[:], in1=mb[:], op=ALU.mult), [pt, mb], [tt8b])
                        V("pool", lambda e, g=g: e.tensor_tensor(out=T8[:, g, :], in0=T8[:, g, :], in1=tt8b[:], op=ALU.add), [tt8b, T8], [T8])
    P.barrier()
    if upto <= 2:
        if debug:
            dbg["T8"] = dout("dbg_T8", [128, 32, 128], BF16)
            dbg["O8"] = dout("dbg_O8", [64, 64, 2, 128], BF16)
            dbg["R8"] = dout("dbg_R8", [128, 64, 2, 64], BF16)
            P.dma(dbg["T8"][:, :, :], T8[:], is_output=True)
            P.dma(dbg["O8"][:, :, :, :], O8[:], is_output=True)
            P.dma(dbg["R8"][:, :, :, :], R8[:], is_output=True)
        P.emit()
        s5es.close()
        top.close()
        return nc


    with contextlib.ExitStack() as es:
        mix = SB(es, [64, 256])
        P.dma(mix[:], cst["mix"][:, :])
        Ugs = [SB(es, [128, NS * 256 + 128], BF16) for _ in range(2)]
        cosr = [SB(es, [64, 256]) for _ in range(2)]
        sinr = [SB(es, [64, 256]) for _ in range(2)]
        angr = SB(es, [64, 256])
        kir = SB(es, [64, 256], I32)
        kfr = SB(es, [64, 256])
        rr = SB(es, [64, 256])
        cc = [SB(es, [64, NS, 255]) for _ in range(2)]
        ta = SB(es, [64, NS, 256])
        tb2 = SB(es, [64, NS, 256])
        W = [SB(es, [64, NS, 256]) for _ in range(2)]
        Wc = [SB(es, [64, NS, 32]) for _ in range(2)]
        h0 = SB(es, [64, 2, 2, NS])
        t4 = SB(es, [64, NS])
        Sin = SB(es, [64, 2, 2, NS * 256], BF16)
        Ysb = SB(es, [128, NS * 256])
        Yout = SB(es, [128, 8, 8, 128])
        psS = [PS(es, [64, NS * 256]) for _ in range(2)]
        psY = PS(es, [128, NS * 256])
        pTr = PS(es, [128, 4, 128])
        psC = PS(es, [64, 2, 128])

        def rot(out_re, out_im, s_re, s_im, cs_, sn_, conj, eng2="pool"):
            V("dve", lambda e: e.tensor_tensor(out=ta_v(out_re), in0=s_re, in1=cs_, op=ALU.mult), [psS[0], psC, W[0], Wc[0], cosr[0], cosr[1]], [ta])
            V("dve", lambda e: e.tensor_tensor(out=tb_v(out_re), in0=s_im, in1=sn_, op=ALU.mult), [psS[1], psC, W[1], Wc[1], sinr[0], sinr[1]], [tb2])
            V(eng2, lambda e: e.tensor_tensor(out=out_re, in0=ta_v(out_re), in1=tb_v(out_re), op=(ALU.add if conj else ALU.subtract)),
              [ta, tb2], [cc[0], Sin])
            V("dve", lambda e: e.tensor_tensor(out=ta_v(out_re), in0=s_im, in1=cs_, op=ALU.mult), [psS[1], psC, W[1], Wc[1], cosr[0], cosr[1]], [ta])
            V("dve", lambda e: e.tensor_tensor(out=tb_v(out_re), in0=s_re, in1=sn_, op=ALU.mult), [psS[0], psC, W[0], Wc[0], sinr[0], sinr[1]], [tb2])
            V(eng2, lambda e: e.tensor_tensor(out=out_im, in0=ta_v(out_re), in1=tb_v(out_re), op=(ALU.subtract if conj else ALU.add)),
              [ta, tb2], [cc[1], Sin])

        def ta_v(like):
            return ta[:, :, 0:like.shape[2]]

        def tb_v(like):
            return tb2[:, :, 0:like.shape[2]]

        for g in range(32):
            Ug = Ugs[g % 2]
            P.dma(Ug[:], U_s[g], q="act" if g % 2 else "sp")
            for d in range(2):
                dg = d * 32 + g
                V("dve", lambda e, dg=dg: e.tensor_scalar(out=angr[:], in0=mix[:], scalar1=th8[:, dg:dg + 1], scalar2=None, op0=ALU.mult),
                  [mix, th8], [angr])
                trig(angr[:], None, (kir[:], kfr[:], rr[:]), sinr[d][:], cosr[d][:])
            for d in range(2):
                dg = d * 32 + g
                for c2 in range(2):
                    mm(psC[:, c2, :], [(R8[:, dg, c2, :], Ug[:, NS * 256:NS * 256 + 128])], [R8, Ug], [psC])
                sre = psC[:, 0, :].rearrange("p (n m) -> p n m", n=NS)
                sim = psC[:, 1, :].rearrange("p (n m) -> p n m", n=NS)
                if d == 1:
                    sre = sre[:, :, ::-1]
                    sim = sim[:, :, ::-1]
                csb = cosr[d][:, 1:33].unsqueeze(1).to_broadcast([64, NS, 32])
                snb = sinr[d][:, 1:33].unsqueeze(1).to_broadcast([64, NS, 32])
                rot(cc[0][:, :, 0:32], cc[1][:, :, 0:32], sre, sim, csb, snb, True)
                for c2 in range(2):
                    for n in range(NS):
                        V("dve", lambda e, c2=c2, n=n, dg=dg: e.tensor_tensor_scan(out=Wc[c2][:, n, :], data0=r8[:, dg:dg + 1].to_broadcast([64, 32]),
                                                                                  data1=cc[c2][:, n, 0:32], initial=0.0, op0=ALU.mult, op1=ALU.add),
                          [r8, cc[c2]], [Wc[c2]])
                c32 = cosr[d][:, 32:33]
                s32 = sinr[d][:, 32:33]
                V("dve", lambda e, s32=s32: e.tensor_scalar(out=t4[:], in0=Wc[1][:, :, 31], scalar1=s32, scalar2=None, op0=ALU.mult), [Wc[1], sinr[d]], [t4])
                V("dve", lambda e, c32=c32, d=d: e.scalar_tensor_tensor(out=h0[:, d, 0, :], in0=Wc[0][:, :, 31], scalar=c32, in1=t4[:], op0=ALU.mult, op1=ALU.subtract),
                  [Wc[0], cosr[d], t4], [h0])
                V("dve", lambda e, c32=c32: e.tensor_scalar(out=t4[:], in0=Wc[1][:, :, 31], scalar1=c32, scalar2=None, op0=ALU.mult), [Wc[1], cosr[d]], [t4])
                V("dve", lambda e, s32=s32, d=d: e.scalar_tensor_tensor(out=h0[:, d, 1, :], in0=Wc[0][:, :, 31], scalar=s32, in1=t4[:], op0=ALU.mult, op1=ALU.add),
                  [Wc[0], sinr[d], t4], [h0])
                if debug:
                    P.dma(dbg["h0"][:, d, g, :], h0[:, d, 0, :], is_output=True)
                    if g == 0 and d == 0:
                        dbg["m"] = dout("dbg_m", [64, 8, 256])
                        dm = SB(es, [64, 8, 256])
                        V("dve", lambda e: e.memset(dm[:], 0.0), [], [dm])
                        V("dve", lambda e: e.tensor_copy(out=dm[:, 0, :], in_=psC[:].rearrange("p a b -> p (a b)")), [psC], [dm])
                        V("dve", lambda e: e.tensor_copy(out=dm[:, 1, 0:128].rearrange("p (n m) -> p n m", n=NS), in_=cc[0][:, :, 0:32]), [cc[0]], [dm])
                        V("dve", lambda e: e.tensor_copy(out=dm[:, 2, 0:128].rearrange("p (n m) -> p n m", n=NS), in_=Wc[0][:]), [Wc[0]], [dm])
                        V("dve", lambda e: e.tensor_copy(out=dm[:, 3, :], in_=cosr[0][:]), [cosr[0]], [dm])
                        V("dve", lambda e: e.tensor_copy(out=dm[:, 4, :], in_=sinr[0][:]), [sinr[0]], [dm])
                        V("dve", lambda e: e.tensor_copy(out=dm[:, 5, 0:64], in_=r8[:]), [r8], [dm])
                        V("dve", lambda e: e.tensor_copy(out=dm[:, 6, 0:64], in_=th8[:]), [th8], [dm])
                        V("dve", lambda e: e.tensor_copy(out=dm[:, 7, 0:128].rearrange("p (n m) -> p n m", n=NS), in_=cc[1][:, :, 0:32]), [cc[1]], [dm])
                        P.dma(dbg["m"][:, :, :], dm[:], is_output=True)
            for d in range(2):
                dg = d * 32 + g
                for c2 in range(2):
                    for hf in range(2):
                        mm(psS[c2][:, hf * 512:(hf + 1) * 512], [(R8[:, dg, c2, :], Ug[:, hf * 512:(hf + 1) * 512])], [R8, Ug], [psS[c2]])
                sre = psS[0][:].rearrange("p (n m) -> p n m", n=NS)
                sim = psS[1][:].rearrange("p (n m) -> p n m", n=NS)
                if d == 0:
                    sre = sre[:, :, 0:255]
                    sim = sim[:, :, 0:255]
                else:
                    sre = sre[:, :, 255:0:-1]
                    sim = sim[:, :, 255:0:-1]
                csb = cosr[d][:, 1:256].unsqueeze(1).to_broadcast([64, NS, 255])
                snb = sinr[d][:, 1:256].unsqueeze(1).to_broadcast([64, NS, 255])
                rot(cc[0][:], cc[1][:], sre, sim, csb, snb, True)
                for c2 in range(2):
                    V("pool", lambda e, c2=c2, d=d: e.tensor_copy(out=W[c2][:, :, 0:1], in_=h0[:, d, c2, :].unsqueeze(2)), [h0], [W[c2]])
                    for n in range(NS):
                        V("dve", lambda e, c2=c2, n=n, dg=dg, d=d: e.tensor_tensor_scan(out=W[c2][:, n, 1:256], data0=r8[:, dg:dg + 1].to_broadcast([64, 255]),
                                                                                       data1=cc[c2][:, n, :], initial=h0[:, d, c2, n:n + 1], op0=ALU.mult, op1=ALU.add),
                          [r8, cc[c2], h0], [W[c2]])
                csb = cosr[d][:].unsqueeze(1).to_broadcast([64, NS, 256])
                snb = sinr[d][:].unsqueeze(1).to_broadcast([64, NS, 256])
                o_re = Sin[:, d, 0, :].rearrange("p (n m) -> p n m", n=NS)
                o_im = Sin[:, d, 1, :].rearrange("p (n m) -> p n m", n=NS)
                if d == 1:
                    o_re = o_re[:, :, ::-1]
                    o_im = o_im[:, :, ::-1]
                rot(o_re, o_im, W[0][:], W[1][:], csb, snb, False)
            for hf in range(2):
                cs_ = slice(hf * 512, (hf + 1) * 512)
                mm(psY[:, cs_], [(T8[:, g, :], Ug[:, cs_]), (O8[:, g, 0, :], Sin[:, 0, 0, cs_]), (O8[:, g, 1, :], Sin[:, 0, 1, cs_]),
                                 (O8[:, 32 + g, 0, :], Sin[:, 1, 0, cs_]), (O8[:, 32 + g, 1, :], Sin[:, 1, 1, cs_])],
                   [T8, Ug, O8, Sin], [psY])
            V("act", lambda e: e.copy(out=Ysb[:], in_=psY[:]), [psY], [Ysb])
            gi = g % 8
            for q4 in range(2):
                for b4 in range(4):
                    blk = q4 * 4 + b4
                    P.op("pe", lambda e, blk=blk, b4=b4: e.transpose(out=pTr[:, b4, :], in_=Ysb[:, blk * 128:(blk + 1) * 128], identity=identf[:]),
                         reads=[Ysb, identf], writes=[pTr])
                V("act", lambda e, q4=q4, gi=gi: e.copy(out=Yout[:, q4 * 4:(q4 + 1) * 4, :, gi * 16:(gi + 1) * 16],
                                                        in_=pTr[:].rearrange("p b (j h) -> p b j h", j=8)), [pTr], [Yout])
            if gi == 7:
                gb = g // 8
                for blk in range(8):
                    dst = y_s[blk * 1024:(blk + 1) * 1024, gb * 128:(gb + 1) * 128].rearrange("(m j) c -> m j c", j=8)
                    P.dma(dst, Yout[:, blk], q="sp" if blk % 2 else "act")
                    if debug:
                        P.dma(dbg["y"][blk * 1024:(blk + 1) * 1024, gb * 128:(gb + 1) * 128].rearrange("(m j) c -> m j c", j=8), Yout[:, blk], is_output=True)
    s5es.close()
    P.barrier()
    if upto <= 3:
        P.emit()
        top.close()
        return nc


    h_s = dscr("h_s", [L, 2048])
    if debug:
        dbg["filt"] = dout("dbg_filt", [L, 2048])
        dbg["z"] = dout("dbg_z", [NS * L, 512])
    hyes = contextlib.ExitStack()
    rn = SB(hyes, [128, 2, 512])
    with contextlib.ExitStack() as es:
        fT = SB(es, [33, 2048])
        w1 = SB(es, [33, 64])
        b1 = SB(es, [64, 1])
        wh = SB(es, [64, 2, 64])
        bh = SB(es, [64, 2])
        fr = SB(es, [64, 1])
        wout = SB(es, [64, 2048])
        absd = SB(es, [128, 2048])
        tneg = SB(es, [128, 16])
        onesf = SB(es, [128, 128])
        P.dma(fT[:], cst["featsT"][:, :])
        P.dma(w1[:], hf_w1[:, :])
        P.dma(b1[:], hf_b1[:, :])
        P.dma(wh[:], hf_wh.rearrange("i a b -> a i b"))
        P.dma(bh[:], hf_bh[:, :])
        P.dma(fr[:], hf_fr[:, :])
        P.dma(wout[:], hf_wout[:, :])
        P.dma(absd[:], hf_dec.partition_broadcast(128))
        P.dma(tneg[:], cst["tneg"][:, :])
        P.dma(onesf[:], cst["onesf"][:, :])
        V("act", lambda e: e.activation(out=absd[:], in_=absd[:], func=AF.Abs), [absd], [absd])
        hid = [SB(es, [64, 2048]) for _ in range(2)]
        pre = SB(es, [64, 2048])
        kiH = SB(es, [64, 2048], I32)
        kfH = SB(es, [64, 2048])
        rH = SB(es, [64, 2048])
        psA = PS(es, [128, 2048])
        psB = PS(es, [128, 2048])
        for layer in range(3):
            for nt in range(4):
                cs_ = slice(nt * 512, (nt + 1) * 512)
                if layer == 0:
                    mm(psA[0:64, cs_], [(w1[:], fT[:, cs_])], [w1, fT], [psA])
                else:
                    mm(psA[0:64, cs_], [(wh[:, layer - 1, :], hid[(layer - 1) % 2][:, cs_])], [wh, hid[(layer - 1) % 2]], [psA])
            bias = b1[:, 0:1] if layer == 0 else bh[:, layer - 1:layer]
            V("dve", lambda e, bias=bias: e.tensor_scalar(out=pre[:], in0=psA[0:64, :], scalar1=bias, scalar2=fr[:, 0:1], op0=ALU.add, op1=ALU.mult),
              [psA, b1, bh, fr], [pre])
            trig(pre[:], None, (kiH[:], kfH[:], rH[:]), hid[layer % 2][:], None)
        hidF = hid[0]
        dec = SB(es, [128, 2048])
        hraw = [SB(es, [128, 2048]) for _ in range(2)]
        hsq = SB(es, [128, 2048])
        for tc in range(16):
            hr = hraw[tc % 2]
            for nt in range(4):
                cs_ = slice(nt * 512, (nt + 1) * 512)
                mm(psA[:, cs_], [(hidF[:, tc * 128:(tc + 1) * 128], wout[:, cs_])], [hidF, wout], [psA])
            V("act", lambda e, tc=tc: e.activation(out=dec[:], in_=absd[:], func=AF.Exp, scale=tneg[:, tc:tc + 1]), [absd, tneg], [dec])
            V("dve", lambda e, hr=hr: e.tensor_tensor(out=hr[:], in0=psA[:], in1=dec[:], op=ALU.mult), [psA, dec], [hr])
            V("pool", lambda e, hr=hr: e.tensor_tensor(out=hsq[:], in0=hr[:], in1=hr[:], op=ALU.mult), [hr], [hsq])
            for nt in range(4):
                cs_ = slice(nt * 512, (nt + 1) * 512)
                P.op("pe", lambda e, cs_=cs_, tc=tc: e.matmul(psB[:, cs_], lhsT=onesf[:], rhs=hsq[:, cs_], start=(tc == 0), stop=(tc == 15)),
                     reads=[onesf, hsq], writes=[psB])
            P.dma(h_s[tc * 128:(tc + 1) * 128, :], hr[:], q="act")
        ssb = SB(es, [128, 2048])
        V("act", lambda e: e.copy(out=ssb[:], in_=psB[:]), [psB], [ssb])
        s4 = ssb[:].rearrange("p (o d c) -> p o d c", o=2, d=2)
        V("dve", lambda e: e.tensor_tensor(out=rn[:], in0=s4[:, :, 0, :], in1=s4[:, :, 1, :], op=ALU.add), [ssb], [rn])
        V("dve", lambda e: e.tensor_scalar(out=rn[:], in0=rn[:], scalar1=EPS, scalar2=None, op0=ALU.add), [rn], [rn])
        mh = SB(es, [128, 1024])
        V("pool", lambda e: e.memset(mh[:], -0.5), [], [mh])
        V("pool", lambda e: e.tensor_tensor(out=rn[:].rearrange("p o c -> p (o c)"), in0=rn[:].rearrange("p o c -> p (o c)"), in1=mh[:], op=ALU.pow), [rn, mh], [rn])
    P.barrier()
    Ct = SB(hyes, [128, 16, 2048], BF16)
    St = SB(hyes, [128, 16, 2048], BF16)
    P.dma(Ct[:], cst["ctab"].rearrange("(tc p) f -> p tc f", p=128))
    P.dma(St[:], cst["stab"].rearrange("(tc p) f -> p tc f", p=128), q="act")
    with contextlib.ExitStack() as es:
        hraw = [SB(es, [128, 2048]) for _ in range(2)]
        psA = PS(es, [128, 2048])
        m0 = SB(es, [128, 1])
        cphi = SB(es, [128, 16])
        sphi = SB(es, [128, 16])
        P.dma(m0[:], cst["m0"][:, :])
        P.dma(cphi[:], cst["cphi"][:, :])
        P.dma(sphi[:], cst["sphi"][:, :])
        PM = SB(es, [128, 16, 2, 512], BF16)
        kre = [SB(es, [128, 512]) for _ in range(2)]
        kim = [SB(es, [128, 512]) for _ in range(2)]
        for o in range(2):
            for tc in range(16):
                hr = hraw[tc % 2]
                P.dma(hr[:], h_s[tc * 128:(tc + 1) * 128, :])
                h4 = hr[:].rearrange("p (o d c) -> p o d c", o=2, d=2)
                V("dve", lambda e, h4=h4, o=o, hr=hr: e.tensor_tensor(out=h4[:, o], in0=h4[:, o], in1=rn[:, o:o + 1, :].to_broadcast([128, 2, 512]), op=ALU.mult),
                  [hr, rn], [hr])
                if debug:
                    P.dma(dbg["filt"][tc * 128:(tc + 1) * 128, o * 1024:(o + 1) * 1024], hr[:, o * 1024:(o + 1) * 1024], is_output=True)
                if tc == 0:
                    V("dve", lambda e, h4=h4, o=o, hr=hr: e.tensor_scalar(out=h4[:, o, 1, :], in0=h4[:, o, 1, :], scalar1=m0[:, 0:1], scalar2=None, op0=ALU.mult),
                      [hr, m0], [hr])
                V("dve", lambda e, h4=h4, o=o, tc=tc, hr=hr: e.tensor_tensor(out=PM[:, tc, 0, :], in0=h4[:, o, 0, :], in1=h4[:, o, 1, :], op=ALU.add), [hr], [PM])
                V("pool", lambda e, h4=h4, o=o, tc=tc, hr=hr: e.tensor_tensor(out=PM[:, tc, 1, :], in0=h4[:, o, 0, :], in1=h4[:, o, 1, :], op=ALU.subtract), [hr], [PM])
            for fc in range(16):
                fs = slice(fc * 128, (fc + 1) * 128)
                mm(psA[:, 0:512], [(Ct[:, tc, fs], PM[:, tc, 0, :]) for tc in range(16)], [Ct, PM], [psA])
                mm(psA[:, 512:1024], [(St[:, tc, fs], PM[:, tc, 0, :]) for tc in range(16)], [St, PM], [psA])
                mm(psA[:, 1024:1536], [(Ct[:, tc, fs], PM[:, tc, 1, :]) for tc in range(16)], [Ct, PM], [psA])
                mm(psA[:, 1536:2048], [(St[:, tc, fs], PM[:, tc, 1, :]) for tc in range(16)], [St, PM], [psA])
                kr = kre[fc % 2]
                kq = kim[fc % 2]
                V("dve", lambda e, kr=kr, fc=fc: e.tensor_scalar(out=kr[:], in0=psA[:, 0:512], scalar1=cphi[:, fc:fc + 1], scalar2=None, op0=ALU.mult), [psA, cphi], [kr])
                V("dve", lambda e, kr=kr, fc=fc: e.scalar_tensor_tensor(out=kr[:], in0=psA[:, 512:1024], scalar=sphi[:, fc:fc + 1], in1=kr[:], op0=ALU.mult, op1=ALU.add),
                  [psA, sphi, kr], [kr])
                V("dve", lambda e, kq=kq, fc=fc: e.tensor_scalar(out=kq[:], in0=psA[:, 1536:2048], scalar1=cphi[:, fc:fc + 1], scalar2=None, op0=ALU.mult), [psA, cphi], [kq])
                V("dve", lambda e, kq=kq, fc=fc: e.scalar_tensor_tensor(out=kq[:], in0=psA[:, 1024:1536], scalar=sphi[:, fc:fc + 1], in1=kq[:], op0=ALU.mult, op1=ALU.subtract),
                  [psA, sphi, kq], [kq])
                P.dma(kspec_s[o, 0, fc], kr[:], q="act")
                P.dma(kspec_s[o, 1, fc], kq[:], q="act")
    P.barrier()
    if upto <= 4:
        P.emit()
        hyes.close()
        top.close()
        return nc


    with contextlib.ExitStack() as es:
        hyd = SB(es, [128, 2, 512])
        P.dma(hyd[:].rearrange("p o c -> p (o c)"), hyd_d.rearrange("o c -> (o c)").partition_broadcast(128))
        zin = SB(es, [128, 16, 512], BF16)
        YY = SB(es, [128, 16, 2, 512], BF16)
        kr2 = [SB(es, [128, 2, 512]) for _ in range(2)]
        gt = [SB(es, [128, 512]) for _ in range(2)]
        vst = gt
        u1 = SB(es, [128, 512])
        u2 = SB(es, [128, 512])
        u3 = SB(es, [128, 512])
        ot = [u1, u2]
        psX = [PS(es, [128, 2, 512]) for _ in range(2)]
        psO = [PS(es, [128, 512]) for _ in range(2)]
        for n in range(NS):
            for tc in range(16):
                vt = vst[tc % 2]
                r0 = n * L + tc * 128
                P.dma(vt[:], q_s[r0:r0 + 128, 0:512], q="act" if tc % 2 else "sp")
                V("act" if tc % 2 else "pool", lambda e, vt=vt, tc=tc: (e.copy if hasattr(e, "copy") else e.tensor_copy)(out=zin[:, tc, :], in_=vt[:]), [vt], [zin])
            for o in range(2):
                for fc in range(16):
                    fs = slice(fc * 128, (fc + 1) * 128)
                    px_ = psX[fc % 2]
                    kk = kr2[fc % 2]
                    P.dma(kk[:, 0, :], kspec_s[o, 0, fc], q="sp")
                    P.dma(kk[:, 1, :], kspec_s[o, 1, fc], q="act")
                    mm(px_[:, 0, :], [(Ct[:, tc, fs], zin[:, tc, :]) for tc in range(16)], [Ct, zin], [px_])
                    mm(px_[:, 1, :], [(St[:, tc, fs], zin[:, tc, :]) for tc in range(16)], [St, zin], [px_])
                    V("dve", lambda e, px_=px_, kk=kk: e.tensor_tensor(out=u1[:], in0=px_[:, 0, :], in1=kk[:, 0, :], op=ALU.mult), [px_, kk], [u1])
                    V("dve", lambda e, px_=px_, kk=kk: e.tensor_tensor(out=u2[:], in0=px_[:, 1, :], in1=kk[:, 1, :], op=ALU.mult), [px_, kk], [u2])
                    V("pool", lambda e, fc=fc: e.tensor_tensor(out=YY[:, fc, 0, :], in0=u1[:], in1=u2[:], op=ALU.add), [u1, u2], [YY])
                    V("dve", lambda e, px_=px_, kk=kk: e.tensor_tensor(out=u1[:], in0=px_[:, 1, :], in1=kk[:, 0, :], op=ALU.mult), [px_, kk], [u1])
                    V("dve", lambda e, px_=px_, kk=kk: e.tensor_tensor(out=u2[:], in0=px_[:, 0, :], in1=kk[:, 1, :], op=ALU.mult), [px_, kk], [u2])
                    V("pool", lambda e, fc=fc: e.tensor_tensor(out=YY[:, fc, 1, :], in0=u1[:], in1=u2[:], op=ALU.subtract), [u1, u2], [YY])
                for tc in range(16):
                    ts_ = slice(tc * 128, (tc + 1) * 128)
                    po = psO[tc % 2]
                    g_ = gt[tc % 2]
                    r0 = n * L + tc * 128
                    P.dma(g_[:], q_s[r0:r0 + 128, 512 * (o + 1):512 * (o + 2)], q="sp")
                    pairs = []
                    for fc in range(16):
                        pairs.append((Ct[:, fc, ts_], YY[:, fc, 0, :]))
                        pairs.append((St[:, fc, ts_], YY[:, fc, 1, :]))
                    mm(po[:], pairs, [Ct, St, YY], [po])
                    V("pool", lambda e, tc=tc, o=o: e.tensor_tensor(out=u3[:], in0=zin[:, tc, :], in1=hyd[:, o, :], op=ALU.mult), [zin, hyd], [u3])
                    V("dve", lambda e, po=po: e.scalar_tensor_tensor(out=u3[:], in0=po[:], scalar=2.0 / NFFT, in1=u3[:], op0=ALU.mult, op1=ALU.add), [po, u3], [u3])
                    if o == 0:
                        V("dve", lambda e, tc=tc, g_=g_: e.tensor_tensor(out=zin[:, tc, :], in0=u3[:], in1=g_[:], op=ALU.mult), [u3, g_], [zin])
                    else:
                        o_ = ot[tc % 2]
                        V("dve", lambda e, o_=o_, g_=g_: e.tensor_tensor(out=o_[:], in0=u3[:], in1=g_[:], op=ALU.mult), [u3, g_], [o_])
                        P.dma(z_s[r0:r0 + 128, :], o_[:], q="act")
                        if debug:
                            P.dma(dbg["z"][r0:r0 + 128, :], o_[:], q="act", is_output=True)
    hyes.close()
    P.barrier()
    if upto <= 5:
        P.emit()
        top.close()
        return nc


    NTG = 2
    TSG = NTG * 128
    x1_s = dscr("x1_s", [NS * L, D])
    hx2T_s = dscr("hx2T_s", [NS * L // TS, 128, 8, TS], BF16)
    sc_s = dscr("sc_s", [NS * L // 128, 128, 16 * 128])
    thr_s = dscr("thr_s", [NS * L // 128, 128, 16])
    with contextlib.ExitStack() as es:
        bglu = SB(es, [128, 512])
        gs5 = SB(es, [128, 512])
        ghy = SB(es, [128, 512])
        g2 = SB(es, [128, D])
        gx1 = SB(es, [128, D])
        g2p = SB(es, [128, D])
        sh2 = SB(es, [128, D])
        P.dma(bglu[:], b_glu_d.partition_broadcast(128))
        P.dma(gs5[:], gs5_d.partition_broadcast(128))
        P.dma(ghy[:], ghy_d.partition_broadcast(128))
        P.dma(g2[:], g2_d.partition_broadcast(128))
        hx2T = SB(es, [128, 8, TSG], BF16)
        x1t = [SB(es, [128, D]) for _ in range(NTG)]
        scs = [SB(es, [128, 16, 128]) for _ in range(NTG)]
        c16s = [SB(es, [128, 8, 16]) for _ in range(NTG)]
        thrs = [SB(es, [128, 16]) for _ in range(NTG)]
        nbs = [t_[:, 0:8] for t_ in thrs]
        ntaus = [t_[:, 8:16] for t_ in thrs]
        woutb = SB(es, [128, 8, 1024], BF16)
        wglub = SB(es, [128, 4, 512], BF16)
        wqb = SB(es, [128, 8, 2048], BF16)
        kTb = SB(es, [128, 16, 128], BF16)
        P.dma(woutb[:], wout_b[:, :, :], q="sp")
        P.dma(wglub[:], wglu_b[:, :, :], q="act")
        P.dma(wqb[:], wq_b[:, :, :], q="sp")
        P.dma(kTb[:], kT_b[:, :, :], q="act")
        qT = SB(es, [128, 16, TSG], BF16)
        yts = [SB(es, [128, 512]) for _ in range(NTG)]
        zts = [SB(es, [128, 512]) for _ in range(NTG)]
        xt4s = [SB(es, [128, D]) for _ in range(NTG)]
        gys = [SB(es, [128, 512]) for _ in range(NTG)]
        gybs = [SB(es, [128, 512], BF16) for _ in range(NTG)]
        gyTs = [SB(es, [128, 4, 128], BF16) for _ in range(NTG)]
        gpres = [SB(es, [128, 512]) for _ in range(NTG)]
        s5os = [SB(es, [128, 512]) for _ in range(NTG)]
        catbs = [SB(es, [128, D], BF16) for _ in range(NTG)]
        catTs = [SB(es, [128, 8, 128], BF16) for _ in range(NTG)]
        tms = [SB(es, [128, D]) for _ in range(NTG)]
        hb4s = [SB(es, [128, D], BF16) for _ in range(NTG)]
        jks = [SB(es, [128, D], BF16) for _ in range(NTG)]
        sss = [[SB(es, [128, 1]) for _ in range(6)] for _ in range(NTG)]
        wks = [SB(es, [128, 256]) for _ in range(NTG)]
        m16s = [SB(es, [128, 16, 16]) for _ in range(NTG)]
        cands = [SB(es, [128, 8, 256]) for _ in range(NTG)]
        ews = [SB(es, [128, 8, 16]) for _ in range(NTG)]
        zss = [SB(es, [128, 8]) for _ in range(NTG)]
        ptrs = [PS(es, [128, 8, 128], BF16) for _ in range(2)]
        pAB = [PS(es, [128, 512]) for _ in range(2)]
        pMs = [PS(es, [128, D]) for _ in range(2)]
        for g_i in range(NS * L // TSG):
            n = (g_i * TSG) // L
            row0 = g_i * TSG
            if (g_i * TSG) % L == 0:
                P.dma(gx1[:], modx_d[n, :, 2 * D:3 * D])
                P.dma(sh2[:], modx_d[n, :, 3 * D:4 * D])
                P.dma(g2p[:], modx_d[n, :, 4 * D:5 * D])
                V("dve", lambda e: e.scalar_tensor_tensor(out=g2p[:], in0=g2p[:], scalar=1.0, in1=g2[:], op0=ALU.add, op1=ALU.mult), [g2p, g2], [g2p])
            TI = list(range(NTG))

            def ctx(ti):
                return dict(r0=row0 + ti * 128, x1=x1t[ti], yt=yts[ti], zt=zts[ti], xt4=xt4s[ti], gy=gys[ti], gyb=gybs[ti], gyT=gyTs[ti], gpre=gpres[ti],
                            s5o=s5os[ti], catb=catbs[ti], catT=catTs[ti], tm=tms[ti], hb4=hb4s[ti], jk=jks[ti], ss=sss[ti], ptr=ptrs[ti % 2], pG=pAB[ti % 2], pM=pMs[ti % 2])

            for ti in TI:
                c_ = ctx(ti); r0, yt, zt, xt4, gy, gyb = c_["r0"], c_["yt"], c_["zt"], c_["xt4"], c_["gy"], c_["gyb"]
                P.dma(yt[:], y_s[r0:r0 + 128, :], q="sp")
                P.dma(zt[:], z_s[r0:r0 + 128, :], q="act")
                P.dma(xt4[:], x_d[r0:r0 + 128, :], q="sp")
                V("act", lambda e: e.activation(out=gy[:], in_=yt[:], func=AF.Gelu_apprx_tanh), [yt], [gy])
                V("pool", lambda e: e.tensor_copy(out=gyb[:], in_=gy[:]), [gy], [gyb])
            for ti in TI:
                c_ = ctx(ti); gyb, ptr, gyT, pG = c_["gyb"], c_["ptr"], c_["gyT"], c_["pG"]
                for k in range(4):
                    P.op("pe", lambda e, k=k: e.transpose(out=ptr[:, k, :], in_=gyb[:, k * 128:(k + 1) * 128], identity=ident[:]), reads=[gyb, ident], writes=[ptr])
                V("act", lambda e: e.copy(out=gyT[:], in_=ptr[:, 0:4, :]), [ptr], [gyT])
            for ti in TI:
                c_ = ctx(ti); gyT, pG = c_["gyT"], c_["pG"]
                mm(pG[:], [(gyT[:, k, :], wglub[:, k, :]) for k in range(4)], [gyT, wglub], [pG])
            for ti in TI:
                c_ = ctx(ti); pG, gpre = c_["pG"], c_["gpre"]
                V("dve", lambda e: e.tensor_tensor(out=gpre[:], in0=pG[:], in1=bglu[:], op=ALU.add), [pG, bglu], [gpre])
                V("act", lambda e: e.activation(out=gpre[:], in_=gpre[:], func=AF.Sigmoid), [gpre], [gpre])
            for ti in TI:
                c_ = ctx(ti); gy, gpre, s5o, jk, zt = c_["gy"], c_["gpre"], c_["s5o"], c_["jk"], c_["zt"]
                ss1, ss2, ss3, rs1, rs2, rs3 = c_["ss"]
                V("dve", lambda e: e.tensor_tensor(out=s5o[:], in0=gy[:], in1=gpre[:], op=ALU.mult), [gy, gpre], [s5o])
                V("act", lambda e: e.activation(out=jk[:, 0:512], in_=s5o[:], func=AF.Square, accum_out=ss1[:]), [s5o], [jk, ss1])
                V("act", lambda e: e.activation(out=jk[:, 512:1024], in_=zt[:], func=AF.Square, accum_out=ss2[:]), [zt], [jk, ss2])
            for ti in TI:
                c_ = ctx(ti)
                ss1, ss2, ss3, rs1, rs2, rs3 = c_["ss"]
                rsqrt_mean(es, ss1, 512, rs1)
                rsqrt_mean(es, ss2, 512, rs2)
            for ti in TI:
                c_ = ctx(ti); s5o, zt, catb = c_["s5o"], c_["zt"], c_["catb"]
                ss1, ss2, ss3, rs1, rs2, rs3 = c_["ss"]
                V("dve", lambda e: e.scalar_tensor_tensor(out=catb[:, 0:512], in0=s5o[:], scalar=rs1[:, 0:1], in1=gs5[:], op0=ALU.mult, op1=ALU.mult),
                  [s5o, rs1, gs5], [catb])
                V("dve", lambda e: e.scalar_tensor_tensor(out=catb[:, 512:1024], in0=zt[:], scalar=rs2[:, 0:1], in1=ghy[:], op0=ALU.mult, op1=ALU.mult),
                  [zt, rs2, ghy], [catb])
            for ti in TI:
                c_ = ctx(ti); catb, ptr, catT = c_["catb"], c_["ptr"], c_["catT"]
                for k in range(8):
                    P.op("pe", lambda e, k=k: e.transpose(out=ptr[:, k, :], in_=catb[:, k * 128:(k + 1) * 128], identity=ident[:]), reads=[catb, ident], writes=[ptr])
                V("act", lambda e: e.copy(out=catT[:], in_=ptr[:]), [ptr], [catT])
            for ti in TI:
                c_ = ctx(ti); catT, pM = c_["catT"], c_["pM"]
                for hf in range(2):
                    mm(pM[:, hf * 512:(hf + 1) * 512], [(catT[:, k, :], woutb[:, k, hf * 512:(hf + 1) * 512]) for k in range(8)], [catT, woutb], [pM])
            for ti in TI:
                c_ = ctx(ti); pM, tm, x1, xt4, jk, r0 = c_["pM"], c_["tm"], c_["x1"], c_["xt4"], c_["jk"], c_["r0"]
                ss1, ss2, ss3, rs1, rs2, rs3 = c_["ss"]
                V("dve", lambda e: e.tensor_tensor(out=tm[:], in0=pM[:], in1=gx1[:], op=ALU.mult), [pM, gx1], [tm])
                V("pool", lambda e: e.tensor_tensor(out=x1[:], in0=tm[:], in1=xt4[:], op=ALU.add), [tm, xt4], [x1])
                if debug:
                    P.dma(dbg["x1"][r0:r0 + 128, :], x1[:], is_output=True)
                V("act", lambda e: e.activation(out=jk[:], in_=x1[:], func=AF.Square, accum_out=ss3[:]), [x1], [jk, ss3])
            for ti in TI:
                c_ = ctx(ti)
                ss1, ss2, ss3, rs1, rs2, rs3 = c_["ss"]
                rsqrt_mean(es, ss3, D, rs3)
            for ti in TI:
                c_ = ctx(ti); tm, x1, hb4, r0 = c_["tm"], c_["x1"], c_["hb4"], c_["r0"]
                ss1, ss2, ss3, rs1, rs2, rs3 = c_["ss"]
                V("dve", lambda e: e.scalar_tensor_tensor(out=tm[:], in0=x1[:], scalar=rs3[:, 0:1], in1=g2p[:], op0=ALU.mult, op1=ALU.mult),
                  [x1, rs3, g2p], [tm])
                V("pool", lambda e: e.tensor_tensor(out=hb4[:], in0=tm[:], in1=sh2[:], op=ALU.add), [tm, sh2], [hb4])
                if debug:
                    P.dma(dbg["hx2"][r0:r0 + 128, :], hb4[:], is_output=True)
            for ti in TI:
                c_ = ctx(ti); hb4, ptr = c_["hb4"], c_["ptr"]
                for k in range(8):
                    P.op("pe", lambda e, k=k: e.transpose(out=ptr[:, k, :], in_=hb4[:, k * 128:(k + 1) * 128], identity=ident[:]), reads=[hb4, ident], writes=[ptr])
                V("act", lambda e: e.copy(out=hx2T[:, :, ti * 128:(ti + 1) * 128], in_=ptr[:]), [ptr], [hx2T])
            for c in range(16):
                pp = pAB[c % 2]
                mm(pp[:, 0:TSG], [(wqb[:, k, c * 128:(c + 1) * 128], hx2T[:, k, :]) for k in range(8)], [wqb, hx2T], [pp])
                if c % 2:
                    V("act", lambda e: e.copy(out=qT[:, c, :], in_=pp[:, 0:TSG]), [pp], [qT])
                else:
                    V("dve", lambda e: e.tensor_copy(out=qT[:, c, :], in_=pp[:, 0:TSG]), [pp], [qT])
            if not lite:
                for hh in range(4):
                    for ti in TI:
                        sc = scs[ti]
                        ps_ = pAB[ti % 2]
                        for c4 in range(4):
                            c = hh * 4 + c4
                            mm(ps_[:, c4 * 128:(c4 + 1) * 128], [(qT[:, c, ti * 128:(ti + 1) * 128], kTb[:, c, :])], [qT, kTb], [ps_])
                        V("act", lambda e: e.copy(out=sc[:, hh * 4:(hh + 1) * 4, :], in_=ps_[:].rearrange("p (a b) -> p a b", a=4)), [ps_], [sc])
                for c in range(16):
                    for ti in TI:
                        sc, m16 = scs[ti], m16s[ti]
                        V("dve", lambda e: e.max(out=m16[:, c, 0:8], in_=sc[:, c, :]), [sc], [m16])
                    for ti in TI:
                        sc, m16, wk = scs[ti], m16s[ti], wks[ti]
                        V("dve", lambda e: e.match_replace(out=wk[:, 0:128], in_to_replace=m16[:, c, 0:8], in_values=sc[:, c, :], imm_value=-1e30), [sc, m16], [wk])
                    for ti in TI:
                        m16, wk = m16s[ti], wks[ti]
                        V("dve", lambda e: e.max(out=m16[:, c, 8:16], in_=wk[:, 0:128]), [wk], [m16])
                for ti in TI:
                    m16, cand = m16s[ti], cands[ti]
                    m4 = m16[:].rearrange("p (h two) k -> p h two k", two=2)
                    V("dve", lambda e: e.tensor_tensor(out=cand[:].rearrange("p h (a b) -> p h a b", a=16),
                                                       in0=m4[:, :, 0, :].unsqueeze(3).to_broadcast([128, 8, 16, 16]),
                                                       in1=m4[:, :, 1, :].unsqueeze(2).to_broadcast([128, 8, 16, 16]), op=ALU.add), [m16], [cand])
                for h in range(8):
                    for ti in TI:
                        cand, c16 = cands[ti], c16s[ti]
                        V("dve", lambda e: e.max(out=c16[:, h, 0:8], in_=cand[:, h, :]), [cand], [c16])
                    for ti in TI:
                        cand, c16, wk = cands[ti], c16s[ti], wks[ti]
                        V("dve", lambda e: e.match_replace(out=wk[:], in_to_replace=c16[:, h, 0:8], in_values=cand[:, h, :], imm_value=-1e30), [cand, c16], [wk])
                    for ti in TI:
                        c16, wk = c16s[ti], wks[ti]
                        V("dve", lambda e: e.max(out=c16[:, h, 8:16], in_=wk[:]), [wk], [c16])
                for ti in TI:
                    c16, ew, zs, nb = c16s[ti], ews[ti], zss[ti], nbs[ti]
                    V("dve", lambda e: e.tensor_tensor(out=ew[:], in0=c16[:], in1=c16[:, :, 0:1].to_broadcast([128, 8, 16]), op=ALU.subtract), [c16], [ew])
                    V("act", lambda e: e.activation(out=ew[:], in_=ew[:], func=AF.Exp), [ew], [ew])
                for ti in TI:
                    c16, ew, zs, nb = c16s[ti], ews[ti], zss[ti], nbs[ti]
                    V("dve", lambda e: e.tensor_reduce(out=zs[:], in_=ew[:], axis=AX.X, op=ALU.add), [ew], [zs])
                    V("act", lambda e: e.activation(out=zs[:], in_=zs[:], func=AF.Ln), [zs], [zs])
                for ti in TI:
                    c16, ew, zs, nb = c16s[ti], ews[ti], zss[ti], nbs[ti]
                    V("dve", lambda e: e.tensor_tensor(out=nb[:], in0=zs[:], in1=c16[:, :, 0], op=ALU.add), [zs, c16], [nb])
                    ntau_ = ntaus[ti]
                    V("dve", lambda e: e.tensor_scalar(out=ntau_[:], in0=c16[:, :, 15], scalar1=-1e-5, scalar2=None, op0=ALU.add), [c16], [ntau_])
                    V("dve", lambda e: e.tensor_tensor(out=nb[:], in0=ntau_[:], in1=nb[:], op=ALU.subtract), [nb, ntau_], [nb])
                    sc = scs[ti]
                    for h in range(8):
                        V("dve", lambda e: e.tensor_scalar(out=sc[:, 2 * h, :], in0=sc[:, 2 * h, :], scalar1=ntau_[:, h:h + 1], scalar2=None, op0=ALU.subtract),
                          [sc, ntau_], [sc])
            for ti in TI:
                r0 = row0 + ti * 128
                P.dma(x1_s[r0:r0 + 128, :], x1t[ti][:], q="sp")
                if not lite:
                    P.dma(sc_s[r0 // 128], scs[ti][:].rearrange("p a b -> p (a b)"), q="act")
                    P.dma(thr_s[r0 // 128], thrs[ti][:], q="sp")
            for hf2 in range(TSG // TS):
                P.dma(hx2T_s[row0 // TS + hf2], hx2T[:, :, hf2 * TS:(hf2 + 1) * TS], q="act")
    P.barrier()
    if not lite:
      with contextlib.ExitStack() as es:
        gx2 = SB(es, [128, D])
        gfin = SB(es, [128, D])
        P.dma(gfin[:], gf_d.partition_broadcast(128))
        hx2Tb = [SB(es, [128, 8, TS], BF16) for _ in range(2)]
        scsb = [[SB(es, [128, 16, 128]) for _ in range(NT)] for _ in range(2)]
        thrb = [[SB(es, [128, 16]) for _ in range(NT)] for _ in range(2)]
        x1t = [SB(es, [128, D]) for _ in range(NT)]
        NB_G = 4
        Sb = [SB(es, [128, 16, 128]) for _ in range(NB_G)]
        Eb = [SB(es, [128, 16, 128], BF16) for _ in range(NB_G)]
        Mb = [SB(es, [128, 16, 128], BF16) for _ in range(NB_G)]
        Gh = Eb
        Gc = [[SB(es, [128, 2048], BF16) for _ in range(2)] for _ in range(NT)]
        uTc = [SB(es, [128, 8, 512], BF16) for _ in range(2)]
        vc = [SB(es, [128, 4, 1024], BF16) for _ in range(3)]
        actb = [SB(es, [128, 512], BF16) for _ in range(2)]
        wab = [SB(es, [128, 512], BF16) for _ in range(2)]
        waT = [SB(es, [128, 4, 128], BF16) for _ in range(2)]
        tm2 = SB(es, [128, D])
        x2 = SB(es, [128, D])
        jk2 = SB(es, [128, D], BF16)
        ss4 = SB(es, [128, 1])
        rs4 = SB(es, [128, 1])
        ob = [SB(es, [128, D]) for _ in range(1)]
        pO = [PS(es, [128, D]) for _ in range(NT)]
        pA = [PS(es, [128, 512]) for _ in range(2)]
        pW = [PS(es, [128, 4, 128], BF16) for _ in range(2)]
        cnt = {"g": 0, "d": 0}
        for st_i in range(NS * L // TS):
            n = (st_i * TS) // L
            row0 = st_i * TS
            par = st_i % 2
            if (st_i * TS) % L == 0:
                P.dma(gx2[:], modx_d[n, :, 5 * D:6 * D])
            def srcs_of(p_):
                return (scsb[p_], [t_[:, 0:8] for t_ in thrb[p_]], [t_[:, 8:16] for t_ in thrb[p_]])

            def preload(si):
                p_ = si % 2
                P.dma(hx2Tb[p_][:], hx2T_s[si], q="sp")
                for ti in range(NT):
                    r0_ = si * TS + ti * 128
                    P.dma(scsb[p_][ti][:].rearrange("p a b -> p (a b)"), sc_s[r0_ // 128], q="sp")
                    P.dma(thrb[p_][ti][:], thr_s[r0_ // 128], q="sp")

            if st_i == 0:
                preload(0)
            if st_i + 1 < NS * L // TS:
                preload(st_i + 1)
            hx2T = hx2Tb[par]
            for ti in range(NT):
                r0 = row0 + ti * 128
                P.dma(x1t[ti][:], x1_s[r0:r0 + 128, :], q="sp")
            cnt_unused = None

            def g_pair(ic, ulist, srcs):
                scs_, nbs_, ntaus_ = srcs
                st = []
                for (ti, h) in ulist:
                    i3 = cnt["g"] % NB_G
                    cnt["g"] += 1
                    st.append((ti, h, Sb[i3], Eb[i3], Mb[i3], Gh[i3]))
                for (ti, h, S_, E_, M_, G_) in st:
                    sc = scs_[ti]
                    V("pool", lambda e: e.tensor_tensor(out=S_[:], in0=sc[:, 2 * h, ic * 16:(ic + 1) * 16].unsqueeze(2).to_broadcast([128, 16, 128]),
                                                        in1=sc[:, 2 * h + 1, :].unsqueeze(1).to_broadcast([128, 16, 128]), op=ALU.add), [sc], [S_])
                for (ti, h, S_, E_, M_, G_) in st:
                    V("act", lambda e: e.activation(out=S_[:], in_=S_[:], func=AF.Prelu, alpha=KMASK), [S_], [S_])
                for (ti, h, S_, E_, M_, G_) in st:
                    nb = nbs_[ti]
                    gdst = Gc[ti][ic % 2]
                    if h == 0:
                        V("act", lambda e: e.activation(out=gdst[:], in_=S_[:].rearrange("p a b -> p (a b)"), func=AF.Exp, bias=nb[:, h:h + 1]), [S_, nb], [gdst])
                    else:
                        V("act", lambda e: e.activation(out=E_[:], in_=S_[:], func=AF.Exp, bias=nb[:, h:h + 1]), [S_, nb], [E_])
                for (ti, h, S_, E_, M_, G_) in st:
                    gdst = Gc[ti][ic % 2]
                    if h != 0:
                        V("dve", lambda e: e.tensor_tensor(out=gdst[:], in0=gdst[:], in1=E_[:].rearrange("p a b -> p (a b)"), op=ALU.add), [E_, gdst], [gdst])

            def item_ctx(j):
                ic, r = divmod(j, 2 * 4)
                e4, ti = divmod(r, NT)
                ec = ic * 4 + e4
                return dict(ic=ic, e4=e4, ti=ti, ec=ec, u_=uTc[ec % 2], v_=vc[ec % 3], pa=pA[j % 2], a_=actb[j % 2], w_=wab[j % 2], wT=waT[j % 2], pw=pW[j % 2],
                            gsrc=Gc[ti][ic % 2])

            def D1(j):
                c_ = item_ctx(j); ec, ti, u_, v_, pa = c_["ec"], c_["ti"], c_["u_"], c_["v_"], c_["pa"]
                if ti == 0:
                    P.dma(u_[:], uTb_s[ec], q="sp")
                    P.dma(v_[:], vb_s[ec], q="sp")
                mm(pa[:], [(hx2T[:, k, ti * 128:(ti + 1) * 128], u_[:, k, :]) for k in range(8)], [hx2T, u_], [pa])

            def D2(j):
                c_ = item_ctx(j); pa, a_ = c_["pa"], c_["a_"]
                V("act", lambda e: e.activation(out=a_[:], in_=pa[:], func=AF.Gelu_apprx_tanh), [pa], [a_])

            def D3(j):
                c_ = item_ctx(j); a_, w_, gsrc, e4 = c_["a_"], c_["w_"], c_["gsrc"], c_["e4"]
                V("dve", lambda e: e.tensor_tensor(out=w_[:], in0=a_[:], in1=gsrc[:, e4 * 512:(e4 + 1) * 512], op=ALU.mult), [a_, gsrc], [w_])

            def D4(j):
                c_ = item_ctx(j); w_, pw = c_["w_"], c_["pw"]
                for eb in range(4):
                    P.op("pe", lambda e, eb=eb: e.transpose(out=pw[:, eb, :], in_=w_[:, eb * 128:(eb + 1) * 128], identity=ident[:]), reads=[w_, ident], writes=[pw])

            def D5(j):
                c_ = item_ctx(j); pw, wT = c_["pw"], c_["wT"]
                V("act", lambda e: e.copy(out=wT[:], in_=pw[:]), [pw], [wT])

            def D6(j):
                c_ = item_ctx(j); ec, ti, wT, v_ = c_["ec"], c_["ti"], c_["wT"], c_["v_"]
                for hf in range(2):
                    for eb in range(4):
                        first = (ec == 0 and eb == 0)
                        last = (ec == NCH - 1 and eb == 3)
                        P.op("pe", lambda e, hf=hf, eb=eb, first=first, last=last:
                             e.matmul(pO[ti][:, hf * 512:(hf + 1) * 512], lhsT=wT[:, eb, :], rhs=v_[:, eb, hf * 512:(hf + 1) * 512], start=first, stop=last),
                             reads=[wT, v_], writes=[pO[ti]])

            units = [(ti, h) for h in range(8) for ti in range(NT)]
            NJ = 64
            upi = 2
            if st_i == 0:
                for j in range(8):
                    g_pair(0, units[j * upi:(j + 1) * upi], srcs_of(0))
            def ok(j):
                return 0 <= j < NJ

            for s in range(NJ + 3):
                if ok(s - 2):
                    D4(s - 2)
                if s % 2 == 0 and ok(s - 1):
                    D2(s - 1)
                    D3(s - 1)
                if s < NJ:
                    ic, r = divmod(s, 8)
                    if ic + 1 < 8:
                        g_pair(ic + 1, units[r * upi:(r + 1) * upi], srcs_of(par))
                    elif st_i + 1 < NS * L // TS:
                        g_pair(0, units[r * upi:(r + 1) * upi], srcs_of(1 - par))
                if s % 2 == 1 and ok(s - 1):
                    D2(s - 1)
                    D3(s - 1)
                if ok(s - 2):
                    D5(s - 2)
                if ok(s - 3):
                    D6(s - 3)
                if s < NJ:
                    D1(s)
            for ti in range(NT):
                r0 = row0 + ti * 128
                x1 = x1t[ti]
                o_ = ob[0]
                if debug:
                    V("act", lambda e: e.copy(out=tm2[:], in_=pO[ti][:]), [pO[ti]], [tm2])
                    P.dma(dbg["pe"][r0:r0 + 128, :], tm2[:], is_output=True)
                V("dve", lambda e: e.tensor_tensor(out=tm2[:], in0=pO[ti][:], in1=gx2[:], op=ALU.mult), [pO[ti], gx2], [tm2])
                V("pool", lambda e: e.tensor_tensor(out=x2[:], in0=tm2[:], in1=x1[:], op=ALU.add), [tm2, x1], [x2])
                V("act", lambda e: e.activation(out=jk2[:], in_=x2[:], func=AF.Square, accum_out=ss4[:]), [x2], [jk2, ss4])
                rsqrt_mean(es, ss4, D, rs4)
                V("dve", lambda e: e.scalar_tensor_tensor(out=o_[:], in0=x2[:], scalar=rs4[:, 0:1], in1=gfin[:], op0=ALU.mult, op1=ALU.mult),
                  [x2, rs4, gfin], [o_])
                P.dma(out_d[r0:r0 + 128, :], o_[:], q="pool", is_output=True)

    P.emit()
    top.close()
    return nc


def _prep_inputs(inp):
    global CONST
    if CONST is None:
        CONST = _constants()
    f = lambda a: np.ascontiguousarray(np.asarray(a, dtype=np.float32))
    shared = {}
    shared["w_ada"] = f(inp["w_ada"][0])
    shared["b_ada"] = f(inp["b_ada"][0])
    shared["g_norm1"] = f(inp["g_norm1"][0])
    shared["g_norm2"] = f(inp["g_norm2"][0])
    shared["w_in"] = f(inp["w_in"][0])
    shared["b_in"] = f(inp["b_in"][0])
    a_re = np.asarray(inp["s5_a_re"][0]).reshape(64, 64).T
    a_im = np.asarray(inp["s5_a_im"][0]).reshape(64, 64).T
    ls = np.broadcast_to(np.asarray(inp["s5_log_step"][0]).reshape(1, 64), (64, 64))
    shared["s5p"] = f(np.stack([a_re, a_im, ls], axis=1))
    bre = np.asarray(inp["s5_b_re"][0]).reshape(64, 64, 16).transpose(1, 0, 2)
    bim = np.asarray(inp["s5_b_im"][0]).reshape(64, 64, 16).transpose(1, 0, 2)
    shared["s5b"] = f(np.stack([bre, bim], axis=1))
    cre = np.asarray(inp["s5_c_re"][0]).reshape(64, 16, 64).transpose(2, 0, 1)
    cim = np.asarray(inp["s5_c_im"][0]).reshape(64, 16, 64).transpose(2, 0, 1)
    shared["s5c"] = f(np.stack([cre, cim], axis=1))
    d5 = np.asarray(inp["s5_d"][0]).reshape(32, 16)
    shared["s5d"] = f(np.broadcast_to(d5.T[None], (8, 16, 32)).reshape(128, 32))
    shared["w_glu"] = f(inp["w_glu"][0])
    shared["b_glu"] = f(inp["b_glu"][0])
    shared["hy_conv_w"] = f(inp["hy_conv_w"][0])
    shared["hy_conv_b"] = f(inp["hy_conv_b"][0])
    shared["hf_w1"] = f(inp["hf_w1"][0])
    shared["hf_b1"] = f(np.asarray(inp["hf_b1"][0]).reshape(64, 1))
    shared["hf_wh"] = f(inp["hf_wh"][0])
    shared["hf_bh"] = f(np.asarray(inp["hf_bh"][0]).T)
    shared["hf_freq"] = f(np.asarray(inp["hf_freq"][0]).reshape(64, 1))
    shared["hf_wout"] = f(inp["hf_wout"][0])
    shared["hf_decay"] = f(inp["hf_decay"][0])
    shared["hy_d"] = f(inp["hy_d"][0])
    shared["g_out_s5"] = f(inp["g_out_s5"][0])
    shared["g_out_hy"] = f(inp["g_out_hy"][0])
    shared["w_out"] = f(inp["w_out"][0])
    shared["peer_wq"] = f(inp["peer_wq"][0])
    k1 = np.asarray(inp["peer_k1"][0])
    k2 = np.asarray(inp["peer_k2"][0])
    kk = np.stack([k1, k2], axis=1).reshape(16, 128, 128)
    shared["peer_kT"] = f(kk.transpose(2, 0, 1))
    shared["peer_uT"] = f(np.asarray(inp["peer_u"][0]).T)
    shared["peer_v"] = f(inp["peer_v"][0])
    shared["g_final"] = f(inp["g_final"])
    for k, v in CONST.items():
        shared["k_" + k] = v
    maps = []
    x = np.asarray(inp["x"])
    ctx = np.asarray(inp["ctx"])
    c = np.asarray(inp["c"])
    cc = np.asarray(inp["c_ctx"])
    for i in range(NCORES):
        m = dict(shared)
        m["x"] = f(x[i * NS:(i + 1) * NS].reshape(NS * L, D))
        m["ctx"] = f(ctx[i * NS:(i + 1) * NS].reshape(NS * LC, D))
        c5 = np.concatenate([c[i * NS:(i + 1) * NS], cc[None]], axis=0)
        m["cT"] = f(c5.reshape(NS + 1, 8, 128).transpose(2, 1, 0))
        maps.append(m)
    return maps


def kernel(**inputs):
    maps = _prep_inputs(inputs)
    nc = build_program()
    res = run_bass_kernel_spmd(nc, maps, core_ids=list(range(NCORES)))
    out = np.concatenate([np.asarray(r["out"]).reshape(NS, L, D) for r in res.results], axis=0)
    return out.astype(np.float32)
```

```python
import contextlib
import math
import types
import numpy as np
import ml_dtypes
import concourse.bass as bass
import concourse.mybir as mybir
from concourse.bass_utils import run_bass_kernel_spmd

F32 = mybir.dt.float32
BF16 = mybir.dt.bfloat16
I32 = mybir.dt.int32
ALU = mybir.AluOpType
AF = mybir.ActivationFunctionType
AX = mybir.AxisListType

NSLOT = 12
NCORES = 8
NS = 4
L = 2048
D = 1024
LC = 256
EPS = 1e-6
NFFT = 4096


def _freeze(fn):
    if fn.__closure__ is None:
        return fn
    cells = []
    for c in fn.__closure__:
        try:
            cells.append(types.CellType(c.cell_contents))
        except ValueError:
            cells.append(c)
    return types.FunctionType(fn.__code__, fn.__globals__, fn.__name__, fn.__defaults__, tuple(cells))


class Prog:
    DMAQ = ("sp", "act", "pool")

    def __init__(self, nc):
        self.nc = nc
        self.streams = {k: [] for k in ("pe", "act", "dve", "pool", "sp")}
        self.cnt = {}
        self.waited = {k: {} for k in self.streams}
        self.lastw = {}
        self.reads = {}
        self.slot_next = {q: 0 for q in self.DMAQ}
        self.ninst = 0
        self.out_deps = []

    def _key(self, x):
        if isinstance(x, (str, tuple)):
            return x
        t = getattr(x, "tensor", x)
        return getattr(t, "name", None) or id(t)

    def _deps(self, reads, writes):
        deps = {}

        def add(s, c):
            if c > deps.get(s, 0):
                deps[s] = c
        for k in reads:
            lw = self.lastw.get(k)
            if lw:
                add(*lw)
        for k in writes:
            lw = self.lastw.get(k)
            if lw:
                add(*lw)
            for s, c in self.reads.get(k, {}).items():
                add(s, c)
        return deps

    def _emit_waits(self, stream, deps):
        w = self.waited[stream]
        for s, c in deps.items():
            if s == stream and stream == "pe":
                continue
            if c > w.get(s, 0):
                w[s] = c
                self.streams[stream].append(("wait", s, c))

    def _commit(self, sem, cnt, reads, writes):
        for k in writes:
            self.lastw[k] = (sem, cnt)
            self.reads[k] = {}
        for k in reads:
            self.reads.setdefault(k, {})
            if self.reads[k].get(sem, 0) < cnt:
                self.reads[k][sem] = cnt

    def op(self, stream, fn, reads=(), writes=()):
        reads = [self._key(r) for r in reads]
        writes = [self._key(r) for r in writes]
        deps = self._deps(reads, writes)
        self._emit_waits(stream, deps)
        c = self.cnt.get(stream, 0) + 1
        self.cnt[stream] = c
        self.streams[stream].append(("op", _freeze(fn), stream, 1))
        self._commit(stream, c, reads, writes)
        self.ninst += 1

    def dma(self, out, in_, reads=None, writes=None, q="sp", is_output=False, **kw):
        reads = [self._key(r) for r in (reads if reads is not None else [in_])]
        writes = [self._key(r) for r in (writes if writes is not None else [out])]
        slot = self.slot_next[q]
        self.slot_next[q] = (slot + 1) % NSLOT
        sem = ("dma", q, slot)
        deps = self._deps(reads, writes)
        prev = self.cnt.get(sem, 0)
        if prev:
            deps[sem] = max(deps.get(sem, 0), prev)
        self._emit_waits(q, deps)
        c = prev + 16
        self.cnt[sem] = c
        self.streams[q].append(("dma", out, in_, kw, sem))
        self._commit(sem, c, reads, writes)
        if is_output:
            self.out_deps.append((sem, c))
        self.ninst += 1

    def barrier(self):
        allc = dict(self.cnt)
        for st in self.streams:
            self._emit_waits(st, allc)

    def emit(self):
        nc = self.nc
        fin = {}
        for s, c in self.out_deps:
            fin[s] = max(fin.get(s, 0), c)
        self._emit_waits("sp", fin)
        semkeys = list(self.cnt.keys())
        with contextlib.ExitStack() as es:
            sems = {}
            for i, k in enumerate(semkeys):
                sems[k] = es.enter_context(nc.semaphore("s%d" % i))
            block = es.enter_context(nc.Block())
            engs = {"pe": block.tensor, "act": block.scalar, "dve": block.vector,
                    "pool": block.gpsimd, "sp": block.sync}

            def make(stream):
                items = self.streams[stream]

                def body(eng):
                    for it in items:
                        if it[0] == "wait":
                            eng.wait_ge(sems[it[1]], it[2])
                        elif it[0] == "op":
                            it[1](eng).then_inc(sems[it[2]], 1)
                        else:
                            _, out, in_, kw, sem = it
                            eng.dma_start(out=out, in_=in_, **kw).then_inc(sems[sem], 16)
                return body
            for st in ("sp", "act", "pool", "dve", "pe"):
                if self.streams[st]:
                    engs[st](make(st))


def _constants():
    c = {}
    c["ident"] = np.eye(128, dtype=np.float32).astype(ml_dtypes.bfloat16)
    c["identf"] = np.eye(128, dtype=np.float32)
    sm = np.zeros((128, 128), np.float32)
    sp = np.zeros((128, 128), np.float32)
    for t in range(128):
        if t % 64 != 0:
            sm[t - 1, t] = 1
        if t % 64 != 63:
            sp[t + 1, t] = 1
    c["shm"] = sm.astype(ml_dtypes.bfloat16)
    c["shp"] = sp.astype(ml_dtypes.bfloat16)
    t = np.arange(L, dtype=np.float64) + 0.5
    ang = 2 * np.pi * np.outer(t, t) / NFFT
    c["ctab"] = np.cos(ang).astype(np.float32).astype(ml_dtypes.bfloat16)
    c["stab"] = np.sin(ang).astype(np.float32).astype(ml_dtypes.bfloat16)
    phi = np.pi * (np.arange(L, dtype=np.float64) + 0.5) / NFFT
    c["cphi"] = np.cos(phi).astype(np.float32).reshape(16, 128).T.copy()
    c["sphi"] = np.sin(phi).astype(np.float32).reshape(16, 128).T.copy()
    pos = np.arange(L, dtype=np.float32)
    tt = pos / np.float32(L - 1)
    bands = np.linspace(1e-4, 15, 16, dtype=np.float32)
    a = (np.float32(2.0 * math.pi / L) * pos[:, None]) * bands[None, :]
    feats = np.concatenate([tt[:, None], np.cos(a), -np.sin(a)], axis=-1).astype(np.float32)
    c["featsT"] = feats.T.copy()
    c["tneg"] = (-tt).reshape(16, 128).T.copy()
    j = np.repeat(np.arange(8), 16).astype(np.float32)
    ex = np.stack([7 - j, j, j + 1, 8 - j, j - 7, -j])
    c["s5exp"] = np.broadcast_to(ex[None], (64, 6, 128)).astype(np.float32).copy()
    jj = np.repeat(np.arange(8), 16)
    c["mf"] = (jj[None, :] >= jj[:, None]).astype(np.float32)
    c["mb"] = (jj[:, None] >= jj[None, :]).astype(np.float32)
    m0 = np.ones((128, 1), np.float32)
    m0[0, 0] = 0.0
    c["m0"] = m0
    c["onesf"] = np.ones((128, 128), np.float32)
    c["mix"] = np.broadcast_to(np.arange(256, dtype=np.float32)[None], (64, 256)).copy()
    return c


CONST = None


def build_program(upto=99, debug=False, lite=False):
    nc = bass.Bass("TRN2", target_bir_lowering=False)
    P = Prog(nc)
    dbg = {}

    def din(name, shape, dt=F32):
        return nc.dram_tensor(name, list(shape), dt, kind="ExternalInput").ap()

    def dscr(name, shape, dt=F32):
        return nc.dram_tensor(name, list(shape), dt).ap()

    def dout(name, shape, dt=F32):
        return nc.dram_tensor(name, list(shape), dt, kind="ExternalOutput").ap()

    x_d = din("x", [NS * L, D])
    ctx_d = din("ctx", [NS * LC, D])
    cT_d = din("cT", [128, 8, NS + 1])
    w_ada = din("w_ada", [D, 6 * D])
    b_ada = din("b_ada", [6 * D])
    g1_d = din("g_norm1", [D])
    g2_d = din("g_norm2", [D])
    w_in_d = din("w_in", [D, 2048])
    b_in_d = din("b_in", [2048])
    s5p_d = din("s5p", [64, 3, 64])
    s5b_d = din("s5b", [64, 2, 64, 16])
    s5c_d = din("s5c", [64, 2, 64, 16])
    s5d_d = din("s5d", [128, 32])
    w_glu_d = din("w_glu", [512, 512])
    b_glu_d = din("b_glu", [512])
    cw_d = din("hy_conv_w", [3, 1536])
    cb_d = din("hy_conv_b", [1536])
    hf_w1 = din("hf_w1", [33, 64])
    hf_b1 = din("hf_b1", [64, 1])
    hf_wh = din("hf_wh", [2, 64, 64])
    hf_bh = din("hf_bh", [64, 2])
    hf_fr = din("hf_freq", [64, 1])
    hf_wout = din("hf_wout", [64, 2048])
    hf_dec = din("hf_decay", [2048])
    hyd_d = din("hy_d", [2, 512])
    gs5_d = din("g_out_s5", [512])
    ghy_d = din("g_out_hy", [512])
    w_out_d = din("w_out", [D, D])
    wq_d = din("peer_wq", [D, 2048])
    kT_d = din("peer_kT", [128, 16, 128])
    uT_d = din("peer_uT", [D, 16384 if not lite else 128])
    v_d = din("peer_v", [16384 if not lite else 128, D])
    gf_d = din("g_final", [D])
    cst = {k: din("k_" + k, list(v.shape), BF16 if v.dtype == ml_dtypes.bfloat16 else F32)
           for k, v in CONST.items()}
    out_d = dout("out", [NS * L, D])

    modx_d = dscr("modx_s", [NS + 1, 128, 6 * D])
    q_s = dscr("q_s", [NS * L, 1536])
    y_s = dscr("y_s", [NS * L, 512])
    z_s = dscr("z_s", [NS * L, 512])
    kspec_s = dscr("kspec_s", [2, 2, 16, 128, 512])
    uTb_s = dscr("uTb_s", [32, 128, 8, 512], BF16)
    vb_s = dscr("vb_s", [32, 128, 4, 1024], BF16)

    if debug:
        dbg["modx"] = dout("dbg_modx", [NS + 1, 6 * D])

    top = contextlib.ExitStack()
    uid = [0]

    def SB(es, shape, dt=F32, name=None):
        uid[0] += 1
        return es.enter_context(nc.sbuf_tensor((name or "t") + str(uid[0]), list(shape), dt))

    def PS(es, shape, dt=F32, name=None):
        uid[0] += 1
        return es.enter_context(nc.psum_tensor((name or "p") + str(uid[0]), list(shape), dt))

    def mm(ps_ap, pairs, reads, writes):
        n = len(pairs)
        for i, (l, r) in enumerate(pairs):
            P.op("pe", lambda e, l=l, r=r, i=i: e.matmul(ps_ap, lhsT=l, rhs=r, start=(i == 0), stop=(i == n - 1)),
                 reads=reads, writes=writes)

    def V(eng, fn, reads, writes):
        P.op(eng, fn, reads=reads, writes=writes)

    ident = SB(top, [128, 128], BF16, "ident")
    identf = SB(top, [128, 128], F32, "identf")
    mhalf = SB(top, [128, 1], F32, "mhalf")
    P.dma(ident[:], cst["ident"][:, :])
    P.dma(identf[:], cst["identf"][:, :])
    V("pool", lambda e: e.memset(mhalf[:], -0.5), [], [mhalf])

    def rsqrt_mean(es, ssum, n, out_rstd):
        V("dve", lambda e: e.tensor_scalar(out=out_rstd[:], in0=ssum[:], scalar1=1.0 / n, scalar2=EPS,
                                           op0=ALU.mult, op1=ALU.add), [ssum], [out_rstd])
        V("pool", lambda e: e.tensor_tensor(out=out_rstd[:], in0=out_rstd[:], in1=mhalf[:], op=ALU.pow),
          [out_rstd, mhalf], [out_rstd])

    with contextlib.ExitStack() as es:
        cT = SB(es, [128, 8, NS + 1])
        sil = SB(es, [128, 8, NS + 1])
        rep = SB(es, [128, NS + 1, 8, 128])
        bada = SB(es, [128, 6 * D])
        P.dma(cT[:], cT_d[:, :, :])
        P.dma(bada[:], b_ada.partition_broadcast(128))
        V("act", lambda e: e.activation(out=sil[:], in_=cT[:], func=AF.Silu), [cT], [sil])
        for n in range(NS + 1):
            V("dve", lambda e, n=n: e.tensor_copy(out=rep[:, n], in_=sil[:, :, n:n + 1].to_broadcast([128, 8, 128])),
              [sil], [rep])
        wts = [SB(es, [128, 8, 512]) for _ in range(2)]
        mps = [PS(es, [128, 512]) for _ in range(2)]
        mo = [SB(es, [128, 512]) for _ in range(2)]
        it = 0
        for nt in range(12):
            wt = wts[nt % 2]
            P.dma(wt[:], w_ada[:, nt * 512:(nt + 1) * 512].rearrange("(k p) n -> p k n", p=128))
            for n in range(NS + 1):
                ps = mps[it % 2]
                o = mo[it % 2]
                it += 1
                mm(ps[:], [(rep[:, n, k, :], wt[:, k, :]) for k in range(8)], [rep, wt], [ps])
                V("dve", lambda e, ps=ps, o=o, nt=nt: e.tensor_tensor(out=o[:], in0=ps[:], in1=bada[:, nt * 512:(nt + 1) * 512],
                                                                      op=ALU.add), [ps, bada], [o])
                P.dma(modx_d[n, :, nt * 512:(nt + 1) * 512], o[:], q="act")
                if debug:
                    P.dma(dbg["modx"][n:n + 1, nt * 512:(nt + 1) * 512], o[0:1, :], q="act", is_output=True)
    P.barrier()
    if upto <= 0:
        P.emit()
        top.close()
        return nc


    KMASK = 1.0e5
    NEXP = 16384 if not lite else 128
    NCH = NEXP // 512 if not lite else 0
    TS = 256
    NT = TS // 128
    wq_b = dscr("wq_b", [128, 8, 2048], BF16)
    wout_b = dscr("wout_b", [128, 8, 1024], BF16)
    wglu_b = dscr("wglu_b", [128, 4, 512], BF16)
    kT_b = dscr("kT_b", [128, 16, 128], BF16)
    if debug:
        dbg["x1"] = dout("dbg_x1", [NS * L, D])
        dbg["hx2"] = dout("dbg_hx2", [NS * L, D], BF16)
        dbg["pe"] = dout("dbg_pe", [NS * L, D])
    jobs = []
    for k in range(8):
        jobs.append((wq_b[:, k, :], wq_d[k * 128:(k + 1) * 128, :], 2048))
    for k in range(8):
        jobs.append((wout_b[:, k, :], w_out_d[k * 128:(k + 1) * 128, :], 1024))
    for k in range(4):
        jobs.append((wglu_b[:, k, :], w_glu_d[k * 128:(k + 1) * 128, :], 512))
    jobs.append((kT_b[:].rearrange("p c n -> p (c n)"), kT_d[:].rearrange("p c n -> p (c n)"), 2048))
    for ec in range(NCH):
        jobs.append((uTb_s[ec].rearrange("p k e -> p (k e)"), uT_d[:, ec * 512:(ec + 1) * 512].rearrange("(k p) e -> p k e", p=128), 4096))
        jobs.append((vb_s[ec].rearrange("p b d -> p (b d)"), v_d[ec * 512:(ec + 1) * 512, :].rearrange("(b p) d -> p b d", p=128), 4096))
    job_i = [0]

    def emit_job(stg, stb):
        i = job_i[0]
        if i >= len(jobs):
            return
        job_i[0] += 1
        dst, srcap, nel = jobs[i]
        a = stg[i % 2]
        b = stb[i % 2]
        if len(srcap.shape) == 3:
            P.dma(a[:, 0:nel].rearrange("p (k e) -> p k e", k=srcap.shape[1]), srcap, q="sp")
        else:
            P.dma(a[:, 0:nel], srcap, q="sp")
        V("act", lambda e: e.copy(out=b[:, 0:nel], in_=a[:, 0:nel]), [a], [b])
        P.dma(dst, b[:, 0:nel], q="sp")

    U_s = dscr("U_s", [32, 128, NS * 256 + 128], BF16)
    if debug:
        dbg["U"] = dout("dbg_U", [32, 128, NS * 256 + 128], BF16)
        dbg["q"] = dout("dbg_q", [NS * L, 1536])
    with contextlib.ExitStack() as es:
        w_in = SB(es, [128, 8, 2048], BF16)
        stage = [SB(es, [128, 2048]) for _ in range(2)]
        for k in range(8):
            st = stage[k % 2]
            P.dma(st[:], w_in_d[k * 128:(k + 1) * 128, :])
            V("dve", lambda e, st=st, k=k: e.tensor_copy(out=w_in[:, k, :], in_=st[:]), [st], [w_in])
        b_in = SB(es, [128, 2048])
        g1 = SB(es, [128, D])
        cw = SB(es, [128, 3, 1536])
        cb = SB(es, [128, 1536])
        shm = SB(es, [128, 128], BF16)
        shp = SB(es, [128, 128], BF16)
        P.dma(b_in[:], b_in_d.partition_broadcast(128))
        P.dma(g1[:], g1_d.partition_broadcast(128))
        for j in range(3):
            P.dma(cw[:, j, :], cw_d[j].partition_broadcast(128))
        P.dma(cb[:], cb_d.partition_broadcast(128))
        P.dma(shm[:], cst["shm"][:, :])
        P.dma(shp[:], cst["shp"][:, :])
        hT = SB(es, [128, 8, 1024], BF16)
        um2 = SB(es, [128, 32, 8, 16], BF16)
        ug = [SB(es, [128, 8, 128], BF16) for _ in range(2)]
        xt = [SB(es, [128, D]) for _ in range(2)]
        tmp = SB(es, [128, D])
        hb = [SB(es, [128, D], BF16) for _ in range(2)]
        g1p = SB(es, [128, D])
        sh1 = SB(es, [128, D])
        ssum = [SB(es, [128, 1]) for _ in range(2)]
        rstd = [SB(es, [128, 1]) for _ in range(2)]
        junk = SB(es, [128, D], BF16)
        pbf = SB(es, [128, 1536], BF16)
        t1 = SB(es, [128, 1536])
        t2 = SB(es, [128, 1536])
        pA = PS(es, [128, 1536])
        pB = PS(es, [128, 1536])
        trp = [PS(es, [128, 8, 128], BF16) for _ in range(1)]
        ps5 = PS(es, [128, 512])

        cstg = [SB(es, [128, 4096]) for _ in range(2)]
        cstb = [SB(es, [128, 4096], BF16) for _ in range(2)]
        blocks = [("x", n, half) for n in range(NS) for half in range(2)] + [("c", NS, 0)]
        cur_mod = None
        for bi, (kind, n, half) in enumerate(blocks):
            if cur_mod != n:
                cur_mod = n
                P.dma(sh1[:], modx_d[n, :, 0:D])
                P.dma(g1p[:], modx_d[n, :, D:2 * D])
                V("dve", lambda e: e.scalar_tensor_tensor(out=g1p[:], in0=g1p[:], scalar=1.0, in1=g1[:],
                                                          op0=ALU.add, op1=ALU.mult), [g1p, g1], [g1p])
            src = x_d if kind == "x" else ctx_d
            row0 = (n * L + half * 1024) if kind == "x" else 0
            for ti in range(8):
                X = xt[ti % 2]
                H = hb[ti % 2]
                ss = ssum[ti % 2]
                rs = rstd[ti % 2]
                P.dma(X[:], src[row0 + ti * 128: row0 + (ti + 1) * 128, :], q="sp" if ti % 2 else "act")
                V("act", lambda e, X=X, ss=ss: e.activation(out=junk[:], in_=X[:], func=AF.Square, accum_out=ss[:]),
                  [X], [junk, ss])
                rsqrt_mean(es, ss, D, rs)
                V("dve", lambda e, X=X, rs=rs: e.scalar_tensor_tensor(out=tmp[:], in0=X[:], scalar=rs[:, 0:1], in1=g1p[:],
                                                                      op0=ALU.mult, op1=ALU.mult), [X, rs, g1p], [tmp])
                V("pool", lambda e, H=H: e.tensor_tensor(out=H[:], in0=tmp[:], in1=sh1[:], op=ALU.add), [tmp, sh1], [H])
                tp = trp[0]
                for k in range(8):
                    P.op("pe", lambda e, k=k, H=H, tp=tp: e.transpose(out=tp[:, k, :], in_=H[:, k * 128:(k + 1) * 128], identity=ident[:]),
                         reads=[H, ident], writes=[tp])
                V("act", lambda e, tp=tp, ti=ti: e.copy(out=hT[:, :, ti * 128:(ti + 1) * 128], in_=tp[:]), [tp], [hT])
                emit_job(cstg, cstb)
            if kind == "x":
                for ti in range(8):
                    for nt in range(3):
                        mm(pA[:, nt * 512:(nt + 1) * 512],
                           [(hT[:, k, ti * 128:(ti + 1) * 128], w_in[:, k, 512 + nt * 512: 512 + (nt + 1) * 512]) for k in range(8)],
                           [hT, w_in], [pA])
                    V("dve", lambda e: e.tensor_tensor(out=pbf[:], in0=pA[:], in1=b_in[:, 512:2048], op=ALU.add), [pA, b_in], [pbf])
                    for nt in range(3):
                        mm(pB[:, nt * 512:(nt + 1) * 512], [(shm[:], pbf[:, nt * 512:(nt + 1) * 512])], [shm, pbf], [pB])
                    for nt in range(3):
                        mm(pA[:, nt * 512:(nt + 1) * 512], [(shp[:], pbf[:, nt * 512:(nt + 1) * 512])], [shp, pbf], [pA])
                    V("pool", lambda e: e.tensor_tensor(out=t1[:], in0=pbf[:], in1=cw[:, 1, :], op=ALU.mult), [pbf, cw], [t1])
                    V("pool", lambda e: e.tensor_tensor(out=t1[:], in0=t1[:], in1=cb[:], op=ALU.add), [t1, cb], [t1])
                    V("dve", lambda e: e.tensor_tensor(out=t2[:], in0=pB[:], in1=cw[:, 0, :], op=ALU.mult), [pB, cw], [t2])
                    V("pool", lambda e: e.tensor_tensor(out=t1[:], in0=t1[:], in1=t2[:], op=ALU.add), [t1, t2], [t1])
                    V("dve", lambda e: e.tensor_tensor(out=t2[:], in0=pA[:], in1=cw[:, 2, :], op=ALU.mult), [pA, cw], [t2])
                    V("pool", lambda e: e.tensor_tensor(out=t1[:], in0=t1[:], in1=t2[:], op=ALU.add), [t1, t2], [t1])
                    r0 = row0 + ti * 128
                    P.dma(q_s[r0:r0 + 128, :], t1[:], q="act")
                    if debug:
                        P.dma(dbg["q"][r0:r0 + 128, :], t1[:], q="act", is_output=True)
            for j in range(8):
                mm(ps5[:], [(hT[:, k, j::8], w_in[:, k, 0:512]) for k in range(8)], [hT, w_in], [ps5])
                V("dve", lambda e, j=j: e.tensor_tensor(out=um2[:, :, j, :], in0=ps5[:].rearrange("p (g h) -> p g h", g=32),
                                                        in1=b_in[:, 0:512].rearrange("p (g h) -> p g h", g=32), op=ALU.add),
                  [ps5, b_in], [um2])
            col0 = (n * 256 + half * 128) if kind == "x" else NS * 256
            for gb in range(4):
                tp = trp[0]
                ugt = ug[gb % 2]
                for gi in range(8):
                    g = gb * 8 + gi
                    P.op("pe", lambda e, g=g, gi=gi, tp=tp: e.transpose(out=tp[:, gi, :], in_=um2[:, g].rearrange("p j h -> p (j h)"),
                                                                        identity=ident[:]), reads=[um2, ident], writes=[tp])
                V("act", lambda e, tp=tp, ugt=ugt: e.copy(out=ugt[:], in_=tp[:]), [tp], [ugt])
                P.dma(U_s[gb * 8:(gb + 1) * 8, :, col0:col0 + 128].rearrange("g p c -> p g c"), ugt[:], q="sp")
                if debug:
                    P.dma(dbg["U"][gb * 8:(gb + 1) * 8, :, col0:col0 + 128].rearrange("g p c -> p g c"), ugt[:], q="sp", is_output=True)
        while job_i[0] < len(jobs):
            emit_job(cstg, cstb)
    P.barrier()
    if upto <= 1:
        P.emit()
        top.close()
        return nc


    TWO_PI = 2.0 * math.pi
    PI_LO = 3.1415925

    def trig(ang, n_free_shape, scratch, out_sin=None, out_cos=None):
        ki, kf, r = scratch
        for out, off in ((out_sin, 0.0), (out_cos, math.pi / 2)):
            if out is None:
                continue
            V("dve", lambda e, off=off: e.tensor_scalar(out=kf, in0=ang, scalar1=off, scalar2=1.0 / TWO_PI, op0=ALU.add, op1=ALU.mult),
              [ang], [kf])
            V("dve", lambda e: e.tensor_copy(out=ki, in_=kf), [kf], [ki])
            V("dve", lambda e: e.tensor_copy(out=kf, in_=ki), [ki], [kf])
            V("dve", lambda e: e.scalar_tensor_tensor(out=r, in0=kf, scalar=-TWO_PI, in1=ang, op0=ALU.mult, op1=ALU.add), [kf, ang], [r])
            V("dve", lambda e, off=off: e.tensor_scalar(out=r, in0=r, scalar1=off, scalar2=PI_LO, op0=ALU.add, op1=ALU.min), [r], [r])
            V("dve", lambda e: e.tensor_scalar(out=r, in0=r, scalar1=-PI_LO, scalar2=None, op0=ALU.max), [r], [r])
            V("act", lambda e, out=out: e.activation(out=out, in_=r, func=AF.Sin), [r], [out])

    if debug:
        dbg["y"] = dout("dbg_y", [NS * L, 512])
        dbg["h0"] = dout("dbg_h0", [64, 2, 64, NS])
    s5es = contextlib.ExitStack()
    R8 = SB(s5es, [128, 64, 2, 64], BF16)
    O8 = SB(s5es, [64, 64, 2, 128], BF16)
    T8 = SB(s5es, [128, 32, 128], BF16)
    th8 = SB(s5es, [64, 64])
    r8 = SB(s5es, [64, 64])
    with contextlib.ExitStack() as es:
        s5p = SB(es, [64, 3, 64])
        s5b = SB(es, [64, 2, 64, 16])
        s5c = SB(es, [64, 2, 64, 16])
        s5d = SB(es, [128, 32])
        sexp = SB(es, [64, 6, 128])
        mf = SB(es, [128, 128])
        mb = SB(es, [128, 128])
        P.dma(s5p[:], s5p_d[:, :, :])
        P.dma(s5b[:], s5b_d[:, :, :, :])
        P.dma(s5c[:], s5c_d[:, :, :, :])
        P.dma(s5d[:], s5d_d[:, :])
        P.dma(sexp[:], cst["s5exp"][:, :, :])
        P.dma(mf[:], cst["mf"][:, :])
        P.dma(mb[:], cst["mb"][:, :])
        step = SB(es, [64, 64])
        rho = SB(es, [64, 64])
        th = SB(es, [64, 64])
        ebase = SB(es, [64, 64])
        V("pool", lambda e: e.memset(ebase[:], math.e), [], [ebase])
        V("pool", lambda e: e.tensor_tensor(out=step[:], in0=ebase[:], in1=s5p[:, 2, :], op=ALU.pow), [ebase, s5p], [step])
        V("dve", lambda e: e.tensor_tensor(out=rho[:], in0=s5p[:, 0, :], in1=step[:], op=ALU.mult), [s5p, step], [rho])
        V("dve", lambda e: e.tensor_tensor(out=th[:], in0=s5p[:, 1, :], in1=step[:], op=ALU.mult), [s5p, step], [th])
        ki_s = SB(es, [64, 1024], I32)
        kf_s = SB(es, [64, 1024])
        r_s = SB(es, [64, 1024])
        sn = SB(es, [64, 1024])
        cs = SB(es, [64, 1024])
        mg = SB(es, [64, 1024])
        ang = SB(es, [64, 1024])
        V("dve", lambda e: e.tensor_copy(out=ang[:, 0:64], in_=th[:]), [th], [ang])
        trig(ang[:, 0:64], None, (ki_s[:, 0:64], kf_s[:, 0:64], r_s[:, 0:64]), sn[:, 0:64], cs[:, 0:64])
        V("act", lambda e: e.activation(out=mg[:, 0:64], in_=rho[:], func=AF.Exp), [rho], [mg])
        l1re = SB(es, [64, 64])
        l1im = SB(es, [64, 64])
        V("dve", lambda e: e.tensor_tensor(out=l1re[:], in0=mg[:, 0:64], in1=cs[:, 0:64], op=ALU.mult), [mg, cs], [l1re])
        V("dve", lambda e: e.tensor_tensor(out=l1im[:], in0=mg[:, 0:64], in1=sn[:, 0:64], op=ALU.mult), [mg, sn], [l1im])
        V("dve", lambda e: e.tensor_scalar(out=l1re[:], in0=l1re[:], scalar1=-1.0, scalar2=None, op0=ALU.add), [l1re], [l1re])
        den = SB(es, [64, 64])
        t64 = SB(es, [64, 64])
        cre = SB(es, [64, 64])
        cim = SB(es, [64, 64])
        are = s5p[:, 0, :]
        aim = s5p[:, 1, :]
        V("dve", lambda e: e.tensor_tensor(out=den[:], in0=are, in1=are, op=ALU.mult), [s5p], [den])
        V("dve", lambda e: e.tensor_tensor(out=t64[:], in0=aim, in1=aim, op=ALU.mult), [s5p], [t64])
        V("dve", lambda e: e.tensor_tensor(out=den[:], in0=den[:], in1=t64[:], op=ALU.add), [den, t64], [den])
        V("dve", lambda e: e.reciprocal(out=den[:], in_=den[:]), [den], [den])
        V("dve", lambda e: e.tensor_tensor(out=cre[:], in0=l1re[:], in1=are, op=ALU.mult), [l1re, s5p], [cre])
        V("dve", lambda e: e.tensor_tensor(out=t64[:], in0=l1im[:], in1=aim, op=ALU.mult), [l1im, s5p], [t64])
        V("dve", lambda e: e.tensor_tensor(out=cre[:], in0=cre[:], in1=t64[:], op=ALU.add), [cre, t64], [cre])
        V("dve", lambda e: e.tensor_tensor(out=cre[:], in0=cre[:], in1=den[:], op=ALU.mult), [cre, den], [cre])
        V("dve", lambda e: e.tensor_tensor(out=cim[:], in0=l1im[:], in1=are, op=ALU.mult), [l1im, s5p], [cim])
        V("dve", lambda e: e.tensor_tensor(out=t64[:], in0=l1re[:], in1=aim, op=ALU.mult), [l1re, s5p], [t64])
        V("dve", lambda e: e.tensor_tensor(out=cim[:], in0=cim[:], in1=t64[:], op=ALU.subtract), [cim, t64], [cim])
        V("dve", lambda e: e.tensor_tensor(out=cim[:], in0=cim[:], in1=den[:], op=ALU.mult), [cim, den], [cim])
        bbre = SB(es, [64, 64, 16])
        bbim = SB(es, [64, 64, 16])
        tb = SB(es, [64, 64, 16])
        creb = cre[:].unsqueeze(2).to_broadcast([64, 64, 16])
        cimb = cim[:].unsqueeze(2).to_broadcast([64, 64, 16])
        V("dve", lambda e: e.tensor_tensor(out=bbre[:], in0=s5b[:, 0], in1=creb, op=ALU.mult), [s5b, cre], [bbre])
        V("dve", lambda e: e.tensor_tensor(out=tb[:], in0=s5b[:, 1], in1=cimb, op=ALU.mult), [s5b, cim], [tb])
        V("dve", lambda e: e.tensor_tensor(out=bbre[:], in0=bbre[:], in1=tb[:], op=ALU.subtract), [bbre, tb], [bbre])
        V("dve", lambda e: e.tensor_tensor(out=bbim[:], in0=s5b[:, 1], in1=creb, op=ALU.mult), [s5b, cre], [bbim])
        V("dve", lambda e: e.tensor_tensor(out=tb[:], in0=s5b[:, 0], in1=cimb, op=ALU.mult), [s5b, cim], [tb])
        V("dve", lambda e: e.tensor_tensor(out=bbim[:], in0=bbim[:], in1=tb[:], op=ALU.add), [bbim, tb], [bbim])
        V("dve", lambda e: e.tensor_scalar(out=ang[:, 0:64], in0=th[:], scalar1=8.0, scalar2=None, op0=ALU.mult), [th], [ang])
        V("dve", lambda e: e.tensor_scalar(out=kf_s[:, 0:64], in0=ang[:, 0:64], scalar1=1.0 / TWO_PI, scalar2=None, op0=ALU.mult), [ang], [kf_s])
        V("dve", lambda e: e.tensor_copy(out=ki_s[:, 0:64], in_=kf_s[:, 0:64]), [kf_s], [ki_s])
        V("dve", lambda e: e.tensor_copy(out=kf_s[:, 0:64], in_=ki_s[:, 0:64]), [ki_s], [kf_s])
        V("dve", lambda e: e.scalar_tensor_tensor(out=th8[:], in0=kf_s[:, 0:64], scalar=-TWO_PI, in1=ang[:, 0:64], op0=ALU.mult, op1=ALU.add),
          [kf_s, ang], [th8])
        V("act", lambda e: e.activation(out=r8[:], in_=rho[:], func=AF.Exp, scale=8.0), [rho], [r8])
        Lre = SB(es, [64, 3, 8, 128])
        Lim = SB(es, [64, 3, 8, 128])
        RT = SB(es, [64, 2, 8, 128])
        OP = SB(es, [64, 2, 8, 128])
        OO = SB(es, [64, 2, 8, 128])
        tw = SB(es, [64, 8, 128])
        pT = [PS(es, [128, 128]) for _ in range(2)]
        pR = PS(es, [128, 4, 64])
        tt8 = SB(es, [128, 128])
        tt8b = SB(es, [128, 128])
        for d in range(2):
            for gb in range(4):
                dg0 = d * 32 + gb * 8
                for kk, kind in enumerate((d, 2 + d, 4 + d)):
                    a3 = ang[:].rearrange("p (a c) -> p a c", a=8)
                    exb = sexp[:, kind, :].unsqueeze(1).to_broadcast([64, 8, 128])
                    V("dve", lambda e, exb=exb, dg0=dg0: e.tensor_tensor(out=a3, in0=exb, in1=th[:, dg0:dg0 + 8].unsqueeze(2).to_broadcast([64, 8, 128]),
                                                                         op=ALU.mult), [sexp, th], [ang])
                    trig(ang[:], None, (ki_s[:], kf_s[:], r_s[:]), sn[:], cs[:])
                    m3 = mg[:].rearrange("p (a c) -> p a c", a=8)
                    V("dve", lambda e, exb=exb, dg0=dg0: e.tensor_tensor(out=m3, in0=exb, in1=rho[:, dg0:dg0 + 8].unsqueeze(2).to_broadcast([64, 8, 128]),
                                                                         op=ALU.mult), [sexp, rho], [mg])
                    V("act", lambda e: e.activation(out=mg[:], in_=mg[:], func=AF.Exp), [mg], [mg])
                    V("dve", lambda e, kk=kk: e.tensor_tensor(out=Lre[:, kk].rearrange("p a c -> p (a c)"), in0=mg[:], in1=cs[:], op=ALU.mult), [mg, cs], [Lre])
                    V("dve", lambda e, kk=kk: e.tensor_tensor(out=Lim[:, kk].rearrange("p a c -> p (a c)"), in0=mg[:], in1=sn[:], op=ALU.mult), [mg, sn], [Lim])
                def cmul(out_re, out_im, kind, vre, vim, neg_im):
                    lre = Lre[:, kind].rearrange("p a (j h) -> p a j h", j=8)
                    lim = Lim[:, kind].rearrange("p a (j h) -> p a j h", j=8)
                    vr = vre.unsqueeze(2).to_broadcast([64, 8, 8, 16])
                    vi = vim.unsqueeze(2).to_broadcast([64, 8, 8, 16])
                    o_re = out_re.rearrange("p a (j h) -> p a j h", j=8)
                    o_im = out_im.rearrange("p a (j h) -> p a j h", j=8)
                    t4 = tw[:].rearrange("p a (j h) -> p a j h", j=8)
                    V("dve", lambda e: e.tensor_tensor(out=o_re, in0=lre, in1=vr, op=ALU.mult), [Lre, bbre, s5c], [out_re])
                    V("dve", lambda e: e.tensor_tensor(out=t4, in0=lim, in1=vi, op=ALU.mult), [Lim, bbim, s5c], [tw])
                    V("dve", lambda e: e.tensor_tensor(out=out_re, in0=out_re, in1=tw[:], op=ALU.subtract), [out_re, tw], [out_re])
                    V("dve", lambda e: e.tensor_tensor(out=o_im, in0=lre, in1=vi, op=ALU.mult), [Lre, bbim, s5c], [out_im])
                    V("dve", lambda e: e.tensor_tensor(out=t4, in0=lim, in1=vr, op=ALU.mult), [Lim, bbre, s5c], [tw])
                    if neg_im:
                        V("dve", lambda e: e.scalar_tensor_tensor(out=out_im.rearrange("p a c -> p (a c)"), in0=out_im.rearrange("p a c -> p (a c)"), scalar=-1.0,
                                                                  in1=tw[:].rearrange("p a c -> p (a c)"), op0=ALU.mult, op1=ALU.subtract), [out_im, tw], [out_im])
                    else:
                        V("dve", lambda e: e.tensor_tensor(out=out_im, in0=out_im, in1=tw[:], op=ALU.add), [out_im, tw], [out_im])
                cmul(RT[:, 0], RT[:, 1], 0, bbre[:, dg0:dg0 + 8, :], bbim[:, dg0:dg0 + 8, :], False)
                cmul(OO[:, 0], OO[:, 1], 1, s5c[:, 0, dg0:dg0 + 8, :], s5c[:, 1, dg0:dg0 + 8, :], True)
                cmul(OP[:, 0], OP[:, 1], 2, s5c[:, 0, dg0:dg0 + 8, :], s5c[:, 1, dg0:dg0 + 8, :], True)
                for i in range(8):
                    dg = dg0 + i
                    g = gb * 8 + i
                    V("act", lambda e, dg=dg, i=i: e.copy(out=O8[:, dg], in_=OO[:, :, i, :]), [OO], [O8])
                    for c2 in range(2):
                        P.op("pe", lambda e, c2=c2, i=i: e.transpose(out=pR[:, c2, :], in_=RT[:, c2, i, :], identity=identf[0:64, 0:64]),
                             reads=[RT, identf], writes=[pR])
                    V("act", lambda e, dg=dg: e.copy(out=R8[:, dg], in_=pR[:, 0:2, :]), [pR], [R8])
                    pt = pT[i % 2]
                    mm(pt[:], [(RT[:, 0, i, :], OP[:, 0, i, :]), (RT[:, 1, i, :], OP[:, 1, i, :])], [RT, OP], [pt])
                    if d == 0:
                        V("dve", lambda e, pt=pt: e.tensor_tensor(out=tt8[:], in0=pt[:], in1=mf[:], op=ALU.mult), [pt, mf], [tt8])
                        V("dve", lambda e, g=g: e.scalar_tensor_tensor(out=tt8[:], in0=identf[:], scalar=s5d[:, g:g + 1], in1=tt8[:],
                                                                       op0=ALU.mult, op1=ALU.add), [identf, s5d, tt8], [tt8])
                        V("pool", lambda e, g=g: e.tensor_copy(out=T8[:, g, :], in_=tt8[:]), [tt8], [T8])
                    else:
                        V("dve", lambda e, pt=pt: e.tensor_tensor(out=tt8b[:], in0=pt[:], in1=mb[:], op=ALU.mult), [pt, mb], [tt8b])
                        V("pool", lambda e, g=g: e.tensor_tensor(out=T8[:, g, :], in0=T8[:, g, :], in1=tt8b[:], op=ALU.add), [tt8b, T8], [T8])
    P.barrier()
    if upto <= 2:
        if debug:
            dbg["T8"] = dout("dbg_T8", [128, 32, 128], BF16)
            dbg["O8"] = dout("dbg_O8", [64, 64, 2, 128], BF16)
            dbg["R8"] = dout("dbg_R8", [128, 64, 2, 64], BF16)
            P.dma(dbg["T8"][:, :, :], T8[:], is_output=True)
            P.dma(dbg["O8"][:, :, :, :], O8[:], is_output=True)
            P.dma(dbg["R8"][:, :, :, :], R8[:], is_output=True)
        P.emit()
        s5es.close()
        top.close()
        return nc


    with contextlib.ExitStack() as es:
        mix = SB(es, [64, 256])
        P.dma(mix[:], cst["mix"][:, :])
        Ugs = [SB(es, [128, NS * 256 + 128], BF16) for _ in range(2)]
        cosr = [SB(es, [64, 256]) for _ in range(2)]
        sinr = [SB(es, [64, 256]) for _ in range(2)]
        angr = SB(es, [64, 256])
        kir = SB(es, [64, 256], I32)
        kfr = SB(es, [64, 256])
        rr = SB(es, [64, 256])
        cc = [SB(es, [64, NS, 255]) for _ in range(2)]
        ta = SB(es, [64, NS, 256])
        tb2 = SB(es, [64, NS, 256])
        W = [SB(es, [64, NS, 256]) for _ in range(2)]
        Wc = [SB(es, [64, NS, 32]) for _ in range(2)]
        h0 = SB(es, [64, 2, 2, NS])
        t4 = SB(es, [64, NS])
        Sin = SB(es, [64, 2, 2, NS * 256], BF16)
        Ysb = SB(es, [128, NS * 256])
        Yout = SB(es, [128, 8, 8, 128])
        psS = [PS(es, [64, NS * 256]) for _ in range(2)]
        psY = PS(es, [128, NS * 256])
        pTr = PS(es, [128, 4, 128])
        psC = PS(es, [64, 2, 128])

        def rot(out_re, out_im, s_re, s_im, cs_, sn_, conj, eng2="pool"):
            V("dve", lambda e: e.tensor_tensor(out=ta_v(out_re), in0=s_re, in1=cs_, op=ALU.mult), [psS[0], psC, W[0], Wc[0], cosr[0], cosr[1]], [ta])
            V("dve", lambda e: e.tensor_tensor(out=tb_v(out_re), in0=s_im, in1=sn_, op=ALU.mult), [psS[1], psC, W[1], Wc[1], sinr[0], sinr[1]], [tb2])
            V(eng2, lambda e: e.tensor_tensor(out=out_re, in0=ta_v(out_re), in1=tb_v(out_re), op=(ALU.add if conj else ALU.subtract)),
              [ta, tb2], [cc[0], Sin])
            V("dve", lambda e: e.tensor_tensor(out=ta_v(out_re), in0=s_im, in1=cs_, op=ALU.mult), [psS[1], psC, W[1], Wc[1], cosr[0], cosr[1]], [ta])
            V("dve", lambda e: e.tensor_tensor(out=tb_v(out_re), in0=s_re, in1=sn_, op=ALU.mult), [psS[0], psC, W[0], Wc[0], sinr[0], sinr[1]], [tb2])
            V(eng2, lambda e: e.tensor_tensor(out=out_im, in0=ta_v(out_re), in1=tb_v(out_re), op=(ALU.subtract if conj else ALU.add)),
              [ta, tb2], [cc[1], Sin])

        def ta_v(like):
            return ta[:, :, 0:like.shape[2]]

        def tb_v(like):
            return tb2[:, :, 0:like.shape[2]]

        for g in range(32):
            Ug = Ugs[g % 2]
            P.dma(Ug[:], U_s[g], q="act" if g % 2 else "sp")
            for d in range(2):
                dg = d * 32 + g
                V("dve", lambda e, dg=dg: e.tensor_scalar(out=angr[:], in0=mix[:], scalar1=th8[:, dg:dg + 1], scalar2=None, op0=ALU.mult),
                  [mix, th8], [angr])
                trig(angr[:], None, (kir[:], kfr[:], rr[:]), sinr[d][:], cosr[d][:])
            for d in range(2):
                dg = d * 32 + g
                for c2 in range(2):
                    mm(psC[:, c2, :], [(R8[:, dg, c2, :], Ug[:, NS * 256:NS * 256 + 128])], [R8, Ug], [psC])
                sre = psC[:, 0, :].rearrange("p (n m) -> p n m", n=NS)
                sim = psC[:, 1, :].rearrange("p (n m) -> p n m", n=NS)
                if d == 1:
                    sre = sre[:, :, ::-1]
                    sim = sim[:, :, ::-1]
                csb = cosr[d][:, 1:33].unsqueeze(1).to_broadcast([64, NS, 32])
                snb = sinr[d][:, 1:33].unsqueeze(1).to_broadcast([64, NS, 32])
                rot(cc[0][:, :, 0:32], cc[1][:, :, 0:32], sre, sim, csb, snb, True)
                for c2 in range(2):
                    for n in range(NS):
                        V("dve", lambda e, c2=c2, n=n, dg=dg: e.tensor_tensor_scan(out=Wc[c2][:, n, :], data0=r8[:, dg:dg + 1].to_broadcast([64, 32]),
                                                                                  data1=cc[c2][:, n, 0:32], initial=0.0, op0=ALU.mult, op1=ALU.add),
                          [r8, cc[c2]], [Wc[c2]])
                c32 = cosr[d][:, 32:33]
                s32 = sinr[d][:, 32:33]
                V("dve", lambda e, s32=s32: e.tensor_scalar(out=t4[:], in0=Wc[1][:, :, 31], scalar1=s32, scalar2=None, op0=ALU.mult), [Wc[1], sinr[d]], [t4])
                V("dve", lambda e, c32=c32, d=d: e.scalar_tensor_tensor(out=h0[:, d, 0, :], in0=Wc[0][:, :, 31], scalar=c32, in1=t4[:], op0=ALU.mult, op1=ALU.subtract),
                  [Wc[0], cosr[d], t4], [h0])
                V("dve", lambda e, c32=c32: e.tensor_scalar(out=t4[:], in0=Wc[1][:, :, 31], scalar1=c32, scalar2=None, op0=ALU.mult), [Wc[1], cosr[d]], [t4])
                V("dve", lambda e, s32=s32, d=d: e.scalar_tensor_tensor(out=h0[:, d, 1, :], in0=Wc[0][:, :, 31], scalar=s32, in1=t4[:], op0=ALU.mult, op1=ALU.add),
                  [Wc[0], sinr[d], t4], [h0])
                if debug:
                    P.dma(dbg["h0"][:, d, g, :], h0[:, d, 0, :], is_output=True)
                    if g == 0 and d == 0:
                        dbg["m"] = dout("dbg_m", [64, 8, 256])
                        dm = SB(es, [64, 8, 256])
                        V("dve", lambda e: e.memset(dm[:], 0.0), [], [dm])
                        V("dve", lambda e: e.tensor_copy(out=dm[:, 0, :], in_=psC[:].rearrange("p a b -> p (a b)")), [psC], [dm])
                        V("dve", lambda e: e.tensor_copy(out=dm[:, 1, 0:128].rearrange("p (n m) -> p n m", n=NS), in_=cc[0][:, :, 0:32]), [cc[0]], [dm])
                        V("dve", lambda e: e.tensor_copy(out=dm[:, 2, 0:128].rearrange("p (n m) -> p n m", n=NS), in_=Wc[0][:]), [Wc[0]], [dm])
                        V("dve", lambda e: e.tensor_copy(out=dm[:, 3, :], in_=cosr[0][:]), [cosr[0]], [dm])
                        V("dve", lambda e: e.tensor_copy(out=dm[:, 4, :], in_=sinr[0][:]), [sinr[0]], [dm])
                        V("dve", lambda e: e.tensor_copy(out=dm[:, 5, 0:64], in_=r8[:]), [r8], [dm])
                        V("dve", lambda e: e.tensor_copy(out=dm[:, 6, 0:64], in_=th8[:]), [th8], [dm])
                        V("dve", lambda e: e.tensor_copy(out=dm[:, 7, 0:128].rearrange("p (n m) -> p n m", n=NS), in_=cc[1][:, :, 0:32]), [cc[1]], [dm])
                        P.dma(dbg["m"][:, :, :], dm[:], is_output=True)
            for d in range(2):
                dg = d * 32 + g
                for c2 in range(2):
                    for hf in range(2):
                        mm(psS[c2][:, hf * 512:(hf + 1) * 512], [(R8[:, dg, c2, :], Ug[:, hf * 512:(hf + 1) * 512])], [R8, Ug], [psS[c2]])
                sre = psS[0][:].rearrange("p (n m) -> p n m", n=NS)
                sim = psS[1][:].rearrange("p (n m) -> p n m", n=NS)
                if d == 0:
                    sre = sre[:, :, 0:255]
                    sim = sim[:, :, 0:255]
                else:
                    sre = sre[:, :, 255:0:-1]
                    sim = sim[:, :, 255:0:-1]
                csb = cosr[d][:, 1:256].unsqueeze(1).to_broadcast([64, NS, 255])
                snb = sinr[d][:, 1:256].unsqueeze(1).to_broadcast([64, NS, 255])
                rot(cc[0][:], cc[1][:], sre, sim, csb, snb, True)
                for c2 in range(2):
                    V("pool", lambda e, c2=c2, d=d: e.tensor_copy(out=W[c2][:, :, 0:1], in_=h0[:, d, c2, :].unsqueeze(2)), [h0], [W[c2]])
                    for n in range(NS):
                        V("dve", lambda e, c2=c2, n=n, dg=dg, d=d: e.tensor_tensor_scan(out=W[c2][:, n, 1:256], data0=r8[:, dg:dg + 1].to_broadcast([64, 255]),
                                                                                       data1=cc[c2][:, n, :], initial=h0[:, d, c2, n:n + 1], op0=ALU.mult, op1=ALU.add),
                          [r8, cc[c2], h0], [W[c2]])
                csb = cosr[d][:].unsqueeze(1).to_broadcast([64, NS, 256])
                snb = sinr[d][:].unsqueeze(1).to_broadcast([64, NS, 256])
                o_re = Sin[:, d, 0, :].rearrange("p (n m) -> p n m", n=NS)
                o_im = Sin[:, d, 1, :].rearrange("p (n m) -> p n m", n=NS)
                if d == 1:
                    o_re = o_re[:, :, ::-1]
                    o_im = o_im[:, :, ::-1]
                rot(o_re, o_im, W[0][:], W[1][:], csb, snb, False)
            for hf in range(2):
                cs_ = slice(hf * 512, (hf + 1) * 512)
                mm(psY[:, cs_], [(T8[:, g, :], Ug[:, cs_]), (O8[:, g, 0, :], Sin[:, 0, 0, cs_]), (O8[:, g, 1, :], Sin[:, 0, 1, cs_]),
                                 (O8[:, 32 + g, 0, :], Sin[:, 1, 0, cs_]), (O8[:, 32 + g, 1, :], Sin[:, 1, 1, cs_])],
                   [T8, Ug, O8, Sin], [psY])
            V("act", lambda e: e.copy(out=Ysb[:], in_=psY[:]), [psY], [Ysb])
            gi = g % 8
            for q4 in range(2):
                for b4 in range(4):
                    blk = q4 * 4 + b4
                    P.op("pe", lambda e, blk=blk, b4=b4: e.transpose(out=pTr[:, b4, :], in_=Ysb[:, blk * 128:(blk + 1) * 128], identity=identf[:]),
                         reads=[Ysb, identf], writes=[pTr])
                V("act", lambda e, q4=q4, gi=gi: e.copy(out=Yout[:, q4 * 4:(q4 + 1) * 4, :, gi * 16:(gi + 1) * 16],
                                                        in_=pTr[:].rearrange("p b (j h) -> p b j h", j=8)), [pTr], [Yout])
            if gi == 7:
                gb = g // 8
                for blk in range(8):
                    dst = y_s[blk * 1024:(blk + 1) * 1024, gb * 128:(gb + 1) * 128].rearrange("(m j) c -> m j c", j=8)
                    P.dma(dst, Yout[:, blk], q="sp" if blk % 2 else "act")
                    if debug:
                        P.dma(dbg["y"][blk * 1024:(blk + 1) * 1024, gb * 128:(gb + 1) * 128].rearrange("(m j) c -> m j c", j=8), Yout[:, blk], is_output=True)
    s5es.close()
    P.barrier()
    if upto <= 3:
        P.emit()
        top.close()
        return nc


    h_s = dscr("h_s", [L, 2048])
    if debug:
        dbg["filt"] = dout("dbg_filt", [L, 2048])
        dbg["z"] = dout("dbg_z", [NS * L, 512])
    hyes = contextlib.ExitStack()
    rn = SB(hyes, [128, 2, 512])
    with contextlib.ExitStack() as es:
        fT = SB(es, [33, 2048])
        w1 = SB(es, [33, 64])
        b1 = SB(es, [64, 1])
        wh = SB(es, [64, 2, 64])
        bh = SB(es, [64, 2])
        fr = SB(es, [64, 1])
        wout = SB(es, [64, 2048])
        absd = SB(es, [128, 2048])
        tneg = SB(es, [128, 16])
        onesf = SB(es, [128, 128])
        P.dma(fT[:], cst["featsT"][:, :])
        P.dma(w1[:], hf_w1[:, :])
        P.dma(b1[:], hf_b1[:, :])
        P.dma(wh[:], hf_wh.rearrange("i a b -> a i b"))
        P.dma(bh[:], hf_bh[:, :])
        P.dma(fr[:], hf_fr[:, :])
        P.dma(wout[:], hf_wout[:, :])
        P.dma(absd[:], hf_dec.partition_broadcast(128))
        P.dma(tneg[:], cst["tneg"][:, :])
        P.dma(onesf[:], cst["onesf"][:, :])
        V("act", lambda e: e.activation(out=absd[:], in_=absd[:], func=AF.Abs), [absd], [absd])
        hid = [SB(es, [64, 2048]) for _ in range(2)]
        pre = SB(es, [64, 2048])
        kiH = SB(es, [64, 2048], I32)
        kfH = SB(es, [64, 2048])
        rH = SB(es, [64, 2048])
        psA = PS(es, [128, 2048])
        psB = PS(es, [128, 2048])
        for layer in range(3):
            for nt in range(4):
                cs_ = slice(nt * 512, (nt + 1) * 512)
                if layer == 0:
                    mm(psA[0:64, cs_], [(w1[:], fT[:, cs_])], [w1, fT], [psA])
                else:
                    mm(psA[0:64, cs_], [(wh[:, layer - 1, :], hid[(layer - 1) % 2][:, cs_])], [wh, hid[(layer - 1) % 2]], [psA])
            bias = b1[:, 0:1] if layer == 0 else bh[:, layer - 1:layer]
            V("dve", lambda e, bias=bias: e.tensor_scalar(out=pre[:], in0=psA[0:64, :], scalar1=bias, scalar2=fr[:, 0:1], op0=ALU.add, op1=ALU.mult),
              [psA, b1, bh, fr], [pre])
            trig(pre[:], None, (kiH[:], kfH[:], rH[:]), hid[layer % 2][:], None)
        hidF = hid[0]
        dec = SB(es, [128, 2048])
        hraw = [SB(es, [128, 2048]) for _ in range(2)]
        hsq = SB(es, [128, 2048])
        for tc in range(16):
            hr = hraw[tc % 2]
            for nt in range(4):
                cs_ = slice(nt * 512, (nt + 1) * 512)
                mm(psA[:, cs_], [(hidF[:, tc * 128:(tc + 1) * 128], wout[:, cs_])], [hidF, wout], [psA])
            V("act", lambda e, tc=tc: e.activation(out=dec[:], in_=absd[:], func=AF.Exp, scale=tneg[:, tc:tc + 1]), [absd, tneg], [dec])
            V("dve", lambda e, hr=hr: e.tensor_tensor(out=hr[:], in0=psA[:], in1=dec[:], op=ALU.mult), [psA, dec], [hr])
            V("pool", lambda e, hr=hr: e.tensor_tensor(out=hsq[:], in0=hr[:], in1=hr[:], op=ALU.mult), [hr], [hsq])
            for nt in range(4):
                cs_ = slice(nt * 512, (nt + 1) * 512)
                P.op("pe", lambda e, cs_=cs_, tc=tc: e.matmul(psB[:, cs_], lhsT=onesf[:], rhs=hsq[:, cs_], start=(tc == 0), stop=(tc == 15)),
                     reads=[onesf, hsq], writes=[psB])
            P.dma(h_s[tc * 128:(tc + 1) * 128, :], hr[:], q="act")
        ssb = SB(es, [128, 2048])
        V("act", lambda e: e.copy(out=ssb[:], in_=psB[:]), [psB], [ssb])
        s4 = ssb[:].rearrange("p (o d c) -> p o d c", o=2, d=2)
        V("dve", lambda e: e.tensor_tensor(out=rn[:], in0=s4[:, :, 0, :], in1=s4[:, :, 1, :], op=ALU.add), [ssb], [rn])
        V("dve", lambda e: e.tensor_scalar(out=rn[:], in0=rn[:], scalar1=EPS, scalar2=None, op0=ALU.add), [rn], [rn])
        mh = SB(es, [128, 1024])
        V("pool", lambda e: e.memset(mh[:], -0.5), [], [mh])
        V("pool", lambda e: e.tensor_tensor(out=rn[:].rearrange("p o c -> p (o c)"), in0=rn[:].rearrange("p o c -> p (o c)"), in1=mh[:], op=ALU.pow), [rn, mh], [rn])
    P.barrier()
    Ct = SB(hyes, [128, 16, 2048], BF16)
    St = SB(hyes, [128, 16, 2048], BF16)
    P.dma(Ct[:], cst["ctab"].rearrange("(tc p) f -> p tc f", p=128))
    P.dma(St[:], cst["stab"].rearrange("(tc p) f -> p tc f", p=128), q="act")
    with contextlib.ExitStack() as es:
        hraw = [SB(es, [128, 2048]) for _ in range(2)]
        psA = PS(es, [128, 2048])
        m0 = SB(es, [128, 1])
        cphi = SB(es, [128, 16])
        sphi = SB(es, [128, 16])
        P.dma(m0[:], cst["m0"][:, :])
        P.dma(cphi[:], cst["cphi"][:, :])
        P.dma(sphi[:], cst["sphi"][:, :])
        PM = SB(es, [128, 16, 2, 512], BF16)
        kre = [SB(es, [128, 512]) for _ in range(2)]
        kim = [SB(es, [128, 512]) for _ in range(2)]
        for o in range(2):
            for tc in range(16):
                hr = hraw[tc % 2]
                P.dma(hr[:], h_s[tc * 128:(tc + 1) * 128, :])
                h4 = hr[:].rearrange("p (o d c) -> p o d c", o=2, d=2)
                V("dve", lambda e, h4=h4, o=o, hr=hr: e.tensor_tensor(out=h4[:, o], in0=h4[:, o], in1=rn[:, o:o + 1, :].to_broadcast([128, 2, 512]), op=ALU.mult),
                  [hr, rn], [hr])
                if debug:
                    P.dma(dbg["filt"][tc * 128:(tc + 1) * 128, o * 1024:(o + 1) * 1024], hr[:, o * 1024:(o + 1) * 1024], is_output=True)
                if tc == 0:
                    V("dve", lambda e, h4=h4, o=o, hr=hr: e.tensor_scalar(out=h4[:, o, 1, :], in0=h4[:, o, 1, :], scalar1=m0[:, 0:1], scalar2=None, op0=ALU.mult),
                      [hr, m0], [hr])
                V("dve", lambda e, h4=h4, o=o, tc=tc, hr=hr: e.tensor_tensor(out=PM[:, tc, 0, :], in0=h4[:, o, 0, :], in1=h4[:, o, 1, :], op=ALU.add), [hr], [PM])
                V("pool", lambda e, h4=h4, o=o, tc=tc, hr=hr: e.tensor_tensor(out=PM[:, tc, 1, :], in0=h4[:, o, 0, :], in1=h4[:, o, 1, :], op=ALU.subtract), [hr], [PM])
            for fc in range(16):
                fs = slice(fc * 128, (fc + 1) * 128)
                mm(psA[:, 0:512], [(Ct[:, tc, fs], PM[:, tc, 0, :]) for tc in range(16)], [Ct, PM], [psA])
                mm(psA[:, 512:1024], [(St[:, tc, fs], PM[:, tc, 0, :]) for tc in range(16)], [St, PM], [psA])
                mm(psA[:, 1024:1536], [(Ct[:, tc, fs], PM[:, tc, 1, :]) for tc in range(16)], [Ct, PM], [psA])
                mm(psA[:, 1536:2048], [(St[:, tc, fs], PM[:, tc, 1, :]) for tc in range(16)], [St, PM], [psA])
                kr = kre[fc % 2]
                kq = kim[fc % 2]
                V("dve", lambda e, kr=kr, fc=fc: e.tensor_scalar(out=kr[:], in0=psA[:, 0:512], scalar1=cphi[:, fc:fc + 1], scalar2=None, op0=ALU.mult), [psA, cphi], [kr])
                V("dve", lambda e, kr=kr, fc=fc: e.scalar_tensor_tensor(out=kr[:], in0=psA[:, 512:1024], scalar=sphi[:, fc:fc + 1], in1=kr[:], op0=ALU.mult, op1=ALU.add),
                  [psA, sphi, kr], [kr])
                V("dve", lambda e, kq=kq, fc=fc: e.tensor_scalar(out=kq[:], in0=psA[:, 1536:2048], scalar1=cphi[:, fc:fc + 1], scalar2=None, op0=ALU.mult), [psA, cphi], [kq])
                V("dve", lambda e, kq=kq, fc=fc: e.scalar_tensor_tensor(out=kq[:], in0=psA[:, 1024:1536], scalar=sphi[:, fc:fc + 1], in1=kq[:], op0=ALU.mult, op1=ALU.subtract),
                  [psA, sphi, kq], [kq])
                P.dma(kspec_s[o, 0, fc], kr[:], q="act")
                P.dma(kspec_s[o, 1, fc], kq[:], q="act")
    P.barrier()
    if upto <= 4:
        P.emit()
        hyes.close()
        top.close()
        return nc


    with contextlib.ExitStack() as es:
        hyd = SB(es, [128, 2, 512])
        P.dma(hyd[:].rearrange("p o c -> p (o c)"), hyd_d.rearrange("o c -> (o c)").partition_broadcast(128))
        zin = SB(es, [128, 16, 512], BF16)
        YY = SB(es, [128, 16, 2, 512], BF16)
        kr2 = [SB(es, [128, 2, 512]) for _ in range(2)]
        gt = [SB(es, [128, 512]) for _ in range(2)]
        vst = gt
        u1 = SB(es, [128, 512])
        u2 = SB(es, [128, 512])
        u3 = SB(es, [128, 512])
        ot = [u1, u2]
        psX = [PS(es, [128, 2, 512]) for _ in range(2)]
        psO = [PS(es, [128, 512]) for _ in range(2)]
        for n in range(NS):
            for tc in range(16):
                vt = vst[tc % 2]
                r0 = n * L + tc * 128
                P.dma(vt[:], q_s[r0:r0 + 128, 0:512], q="act" if tc % 2 else "sp")
                V("act" if tc % 2 else "pool", lambda e, vt=vt, tc=tc: (e.copy if hasattr(e, "copy") else e.tensor_copy)(out=zin[:, tc, :], in_=vt[:]), [vt], [zin])
            for o in range(2):
                for fc in range(16):
                    fs = slice(fc * 128, (fc + 1) * 128)
                    px_ = psX[fc % 2]
                    kk = kr2[fc % 2]
                    P.dma(kk[:, 0, :], kspec_s[o, 0, fc], q="sp")
                    P.dma(kk[:, 1, :], kspec_s[o, 1, fc], q="act")
                    mm(px_[:, 0, :], [(Ct[:, tc, fs], zin[:, tc, :]) for tc in range(16)], [Ct, zin], [px_])
                    mm(px_[:, 1, :], [(St[:, tc, fs], zin[:, tc, :]) for tc in range(16)], [St, zin], [px_])
                    V("dve", lambda e, px_=px_, kk=kk: e.tensor_tensor(out=u1[:], in0=px_[:, 0, :], in1=kk[:, 0, :], op=ALU.mult), [px_, kk], [u1])
                    V("dve", lambda e, px_=px_, kk=kk: e.tensor_tensor(out=u2[:], in0=px_[:, 1, :], in1=kk[:, 1, :], op=ALU.mult), [px_, kk], [u2])
                    V("pool", lambda e, fc=fc: e.tensor_tensor(out=YY[:, fc, 0, :], in0=u1[:], in1=u2[:], op=ALU.add), [u1, u2], [YY])
                    V("dve", lambda e, px_=px_, kk=kk: e.tensor_tensor(out=u1[:], in0=px_[:, 1, :], in1=kk[:, 0, :], op=ALU.mult), [px_, kk], [u1])
                    V("dve", lambda e, px_=px_, kk=kk: e.tensor_tensor(out=u2[:], in0=px_[:, 0, :], in1=kk[:, 1, :], op=ALU.mult), [px_, kk], [u2])
                    V("pool", lambda e, fc=fc: e.tensor_tensor(out=YY[:, fc, 1, :], in0=u1[:], in1=u2[:], op=ALU.subtract), [u1, u2], [YY])
                for tc in range(16):
                    ts_ = slice(tc * 128, (tc + 1) * 128)
                    po = psO[tc % 2]
                    g_ = gt[tc % 2]
                    r0 = n * L + tc * 128
                    P.dma(g_[:], q_s[r0:r0 + 128, 512 * (o + 1):512 * (o + 2)], q="sp")
                    pairs = []
                    for fc in range(16):
                        pairs.append((Ct[:, fc, ts_], YY[:, fc, 0, :]))
                        pairs.append((St[:, fc, ts_], YY[:, fc, 1, :]))
                    mm(po[:], pairs, [Ct, St, YY], [po])
                    V("pool", lambda e, tc=tc, o=o: e.tensor_tensor(out=u3[:], in0=zin[:, tc, :], in1=hyd[:, o, :], op=ALU.mult), [zin, hyd], [u3])
                    V("dve", lambda e, po=po: e.scalar_tensor_tensor(out=u3[:], in0=po[:], scalar=2.0 / NFFT, in1=u3[:], op0=ALU.mult, op1=ALU.add), [po, u3], [u3])
                    if o == 0:
                        V("dve", lambda e, tc=tc, g_=g_: e.tensor_tensor(out=zin[:, tc, :], in0=u3[:], in1=g_[:], op=ALU.mult), [u3, g_], [zin])
                    else:
                        o_ = ot[tc % 2]
                        V("dve", lambda e, o_=o_, g_=g_: e.tensor_tensor(out=o_[:], in0=u3[:], in1=g_[:], op=ALU.mult), [u3, g_], [o_])
                        P.dma(z_s[r0:r0 + 128, :], o_[:], q="act")
                        if debug:
                            P.dma(dbg["z"][r0:r0 + 128, :], o_[:], q="act", is_output=True)
    hyes.close()
    P.barrier()
    if upto <= 5:
        P.emit()
        top.close()
        return nc


    NTG = 2
    TSG = NTG * 128
    x1_s = dscr("x1_s", [NS * L, D])
    hx2T_s = dscr("hx2T_s", [NS * L // TS, 128, 8, TS], BF16)
    sc_s = dscr("sc_s", [NS * L // 128, 128, 16 * 128])
    thr_s = dscr("thr_s", [NS * L // 128, 128, 16])
    with contextlib.ExitStack() as es:
        bglu = SB(es, [128, 512])
        gs5 = SB(es, [128, 512])
        ghy = SB(es, [128, 512])
        g2 = SB(es, [128, D])
        gx1 = SB(es, [128, D])
        g2p = SB(es, [128, D])
        sh2 = SB(es, [128, D])
        P.dma(bglu[:], b_glu_d.partition_broadcast(128))
        P.dma(gs5[:], gs5_d.partition_broadcast(128))
        P.dma(ghy[:], ghy_d.partition_broadcast(128))
        P.dma(g2[:], g2_d.partition_broadcast(128))
        hx2T = SB(es, [128, 8, TSG], BF16)
        x1t = [SB(es, [128, D]) for _ in range(NTG)]
        scs = [SB(es, [128, 16, 128]) for _ in range(NTG)]
        c16s = [SB(es, [128, 8, 16]) for _ in range(NTG)]
        thrs = [SB(es, [128, 16]) for _ in range(NTG)]
        nbs = [t_[:, 0:8] for t_ in thrs]
        ntaus = [t_[:, 8:16] for t_ in thrs]
        woutb = SB(es, [128, 8, 1024], BF16)
        wglub = SB(es, [128, 4, 512], BF16)
        wqb = SB(es, [128, 8, 2048], BF16)
        kTb = SB(es, [128, 16, 128], BF16)
        P.dma(woutb[:], wout_b[:, :, :], q="sp")
        P.dma(wglub[:], wglu_b[:, :, :], q="act")
        P.dma(wqb[:], wq_b[:, :, :], q="sp")
        P.dma(kTb[:], kT_b[:, :, :], q="act")
        qT = SB(es, [128, 16, TSG], BF16)
        yts = [SB(es, [128, 512]) for _ in range(NTG)]
        zts = [SB(es, [128, 512]) for _ in range(NTG)]
        xt4s = [SB(es, [128, D]) for _ in range(NTG)]
        gys = [SB(es, [128, 512]) for _ in range(NTG)]
        gybs = [SB(es, [128, 512], BF16) for _ in range(NTG)]
        gyTs = [SB(es, [128, 4, 128], BF16) for _ in range(NTG)]
        gpres = [SB(es, [128, 512]) for _ in range(NTG)]
        s5os = [SB(es, [128, 512]) for _ in range(NTG)]
        catbs = [SB(es, [128, D], BF16) for _ in range(NTG)]
        catTs = [SB(es, [128, 8, 128], BF16) for _ in range(NTG)]
        tms = [SB(es, [128, D]) for _ in range(NTG)]
        hb4s = [SB(es, [128, D], BF16) for _ in range(NTG)]
        jks = [SB(es, [128, D], BF16) for _ in range(NTG)]
        sss = [[SB(es, [128, 1]) for _ in range(6)] for _ in range(NTG)]
        wks = [SB(es, [128, 256]) for _ in range(NTG)]
        m16s = [SB(es, [128, 16, 16]) for _ in range(NTG)]
        cands = [SB(es, [128, 8, 256]) for _ in range(NTG)]
        ews = [SB(es, [128, 8, 16]) for _ in range(NTG)]
        zss = [SB(es, [128, 8]) for _ in range(NTG)]
        ptrs = [PS(es, [128, 8, 128], BF16) for _ in range(2)]
        pAB = [PS(es, [128, 512]) for _ in range(2)]
        pMs = [PS(es, [128, D]) for _ in range(2)]
        for g_i in range(NS * L // TSG):
            n = (g_i * TSG) // L
            row0 = g_i * TSG
            if (g_i * TSG) % L == 0:
                P.dma(gx1[:], modx_d[n, :, 2 * D:3 * D])
                P.dma(sh2[:], modx_d[n, :, 3 * D:4 * D])
                P.dma(g2p[:], modx_d[n, :, 4 * D:5 * D])
                V("dve", lambda e: e.scalar_tensor_tensor(out=g2p[:], in0=g2p[:], scalar=1.0, in1=g2[:], op0=ALU.add, op1=ALU.mult), [g2p, g2], [g2p])
            TI = list(range(NTG))

            def ctx(ti):
                return dict(r0=row0 + ti * 128, x1=x1t[ti], yt=yts[ti], zt=zts[ti], xt4=xt4s[ti], gy=gys[ti], gyb=gybs[ti], gyT=gyTs[ti], gpre=gpres[ti],
                            s5o=s5os[ti], catb=catbs[ti], catT=catTs[ti], tm=tms[ti], hb4=hb4s[ti], jk=jks[ti], ss=sss[ti], ptr=ptrs[ti % 2], pG=pAB[ti % 2], pM=pMs[ti % 2])

            for ti in TI:
                c_ = ctx(ti); r0, yt, zt, xt4, gy, gyb = c_["r0"], c_["yt"], c_["zt"], c_["xt4"], c_["gy"], c_["gyb"]
                P.dma(yt[:], y_s[r0:r0 + 128, :], q="sp")
                P.dma(zt[:], z_s[r0:r0 + 128, :], q="act")
                P.dma(xt4[:], x_d[r0:r0 + 128, :], q="sp")
                V("act", lambda e: e.activation(out=gy[:], in_=yt[:], func=AF.Gelu_apprx_tanh), [yt], [gy])
                V("pool", lambda e: e.tensor_copy(out=gyb[:], in_=gy[:]), [gy], [gyb])
            for ti in TI:
                c_ = ctx(ti); gyb, ptr, gyT, pG = c_["gyb"], c_["ptr"], c_["gyT"], c_["pG"]
                for k in range(4):
                    P.op("pe", lambda e, k=k: e.transpose(out=ptr[:, k, :], in_=gyb[:, k * 128:(k + 1) * 128], identity=ident[:]), reads=[gyb, ident], writes=[ptr])
                V("act", lambda e: e.copy(out=gyT[:], in_=ptr[:, 0:4, :]), [ptr], [gyT])
            for ti in TI:
                c_ = ctx(ti); gyT, pG = c_["gyT"], c_["pG"]
                mm(pG[:], [(gyT[:, k, :], wglub[:, k, :]) for k in range(4)], [gyT, wglub], [pG])
            for ti in TI:
                c_ = ctx(ti); pG, gpre = c_["pG"], c_["gpre"]
                V("dve", lambda e: e.tensor_tensor(out=gpre[:], in0=pG[:], in1=bglu[:], op=ALU.add), [pG, bglu], [gpre])
                V("act", lambda e: e.activation(out=gpre[:], in_=gpre[:], func=AF.Sigmoid), [gpre], [gpre])
            for ti in TI:
                c_ = ctx(ti); gy, gpre, s5o, jk, zt = c_["gy"], c_["gpre"], c_["s5o"], c_["jk"], c_["zt"]
                ss1, ss2, ss3, rs1, rs2, rs3 = c_["ss"]
                V("dve", lambda e: e.tensor_tensor(out=s5o[:], in0=gy[:], in1=gpre[:], op=ALU.mult), [gy, gpre], [s5o])
                V("act", lambda e: e.activation(out=jk[:, 0:512], in_=s5o[:], func=AF.Square, accum_out=ss1[:]), [s5o], [jk, ss1])
                V("act", lambda e: e.activation(out=jk[:, 512:1024], in_=zt[:], func=AF.Square, accum_out=ss2[:]), [zt], [jk, ss2])
            for ti in TI:
                c_ = ctx(ti)
                ss1, ss2, ss3, rs1, rs2, rs3 = c_["ss"]
                rsqrt_mean(es, ss1, 512, rs1)
                rsqrt_mean(es, ss2, 512, rs2)
            for ti in TI:
                c_ = ctx(ti); s5o, zt, catb = c_["s5o"], c_["zt"], c_["catb"]
                ss1, ss2, ss3, rs1, rs2, rs3 = c_["ss"]
                V("dve", lambda e: e.scalar_tensor_tensor(out=catb[:, 0:512], in0=s5o[:], scalar=rs1[:, 0:1], in1=gs5[:], op0=ALU.mult, op1=ALU.mult),
                  [s5o, rs1, gs5], [catb])
                V("dve", lambda e: e.scalar_tensor_tensor(out=catb[:, 512:1024], in0=zt[:], scalar=rs2[:, 0:1], in1=ghy[:], op0=ALU.mult, op1=ALU.mult),
                  [zt, rs2, ghy], [catb])
            for ti in TI:
                c_ = ctx(ti); catb, ptr, catT = c_["catb"], c_["ptr"], c_["catT"]
                for k in range(8):
                    P.op("pe", lambda e, k=k: e.transpose(out=ptr[:, k, :], in_=catb[:, k * 128:(k + 1) * 128], identity=ident[:]), reads=[catb, ident], writes=[ptr])
                V("act", lambda e: e.copy(out=catT[:], in_=ptr[:]), [ptr], [catT])
            for ti in TI:
                c_ = ctx(ti); catT, pM = c_["catT"], c_["pM"]
                for hf in range(2):
                    mm(pM[:, hf * 512:(hf + 1) * 512], [(catT[:, k, :], woutb[:, k, hf * 512:(hf + 1) * 512]) for k in range(8)], [catT, woutb], [pM])
            for ti in TI:
                c_ = ctx(ti); pM, tm, x1, xt4, jk, r0 = c_["pM"], c_["tm"], c_["x1"], c_["xt4"], c_["jk"], c_["r0"]
                ss1, ss2, ss3, rs1, rs2, rs3 = c_["ss"]
                V("dve", lambda e: e.tensor_tensor(out=tm[:], in0=pM[:], in1=gx1[:], op=ALU.mult), [pM, gx1], [tm])
                V("pool", lambda e: e.tensor_tensor(out=x1[:], in0=tm[:], in1=xt4[:], op=ALU.add), [tm, xt4], [x1])
                if debug:
                    P.dma(dbg["x1"][r0:r0 + 128, :], x1[:], is_output=True)
                V("act", lambda e: e.activation(out=jk[:], in_=x1[:], func=AF.Square, accum_out=ss3[:]), [x1], [jk, ss3])
            for ti in TI:
                c_ = ctx(ti)
                ss1, ss2, ss3, rs1, rs2, rs3 = c_["ss"]
                rsqrt_mean(es, ss3, D, rs3)
            for ti in TI:
                c_ = ctx(ti); tm, x1, hb4, r0 = c_["tm"], c_["x1"], c_["hb4"], c_["r0"]
                ss1, ss2, ss3, rs1, rs2, rs3 = c_["ss"]
                V("dve", lambda e: e.scalar_tensor_tensor(out=tm[:], in0=x1[:], scalar=rs3[:, 0:1], in1=g2p[:], op0=ALU.mult, op1=ALU.mult),
                  [x1, rs3, g2p], [tm])
                V("pool", lambda e: e.tensor_tensor(out=hb4[:], in0=tm[:], in1=sh2[:], op=ALU.add), [tm, sh2], [hb4])
                if debug:
                    P.dma(dbg["hx2"][r0:r0 + 128, :], hb4[:], is_output=True)
            for ti in TI:
                c_ = ctx(ti); hb4, ptr = c_["hb4"], c_["ptr"]
                for k in range(8):
                    P.op("pe", lambda e, k=k: e.transpose(out=ptr[:, k, :], in_=hb4[:, k * 128:(k + 1) * 128], identity=ident[:]), reads=[hb4, ident], writes=[ptr])
                V("act", lambda e: e.copy(out=hx2T[:, :, ti * 128:(ti + 1) * 128], in_=ptr[:]), [ptr], [hx2T])
            for c in range(16):
                pp = pAB[c % 2]
                mm(pp[:, 0:TSG], [(wqb[:, k, c * 128:(c + 1) * 128], hx2T[:, k, :]) for k in range(8)], [wqb, hx2T], [pp])
                if c % 2:
                    V("act", lambda e: e.copy(out=qT[:, c, :], in_=pp[:, 0:TSG]), [pp], [qT])
                else:
                    V("dve", lambda e: e.tensor_copy(out=qT[:, c, :], in_=pp[:, 0:TSG]), [pp], [qT])
            if not lite:
                for hh in range(4):
                    for ti in TI:
                        sc = scs[ti]
                        ps_ = pAB[ti % 2]
                        for c4 in range(4):
                            c = hh * 4 + c4
                            mm(ps_[:, c4 * 128:(c4 + 1) * 128], [(qT[:, c, ti * 128:(ti + 1) * 128], kTb[:, c, :])], [qT, kTb], [ps_])
                        V("act", lambda e: e.copy(out=sc[:, hh * 4:(hh + 1) * 4, :], in_=ps_[:].rearrange("p (a b) -> p a b", a=4)), [ps_], [sc])
                for c in range(16):
                    for ti in TI:
                        sc, m16 = scs[ti], m16s[ti]
                        V("dve", lambda e: e.max(out=m16[:, c, 0:8], in_=sc[:, c, :]), [sc], [m16])
                    for ti in TI:
                        sc, m16, wk = scs[ti], m16s[ti], wks[ti]
                        V("dve", lambda e: e.match_replace(out=wk[:, 0:128], in_to_replace=m16[:, c, 0:8], in_values=sc[:, c, :], imm_value=-1e30), [sc, m16], [wk])
                    for ti in TI:
                        m16, wk = m16s[ti], wks[ti]
                        V("dve", lambda e: e.max(out=m16[:, c, 8:16], in_=wk[:, 0:128]), [wk], [m16])
                for ti in TI:
                    m16, cand = m16s[ti], cands[ti]
                    m4 = m16[:].rearrange("p (h two) k -> p h two k", two=2)
                    V("dve", lambda e: e.tensor_tensor(out=cand[:].rearrange("p h (a b) -> p h a b", a=16),
                                                       in0=m4[:, :, 0, :].unsqueeze(3).to_broadcast([128, 8, 16, 16]),
                                                       in1=m4[:, :, 1, :].unsqueeze(2).to_broadcast([128, 8, 16, 16]), op=ALU.add), [m16], [cand])
                for h in range(8):
                    for ti in TI:
                        cand, c16 = cands[ti], c16s[ti]
                        V("dve", lambda e: e.max(out=c16[:, h, 0:8], in_=cand[:, h, :]), [cand], [c16])
                    for ti in TI:
                        cand, c16, wk = cands[ti], c16s[ti], wks[ti]
                        V("dve", lambda e: e.match_replace(out=wk[:], in_to_replace=c16[:, h, 0:8], in_values=cand[:, h, :], imm_value=-1e30), [cand, c16], [wk])
                    for ti in TI:
                        c16, wk = c16s[ti], wks[ti]
                        V("dve", lambda e: e.max(out=c16[:, h, 8:16], in_=wk[:]), [wk], [c16])
                for ti in TI:
                    c16, ew, zs, nb = c16s[ti], ews[ti], zss[ti], nbs[ti]
                    V("dve", lambda e: e.tensor_tensor(out=ew[:], in0=c16[:], in1=c16[:, :, 0:1].to_broadcast([128, 8, 16]), op=ALU.subtract), [c16], [ew])
                    V("act", lambda e: e.activation(out=ew[:], in_=ew[:], func=AF.Exp), [ew], [ew])
                for ti in TI:
                    c16, ew, zs, nb = c16s[ti], ews[ti], zss[ti], nbs[ti]
                    V("dve", lambda e: e.tensor_reduce(out=zs[:], in_=ew[:], axis=AX.X, op=ALU.add), [ew], [zs])
                    V("act", lambda e: e.activation(out=zs[:], in_=zs[:], func=AF.Ln), [zs], [zs])
                for ti in TI:
                    c16, ew, zs, nb = c16s[ti], ews[ti], zss[ti], nbs[ti]
                    V("dve", lambda e: e.tensor_tensor(out=nb[:], in0=zs[:], in1=c16[:, :, 0], op=ALU.add), [zs, c16], [nb])
                    ntau_ = ntaus[ti]
                    V("dve", lambda e: e.tensor_scalar(out=ntau_[:], in0=c16[:, :, 15], scalar1=-1e-5, scalar2=None, op0=ALU.add), [c16], [ntau_])
                    V("dve", lambda e: e.tensor_tensor(out=nb[:], in0=ntau_[:], in1=nb[:], op=ALU.subtract), [nb, ntau_], [nb])
                    sc = scs[ti]
                    for h in range(8):
                        V("dve", lambda e: e.tensor_scalar(out=sc[:, 2 * h, :], in0=sc[:, 2 * h, :], scalar1=ntau_[:, h:h + 1], scalar2=None, op0=ALU.subtract),
                          [sc, ntau_], [sc])
            for ti in TI:
                r0 = row0 + ti * 128
                P.dma(x1_s[r0:r0 + 128, :], x1t[ti][:], q="sp")
                if not lite:
                    P.dma(sc_s[r0 // 128], scs[ti][:].rearrange("p a b -> p (a b)"), q="act")
                    P.dma(thr_s[r0 // 128], thrs[ti][:], q="sp")
            for hf2 in range(TSG // TS):
                P.dma(hx2T_s[row0 // TS + hf2], hx2T[:, :, hf2 * TS:(hf2 + 1) * TS], q="act")
    P.barrier()
    if not lite:
      with contextlib.ExitStack() as es:
        gx2 = SB(es, [128, D])
        gfin = SB(es, [128, D])
        P.dma(gfin[:], gf_d.partition_broadcast(128))
        hx2Tb = [SB(es, [128, 8, TS], BF16) for _ in range(2)]
        scsb = [[SB(es, [128, 16, 128]) for _ in range(NT)] for _ in range(2)]
        thrb = [[SB(es, [128, 16]) for _ in range(NT)] for _ in range(2)]
        x1t = [SB(es, [128, D]) for _ in range(NT)]
        NB_G = 4
        Sb = [SB(es, [128, 16, 128]) for _ in range(NB_G)]
        Eb = [SB(es, [128, 16, 128], BF16) for _ in range(NB_G)]
        Mb = [SB(es, [128, 16, 128], BF16) for _ in range(NB_G)]
        Gh = Eb
        Gc = [[SB(es, [128, 2048], BF16) for _ in range(2)] for _ in range(NT)]
        uTc = [SB(es, [128, 8, 512], BF16) for _ in range(2)]
        vc = [SB(es, [128, 4, 1024], BF16) for _ in range(3)]
        actb = [SB(es, [128, 512], BF16) for _ in range(2)]
        wab = [SB(es, [128, 512], BF16) for _ in range(2)]
        waT = [SB(es, [128, 4, 128], BF16) for _ in range(2)]
        tm2 = SB(es, [128, D])
        x2 = SB(es, [128, D])
        jk2 = SB(es, [128, D], BF16)
        ss4 = SB(es, [128, 1])
        rs4 = SB(es, [128, 1])
        ob = [SB(es, [128, D]) for _ in range(1)]
        pO = [PS(es, [128, D]) for _ in range(NT)]
        pA = [PS(es, [128, 512]) for _ in range(2)]
        pW = [PS(es, [128, 4, 128], BF16) for _ in range(2)]
        cnt = {"g": 0, "d": 0}
        for st_i in range(NS * L // TS):
            n = (st_i * TS) // L
            row0 = st_i * TS
            par = st_i % 2
            if (st_i * TS) % L == 0:
                P.dma(gx2[:], modx_d[n, :, 5 * D:6 * D])
            def srcs_of(p_):
                return (scsb[p_], [t_[:, 0:8] for t_ in thrb[p_]], [t_[:, 8:16] for t_ in thrb[p_]])

            def preload(si):
                p_ = si % 2
                P.dma(hx2Tb[p_][:], hx2T_s[si], q="sp")
                for ti in range(NT):
                    r0_ = si * TS + ti * 128
                    P.dma(scsb[p_][ti][:].rearrange("p a b -> p (a b)"), sc_s[r0_ // 128], q="act" if ti else "sp")
                    P.dma(thrb[p_][ti][:], thr_s[r0_ // 128], q="sp")

            if st_i == 0:
                preload(0)
            if st_i + 1 < NS * L // TS:
                preload(st_i + 1)
            hx2T = hx2Tb[par]
            for ti in range(NT):
                r0 = row0 + ti * 128
                P.dma(x1t[ti][:], x1_s[r0:r0 + 128, :], q="act")
            cnt_unused = None

            def g_pair(ic, ulist, srcs):
                scs_, nbs_, ntaus_ = srcs
                st = []
                for (ti, h) in ulist:
                    i3 = cnt["g"] % NB_G
                    cnt["g"] += 1
                    st.append((ti, h, Sb[i3], Eb[i3], Mb[i3], Gh[i3]))
                for (ti, h, S_, E_, M_, G_) in st:
                    sc = scs_[ti]
                    V("pool", lambda e: e.tensor_tensor(out=S_[:], in0=sc[:, 2 * h, ic * 16:(ic + 1) * 16].unsqueeze(2).to_broadcast([128, 16, 128]),
                                                        in1=sc[:, 2 * h + 1, :].unsqueeze(1).to_broadcast([128, 16, 128]), op=ALU.add), [sc], [S_])
                for (ti, h, S_, E_, M_, G_) in st:
                    V("act", lambda e: e.activation(out=S_[:], in_=S_[:], func=AF.Prelu, alpha=KMASK), [S_], [S_])
                for (ti, h, S_, E_, M_, G_) in st:
                    nb = nbs_[ti]
                    gdst = Gc[ti][ic % 2]
                    if h == 0:
                        V("act", lambda e: e.activation(out=gdst[:], in_=S_[:].rearrange("p a b -> p (a b)"), func=AF.Exp, bias=nb[:, h:h + 1]), [S_, nb], [gdst])
                    else:
                        V("act", lambda e: e.activation(out=E_[:], in_=S_[:], func=AF.Exp, bias=nb[:, h:h + 1]), [S_, nb], [E_])
                for (ti, h, S_, E_, M_, G_) in st:
                    gdst = Gc[ti][ic % 2]
                    if h != 0:
                        V("dve", lambda e: e.tensor_tensor(out=gdst[:], in0=gdst[:], in1=E_[:].rearrange("p a b -> p (a b)"), op=ALU.add), [E_, gdst], [gdst])

            def item_ctx(j):
                ic, r = divmod(j, 2 * 4)
                e4, ti = divmod(r, NT)
                ec = ic * 4 + e4
                return dict(ic=ic, e4=e4, ti=ti, ec=ec, u_=uTc[ec % 2], v_=vc[ec % 3], pa=pA[j % 2], a_=actb[j % 2], w_=wab[j % 2], wT=waT[j % 2], pw=pW[j % 2],
                            gsrc=Gc[ti][ic % 2])

            def D1(j):
                c_ = item_ctx(j); ec, ti, u_, v_, pa = c_["ec"], c_["ti"], c_["u_"], c_["v_"], c_["pa"]
                if ti == 0:
                    P.dma(u_[:], uTb_s[ec], q="sp")
                    P.dma(v_[:], vb_s[ec], q="act")
                mm(pa[:], [(hx2T[:, k, ti * 128:(ti + 1) * 128], u_[:, k, :]) for k in range(8)], [hx2T, u_], [pa])

            def D2(j):
                c_ = item_ctx(j); pa, a_ = c_["pa"], c_["a_"]
                V("act", lambda e: e.activation(out=a_[:], in_=pa[:], func=AF.Gelu_apprx_tanh), [pa], [a_])

            def D3(j):
                c_ = item_ctx(j); a_, w_, gsrc, e4 = c_["a_"], c_["w_"], c_["gsrc"], c_["e4"]
                V("dve", lambda e: e.tensor_tensor(out=w_[:], in0=a_[:], in1=gsrc[:, e4 * 512:(e4 + 1) * 512], op=ALU.mult), [a_, gsrc], [w_])

            def D4(j):
                c_ = item_ctx(j); w_, pw = c_["w_"], c_["pw"]
                for eb in range(4):
                    P.op("pe", lambda e, eb=eb: e.transpose(out=pw[:, eb, :], in_=w_[:, eb * 128:(eb + 1) * 128], identity=ident[:]), reads=[w_, ident], writes=[pw])

            def D5(j):
                c_ = item_ctx(j); pw, wT = c_["pw"], c_["wT"]
                if j % 4 == 3:
                    V("dve", lambda e: e.tensor_copy(out=wT[:], in_=pw[:]), [pw], [wT])
                else:
                    V("act", lambda e: e.copy(out=wT[:], in_=pw[:]), [pw], [wT])

            def D6(j):
                c_ = item_ctx(j); ec, ti, wT, v_ = c_["ec"], c_["ti"], c_["wT"], c_["v_"]
                for hf in range(2):
                    for eb in range(4):
                        first = (ec == 0 and eb == 0)
                        last = (ec == NCH - 1 and eb == 3)
                        P.op("pe", lambda e, hf=hf, eb=eb, first=first, last=last:
                             e.matmul(pO[ti][:, hf * 512:(hf + 1) * 512], lhsT=wT[:, eb, :], rhs=v_[:, eb, hf * 512:(hf + 1) * 512], start=first, stop=last),
                             reads=[wT, v_], writes=[pO[ti]])

            units = [(ti, h) for h in range(8) for ti in range(NT)]
            NJ = 64
            upi = 2
            if st_i == 0:
                for j in range(8):
                    g_pair(0, units[j * upi:(j + 1) * upi], srcs_of(0))
            def ok(j):
                return 0 <= j < NJ

            for s in range(NJ + 3):
                if ok(s - 2):
                    D4(s - 2)
                if s % 2 == 0 and ok(s - 1):
                    D2(s - 1)
                    D3(s - 1)
                if s < NJ:
                    ic, r = divmod(s, 8)
                    if ic + 1 < 8:
                        g_pair(ic + 1, units[r * upi:(r + 1) * upi], srcs_of(par))
                    elif st_i + 1 < NS * L // TS:
                        g_pair(0, units[r * upi:(r + 1) * upi], srcs_of(1 - par))
                if s % 2 == 1 and ok(s - 1):
                    D2(s - 1)
                    D3(s - 1)
                if ok(s - 2):
                    D5(s - 2)
                if ok(s - 3):
                    D6(s - 3)
                if s < NJ:
                    D1(s)
            for ti in range(NT):
                r0 = row0 + ti * 128
                x1 = x1t[ti]
                o_ = ob[0]
                if debug:
                    V("act", lambda e: e.copy(out=tm2[:], in_=pO[ti][:]), [pO[ti]], [tm2])
                    P.dma(dbg["pe"][r0:r0 + 128, :], tm2[:], is_output=True)
                V("dve", lambda e: e.tensor_tensor(out=tm2[:], in0=pO[ti][:], in1=gx2[:], op=ALU.mult), [pO[ti], gx2], [tm2])
                V("pool", lambda e: e.tensor_tensor(out=x2[:], in0=tm2[:], in1=x1[:], op=ALU.add), [tm2, x1], [x2])
                V("act", lambda e: e.activation(out=jk2[:], in_=x2[:], func=AF.Square, accum_out=ss4[:]), [x2], [jk2, ss4])
                rsqrt_mean(es, ss4, D, rs4)
                V("dve", lambda e: e.scalar_tensor_tensor(out=o_[:], in0=x2[:], scalar=rs4[:, 0:1], in1=gfin[:], op0=ALU.mult, op1=ALU.mult),
                  [x2, rs4, gfin], [o_])
                P.dma(out_d[r0:r0 + 128, :], o_[:], q="sp", is_output=True)

    P.emit()
    top.close()
    return nc


def _prep_inputs(inp):
    global CONST
    if CONST is None:
        CONST = _constants()
    f = lambda a: np.ascontiguousarray(np.asarray(a, dtype=np.float32))
    shared = {}
    shared["w_ada"] = f(inp["w_ada"][0])
    shared["b_ada"] = f(inp["b_ada"][0])
    shared["g_norm1"] = f(inp["g_norm1"][0])
    shared["g_norm2"] = f(inp["g_norm2"][0])
    shared["w_in"] = f(inp["w_in"][0])
    shared["b_in"] = f(inp["b_in"][0])
    a_re = np.asarray(inp["s5_a_re"][0]).reshape(64, 64).T
    a_im = np.asarray(inp["s5_a_im"][0]).reshape(64, 64).T
    ls = np.broadcast_to(np.asarray(inp["s5_log_step"][0]).reshape(1, 64), (64, 64))
    shared["s5p"] = f(np.stack([a_re, a_im, ls], axis=1))
    bre = np.asarray(inp["s5_b_re"][0]).reshape(64, 64, 16).transpose(1, 0, 2)
    bim = np.asarray(inp["s5_b_im"][0]).reshape(64, 64, 16).transpose(1, 0, 2)
    shared["s5b"] = f(np.stack([bre, bim], axis=1))
    cre = np.asarray(inp["s5_c_re"][0]).reshape(64, 16, 64).transpose(2, 0, 1)
    cim = np.asarray(inp["s5_c_im"][0]).reshape(64, 16, 64).transpose(2, 0, 1)
    shared["s5c"] = f(np.stack([cre, cim], axis=1))
    d5 = np.asarray(inp["s5_d"][0]).reshape(32, 16)
    shared["s5d"] = f(np.broadcast_to(d5.T[None], (8, 16, 32)).reshape(128, 32))
    shared["w_glu"] = f(inp["w_glu"][0])
    shared["b_glu"] = f(inp["b_glu"][0])
    shared["hy_conv_w"] = f(inp["hy_conv_w"][0])
    shared["hy_conv_b"] = f(inp["hy_conv_b"][0])
    shared["hf_w1"] = f(inp["hf_w1"][0])
    shared["hf_b1"] = f(np.asarray(inp["hf_b1"][0]).reshape(64, 1))
    shared["hf_wh"] = f(inp["hf_wh"][0])
    shared["hf_bh"] = f(np.asarray(inp["hf_bh"][0]).T)
    shared["hf_freq"] = f(np.asarray(inp["hf_freq"][0]).reshape(64, 1))
    shared["hf_wout"] = f(inp["hf_wout"][0])
    shared["hf_decay"] = f(inp["hf_decay"][0])
    shared["hy_d"] = f(inp["hy_d"][0])
    shared["g_out_s5"] = f(inp["g_out_s5"][0])
    shared["g_out_hy"] = f(inp["g_out_hy"][0])
    shared["w_out"] = f(inp["w_out"][0])
    shared["peer_wq"] = f(inp["peer_wq"][0])
    k1 = np.asarray(inp["peer_k1"][0])
    k2 = np.asarray(inp["peer_k2"][0])
    kk = np.stack([k1, k2], axis=1).reshape(16, 128, 128)
    shared["peer_kT"] = f(kk.transpose(2, 0, 1))
    shared["peer_uT"] = f(np.asarray(inp["peer_u"][0]).T)
    shared["peer_v"] = f(inp["peer_v"][0])
    shared["g_final"] = f(inp["g_final"])
    for k, v in CONST.items():
        shared["k_" + k] = v
    maps = []
    x = np.asarray(inp["x"])
    ctx = np.asarray(inp["ctx"])
    c = np.asarray(inp["c"])
    cc = np.asarray(inp["c_ctx"])
    for i in range(NCORES):
        m = dict(shared)
        m["x"] = f(x[i * NS:(i + 1) * NS].reshape(NS * L, D))
        m["ctx"] = f(ctx[i * NS:(i + 1) * NS].reshape(NS * LC, D))
        c5 = np.concatenate([c[i * NS:(i + 1) * NS], cc[None]], axis=0)
        m["cT"] = f(c5.reshape(NS + 1, 8, 128).transpose(2, 1, 0))
        maps.append(m)
    return maps


def kernel(**inputs):
    maps = _prep_inputs(inputs)
    nc = build_program()
    res = run_bass_kernel_spmd(nc, maps, core_ids=list(range(NCORES)))
    out = np.concatenate([np.asarray(r["out"]).reshape(NS, L, D) for r in res.results], axis=0)
    return out.astype(np.float32)
```

```python
import contextlib
import math
import types
import numpy as np
import ml_dtypes
import concourse.bass as bass
import concourse.mybir as mybir
from concourse.bass_utils import run_bass_kernel_spmd

F32 = mybir.dt.float32
BF16 = mybir.dt.bfloat16
I32 = mybir.dt.int32
ALU = mybir.AluOpType
AF = mybir.ActivationFunctionType
AX = mybir.AxisListType

NSLOT = 12
NCORES = 8
NS = 4
L = 2048
D = 1024
LC = 256
EPS = 1e-6
NFFT = 4096


def _freeze(fn):
    if fn.__closure__ is None:
        return fn
    cells = []
    for c in fn.__closure__:
        try:
            cells.append(types.CellType(c.cell_contents))
        except ValueError:
            cells.append(c)
    return types.FunctionType(fn.__code__, fn.__globals__, fn.__name__, fn.__defaults__, tuple(cells))


class Prog:
    DMAQ = ("sp", "act", "pool")

    def __init__(self, nc):
        self.nc = nc
        self.streams = {k: [] for k in ("pe", "act", "dve", "pool", "sp")}
        self.cnt = {}
        self.waited = {k: {} for k in self.streams}
        self.lastw = {}
        self.reads = {}
        self.slot_next = {q: 0 for q in self.DMAQ}
        self.ninst = 0
        self.out_deps = []

    def _key(self, x):
        if isinstance(x, (str, tuple)):
            return x
        t = getattr(x, "tensor", x)
        return getattr(t, "name", None) or id(t)

    def _deps(self, reads, writes):
        deps = {}

        def add(s, c):
            if c > deps.get(s, 0):
                deps[s] = c
        for k in reads:
            lw = self.lastw.get(k)
            if lw:
                add(*lw)
        for k in writes:
            lw = self.lastw.get(k)
            if lw:
                add(*lw)
            for s, c in self.reads.get(k, {}).items():
                add(s, c)
        return deps

    def _emit_waits(self, stream, deps):
        w = self.waited[stream]
        for s, c in deps.items():
            if s == stream and stream == "pe":
                continue
            if c > w.get(s, 0):
                w[s] = c
                self.streams[stream].append(("wait", s, c))

    def _commit(self, sem, cnt, reads, writes):
        for k in writes:
            self.lastw[k] = (sem, cnt)
            self.reads[k] = {}
        for k in reads:
            self.reads.setdefault(k, {})
            if self.reads[k].get(sem, 0) < cnt:
                self.reads[k][sem] = cnt

    def op(self, stream, fn, reads=(), writes=()):
        reads = [self._key(r) for r in reads]
        writes = [self._key(r) for r in writes]
        deps = self._deps(reads, writes)
        self._emit_waits(stream, deps)
        c = self.cnt.get(stream, 0) + 1
        self.cnt[stream] = c
        self.streams[stream].append(("op", _freeze(fn), stream, 1))
        self._commit(stream, c, reads, writes)
        self.ninst += 1

    def dma(self, out, in_, reads=None, writes=None, q="sp", is_output=False, **kw):
        reads = [self._key(r) for r in (reads if reads is not None else [in_])]
        writes = [self._key(r) for r in (writes if writes is not None else [out])]
        slot = self.slot_next[q]
        self.slot_next[q] = (slot + 1) % NSLOT
        sem = ("dma", q, slot)
        deps = self._deps(reads, writes)
        prev = self.cnt.get(sem, 0)
        if prev:
            deps[sem] = max(deps.get(sem, 0), prev)
        self._emit_waits(q, deps)
        c = prev + 16
        self.cnt[sem] = c
        self.streams[q].append(("dma", out, in_, kw, sem))
        self._commit(sem, c, reads, writes)
        if is_output:
            self.out_deps.append((sem, c))
        self.ninst += 1

    def barrier(self):
        allc = dict(self.cnt)
        for st in self.streams:
            self._emit_waits(st, allc)

    def emit(self):
        nc = self.nc
        fin = {}
        for s, c in self.out_deps:
            fin[s] = max(fin.get(s, 0), c)
        self._emit_waits("sp", fin)
        semkeys = list(self.cnt.keys())
        with contextlib.ExitStack() as es:
            sems = {}
            for i, k in enumerate(semkeys):
                sems[k] = es.enter_context(nc.semaphore("s%d" % i))
            block = es.enter_context(nc.Block())
            engs = {"pe": block.tensor, "act": block.scalar, "dve": block.vector,
                    "pool": block.gpsimd, "sp": block.sync}

            def make(stream):
                items = self.streams[stream]

                def body(eng):
                    for it in items:
                        if it[0] == "wait":
                            eng.wait_ge(sems[it[1]], it[2])
                        elif it[0] == "op":
                            it[1](eng).then_inc(sems[it[2]], 1)
                        else:
                            _, out, in_, kw, sem = it
                            eng.dma_start(out=out, in_=in_, **kw).then_inc(sems[sem], 16)
                return body
            for st in ("sp", "act", "pool", "dve", "pe"):
                if self.streams[st]:
                    engs[st](make(st))


def _constants():
    c = {}
    c["ident"] = np.eye(128, dtype=np.float32).astype(ml_dtypes.bfloat16)
    c["identf"] = np.eye(128, dtype=np.float32)
    sm = np.zeros((128, 128), np.float32)
    sp = np.zeros((128, 128), np.float32)
    for t in range(128):
        if t % 64 != 0:
            sm[t - 1, t] = 1
        if t % 64 != 63:
            sp[t + 1, t] = 1
    c["shm"] = sm.astype(ml_dtypes.bfloat16)
    c["shp"] = sp.astype(ml_dtypes.bfloat16)
    t = np.arange(L, dtype=np.float64) + 0.5
    ang = 2 * np.pi * np.outer(t, t) / NFFT
    c["ctab"] = np.cos(ang).astype(np.float32).astype(ml_dtypes.bfloat16)
    c["stab"] = np.sin(ang).astype(np.float32).astype(ml_dtypes.bfloat16)
    phi = np.pi * (np.arange(L, dtype=np.float64) + 0.5) / NFFT
    c["cphi"] = np.cos(phi).astype(np.float32).reshape(16, 128).T.copy()
    c["sphi"] = np.sin(phi).astype(np.float32).reshape(16, 128).T.copy()
    pos = np.arange(L, dtype=np.float32)
    tt = pos / np.float32(L - 1)
    bands = np.linspace(1e-4, 15, 16, dtype=np.float32)
    a = (np.float32(2.0 * math.pi / L) * pos[:, None]) * bands[None, :]
    feats = np.concatenate([tt[:, None], np.cos(a), -np.sin(a)], axis=-1).astype(np.float32)
    c["featsT"] = feats.T.copy()
    c["tneg"] = (-tt).reshape(16, 128).T.copy()
    j = np.repeat(np.arange(8), 16).astype(np.float32)
    ex = np.stack([7 - j, j, j + 1, 8 - j, j - 7, -j])
    c["s5exp"] = np.broadcast_to(ex[None], (64, 6, 128)).astype(np.float32).copy()
    jj = np.repeat(np.arange(8), 16)
    c["mf"] = (jj[None, :] >= jj[:, None]).astype(np.float32)
    c["mb"] = (jj[:, None] >= jj[None, :]).astype(np.float32)
    m0 = np.ones((128, 1), np.float32)
    m0[0, 0] = 0.0
    c["m0"] = m0
    c["onesf"] = np.ones((128, 128), np.float32)
    c["mix"] = np.broadcast_to(np.arange(256, dtype=np.float32)[None], (64, 256)).copy()
    return c


CONST = None


def build_program(upto=99, debug=False, lite=False):
    nc = bass.Bass("TRN2", target_bir_lowering=False)
    P = Prog(nc)
    dbg = {}

    def din(name, shape, dt=F32):
        return nc.dram_tensor(name, list(shape), dt, kind="ExternalInput").ap()

    def dscr(name, shape, dt=F32):
        return nc.dram_tensor(name, list(shape), dt).ap()

    def dout(name, shape, dt=F32):
        return nc.dram_tensor(name, list(shape), dt, kind="ExternalOutput").ap()

    x_d = din("x", [NS * L, D])
    ctx_d = din("ctx", [NS * LC, D])
    cT_d = din("cT", [128, 8, NS + 1])
    w_ada = din("w_ada", [D, 6 * D])
    b_ada = din("b_ada", [6 * D])
    g1_d = din("g_norm1", [D])
    g2_d = din("g_norm2", [D])
    w_in_d = din("w_in", [D, 2048])
    b_in_d = din("b_in", [2048])
    s5p_d = din("s5p", [64, 3, 64])
    s5b_d = din("s5b", [64, 2, 64, 16])
    s5c_d = din("s5c", [64, 2, 64, 16])
    s5d_d = din("s5d", [128, 32])
    w_glu_d = din("w_glu", [512, 512])
    b_glu_d = din("b_glu", [512])
    cw_d = din("hy_conv_w", [3, 1536])
    cb_d = din("hy_conv_b", [1536])
    hf_w1 = din("hf_w1", [33, 64])
    hf_b1 = din("hf_b1", [64, 1])
    hf_wh = din("hf_wh", [2, 64, 64])
    hf_bh = din("hf_bh", [64, 2])
    hf_fr = din("hf_freq", [64, 1])
    hf_wout = din("hf_wout", [64, 2048])
    hf_dec = din("hf_decay", [2048])
    hyd_d = din("hy_d", [2, 512])
    gs5_d = din("g_out_s5", [512])
    ghy_d = din("g_out_hy", [512])
    w_out_d = din("w_out", [D, D])
    wq_d = din("peer_wq", [D, 2048])
    kT_d = din("peer_kT", [128, 16, 128])
    uT_d = din("peer_uT", [D, 16384 if not lite else 128])
    v_d = din("peer_v", [16384 if not lite else 128, D])
    gf_d = din("g_final", [D])
    cst = {k: din("k_" + k, list(v.shape), BF16 if v.dtype == ml_dtypes.bfloat16 else F32)
           for k, v in CONST.items()}
    out_d = dout("out", [NS * L, D])

    modx_d = dscr("modx_s", [NS + 1, 128, 6 * D])
    q_s = dscr("q_s", [NS * L, 1536])
    y_s = dscr("y_s", [NS * L, 512])
    z_s = dscr("z_s", [NS * L, 512])
    kspec_s = dscr("kspec_s", [2, 2, 16, 128, 512])
    uTb_s = dscr("uTb_s", [32, 128, 8, 512], BF16)
    vb_s = dscr("vb_s", [32, 128, 4, 1024], BF16)

    if debug:
        dbg["modx"] = dout("dbg_modx", [NS + 1, 6 * D])

    top = contextlib.ExitStack()
    uid = [0]

    def SB(es, shape, dt=F32, name=None):
        uid[0] += 1
        return es.enter_context(nc.sbuf_tensor((name or "t") + str(uid[0]), list(shape), dt))

    def PS(es, shape, dt=F32, name=None):
        uid[0] += 1
        return es.enter_context(nc.psum_tensor((name or "p") + str(uid[0]), list(shape), dt))

    def mm(ps_ap, pairs, reads, writes):
        n = len(pairs)
        for i, (l, r) in enumerate(pairs):
            P.op("pe", lambda e, l=l, r=r, i=i: e.matmul(ps_ap, lhsT=l, rhs=r, start=(i == 0), stop=(i == n - 1)),
                 reads=reads, writes=writes)

    def V(eng, fn, reads, writes):
        P.op(eng, fn, reads=reads, writes=writes)

    ident = SB(top, [128, 128], BF16, "ident")
    identf = SB(top, [128, 128], F32, "identf")
    mhalf = SB(top, [128, 1], F32, "mhalf")
    P.dma(ident[:], cst["ident"][:, :])
    P.dma(identf[:], cst["identf"][:, :])
    V("pool", lambda e: e.memset(mhalf[:], -0.5), [], [mhalf])

    def rsqrt_mean(es, ssum, n, out_rstd):
        V("dve", lambda e: e.tensor_scalar(out=out_rstd[:], in0=ssum[:], scalar1=1.0 / n, scalar2=EPS,
                                           op0=ALU.mult, op1=ALU.add), [ssum], [out_rstd])
        V("pool", lambda e: e.tensor_tensor(out=out_rstd[:], in0=out_rstd[:], in1=mhalf[:], op=ALU.pow),
          [out_rstd, mhalf], [out_rstd])

    with contextlib.ExitStack() as es:
        cT = SB(es, [128, 8, NS + 1])
        sil = SB(es, [128, 8, NS + 1])
        rep = SB(es, [128, NS + 1, 8, 128])
        bada = SB(es, [128, 6 * D])
        P.dma(cT[:], cT_d[:, :, :])
        P.dma(bada[:], b_ada.partition_broadcast(128))
        V("act", lambda e: e.activation(out=sil[:], in_=cT[:], func=AF.Silu), [cT], [sil])
        for n in range(NS + 1):
            V("dve", lambda e, n=n: e.tensor_copy(out=rep[:, n], in_=sil[:, :, n:n + 1].to_broadcast([128, 8, 128])),
              [sil], [rep])
        wts = [SB(es, [128, 8, 512]) for _ in range(2)]
        mps = [PS(es, [128, 512]) for _ in range(2)]
        mo = [SB(es, [128, 512]) for _ in range(2)]
        it = 0
        for nt in range(12):
            wt = wts[nt % 2]
            P.dma(wt[:], w_ada[:, nt * 512:(nt + 1) * 512].rearrange("(k p) n -> p k n", p=128))
            for n in range(NS + 1):
                ps = mps[it % 2]
                o = mo[it % 2]
                it += 1
                mm(ps[:], [(rep[:, n, k, :], wt[:, k, :]) for k in range(8)], [rep, wt], [ps])
                V("dve", lambda e, ps=ps, o=o, nt=nt: e.tensor_tensor(out=o[:], in0=ps[:], in1=bada[:, nt * 512:(nt + 1) * 512],
                                                                      op=ALU.add), [ps, bada], [o])
                P.dma(modx_d[n, :, nt * 512:(nt + 1) * 512], o[:], q="act")
                if debug:
                    P.dma(dbg["modx"][n:n + 1, nt * 512:(nt + 1) * 512], o[0:1, :], q="act", is_output=True)
    P.barrier()
    if upto <= 0:
        P.emit()
        top.close()
        return nc


    KMASK = 1.0e5
    NEXP = 16384 if not lite else 128
    NCH = NEXP // 512 if not lite else 0
    TS = 256
    NT = TS // 128
    wq_b = dscr("wq_b", [128, 8, 2048], BF16)
    wout_b = dscr("wout_b", [128, 8, 1024], BF16)
    wglu_b = dscr("wglu_b", [128, 4, 512], BF16)
    kT_b = dscr("kT_b", [128, 16, 128], BF16)
    if debug:
        dbg["x1"] = dout("dbg_x1", [NS * L, D])
        dbg["hx2"] = dout("dbg_hx2", [NS * L, D], BF16)
        dbg["pe"] = dout("dbg_pe", [NS * L, D])
    jobs = []
    for k in range(8):
        jobs.append((wq_b[:, k, :], wq_d[k * 128:(k + 1) * 128, :], 2048))
    for k in range(8):
        jobs.append((wout_b[:, k, :], w_out_d[k * 128:(k + 1) * 128, :], 1024))
    for k in range(4):
        jobs.append((wglu_b[:, k, :], w_glu_d[k * 128:(k + 1) * 128, :], 512))
    jobs.append((kT_b[:].rearrange("p c n -> p (c n)"), kT_d[:].rearrange("p c n -> p (c n)"), 2048))
    for ec in range(NCH):
        jobs.append((uTb_s[ec].rearrange("p k e -> p (k e)"), uT_d[:, ec * 512:(ec + 1) * 512].rearrange("(k p) e -> p k e", p=128), 4096))
        jobs.append((vb_s[ec].rearrange("p b d -> p (b d)"), v_d[ec * 512:(ec + 1) * 512, :].rearrange("(b p) d -> p b d", p=128), 4096))
    job_i = [0]

    def emit_job(stg, stb):
        i = job_i[0]
        if i >= len(jobs):
            return
        job_i[0] += 1
        dst, srcap, nel = jobs[i]
        a = stg[i % 2]
        b = stb[i % 2]
        if len(srcap.shape) == 3:
            P.dma(a[:, 0:nel].rearrange("p (k e) -> p k e", k=srcap.shape[1]), srcap, q="sp")
        else:
            P.dma(a[:, 0:nel], srcap, q="sp")
        V("act", lambda e: e.copy(out=b[:, 0:nel], in_=a[:, 0:nel]), [a], [b])
        P.dma(dst, b[:, 0:nel], q="sp")

    U_s = dscr("U_s", [32, 128, NS * 256 + 128], BF16)
    if debug:
        dbg["U"] = dout("dbg_U", [32, 128, NS * 256 + 128], BF16)
        dbg["q"] = dout("dbg_q", [NS * L, 1536])
    with contextlib.ExitStack() as es:
        w_in = SB(es, [128, 8, 2048], BF16)
        stage = [SB(es, [128, 2048]) for _ in range(2)]
        for k in range(8):
            st = stage[k % 2]
            P.dma(st[:], w_in_d[k * 128:(k + 1) * 128, :])
            V("dve", lambda e, st=st, k=k: e.tensor_copy(out=w_in[:, k, :], in_=st[:]), [st], [w_in])
        b_in = SB(es, [128, 2048])
        g1 = SB(es, [128, D])
        cw = SB(es, [128, 3, 1536])
        cb = SB(es, [128, 1536])
        shm = SB(es, [128, 128], BF16)
        shp = SB(es, [128, 128], BF16)
        P.dma(b_in[:], b_in_d.partition_broadcast(128))
        P.dma(g1[:], g1_d.partition_broadcast(128))
        for j in range(3):
            P.dma(cw[:, j, :], cw_d[j].partition_broadcast(128))
        P.dma(cb[:], cb_d.partition_broadcast(128))
        P.dma(shm[:], cst["shm"][:, :])
        P.dma(shp[:], cst["shp"][:, :])
        hT = SB(es, [128, 8, 1024], BF16)
        um2 = SB(es, [128, 32, 8, 16], BF16)
        ug = [SB(es, [128, 8, 128], BF16) for _ in range(2)]
        xt = [SB(es, [128, D]) for _ in range(2)]
        tmp = SB(es, [128, D])
        hb = [SB(es, [128, D], BF16) for _ in range(2)]
        g1p = SB(es, [128, D])
        sh1 = SB(es, [128, D])
        ssum = [SB(es, [128, 1]) for _ in range(2)]
        rstd = [SB(es, [128, 1]) for _ in range(2)]
        junk = SB(es, [128, D], BF16)
        pbf = SB(es, [128, 1536], BF16)
        t1 = SB(es, [128, 1536])
        t2 = SB(es, [128, 1536])
        pA = PS(es, [128, 1536])
        pB = PS(es, [128, 1536])
        trp = [PS(es, [128, 8, 128], BF16) for _ in range(1)]
        ps5 = PS(es, [128, 512])

        cstg = [SB(es, [128, 4096]) for _ in range(2)]
        cstb = [SB(es, [128, 4096], BF16) for _ in range(2)]
        blocks = [("x", n, half) for n in range(NS) for half in range(2)] + [("c", NS, 0)]
        cur_mod = None
        for bi, (kind, n, half) in enumerate(blocks):
            if cur_mod != n:
                cur_mod = n
                P.dma(sh1[:], modx_d[n, :, 0:D])
                P.dma(g1p[:], modx_d[n, :, D:2 * D])
                V("dve", lambda e: e.scalar_tensor_tensor(out=g1p[:], in0=g1p[:], scalar=1.0, in1=g1[:],
                                                          op0=ALU.add, op1=ALU.mult), [g1p, g1], [g1p])
            src = x_d if kind == "x" else ctx_d
            row0 = (n * L + half * 1024) if kind == "x" else 0
            for ti in range(8):
                X = xt[ti % 2]
                H = hb[ti % 2]
                ss = ssum[ti % 2]
                rs = rstd[ti % 2]
                P.dma(X[:], src[row0 + ti * 128: row0 + (ti + 1) * 128, :], q="sp" if ti % 2 else "act")
                V("act", lambda e, X=X, ss=ss: e.activation(out=junk[:], in_=X[:], func=AF.Square, accum_out=ss[:]),
                  [X], [junk, ss])
                rsqrt_mean(es, ss, D, rs)
                V("dve", lambda e, X=X, rs=rs: e.scalar_tensor_tensor(out=tmp[:], in0=X[:], scalar=rs[:, 0:1], in1=g1p[:],
                                                                      op0=ALU.mult, op1=ALU.mult), [X, rs, g1p], [tmp])
                V("pool", lambda e, H=H: e.tensor_tensor(out=H[:], in0=tmp[:], in1=sh1[:], op=ALU.add), [tmp, sh1], [H])
                tp = trp[0]
                for k in range(8):
                    P.op("pe", lambda e, k=k, H=H, tp=tp: e.transpose(out=tp[:, k, :], in_=H[:, k * 128:(k + 1) * 128], identity=ident[:]),
                         reads=[H, ident], writes=[tp])
                V("act", lambda e, tp=tp, ti=ti: e.copy(out=hT[:, :, ti * 128:(ti + 1) * 128], in_=tp[:]), [tp], [hT])
                emit_job(cstg, cstb)
            if kind == "x":
                for ti in range(8):
                    for nt in range(3):
                        mm(pA[:, nt * 512:(nt + 1) * 512],
                           [(hT[:, k, ti * 128:(ti + 1) * 128], w_in[:, k, 512 + nt * 512: 512 + (nt + 1) * 512]) for k in range(8)],
                           [hT, w_in], [pA])
                    V("dve", lambda e: e.tensor_tensor(out=pbf[:], in0=pA[:], in1=b_in[:, 512:2048], op=ALU.add), [pA, b_in], [pbf])
                    for nt in range(3):
                        mm(pB[:, nt * 512:(nt + 1) * 512], [(shm[:], pbf[:, nt * 512:(nt + 1) * 512])], [shm, pbf], [pB])
                    for nt in range(3):
                        mm(pA[:, nt * 512:(nt + 1) * 512], [(shp[:], pbf[:, nt * 512:(nt + 1) * 512])], [shp, pbf], [pA])
                    V("pool", lambda e: e.tensor_tensor(out=t1[:], in0=pbf[:], in1=cw[:, 1, :], op=ALU.mult), [pbf, cw], [t1])
                    V("pool", lambda e: e.tensor_tensor(out=t1[:], in0=t1[:], in1=cb[:], op=ALU.add), [t1, cb], [t1])
                    V("dve", lambda e: e.tensor_tensor(out=t2[:], in0=pB[:], in1=cw[:, 0, :], op=ALU.mult), [pB, cw], [t2])
                    V("pool", lambda e: e.tensor_tensor(out=t1[:], in0=t1[:], in1=t2[:], op=ALU.add), [t1, t2], [t1])
                    V("dve", lambda e: e.tensor_tensor(out=t2[:], in0=pA[:], in1=cw[:, 2, :], op=ALU.mult), [pA, cw], [t2])
                    V("pool", lambda e: e.tensor_tensor(out=t1[:], in0=t1[:], in1=t2[:], op=ALU.add), [t1, t2], [t1])
                    r0 = row0 + ti * 128
                    P.dma(q_s[r0:r0 + 128, :], t1[:], q="act")
                    if debug:
                        P.dma(dbg["q"][r0:r0 + 128, :], t1[:], q="act", is_output=True)
            for j in range(8):
                mm(ps5[:], [(hT[:, k, j::8], w_in[:, k, 0:512]) for k in range(8)], [hT, w_in], [ps5])
                V("dve", lambda e, j=j: e.tensor_tensor(out=um2[:, :, j, :], in0=ps5[:].rearrange("p (g h) -> p g h", g=32),
                                                        in1=b_in[:, 0:512].rearrange("p (g h) -> p g h", g=32), op=ALU.add),
                  [ps5, b_in], [um2])
            col0 = (n * 256 + half * 128) if kind == "x" else NS * 256
            for gb in range(4):
                tp = trp[0]
                ugt = ug[gb % 2]
                for gi in range(8):
                    g = gb * 8 + gi
                    P.op("pe", lambda e, g=g, gi=gi, tp=tp: e.transpose(out=tp[:, gi, :], in_=um2[:, g].rearrange("p j h -> p (j h)"),
                                                                        identity=ident[:]), reads=[um2, ident], writes=[tp])
                V("act", lambda e, tp=tp, ugt=ugt: e.copy(out=ugt[:], in_=tp[:]), [tp], [ugt])
                P.dma(U_s[gb * 8:(gb + 1) * 8, :, col0:col0 + 128].rearrange("g p c -> p g c"), ugt[:], q="sp")
                if debug:
                    P.dma(dbg["U"][gb * 8:(gb + 1) * 8, :, col0:col0 + 128].rearrange("g p c -> p g c"), ugt[:], q="sp", is_output=True)
        while job_i[0] < len(jobs):
            emit_job(cstg, cstb)
    P.barrier()
    if upto <= 1:
        P.emit()
        top.close()
        return nc


    TWO_PI = 2.0 * math.pi
    PI_LO = 3.1415925

    def trig(ang, n_free_shape, scratch, out_sin=None, out_cos=None):
        ki, kf, r = scratch
        for out, off in ((out_sin, 0.0), (out_cos, math.pi / 2)):
            if out is None:
                continue
            V("dve", lambda e, off=off: e.tensor_scalar(out=kf, in0=ang, scalar1=off, scalar2=1.0 / TWO_PI, op0=ALU.add, op1=ALU.mult),
              [ang], [kf])
            V("dve", lambda e: e.tensor_copy(out=ki, in_=kf), [kf], [ki])
            V("dve", lambda e: e.tensor_copy(out=kf, in_=ki), [ki], [kf])
            V("dve", lambda e: e.scalar_tensor_tensor(out=r, in0=kf, scalar=-TWO_PI, in1=ang, op0=ALU.mult, op1=ALU.add), [kf, ang], [r])
            V("dve", lambda e, off=off: e.tensor_scalar(out=r, in0=r, scalar1=off, scalar2=PI_LO, op0=ALU.add, op1=ALU.min), [r], [r])
            V("dve", lambda e: e.tensor_scalar(out=r, in0=r, scalar1=-PI_LO, scalar2=None, op0=ALU.max), [r], [r])
            V("act", lambda e, out=out: e.activation(out=out, in_=r, func=AF.Sin), [r], [out])

    if debug:
        dbg["y"] = dout("dbg_y", [NS * L, 512])
        dbg["h0"] = dout("dbg_h0", [64, 2, 64, NS])
    s5es = contextlib.ExitStack()
    R8 = SB(s5es, [128, 64, 2, 64], BF16)
    O8 = SB(s5es, [64, 64, 2, 128], BF16)
    T8 = SB(s5es, [128, 32, 128], BF16)
    th8 = SB(s5es, [64, 64])
    r8 = SB(s5es, [64, 64])
    with contextlib.ExitStack() as es:
        s5p = SB(es, [64, 3, 64])
        s5b = SB(es, [64, 2, 64, 16])
        s5c = SB(es, [64, 2, 64, 16])
        s5d = SB(es, [128, 32])
        sexp = SB(es, [64, 6, 128])
        mf = SB(es, [128, 128])
        mb = SB(es, [128, 128])
        P.dma(s5p[:], s5p_d[:, :, :])
        P.dma(s5b[:], s5b_d[:, :, :, :])
        P.dma(s5c[:], s5c_d[:, :, :, :])
        P.dma(s5d[:], s5d_d[:, :])
        P.dma(sexp[:], cst["s5exp"][:, :, :])
        P.dma(mf[:], cst["mf"][:, :])
        P.dma(mb[:], cst["mb"][:, :])
        step = SB(es, [64, 64])
        rho = SB(es, [64, 64])
        th = SB(es, [64, 64])
        ebase = SB(es, [64, 64])
        V("pool", lambda e: e.memset(ebase[:], math.e), [], [ebase])
        V("pool", lambda e: e.tensor_tensor(out=step[:], in0=ebase[:], in1=s5p[:, 2, :], op=ALU.pow), [ebase, s5p], [step])
        V("dve", lambda e: e.tensor_tensor(out=rho[:], in0=s5p[:, 0, :], in1=step[:], op=ALU.mult), [s5p, step], [rho])
        V("dve", lambda e: e.tensor_tensor(out=th[:], in0=s5p[:, 1, :], in1=step[:], op=ALU.mult), [s5p, step], [th])
        ki_s = SB(es, [64, 1024], I32)
        kf_s = SB(es, [64, 1024])
        r_s = SB(es, [64, 1024])
        sn = SB(es, [64, 1024])
        cs = SB(es, [64, 1024])
        mg = SB(es, [64, 1024])
        ang = SB(es, [64, 1024])
        V("dve", lambda e: e.tensor_copy(out=ang[:, 0:64], in_=th[:]), [th], [ang])
        trig(ang[:, 0:64], None, (ki_s[:, 0:64], kf_s[:, 0:64], r_s[:, 0:64]), sn[:, 0:64], cs[:, 0:64])
        V("act", lambda e: e.activation(out=mg[:, 0:64], in_=rho[:], func=AF.Exp), [rho], [mg])
        l1re = SB(es, [64, 64])
        l1im = SB(es, [64, 64])
        V("dve", lambda e: e.tensor_tensor(out=l1re[:], in0=mg[:, 0:64], in1=cs[:, 0:64], op=ALU.mult), [mg, cs], [l1re])
        V("dve", lambda e: e.tensor_tensor(out=l1im[:], in0=mg[:, 0:64], in1=sn[:, 0:64], op=ALU.mult), [mg, sn], [l1im])
        V("dve", lambda e: e.tensor_scalar(out=l1re[:], in0=l1re[:], scalar1=-1.0, scalar2=None, op0=ALU.add), [l1re], [l1re])
        den = SB(es, [64, 64])
        t64 = SB(es, [64, 64])
        cre = SB(es, [64, 64])
        cim = SB(es, [64, 64])
        are = s5p[:, 0, :]
        aim = s5p[:, 1, :]
        V("dve", lambda e: e.tensor_tensor(out=den[:], in0=are, in1=are, op=ALU.mult), [s5p], [den])
        V("dve", lambda e: e.tensor_tensor(out=t64[:], in0=aim, in1=aim, op=ALU.mult), [s5p], [t64])
        V("dve", lambda e: e.tensor_tensor(out=den[:], in0=den[:], in1=t64[:], op=ALU.add), [den, t64], [den])
        V("dve", lambda e: e.reciprocal(out=den[:], in_=den[:]), [den], [den])
        V("dve", lambda e: e.tensor_tensor(out=cre[:], in0=l1re[:], in1=are, op=ALU.mult), [l1re, s5p], [cre])
        V("dve", lambda e: e.tensor_tensor(out=t64[:], in0=l1im[:], in1=aim, op=ALU.mult), [l1im, s5p], [t64])
        V("dve", lambda e: e.tensor_tensor(out=cre[:], in0=cre[:], in1=t64[:], op=ALU.add), [cre, t64], [cre])
        V("dve", lambda e: e.tensor_tensor(out=cre[:], in0=cre[:], in1=den[:], op=ALU.mult), [cre, den], [cre])
        V("dve", lambda e: e.tensor_tensor(out=cim[:], in0=l1im[:], in1=are, op=ALU.mult), [l1im, s5p], [cim])
        V("dve", lambda e: e.tensor_tensor(out=t64[:], in0=l1re[:], in1=aim, op=ALU.mult), [l1re, s5p], [t64])
        V("dve", lambda e: e.tensor_tensor(out=cim[:], in0=cim[:], in1=t64[:], op=ALU.subtract), [cim, t64], [cim])
        V("dve", lambda e: e.tensor_tensor(out=cim[:], in0=cim[:], in1=den[:], op=ALU.mult), [cim, den], [cim])
        bbre = SB(es, [64, 64, 16])
        bbim = SB(es, [64, 64, 16])
        tb = SB(es, [64, 64, 16])
        creb = cre[:].unsqueeze(2).to_broadcast([64, 64, 16])
        cimb = cim[:].unsqueeze(2).to_broadcast([64, 64, 16])
        V("dve", lambda e: e.tensor_tensor(out=bbre[:], in0=s5b[:, 0], in1=creb, op=ALU.mult), [s5b, cre], [bbre])
        V("dve", lambda e: e.tensor_tensor(out=tb[:], in0=s5b[:, 1], in1=cimb, op=ALU.mult), [s5b, cim], [tb])
        V("dve", lambda e: e.tensor_tensor(out=bbre[:], in0=bbre[:], in1=tb[:], op=ALU.subtract), [bbre, tb], [bbre])
        V("dve", lambda e: e.tensor_tensor(out=bbim[:], in0=s5b[:, 1], in1=creb, op=ALU.mult), [s5b, cre], [bbim])
        V("dve", lambda e: e.tensor_tensor(out=tb[:], in0=s5b[:, 0], in1=cimb, op=ALU.mult), [s5b, cim], [tb])
        V("dve", lambda e: e.tensor_tensor(out=bbim[:], in0=bbim[:], in1=tb[:], op=ALU.add), [bbim, tb], [bbim])
        V("dve", lambda e: e.tensor_scalar(out=ang[:, 0:64], in0=th[:], scalar1=8.0, scalar2=None, op0=ALU.mult), [th], [ang])
        V("dve", lambda e: e.tensor_scalar(out=kf_s[:, 0:64], in0=ang[:, 0:64], scalar1=1.0 / TWO_PI, scalar2=None, op0=ALU.mult), [ang], [kf_s])
        V("dve", lambda e: e.tensor_copy(out=ki_s[:, 0:64], in_=kf_s[:, 0:64]), [kf_s], [ki_s])
        V("dve", lambda e: e.tensor_copy(out=kf_s[:, 0:64], in_=ki_s[:, 0:64]), [ki_s], [kf_s])
        V("dve", lambda e: e.scalar_tensor_tensor(out=th8[:], in0=kf_s[:, 0:64], scalar=-TWO_PI, in1=ang[:, 0:64], op0=ALU.mult, op1=ALU.add),
          [kf_s, ang], [th8])
        V("act", lambda e: e.activation(out=r8[:], in_=rho[:], func=AF.Exp, scale=8.0), [rho], [r8])
        Lre = SB(es, [64, 3, 8, 128])
        Lim = SB(es, [64, 3, 8, 128])
        RT = SB(es, [64, 2, 8, 128])
        OP = SB(es, [64, 2, 8, 128])
        OO = SB(es, [64, 2, 8, 128])
        tw = SB(es, [64, 8, 128])
        pT = [PS(es, [128, 128]) for _ in range(2)]
        pR = PS(es, [128, 4, 64])
        tt8 = SB(es, [128, 128])
        tt8b = SB(es, [128, 128])
        for d in range(2):
            for gb in range(4):
                dg0 = d * 32 + gb * 8
                for kk, kind in enumerate((d, 2 + d, 4 + d)):
                    a3 = ang[:].rearrange("p (a c) -> p a c", a=8)
                    exb = sexp[:, kind, :].unsqueeze(1).to_broadcast([64, 8, 128])
                    V("dve", lambda e, exb=exb, dg0=dg0: e.tensor_tensor(out=a3, in0=exb, in1=th[:, dg0:dg0 + 8].unsqueeze(2).to_broadcast([64, 8, 128]),
                                                                         op=ALU.mult), [sexp, th], [ang])
                    trig(ang[:], None, (ki_s[:], kf_s[:], r_s[:]), sn[:], cs[:])
                    m3 = mg[:].rearrange("p (a c) -> p a c", a=8)
                    V("dve", lambda e, exb=exb, dg0=dg0: e.tensor_tensor(out=m3, in0=exb, in1=rho[:, dg0:dg0 + 8].unsqueeze(2).to_broadcast([64, 8, 128]),
                                                                         op=ALU.mult), [sexp, rho], [mg])
                    V("act", lambda e: e.activation(out=mg[:], in_=mg[:], func=AF.Exp), [mg], [mg])
                    V("dve", lambda e, kk=kk: e.tensor_tensor(out=Lre[:, kk].rearrange("p a c -> p (a c)"), in0=mg[:], in1=cs[:], op=ALU.mult), [mg, cs], [Lre])
                    V("dve", lambda e, kk=kk: e.tensor_tensor(out=Lim[:, kk].rearrange("p a c -> p (a c)"), in0=mg[:], in1=sn[:], op=ALU.mult), [mg, sn], [Lim])
                def cmul(out_re, out_im, kind, vre, vim, neg_im):
                    lre = Lre[:, kind].rearrange("p a (j h) -> p a j h", j=8)
                    lim = Lim[:, kind].rearrange("p a (j h) -> p a j h", j=8)
                    vr = vre.unsqueeze(2).to_broadcast([64, 8, 8, 16])
                    vi = vim.unsqueeze(2).to_broadcast([64, 8, 8, 16])
                    o_re = out_re.rearrange("p a (j h) -> p a j h", j=8)
                    o_im = out_im.rearrange("p a (j h) -> p a j h", j=8)
                    t4 = tw[:].rearrange("p a (j h) -> p a j h", j=8)
                    V("dve", lambda e: e.tensor_tensor(out=o_re, in0=lre, in1=vr, op=ALU.mult), [Lre, bbre, s5c], [out_re])
                    V("dve", lambda e: e.tensor_tensor(out=t4, in0=lim, in1=vi, op=ALU.mult), [Lim, bbim, s5c], [tw])
                    V("dve", lambda e: e.tensor_tensor(out=out_re, in0=out_re, in1=tw[:], op=ALU.subtract), [out_re, tw], [out_re])
                    V("dve", lambda e: e.tensor_tensor(out=o_im, in0=lre, in1=vi, op=ALU.mult), [Lre, bbim, s5c], [out_im])
                    V("dve", lambda e: e.tensor_tensor(out=t4, in0=lim, in1=vr, op=ALU.mult), [Lim, bbre, s5c], [tw])
                    if neg_im:
                        V("dve", lambda e: e.scalar_tensor_tensor(out=out_im.rearrange("p a c -> p (a c)"), in0=out_im.rearrange("p a c -> p (a c)"), scalar=-1.0,
                                                                  in1=tw[:].rearrange("p a c -> p (a c)"), op0=ALU.mult, op1=ALU.subtract), [out_im, tw], [out_im])
                    else:
                        V("dve", lambda e: e.tensor_tensor(out=out_im, in0=out_im, in1=tw[:], op=ALU.add), [out_im, tw], [out_im])
                cmul(RT[:, 0], RT[:, 1], 0, bbre[:, dg0:dg0 + 8, :], bbim[:, dg0:dg0 + 8, :], False)
                cmul(OO[:, 0], OO[:, 1], 1, s5c[:, 0, dg0:dg0 + 8, :], s5c[:, 1, dg0:dg0 + 8, :], True)
                cmul(OP[:, 0], OP[:, 1], 2, s5c[:, 0, dg0:dg0 + 8, :], s5c[:, 1, dg0:dg0 + 8, :], True)
                for i in range(8):
                    dg = dg0 + i
                    g = gb * 8 + i
                    V("act", lambda e, dg=dg, i=i: e.copy(out=O8[:, dg], in_=OO[:, :, i, :]), [OO], [O8])
                    for c2 in range(2):
                        P.op("pe", lambda e, c2=c2, i=i: e.transpose(out=pR[:, c2, :], in_=RT[:, c2, i, :], identity=identf[0:64, 0:64]),
                             reads=[RT, identf], writes=[pR])
                    V("act", lambda e, dg=dg: e.copy(out=R8[:, dg], in_=pR[:, 0:2, :]), [pR], [R8])
                    pt = pT[i % 2]
                    mm(pt[:], [(RT[:, 0, i, :], OP[:, 0, i, :]), (RT[:, 1, i, :], OP[:, 1, i, :])], [RT, OP], [pt])
                    if d == 0:
                        V("dve", lambda e, pt=pt: e.tensor_tensor(out=tt8[:], in0=pt[:], in1=mf[:], op=ALU.mult), [pt, mf], [tt8])
                        V("dve", lambda e, g=g: e.scalar_tensor_tensor(out=tt8[:], in0=identf[:], scalar=s5d[:, g:g + 1], in1=tt8[:],
                                                                       op0=ALU.mult, op1=ALU.add), [identf, s5d, tt8], [tt8])
                        V("pool", lambda e, g=g: e.tensor_copy(out=T8[:, g, :], in_=tt8[:]), [tt8], [T8])
                    else:
                        V("dve", lambda e, pt=pt: e.tensor_tensor(out=tt8b[:], in0=pt[:], in1=mb[:], op=ALU.mult), [pt, mb], [tt8b])
                        V("pool", lambda e, g=g: e.tensor_tensor(out=T8[:, g, :], in0=T8[:, g, :], in1=tt8b[:], op=ALU.add), [tt8b, T8], [T8])
    P.barrier()
    if upto <= 2:
        if debug:
            dbg["T8"] = dout("dbg_T8", [128, 32, 128], BF16)
            dbg["O8"] = dout("dbg_O8", [64, 64, 2, 128], BF16)
            dbg["R8"] = dout("dbg_R8", [128, 64, 2, 64], BF16)
            P.dma(dbg["T8"][:, :, :], T8[:], is_output=True)
            P.dma(dbg["O8"][:, :, :, :], O8[:], is_output=True)
            P.dma(dbg["R8"][:, :, :, :], R8[:], is_output=True)
        P.emit()
        s5es.close()
        top.close()
        return nc


    with contextlib.ExitStack() as es:
        mix = SB(es, [64, 256])
        P.dma(mix[:], cst["mix"][:, :])
        Ugs = [SB(es, [128, NS * 256 + 128], BF16) for _ in range(2)]
        cosr = [SB(es, [64, 256]) for _ in range(2)]
        sinr = [SB(es, [64, 256]) for _ in range(2)]
        angr = SB(es, [64, 256])
        kir = SB(es, [64, 256], I32)
        kfr = SB(es, [64, 256])
        rr = SB(es, [64, 256])
        cc = [SB(es, [64, NS, 255]) for _ in range(2)]
        ta = SB(es, [64, NS, 256])
        tb2 = SB(es, [64, NS, 256])
        W = [SB(es, [64, NS, 256]) for _ in range(2)]
        Wc = [SB(es, [64, NS, 32]) for _ in range(2)]
        h0 = SB(es, [64, 2, 2, NS])
        t4 = SB(es, [64, NS])
        Sin = SB(es, [64, 2, 2, NS * 256], BF16)
        Ysb = SB(es, [128, NS * 256])
        Yout = SB(es, [128, 8, 8, 128])
        psS = [PS(es, [64, NS * 256]) for _ in range(2)]
        psY = PS(es, [128, NS * 256])
        pTr = PS(es, [128, 4, 128])
        psC = PS(es, [64, 2, 128])

        def rot(out_re, out_im, s_re, s_im, cs_, sn_, conj, eng2="pool"):
            V("dve", lambda e: e.tensor_tensor(out=ta_v(out_re), in0=s_re, in1=cs_, op=ALU.mult), [psS[0], psC, W[0], Wc[0], cosr[0], cosr[1]], [ta])
            V("dve", lambda e: e.tensor_tensor(out=tb_v(out_re), in0=s_im, in1=sn_, op=ALU.mult), [psS[1], psC, W[1], Wc[1], sinr[0], sinr[1]], [tb2])
            V(eng2, lambda e: e.tensor_tensor(out=out_re, in0=ta_v(out_re), in1=tb_v(out_re), op=(ALU.add if conj else ALU.subtract)),
              [ta, tb2], [cc[0], Sin])
            V("dve", lambda e: e.tensor_tensor(out=ta_v(out_re), in0=s_im, in1=cs_, op=ALU.mult), [psS[1], psC, W[1], Wc[1], cosr[0], cosr[1]], [ta])
            V("dve", lambda e: e.tensor_tensor(out=tb_v(out_re), in0=s_re, in1=sn_, op=ALU.mult), [psS[0], psC, W[0], Wc[0], sinr[0], sinr[1]], [tb2])
            V(eng2, lambda e: e.tensor_tensor(out=out_im, in0=ta_v(out_re), in1=tb_v(out_re), op=(ALU.subtract if conj else ALU.add)),
              [ta, tb2], [cc[1], Sin])

        def ta_v(like):
            return ta[:, :, 0:like.shape[2]]

        def tb_v(like):
            return tb2[:, :, 0:like.shape[2]]

        for g in range(32):
            Ug = Ugs[g % 2]
            P.dma(Ug[:], U_s[g], q="act" if g % 2 else "sp")
            for d in range(2):
                dg = d * 32 + g
                V("dve", lambda e, dg=dg: e.tensor_scalar(out=angr[:], in0=mix[:], scalar1=th8[:, dg:dg + 1], scalar2=None, op0=ALU.mult),
                  [mix, th8], [angr])
                trig(angr[:], None, (kir[:], kfr[:], rr[:]), sinr[d][:], cosr[d][:])
            for d in range(2):
                dg = d * 32 + g
                for c2 in range(2):
                    mm(psC[:, c2, :], [(R8[:, dg, c2, :], Ug[:, NS * 256:NS * 256 + 128])], [R8, Ug], [psC])
                sre = psC[:, 0, :].rearrange("p (n m) -> p n m", n=NS)
                sim = psC[:, 1, :].rearrange("p (n m) -> p n m", n=NS)
                if d == 1:
                    sre = sre[:, :, ::-1]
                    sim = sim[:, :, ::-1]
                csb = cosr[d][:, 1:33].unsqueeze(1).to_broadcast([64, NS, 32])
                snb = sinr[d][:, 1:33].unsqueeze(1).to_broadcast([64, NS, 32])
                rot(cc[0][:, :, 0:32], cc[1][:, :, 0:32], sre, sim, csb, snb, True)
                for c2 in range(2):
                    for n in range(NS):
                        V("dve", lambda e, c2=c2, n=n, dg=dg: e.tensor_tensor_scan(out=Wc[c2][:, n, :], data0=r8[:, dg:dg + 1].to_broadcast([64, 32]),
                                                                                  data1=cc[c2][:, n, 0:32], initial=0.0, op0=ALU.mult, op1=ALU.add),
                          [r8, cc[c2]], [Wc[c2]])
                c32 = cosr[d][:, 32:33]
                s32 = sinr[d][:, 32:33]
                V("dve", lambda e, s32=s32: e.tensor_scalar(out=t4[:], in0=Wc[1][:, :, 31], scalar1=s32, scalar2=None, op0=ALU.mult), [Wc[1], sinr[d]], [t4])
                V("dve", lambda e, c32=c32, d=d: e.scalar_tensor_tensor(out=h0[:, d, 0, :], in0=Wc[0][:, :, 31], scalar=c32, in1=t4[:], op0=ALU.mult, op1=ALU.subtract),
                  [Wc[0], cosr[d], t4], [h0])
                V("dve", lambda e, c32=c32: e.tensor_scalar(out=t4[:], in0=Wc[1][:, :, 31], scalar1=c32, scalar2=None, op0=ALU.mult), [Wc[1], cosr[d]], [t4])
                V("dve", lambda e, s32=s32, d=d: e.scalar_tensor_tensor(out=h0[:, d, 1, :], in0=Wc[0][:, :, 31], scalar=s32, in1=t4[:], op0=ALU.mult, op1=ALU.add),
                  [Wc[0], sinr[d], t4], [h0])
                if debug:
                    P.dma(dbg["h0"][:, d, g, :], h0[:, d, 0, :], is_output=True)
                    if g == 0 and d == 0:
                        dbg["m"] = dout("dbg_m", [64, 8, 256])
                        dm = SB(es, [64, 8, 256])
                        V("dve", lambda e: e.memset(dm[:], 0.0), [], [dm])
                        V("dve", lambda e: e.tensor_copy(out=dm[:, 0, :], in_=psC[:].rearrange("p a b -> p (a b)")), [psC], [dm])
                        V("dve", lambda e: e.tensor_copy(out=dm[:, 1, 0:128].rearrange("p (n m) -> p n m", n=NS), in_=cc[0][:, :, 0:32]), [cc[0]], [dm])
                        V("dve", lambda e: e.tensor_copy(out=dm[:, 2, 0:128].rearrange("p (n m) -> p n m", n=NS), in_=Wc[0][:]), [Wc[0]], [dm])
                        V("dve", lambda e: e.tensor_copy(out=dm[:, 3, :], in_=cosr[0][:]), [cosr[0]], [dm])
                        V("dve", lambda e: e.tensor_copy(out=dm[:, 4, :], in_=sinr[0][:]), [sinr[0]], [dm])
                        V("dve", lambda e: e.tensor_copy(out=dm[:, 5, 0:64], in_=r8[:]), [r8], [dm])
                        V("dve", lambda e: e.tensor_copy(out=dm[:, 6, 0:64], in_=th8[:]), [th8], [dm])
                        V("dve", lambda e: e.tensor_copy(out=dm[:, 7, 0:128].rearrange("p (n m) -> p n m", n=NS), in_=cc[1][:, :, 0:32]), [cc[1]], [dm])
                        P.dma(dbg["m"][:, :, :], dm[:], is_output=True)
            for d in range(2):
                dg = d * 32 + g
                for c2 in range(2):
                    for hf in range(2):
                        mm(psS[c2][:, hf * 512:(hf + 1) * 512], [(R8[:, dg, c2, :], Ug[:, hf * 512:(hf + 1) * 512])], [R8, Ug], [psS[c2]])
                sre = psS[0][:].rearrange("p (n m) -> p n m", n=NS)
                sim = psS[1][:].rearrange("p (n m) -> p n m", n=NS)
                if d == 0:
                    sre = sre[:, :, 0:255]
                    sim = sim[:, :, 0:255]
                else:
                    sre = sre[:, :, 255:0:-1]
                    sim = sim[:, :, 255:0:-1]
                csb = cosr[d][:, 1:256].unsqueeze(1).to_broadcast([64, NS, 255])
                snb = sinr[d][:, 1:256].unsqueeze(1).to_broadcast([64, NS, 255])
                rot(cc[0][:], cc[1][:], sre, sim, csb, snb, True)
                for c2 in range(2):
                    V("pool", lambda e, c2=c2, d=d: e.tensor_copy(out=W[c2][:, :, 0:1], in_=h0[:, d, c2, :].unsqueeze(2)), [h0], [W[c2]])
                    for n in range(NS):
                        V("dve", lambda e, c2=c2, n=n, dg=dg, d=d: e.tensor_tensor_scan(out=W[c2][:, n, 1:256], data0=r8[:, dg:dg + 1].to_broadcast([64, 255]),
                                                                                       data1=cc[c2][:, n, :], initial=h0[:, d, c2, n:n + 1], op0=ALU.mult, op1=ALU.add),
                          [r8, cc[c2], h0], [W[c2]])
                csb = cosr[d][:].unsqueeze(1).to_broadcast([64, NS, 256])
                snb = sinr[d][:].unsqueeze(1).to_broadcast([64, NS, 256])
                o_re = Sin[:, d, 0, :].rearrange("p (n m) -> p n m", n=NS)
                o_im = Sin[:, d, 1, :].rearrange("p (n m) -> p n m", n=NS)
                if d == 1:
                    o_re = o_re[:, :, ::-1]
                    o_im = o_im[:, :, ::-1]
                rot(o_re, o_im, W[0][:], W[1][:], csb, snb, False)
            for hf in range(2):
                cs_ = slice(hf * 512, (hf + 1) * 512)
                mm(psY[:, cs_], [(T8[:, g, :], Ug[:, cs_]), (O8[:, g, 0, :], Sin[:, 0, 0, cs_]), (O8[:, g, 1, :], Sin[:, 0, 1, cs_]),
                                 (O8[:, 32 + g, 0, :], Sin[:, 1, 0, cs_]), (O8[:, 32 + g, 1, :], Sin[:, 1, 1, cs_])],
                   [T8, Ug, O8, Sin], [psY])
            V("act", lambda e: e.copy(out=Ysb[:], in_=psY[:]), [psY], [Ysb])
            gi = g % 8
            for q4 in range(2):
                for b4 in range(4):
                    blk = q4 * 4 + b4
                    P.op("pe", lambda e, blk=blk, b4=b4: e.transpose(out=pTr[:, b4, :], in_=Ysb[:, blk * 128:(blk + 1) * 128], identity=identf[:]),
                         reads=[Ysb, identf], writes=[pTr])
                V("act", lambda e, q4=q4, gi=gi: e.copy(out=Yout[:, q4 * 4:(q4 + 1) * 4, :, gi * 16:(gi + 1) * 16],
                                                        in_=pTr[:].rearrange("p b (j h) -> p b j h", j=8)), [pTr], [Yout])
            if gi == 7:
                gb = g // 8
                for blk in range(8):
                    dst = y_s[blk * 1024:(blk + 1) * 1024, gb * 128:(gb + 1) * 128].rearrange("(m j) c -> m j c", j=8)
                    P.dma(dst, Yout[:, blk], q="sp" if blk % 2 else "act")
                    if debug:
                        P.dma(dbg["y"][blk * 1024:(blk + 1) * 1024, gb * 128:(gb + 1) * 128].rearrange("(m j) c -> m j c", j=8), Yout[:, blk], is_output=True)
    s5es.close()
    P.barrier()
    if upto <= 3:
        P.emit()
        top.close()
        return nc


    h_s = dscr("h_s", [L, 2048])
    if debug:
        dbg["filt"] = dout("dbg_filt", [L, 2048])
        dbg["z"] = dout("dbg_z", [NS * L, 512])
    hyes = contextlib.ExitStack()
    rn = SB(hyes, [128, 2, 512])
    with contextlib.ExitStack() as es:
        fT = SB(es, [33, 2048])
        w1 = SB(es, [33, 64])
        b1 = SB(es, [64, 1])
        wh = SB(es, [64, 2, 64])
        bh = SB(es, [64, 2])
        fr = SB(es, [64, 1])
        wout = SB(es, [64, 2048])
        absd = SB(es, [128, 2048])
        tneg = SB(es, [128, 16])
        onesf = SB(es, [128, 128])
        P.dma(fT[:], cst["featsT"][:, :])
        P.dma(w1[:], hf_w1[:, :])
        P.dma(b1[:], hf_b1[:, :])
        P.dma(wh[:], hf_wh.rearrange("i a b -> a i b"))
        P.dma(bh[:], hf_bh[:, :])
        P.dma(fr[:], hf_fr[:, :])
        P.dma(wout[:], hf_wout[:, :])
        P.dma(absd[:], hf_dec.partition_broadcast(128))
        P.dma(tneg[:], cst["tneg"][:, :])
        P.dma(onesf[:], cst["onesf"][:, :])
        V("act", lambda e: e.activation(out=absd[:], in_=absd[:], func=AF.Abs), [absd], [absd])
        hid = [SB(es, [64, 2048]) for _ in range(2)]
        pre = SB(es, [64, 2048])
        kiH = SB(es, [64, 2048], I32)
        kfH = SB(es, [64, 2048])
        rH = SB(es, [64, 2048])
        psA = PS(es, [128, 2048])
        psB = PS(es, [128, 2048])
        for layer in range(3):
            for nt in range(4):
                cs_ = slice(nt * 512, (nt + 1) * 512)
                if layer == 0:
                    mm(psA[0:64, cs_], [(w1[:], fT[:, cs_])], [w1, fT], [psA])
                else:
                    mm(psA[0:64, cs_], [(wh[:, layer - 1, :], hid[(layer - 1) % 2][:, cs_])], [wh, hid[(layer - 1) % 2]], [psA])
            bias = b1[:, 0:1] if layer == 0 else bh[:, layer - 1:layer]
            V("dve", lambda e, bias=bias: e.tensor_scalar(out=pre[:], in0=psA[0:64, :], scalar1=bias, scalar2=fr[:, 0:1], op0=ALU.add, op1=ALU.mult),
              [psA, b1, bh, fr], [pre])
            trig(pre[:], None, (kiH[:], kfH[:], rH[:]), hid[layer % 2][:], None)
        hidF = hid[0]
        dec = SB(es, [128, 2048])
        hraw = [SB(es, [128, 2048]) for _ in range(2)]
        hsq = SB(es, [128, 2048])
        for tc in range(16):
            hr = hraw[tc % 2]
            for nt in range(4):
                cs_ = slice(nt * 512, (nt + 1) * 512)
                mm(psA[:, cs_], [(hidF[:, tc * 128:(tc + 1) * 128], wout[:, cs_])], [hidF, wout], [psA])
            V("act", lambda e, tc=tc: e.activation(out=dec[:], in_=absd[:], func=AF.Exp, scale=tneg[:, tc:tc + 1]), [absd, tneg], [dec])
            V("dve", lambda e, hr=hr: e.tensor_tensor(out=hr[:], in0=psA[:], in1=dec[:], op=ALU.mult), [psA, dec], [hr])
            V("pool", lambda e, hr=hr: e.tensor_tensor(out=hsq[:], in0=hr[:], in1=hr[:], op=ALU.mult), [hr], [hsq])
            for nt in range(4):
                cs_ = slice(nt * 512, (nt + 1) * 512)
                P.op("pe", lambda e, cs_=cs_, tc=tc: e.matmul(psB[:, cs_], lhsT=onesf[:], rhs=hsq[:, cs_], start=(tc == 0), stop=(tc == 15)),
                     reads=[onesf, hsq], writes=[psB])
            P.dma(h_s[tc * 128:(tc + 1) * 128, :], hr[:], q="act")
        ssb = SB(es, [128, 2048])
        V("act", lambda e: e.copy(out=ssb[:], in_=psB[:]), [psB], [ssb])
        s4 = ssb[:].rearrange("p (o d c) -> p o d c", o=2, d=2)
        V("dve", lambda e: e.tensor_tensor(out=rn[:], in0=s4[:, :, 0, :], in1=s4[:, :, 1, :], op=ALU.add), [ssb], [rn])
        V("dve", lambda e: e.tensor_scalar(out=rn[:], in0=rn[:], scalar1=EPS, scalar2=None, op0=ALU.add), [rn], [rn])
        mh = SB(es, [128, 1024])
        V("pool", lambda e: e.memset(mh[:], -0.5), [], [mh])
        V("pool", lambda e: e.tensor_tensor(out=rn[:].rearrange("p o c -> p (o c)"), in0=rn[:].rearrange("p o c -> p (o c)"), in1=mh[:], op=ALU.pow), [rn, mh], [rn])
    P.barrier()
    Ct = SB(hyes, [128, 16, 2048], BF16)
    St = SB(hyes, [128, 16, 2048], BF16)
    P.dma(Ct[:], cst["ctab"].rearrange("(tc p) f -> p tc f", p=128))
    P.dma(St[:], cst["stab"].rearrange("(tc p) f -> p tc f", p=128), q="act")
    with contextlib.ExitStack() as es:
        hraw = [SB(es, [128, 2048]) for _ in range(2)]
        psA = PS(es, [128, 2048])
        m0 = SB(es, [128, 1])
        cphi = SB(es, [128, 16])
        sphi = SB(es, [128, 16])
        P.dma(m0[:], cst["m0"][:, :])
        P.dma(cphi[:], cst["cphi"][:, :])
        P.dma(sphi[:], cst["sphi"][:, :])
        PM = SB(es, [128, 16, 2, 512], BF16)
        kre = [SB(es, [128, 512]) for _ in range(2)]
        kim = [SB(es, [128, 512]) for _ in range(2)]
        for o in range(2):
            for tc in range(16):
                hr = hraw[tc % 2]
                P.dma(hr[:], h_s[tc * 128:(tc + 1) * 128, :])
                h4 = hr[:].rearrange("p (o d c) -> p o d c", o=2, d=2)
                V("dve", lambda e, h4=h4, o=o, hr=hr: e.tensor_tensor(out=h4[:, o], in0=h4[:, o], in1=rn[:, o:o + 1, :].to_broadcast([128, 2, 512]), op=ALU.mult),
                  [hr, rn], [hr])
                if debug:
                    P.dma(dbg["filt"][tc * 128:(tc + 1) * 128, o * 1024:(o + 1) * 1024], hr[:, o * 1024:(o + 1) * 1024], is_output=True)
                if tc == 0:
                    V("dve", lambda e, h4=h4, o=o, hr=hr: e.tensor_scalar(out=h4[:, o, 1, :], in0=h4[:, o, 1, :], scalar1=m0[:, 0:1], scalar2=None, op0=ALU.mult),
                      [hr, m0], [hr])
                V("dve", lambda e, h4=h4, o=o, tc=tc, hr=hr: e.tensor_tensor(out=PM[:, tc, 0, :], in0=h4[:, o, 0, :], in1=h4[:, o, 1, :], op=ALU.add), [hr], [PM])
                V("pool", lambda e, h4=h4, o=o, tc=tc, hr=hr: e.tensor_tensor(out=PM[:, tc, 1, :], in0=h4[:, o, 0, :], in1=h4[:, o, 1, :], op=ALU.subtract), [hr], [PM])
            for fc in range(16):
                fs = slice(fc * 128, (fc + 1) * 128)
                mm(psA[:, 0:512], [(Ct[:, tc, fs], PM[:, tc, 0, :]) for tc in range(16)], [Ct, PM], [psA])
                mm(psA[:, 512:1024], [(St[:, tc, fs], PM[:, tc, 0, :]) for tc in range(16)], [St, PM], [psA])
                mm(psA[:, 1024:1536], [(Ct[:, tc, fs], PM[:, tc, 1, :]) for tc in range(16)], [Ct, PM], [psA])
                mm(psA[:, 1536:2048], [(St[:, tc, fs], PM[:, tc, 1, :]) for tc in range(16)], [St, PM], [psA])
                kr = kre[fc % 2]
                kq = kim[fc % 2]
                V("dve", lambda e, kr=kr, fc=fc: e.tensor_scalar(out=kr[:], in0=psA[:, 0:512], scalar1=cphi[:, fc:fc + 1], scalar2=None, op0=ALU.mult), [psA, cphi], [kr])
                V("dve", lambda e, kr=kr, fc=fc: e.scalar_tensor_tensor(out=kr[:], in0=psA[:, 512:1024], scalar=sphi[:, fc:fc + 1], in1=kr[:], op0=ALU.mult, op1=ALU.add),
                  [psA, sphi, kr], [kr])
                V("dve", lambda e, kq=kq, fc=fc: e.tensor_scalar(out=kq[:], in0=psA[:, 1536:2048], scalar1=cphi[:, fc:fc + 1], scalar2=None, op0=ALU.mult), [psA, cphi], [kq])
                V("dve", lambda e, kq=kq, fc=fc: e.scalar_tensor_tensor(out=kq[:], in0=psA[:, 1024:1536], scalar=sphi[:, fc:fc + 1], in1=kq[:], op0=ALU.mult, op1=ALU.subtract),
                  [psA, sphi, kq], [kq])
                P.dma(kspec_s[o, 0, fc], kr[:], q="act")
                P.dma(kspec_s[o, 1, fc], kq[:], q="act")
    P.barrier()
    if upto <= 4:
        P.emit()
        hyes.close()
        top.close()
        return nc


    with contextlib.ExitStack() as es:
        hyd = SB(es, [128, 2, 512])
        P.dma(hyd[:].rearrange("p o c -> p (o c)"), hyd_d.rearrange("o c -> (o c)").partition_broadcast(128))
        zin = SB(es, [128, 16, 512], BF16)
        YY = SB(es, [128, 16, 2, 512], BF16)
        kr2 = [SB(es, [128, 2, 512]) for _ in range(2)]
        gt = [SB(es, [128, 512]) for _ in range(2)]
        vst = gt
        u1 = SB(es, [128, 512])
        u2 = SB(es, [128, 512])
        u3 = SB(es, [128, 512])
        ot = [u1, u2]
        psX = [PS(es, [128, 2, 512]) for _ in range(2)]
        psO = [PS(es, [128, 512]) for _ in range(2)]
        for n in range(NS):
            for tc in range(16):
                vt = vst[tc % 2]
                r0 = n * L + tc * 128
                P.dma(vt[:], q_s[r0:r0 + 128, 0:512], q="act" if tc % 2 else "sp")
                V("act" if tc % 2 else "pool", lambda e, vt=vt, tc=tc: (e.copy if hasattr(e, "copy") else e.tensor_copy)(out=zin[:, tc, :], in_=vt[:]), [vt], [zin])
            for o in range(2):
                for fc in range(16):
                    fs = slice(fc * 128, (fc + 1) * 128)
                    px_ = psX[fc % 2]
                    kk = kr2[fc % 2]
                    P.dma(kk[:, 0, :], kspec_s[o, 0, fc], q="sp")
                    P.dma(kk[:, 1, :], kspec_s[o, 1, fc], q="act")
                    mm(px_[:, 0, :], [(Ct[:, tc, fs], zin[:, tc, :]) for tc in range(16)], [Ct, zin], [px_])
                    mm(px_[:, 1, :], [(St[:, tc, fs], zin[:, tc, :]) for tc in range(16)], [St, zin], [px_])
                    V("dve", lambda e, px_=px_, kk=kk: e.tensor_tensor(out=u1[:], in0=px_[:, 0, :], in1=kk[:, 0, :], op=ALU.mult), [px_, kk], [u1])
                    V("dve", lambda e, px_=px_, kk=kk: e.tensor_tensor(out=u2[:], in0=px_[:, 1, :], in1=kk[:, 1, :], op=ALU.mult), [px_, kk], [u2])
                    V("pool", lambda e, fc=fc: e.tensor_tensor(out=YY[:, fc, 0, :], in0=u1[:], in1=u2[:], op=ALU.add), [u1, u2], [YY])
                    V("dve", lambda e, px_=px_, kk=kk: e.tensor_tensor(out=u1[:], in0=px_[:, 1, :], in1=kk[:, 0, :], op=ALU.mult), [px_, kk], [u1])
                    V("dve", lambda e, px_=px_, kk=kk: e.tensor_tensor(out=u2[:], in0=px_[:, 0, :], in1=kk[:, 1, :], op=ALU.mult), [px_, kk], [u2])
                    V("pool", lambda e, fc=fc: e.tensor_tensor(out=YY[:, fc, 1, :], in0=u1[:], in1=u2[:], op=ALU.subtract), [u1, u2], [YY])
                for tc in range(16):
                    ts_ = slice(tc * 128, (tc + 1) * 128)
                    po = psO[tc % 2]
                    g_ = gt[tc % 2]
                    r0 = n * L + tc * 128
                    P.dma(g_[:], q_s[r0:r0 + 128, 512 * (o + 1):512 * (o + 2)], q="sp")
                    pairs = []
                    for fc in range(16):
                        pairs.append((Ct[:, fc, ts_], YY[:, fc, 0, :]))
                        pairs.append((St[:, fc, ts_], YY[:, fc, 1, :]))
                    mm(po[:], pairs, [Ct, St, YY], [po])
                    V("pool", lambda e, tc=tc, o=o: e.tensor_tensor(out=u3[:], in0=zin[:, tc, :], in1=hyd[:, o, :], op=ALU.mult), [zin, hyd], [u3])
                    V("dve", lambda e, po=po: e.scalar_tensor_tensor(out=u3[:], in0=po[:], scalar=2.0 / NFFT, in1=u3[:], op0=ALU.mult, op1=ALU.add), [po, u3], [u3])
                    if o == 0:
                        V("dve", lambda e, tc=tc, g_=g_: e.tensor_tensor(out=zin[:, tc, :], in0=u3[:], in1=g_[:], op=ALU.mult), [u3, g_], [zin])
                    else:
                        o_ = ot[tc % 2]
                        V("dve", lambda e, o_=o_, g_=g_: e.tensor_tensor(out=o_[:], in0=u3[:], in1=g_[:], op=ALU.mult), [u3, g_], [o_])
                        P.dma(z_s[r0:r0 + 128, :], o_[:], q="act")
                        if debug:
                            P.dma(dbg["z"][r0:r0 + 128, :], o_[:], q="act", is_output=True)
    hyes.close()
    P.barrier()
    if upto <= 5:
        P.emit()
        top.close()
        return nc


    NTG = 2
    TSG = NTG * 128
    x1_s = dscr("x1_s", [NS * L, D])
    hx2T_s = dscr("hx2T_s", [NS * L // TS, 128, 8, TS], BF16)
    sc_s = dscr("sc_s", [NS * L // 128, 128, 16 * 128])
    thr_s = dscr("thr_s", [NS * L // 128, 128, 16])
    with contextlib.ExitStack() as es:
        bglu = SB(es, [128, 512])
        gs5 = SB(es, [128, 512])
        ghy = SB(es, [128, 512])
        g2 = SB(es, [128, D])
        gx1 = SB(es, [128, D])
        g2p = SB(es, [128, D])
        sh2 = SB(es, [128, D])
        P.dma(bglu[:], b_glu_d.partition_broadcast(128))
        P.dma(gs5[:], gs5_d.partition_broadcast(128))
        P.dma(ghy[:], ghy_d.partition_broadcast(128))
        P.dma(g2[:], g2_d.partition_broadcast(128))
        hx2T = SB(es, [128, 8, TSG], BF16)
        x1t = [SB(es, [128, D]) for _ in range(NTG)]
        scs = [SB(es, [128, 16, 128]) for _ in range(NTG)]
        c16s = [SB(es, [128, 8, 16]) for _ in range(NTG)]
        thrs = [SB(es, [128, 16]) for _ in range(NTG)]
        nbs = [t_[:, 0:8] for t_ in thrs]
        ntaus = [t_[:, 8:16] for t_ in thrs]
        woutb = SB(es, [128, 8, 1024], BF16)
        wglub = SB(es, [128, 4, 512], BF16)
        wqb = SB(es, [128, 8, 2048], BF16)
        kTb = SB(es, [128, 16, 128], BF16)
        P.dma(woutb[:], wout_b[:, :, :], q="sp")
        P.dma(wglub[:], wglu_b[:, :, :], q="act")
        P.dma(wqb[:], wq_b[:, :, :], q="sp")
        P.dma(kTb[:], kT_b[:, :, :], q="act")
        qT = SB(es, [128, 16, TSG], BF16)
        yts = [SB(es, [128, 512]) for _ in range(NTG)]
        zts = [SB(es, [128, 512]) for _ in range(NTG)]
        xt4s = [SB(es, [128, D]) for _ in range(NTG)]
        gys = [SB(es, [128, 512]) for _ in range(NTG)]
        gybs = [SB(es, [128, 512], BF16) for _ in range(NTG)]
        gyTs = [SB(es, [128, 4, 128], BF16) for _ in range(NTG)]
        gpres = [SB(es, [128, 512]) for _ in range(NTG)]
        s5os = [SB(es, [128, 512]) for _ in range(NTG)]
        catbs = [SB(es, [128, D], BF16) for _ in range(NTG)]
        catTs = [SB(es, [128, 8, 128], BF16) for _ in range(NTG)]
        tms = [SB(es, [128, D]) for _ in range(NTG)]
        hb4s = [SB(es, [128, D], BF16) for _ in range(NTG)]
        jks = [SB(es, [128, D], BF16) for _ in range(NTG)]
        sss = [[SB(es, [128, 1]) for _ in range(6)] for _ in range(NTG)]
        wks = [SB(es, [128, 256]) for _ in range(NTG)]
        m16s = [SB(es, [128, 16, 16]) for _ in range(NTG)]
        cands = [SB(es, [128, 8, 256]) for _ in range(NTG)]
        ews = [SB(es, [128, 8, 16]) for _ in range(NTG)]
        zss = [SB(es, [128, 8]) for _ in range(NTG)]
        ptrs = [PS(es, [128, 8, 128], BF16) for _ in range(2)]
        pAB = [PS(es, [128, 512]) for _ in range(2)]
        pMs = [PS(es, [128, D]) for _ in range(2)]
        for g_i in range(NS * L // TSG):
            n = (g_i * TSG) // L
            row0 = g_i * TSG
            if (g_i * TSG) % L == 0:
                P.dma(gx1[:], modx_d[n, :, 2 * D:3 * D])
                P.dma(sh2[:], modx_d[n, :, 3 * D:4 * D])
                P.dma(g2p[:], modx_d[n, :, 4 * D:5 * D])
                V("dve", lambda e: e.scalar_tensor_tensor(out=g2p[:], in0=g2p[:], scalar=1.0, in1=g2[:], op0=ALU.add, op1=ALU.mult), [g2p, g2], [g2p])
            TI = list(range(NTG))

            def ctx(ti):
                return dict(r0=row0 + ti * 128, x1=x1t[ti], yt=yts[ti], zt=zts[ti], xt4=xt4s[ti], gy=gys[ti], gyb=gybs[ti], gyT=gyTs[ti], gpre=gpres[ti],
                            s5o=s5os[ti], catb=catbs[ti], catT=catTs[ti], tm=tms[ti], hb4=hb4s[ti], jk=jks[ti], ss=sss[ti], ptr=ptrs[ti % 2], pG=pAB[ti % 2], pM=pMs[ti % 2])

            for ti in TI:
                c_ = ctx(ti); r0, yt, zt, xt4, gy, gyb = c_["r0"], c_["yt"], c_["zt"], c_["xt4"], c_["gy"], c_["gyb"]
                P.dma(yt[:], y_s[r0:r0 + 128, :], q="sp")
                P.dma(zt[:], z_s[r0:r0 + 128, :], q="act")
                P.dma(xt4[:], x_d[r0:r0 + 128, :], q="sp")
                V("act", lambda e: e.activation(out=gy[:], in_=yt[:], func=AF.Gelu_apprx_tanh), [yt], [gy])
                V("pool", lambda e: e.tensor_copy(out=gyb[:], in_=gy[:]), [gy], [gyb])
            for ti in TI:
                c_ = ctx(ti); gyb, ptr, gyT, pG = c_["gyb"], c_["ptr"], c_["gyT"], c_["pG"]
                for k in range(4):
                    P.op("pe", lambda e, k=k: e.transpose(out=ptr[:, k, :], in_=gyb[:, k * 128:(k + 1) * 128], identity=ident[:]), reads=[gyb, ident], writes=[ptr])
                V("act", lambda e: e.copy(out=gyT[:], in_=ptr[:, 0:4, :]), [ptr], [gyT])
            for ti in TI:
                c_ = ctx(ti); gyT, pG = c_["gyT"], c_["pG"]
                mm(pG[:], [(gyT[:, k, :], wglub[:, k, :]) for k in range(4)], [gyT, wglub], [pG])
            for ti in TI:
                c_ = ctx(ti); pG, gpre = c_["pG"], c_["gpre"]
                V("dve", lambda e: e.tensor_tensor(out=gpre[:], in0=pG[:], in1=bglu[:], op=ALU.add), [pG, bglu], [gpre])
                V("act", lambda e: e.activation(out=gpre[:], in_=gpre[:], func=AF.Sigmoid), [gpre], [gpre])
            for ti in TI:
                c_ = ctx(ti); gy, gpre, s5o, jk, zt = c_["gy"], c_["gpre"], c_["s5o"], c_["jk"], c_["zt"]
                ss1, ss2, ss3, rs1, rs2, rs3 = c_["ss"]
                V("dve", lambda e: e.tensor_tensor(out=s5o[:], in0=gy[:], in1=gpre[:], op=ALU.mult), [gy, gpre], [s5o])
                V("act", lambda e: e.activation(out=jk[:, 0:512], in_=s5o[:], func=AF.Square, accum_out=ss1[:]), [s5o], [jk, ss1])
                V("act", lambda e: e.activation(out=jk[:, 512:1024], in_=zt[:], func=AF.Square, accum_out=ss2[:]), [zt], [jk, ss2])
            for ti in TI:
                c_ = ctx(ti)
                ss1, ss2, ss3, rs1, rs2, rs3 = c_["ss"]
                rsqrt_mean(es, ss1, 512, rs1)
                rsqrt_mean(es, ss2, 512, rs2)
            for ti in TI:
                c_ = ctx(ti); s5o, zt, catb = c_["s5o"], c_["zt"], c_["catb"]
                ss1, ss2, ss3, rs1, rs2, rs3 = c_["ss"]
                V("dve", lambda e: e.scalar_tensor_tensor(out=catb[:, 0:512], in0=s5o[:], scalar=rs1[:, 0:1], in1=gs5[:], op0=ALU.mult, op1=ALU.mult),
                  [s5o, rs1, gs5], [catb])
                V("dve", lambda e: e.scalar_tensor_tensor(out=catb[:, 512:1024], in0=zt[:], scalar=rs2[:, 0:1], in1=ghy[:], op0=ALU.mult, op1=ALU.mult),
                  [zt, rs2, ghy], [catb])
            for ti in TI:
                c_ = ctx(ti); catb, ptr, catT = c_["catb"], c_["ptr"], c_["catT"]
                for k in range(8):
                    P.op("pe", lambda e, k=k: e.transpose(out=ptr[:, k, :], in_=catb[:, k * 128:(k + 1) * 128], identity=ident[:]), reads=[catb, ident], writes=[ptr])
                V("act", lambda e: e.copy(out=catT[:], in_=ptr[:]), [ptr], [catT])
            for ti in TI:
                c_ = ctx(ti); catT, pM = c_["catT"], c_["pM"]
                for hf in range(2):
                    mm(pM[:, hf * 512:(hf + 1) * 512], [(catT[:, k, :], woutb[:, k, hf * 512:(hf + 1) * 512]) for k in range(8)], [catT, woutb], [pM])
            for ti in TI:
                c_ = ctx(ti); pM, tm, x1, xt4, jk, r0 = c_["pM"], c_["tm"], c_["x1"], c_["xt4"], c_["jk"], c_["r0"]
                ss1, ss2, ss3, rs1, rs2, rs3 = c_["ss"]
                V("dve", lambda e: e.tensor_tensor(out=tm[:], in0=pM[:], in1=gx1[:], op=ALU.mult), [pM, gx1], [tm])
                V("pool", lambda e: e.tensor_tensor(out=x1[:], in0=tm[:], in1=xt4[:], op=ALU.add), [tm, xt4], [x1])
                if debug:
                    P.dma(dbg["x1"][r0:r0 + 128, :], x1[:], is_output=True)
                V("act", lambda e: e.activation(out=jk[:], in_=x1[:], func=AF.Square, accum_out=ss3[:]), [x1], [jk, ss3])
            for ti in TI:
                c_ = ctx(ti)
                ss1, ss2, ss3, rs1, rs2, rs3 = c_["ss"]
                rsqrt_mean(es, ss3, D, rs3)
            for ti in TI:
                c_ = ctx(ti); tm, x1, hb4, r0 = c_["tm"], c_["x1"], c_["hb4"], c_["r0"]
                ss1, ss2, ss3, rs1, rs2, rs3 = c_["ss"]
                V("dve", lambda e: e.scalar_tensor_tensor(out=tm[:], in0=x1[:], scalar=rs3[:, 0:1], in1=g2p[:], op0=ALU.mult, op1=ALU.mult),
                  [x1, rs3, g2p], [tm])
                V("pool", lambda e: e.tensor_tensor(out=hb4[:], in0=tm[:], in1=sh2[:], op=ALU.add), [tm, sh2], [hb4])
                if debug:
                    P.dma(dbg["hx2"][r0:r0 + 128, :], hb4[:], is_output=True)
            for ti in TI:
                c_ = ctx(ti); hb4, ptr = c_["hb4"], c_["ptr"]
                for k in range(8):
                    P.op("pe", lambda e, k=k: e.transpose(out=ptr[:, k, :], in_=hb4[:, k * 128:(k + 1) * 128], identity=ident[:]), reads=[hb4, ident], writes=[ptr])
                V("act", lambda e: e.copy(out=hx2T[:, :, ti * 128:(ti + 1) * 128], in_=ptr[:]), [ptr], [hx2T])
            for c in range(16):
                pp = pAB[c % 2]
                mm(pp[:, 0:TSG], [(wqb[:, k, c * 128:(c + 1) * 128], hx2T[:, k, :]) for k in range(8)], [wqb, hx2T], [pp])
                if c % 2:
                    V("act", lambda e: e.copy(out=qT[:, c, :], in_=pp[:, 0:TSG]), [pp], [qT])
                else:
                    V("dve", lambda e: e.tensor_copy(out=qT[:, c, :], in_=pp[:, 0:TSG]), [pp], [qT])
            if not lite:
                for hh in range(4):
                    for ti in TI:
                        sc = scs[ti]
                        ps_ = pAB[ti % 2]
                        for c4 in range(4):
                            c = hh * 4 + c4
                            mm(ps_[:, c4 * 128:(c4 + 1) * 128], [(qT[:, c, ti * 128:(ti + 1) * 128], kTb[:, c, :])], [qT, kTb], [ps_])
                        V("act", lambda e: e.copy(out=sc[:, hh * 4:(hh + 1) * 4, :], in_=ps_[:].rearrange("p (a b) -> p a b", a=4)), [ps_], [sc])
                for c in range(16):
                    for ti in TI:
                        sc, m16 = scs[ti], m16s[ti]
                        V("dve", lambda e: e.max(out=m16[:, c, 0:8], in_=sc[:, c, :]), [sc], [m16])
                    for ti in TI:
                        sc, m16, wk = scs[ti], m16s[ti], wks[ti]
                        V("dve", lambda e: e.match_replace(out=wk[:, 0:128], in_to_replace=m16[:, c, 0:8], in_values=sc[:, c, :], imm_value=-1e30), [sc, m16], [wk])
                    for ti in TI:
                        m16, wk = m16s[ti], wks[ti]
                        V("dve", lambda e: e.max(out=m16[:, c, 8:16], in_=wk[:, 0:128]), [wk], [m16])
                for ti in TI:
                    m16, cand = m16s[ti], cands[ti]
                    m4 = m16[:].rearrange("p (h two) k -> p h two k", two=2)
                    V("dve", lambda e: e.tensor_tensor(out=cand[:].rearrange("p h (a b) -> p h a b", a=16),
                                                       in0=m4[:, :, 0, :].unsqueeze(3).to_broadcast([128, 8, 16, 16]),
                                                       in1=m4[:, :, 1, :].unsqueeze(2).to_broadcast([128, 8, 16, 16]), op=ALU.add), [m16], [cand])
                for h in range(8):
                    for ti in TI:
                        cand, c16 = cands[ti], c16s[ti]
                        V("dve", lambda e: e.max(out=c16[:, h, 0:8], in_=cand[:, h, :]), [cand], [c16])
                    for ti in TI:
                        cand, c16, wk = cands[ti], c16s[ti], wks[ti]
                        V("dve", lambda e: e.match_replace(out=wk[:], in_to_replace=c16[:, h, 0:8], in_values=cand[:, h, :], imm_value=-1e30), [cand, c16], [wk])
                    for ti in TI:
                        c16, wk = c16s[ti], wks[ti]
                        V("dve", lambda e: e.max(out=c16[:, h, 8:16], in_=wk[:]), [wk], [c16])
                for ti in TI:
                    c16, ew, zs, nb = c16s[ti], ews[ti], zss[ti], nbs[ti]
                    V("dve", lambda e: e.tensor_tensor(out=ew[:], in0=c16[:], in1=c16[:, :, 0:1].to_broadcast([128, 8, 16]), op=ALU.subtract), [c16], [ew])
                    V("act", lambda e: e.activation(out=ew[:], in_=ew[:], func=AF.Exp), [ew], [ew])
                for ti in TI:
                    c16, ew, zs, nb = c16s[ti], ews[ti], zss[ti], nbs[ti]
                    V("dve", lambda e: e.tensor_reduce(out=zs[:], in_=ew[:], axis=AX.X, op=ALU.add), [ew], [zs])
                    V("act", lambda e: e.activation(out=zs[:], in_=zs[:], func=AF.Ln), [zs], [zs])
                for ti in TI:
                    c16, ew, zs, nb = c16s[ti], ews[ti], zss[ti], nbs[ti]
                    V("dve", lambda e: e.tensor_tensor(out=nb[:], in0=zs[:], in1=c16[:, :, 0], op=ALU.add), [zs, c16], [nb])
                    ntau_ = ntaus[ti]
                    V("dve", lambda e: e.tensor_scalar(out=ntau_[:], in0=c16[:, :, 15], scalar1=-1e-5, scalar2=None, op0=ALU.add), [c16], [ntau_])
                    V("dve", lambda e: e.tensor_tensor(out=nb[:], in0=ntau_[:], in1=nb[:], op=ALU.subtract), [nb, ntau_], [nb])
                    sc = scs[ti]
                    for h in range(8):
                        V("dve", lambda e: e.tensor_scalar(out=sc[:, 2 * h, :], in0=sc[:, 2 * h, :], scalar1=ntau_[:, h:h + 1], scalar2=None, op0=ALU.subtract),
                          [sc, ntau_], [sc])
            for ti in TI:
                r0 = row0 + ti * 128
                P.dma(x1_s[r0:r0 + 128, :], x1t[ti][:], q="sp")
                if not lite:
                    P.dma(sc_s[r0 // 128], scs[ti][:].rearrange("p a b -> p (a b)"), q="act")
                    P.dma(thr_s[r0 // 128], thrs[ti][:], q="sp")
            for hf2 in range(TSG // TS):
                P.dma(hx2T_s[row0 // TS + hf2], hx2T[:, :, hf2 * TS:(hf2 + 1) * TS], q="act")
    P.barrier()
    if not lite:
      with contextlib.ExitStack() as es:
        gx2 = SB(es, [128, D])
        gfin = SB(es, [128, D])
        P.dma(gfin[:], gf_d.partition_broadcast(128))
        hx2Tb = [SB(es, [128, 8, TS], BF16) for _ in range(2)]
        scsb = [[SB(es, [128, 16, 128]) for _ in range(NT)] for _ in range(2)]
        thrb = [[SB(es, [128, 16]) for _ in range(NT)] for _ in range(2)]
        x1t = [SB(es, [128, D]) for _ in range(NT)]
        NB_G = 4
        Sb = [SB(es, [128, 16, 128]) for _ in range(NB_G)]
        Eb = [SB(es, [128, 16, 128], BF16) for _ in range(NB_G)]
        Mb = [SB(es, [128, 16, 128], BF16) for _ in range(NB_G)]
        Gh = Eb
        Gc = [[SB(es, [128, 2048], BF16) for _ in range(2)] for _ in range(NT)]
        uTc = [SB(es, [128, 8, 512], BF16) for _ in range(2)]
        vc = [SB(es, [128, 4, 1024], BF16) for _ in range(3)]
        actb = [SB(es, [128, 512], BF16) for _ in range(2)]
        wab = [SB(es, [128, 512], BF16) for _ in range(2)]
        waT = [SB(es, [128, 4, 128], BF16) for _ in range(2)]
        tm2 = SB(es, [128, D])
        x2 = SB(es, [128, D])
        jk2 = SB(es, [128, D], BF16)
        ss4 = SB(es, [128, 1])
        rs4 = SB(es, [128, 1])
        ob = [SB(es, [128, D]) for _ in range(1)]
        pO = [PS(es, [128, D]) for _ in range(NT)]
        pA = [PS(es, [128, 512]) for _ in range(2)]
        pW = [PS(es, [128, 4, 128], BF16) for _ in range(2)]
        cnt = {"g": 0, "d": 0}
        for st_i in range(NS * L // TS):
            n = (st_i * TS) // L
            row0 = st_i * TS
            par = st_i % 2
            if (st_i * TS) % L == 0:
                P.dma(gx2[:], modx_d[n, :, 5 * D:6 * D])
            def srcs_of(p_):
                return (scsb[p_], [t_[:, 0:8] for t_ in thrb[p_]], [t_[:, 8:16] for t_ in thrb[p_]])

            def preload(si):
                p_ = si % 2
                P.dma(hx2Tb[p_][:], hx2T_s[si], q="sp")
                for ti in range(NT):
                    r0_ = si * TS + ti * 128
                    P.dma(scsb[p_][ti][:].rearrange("p a b -> p (a b)"), sc_s[r0_ // 128], q="act" if ti else "sp")
                    P.dma(thrb[p_][ti][:], thr_s[r0_ // 128], q="sp")

            if st_i == 0:
                preload(0)
            if st_i + 1 < NS * L // TS:
                preload(st_i + 1)
            hx2T = hx2Tb[par]
            for ti in range(NT):
                r0 = row0 + ti * 128
                P.dma(x1t[ti][:], x1_s[r0:r0 + 128, :], q="act")
            cnt_unused = None

            def g_pair(ic, ulist, srcs):
                scs_, nbs_, ntaus_ = srcs
                st = []
                for (ti, h) in ulist:
                    i3 = cnt["g"] % NB_G
                    cnt["g"] += 1
                    st.append((ti, h, Sb[i3], Eb[i3], Mb[i3], Gh[i3]))
                for (ti, h, S_, E_, M_, G_) in st:
                    sc = scs_[ti]
                    V("pool", lambda e: e.tensor_tensor(out=S_[:], in0=sc[:, 2 * h, ic * 16:(ic + 1) * 16].unsqueeze(2).to_broadcast([128, 16, 128]),
                                                        in1=sc[:, 2 * h + 1, :].unsqueeze(1).to_broadcast([128, 16, 128]), op=ALU.add), [sc], [S_])
                for (ti, h, S_, E_, M_, G_) in st:
                    V("act", lambda e: e.activation(out=S_[:], in_=S_[:], func=AF.Prelu, alpha=KMASK), [S_], [S_])
                for (ti, h, S_, E_, M_, G_) in st:
                    nb = nbs_[ti]
                    gdst = Gc[ti][ic % 2]
                    if h == 0:
                        V("act", lambda e: e.activation(out=gdst[:], in_=S_[:].rearrange("p a b -> p (a b)"), func=AF.Exp, bias=nb[:, h:h + 1]), [S_, nb], [gdst])
                    else:
                        V("act", lambda e: e.activation(out=E_[:], in_=S_[:], func=AF.Exp, bias=nb[:, h:h + 1]), [S_, nb], [E_])
                for (ti, h, S_, E_, M_, G_) in st:
                    gdst = Gc[ti][ic % 2]
                    if h != 0:
                        V("dve", lambda e: e.tensor_tensor(out=gdst[:], in0=gdst[:], in1=E_[:].rearrange("p a b -> p (a b)"), op=ALU.add), [E_, gdst], [gdst])

            def item_ctx(j):
                ic, r = divmod(j, 2 * 4)
                e4, ti = divmod(r, NT)
                ec = ic * 4 + e4
                return dict(ic=ic, e4=e4, ti=ti, ec=ec, u_=uTc[ec % 2], v_=vc[ec % 3], pa=pA[j % 2], a_=actb[j % 2], w_=wab[j % 2], wT=waT[j % 2], pw=pW[j % 2],
                            gsrc=Gc[ti][ic % 2])

            def D1(j):
                c_ = item_ctx(j); ec, ti, u_, v_, pa = c_["ec"], c_["ti"], c_["u_"], c_["v_"], c_["pa"]
                if ti == 0:
                    P.dma(u_[:], uTb_s[ec], q="sp")
                    P.dma(v_[:], vb_s[ec], q="act")
                mm(pa[:], [(hx2T[:, k, ti * 128:(ti + 1) * 128], u_[:, k, :]) for k in range(8)], [hx2T, u_], [pa])

            def D2(j):
                c_ = item_ctx(j); pa, a_ = c_["pa"], c_["a_"]
                V("act", lambda e: e.activation(out=a_[:], in_=pa[:], func=AF.Gelu_apprx_tanh), [pa], [a_])

            def D3(j):
                c_ = item_ctx(j); a_, w_, gsrc, e4 = c_["a_"], c_["w_"], c_["gsrc"], c_["e4"]
                V("dve", lambda e: e.tensor_tensor(out=w_[:], in0=a_[:], in1=gsrc[:, e4 * 512:(e4 + 1) * 512], op=ALU.mult), [a_, gsrc], [w_])

            def D4(j):
                c_ = item_ctx(j); w_, pw = c_["w_"], c_["pw"]
                for eb in range(4):
                    P.op("pe", lambda e, eb=eb: e.transpose(out=pw[:, eb, :], in_=w_[:, eb * 128:(eb + 1) * 128], identity=ident[:]), reads=[w_, ident], writes=[pw])

            def D5(j):
                c_ = item_ctx(j); pw, wT = c_["pw"], c_["wT"]
                if j % 2 == 1:
                    V("dve", lambda e: e.tensor_copy(out=wT[:], in_=pw[:]), [pw], [wT])
                else:
                    V("act", lambda e: e.copy(out=wT[:], in_=pw[:]), [pw], [wT])

            def D6(j):
                c_ = item_ctx(j); ec, ti, wT, v_ = c_["ec"], c_["ti"], c_["wT"], c_["v_"]
                for hf in range(2):
                    for eb in range(4):
                        first = (ec == 0 and eb == 0)
                        last = (ec == NCH - 1 and eb == 3)
                        P.op("pe", lambda e, hf=hf, eb=eb, first=first, last=last:
                             e.matmul(pO[ti][:, hf * 512:(hf + 1) * 512], lhsT=wT[:, eb, :], rhs=v_[:, eb, hf * 512:(hf + 1) * 512], start=first, stop=last),
                             reads=[wT, v_], writes=[pO[ti]])

            units = [(ti, h) for h in range(8) for ti in range(NT)]
            NJ = 64
            upi = 2
            if st_i == 0:
                for j in range(8):
                    g_pair(0, units[j * upi:(j + 1) * upi], srcs_of(0))
            def ok(j):
                return 0 <= j < NJ

            for s in range(NJ + 3):
                if ok(s - 2):
                    D4(s - 2)
                if s % 2 == 0 and ok(s - 1):
                    D2(s - 1)
                    D3(s - 1)
                if s < NJ:
                    ic, r = divmod(s, 8)
                    if ic + 1 < 8:
                        g_pair(ic + 1, units[r * upi:(r + 1) * upi], srcs_of(par))
                    elif st_i + 1 < NS * L // TS:
                        g_pair(0, units[r * upi:(r + 1) * upi], srcs_of(1 - par))
                if s % 2 == 1 and ok(s - 1):
                    D2(s - 1)
                    D3(s - 1)
                if ok(s - 2):
                    D5(s - 2)
                if ok(s - 3):
                    D6(s - 3)
                if s < NJ:
                    D1(s)
            for ti in range(NT):
                r0 = row0 + ti * 128
                x1 = x1t[ti]
                o_ = ob[0]
                if debug:
                    V("act", lambda e: e.copy(out=tm2[:], in_=pO[ti][:]), [pO[ti]], [tm2])
                    P.dma(dbg["pe"][r0:r0 + 128, :], tm2[:], is_output=True)
                V("dve", lambda e: e.tensor_tensor(out=tm2[:], in0=pO[ti][:], in1=gx2[:], op=ALU.mult), [pO[ti], gx2], [tm2])
                V("pool", lambda e: e.tensor_tensor(out=x2[:], in0=tm2[:], in1=x1[:], op=ALU.add), [tm2, x1], [x2])
                V("act", lambda e: e.activation(out=jk2[:], in_=x2[:], func=AF.Square, accum_out=ss4[:]), [x2], [jk2, ss4])
                rsqrt_mean(es, ss4, D, rs4)
                V("dve", lambda e: e.scalar_tensor_tensor(out=o_[:], in0=x2[:], scalar=rs4[:, 0:1], in1=gfin[:], op0=ALU.mult, op1=ALU.mult),
                  [x2, rs4, gfin], [o_])
                P.dma(out_d[r0:r0 + 128, :], o_[:], q="sp", is_output=True)

    P.emit()
    top.close()
    return nc


def _prep_inputs(inp):
    global CONST
    if CONST is None:
        CONST = _constants()
    f = lambda a: np.ascontiguousarray(np.asarray(a, dtype=np.float32))
    shared = {}
    shared["w_ada"] = f(inp["w_ada"][0])
    shared["b_ada"] = f(inp["b_ada"][0])
    shared["g_norm1"] = f(inp["g_norm1"][0])
    shared["g_norm2"] = f(inp["g_norm2"][0])
    shared["w_in"] = f(inp["w_in"][0])
    shared["b_in"] = f(inp["b_in"][0])
    a_re = np.asarray(inp["s5_a_re"][0]).reshape(64, 64).T
    a_im = np.asarray(inp["s5_a_im"][0]).reshape(64, 64).T
    ls = np.broadcast_to(np.asarray(inp["s5_log_step"][0]).reshape(1, 64), (64, 64))
    shared["s5p"] = f(np.stack([a_re, a_im, ls], axis=1))
    bre = np.asarray(inp["s5_b_re"][0]).reshape(64, 64, 16).transpose(1, 0, 2)
    bim = np.asarray(inp["s5_b_im"][0]).reshape(64, 64, 16).transpose(1, 0, 2)
    shared["s5b"] = f(np.stack([bre, bim], axis=1))
    cre = np.asarray(inp["s5_c_re"][0]).reshape(64, 16, 64).transpose(2, 0, 1)
    cim = np.asarray(inp["s5_c_im"][0]).reshape(64, 16, 64).transpose(2, 0, 1)
    shared["s5c"] = f(np.stack([cre, cim], axis=1))
    d5 = np.asarray(inp["s5_d"][0]).reshape(32, 16)
    shared["s5d"] = f(np.broadcast_to(d5.T[None], (8, 16, 32)).reshape(128, 32))
    shared["w_glu"] = f(inp["w_glu"][0])
    shared["b_glu"] = f(inp["b_glu"][0])
    shared["hy_conv_w"] = f(inp["hy_conv_w"][0])
    shared["hy_conv_b"] = f(inp["hy_conv_b"][0])
    shared["hf_w1"] = f(inp["hf_w1"][0])
    shared["hf_b1"] = f(np.asarray(inp["hf_b1"][0]).reshape(64, 1))
    shared["hf_wh"] = f(inp["hf_wh"][0])
    shared["hf_bh"] = f(np.asarray(inp["hf_bh"][0]).T)
    shared["hf_freq"] = f(np.asarray(inp["hf_freq"][0]).reshape(64, 1))
    shared["hf_wout"] = f(inp["hf_wout"][0])
    shared["hf_decay"] = f(inp["hf_decay"][0])
    shared["hy_d"] = f(inp["hy_d"][0])
    shared["g_out_s5"] = f(inp["g_out_s5"][0])
    shared["g_out_hy"] = f(inp["g_out_hy"][0])
    shared["w_out"] = f(inp["w_out"][0])
    shared["peer_wq"] = f(inp["peer_wq"][0])
    k1 = np.asarray(inp["peer_k1"][0])
    k2 = np.asarray(inp["peer_k2"][0])
    kk = np.stack([k1, k2], axis=1).reshape(16, 128, 128)
    shared["peer_kT"] = f(kk.transpose(2, 0, 1))
    shared["peer_uT"] = f(np.asarray(inp["peer_u"][0]).T)
    shared["peer_v"] = f(inp["peer_v"][0])
    shared["g_final"] = f(inp["g_final"])
    for k, v in CONST.items():
        shared["k_" + k] = v
    maps = []
    x = np.asarray(inp["x"])
    ctx = np.asarray(inp["ctx"])
    c = np.asarray(inp["c"])
    cc = np.asarray(inp["c_ctx"])
    for i in range(NCORES):
        m = dict(shared)
        m["x"] = f(x[i * NS:(i + 1) * NS].reshape(NS * L, D))
        m["ctx"] = f(ctx[i * NS:(i + 1) * NS].reshape(NS * LC, D))
        c5 = np.concatenate([c[i * NS:(i + 1) * NS], cc[None]], axis=0)
        m["cT"] = f(c5.reshape(NS + 1, 8, 128).transpose(2, 1, 0))
        maps.append(m)
    return maps


def kernel(**inputs):
    maps = _prep_inputs(inputs)
    nc = build_program()
    res = run_bass_kernel_spmd(nc, maps, core_ids=list(range(NCORES)))
    out = np.concatenate([np.asarray(r["out"]).reshape(NS, L, D) for r in res.results], axis=0)
    return out.astype(np.float32)
```

```python
import contextlib
import math
import types
import numpy as np
import ml_dtypes
import concourse.bass as bass
import concourse.mybir as mybir
from concourse.bass_utils import run_bass_kernel_spmd

F32 = mybir.dt.float32
BF16 = mybir.dt.bfloat16
I32 = mybir.dt.int32
ALU = mybir.AluOpType
AF = mybir.ActivationFunctionType
AX = mybir.AxisListType

NSLOT = 12
NCORES = 8
NS = 4
L = 2048
D = 1024
LC = 256
EPS = 1e-6
NFFT = 4096


def _freeze(fn):
    if fn.__closure__ is None:
        return fn
    cells = []
    for c in fn.__closure__:
        try:
            cells.append(types.CellType(c.cell_contents))
        except ValueError:
            cells.append(c)
    return types.FunctionType(fn.__code__, fn.__globals__, fn.__name__, fn.__defaults__, tuple(cells))


class Prog:
    DMAQ = ("sp", "act", "pool")

    def __init__(self, nc):
        self.nc = nc
        self.streams = {k: [] for k in ("pe", "act", "dve", "pool", "sp")}
        self.cnt = {}
        self.waited = {k: {} for k in self.streams}
        self.lastw = {}
        self.reads = {}
        self.slot_next = {q: 0 for q in self.DMAQ}
        self.ninst = 0
        self.out_deps = []

    def _key(self, x):
        if isinstance(x, (str, tuple)):
            return x
        t = getattr(x, "tensor", x)
        return getattr(t, "name", None) or id(t)

    def _deps(self, reads, writes):
        deps = {}

        def add(s, c):
            if c > deps.get(s, 0):
                deps[s] = c
        for k in reads:
            lw = self.lastw.get(k)
            if lw:
                add(*lw)
        for k in writes:
            lw = self.lastw.get(k)
            if lw:
                add(*lw)
            for s, c in self.reads.get(k, {}).items():
                add(s, c)
        return deps

    def _emit_waits(self, stream, deps):
        w = self.waited[stream]
        for s, c in deps.items():
            if s == stream and stream == "pe":
                continue
            if c > w.get(s, 0):
                w[s] = c
                self.streams[stream].append(("wait", s, c))

    def _commit(self, sem, cnt, reads, writes):
        for k in writes:
            self.lastw[k] = (sem, cnt)
            self.reads[k] = {}
        for k in reads:
            self.reads.setdefault(k, {})
            if self.reads[k].get(sem, 0) < cnt:
                self.reads[k][sem] = cnt

    def op(self, stream, fn, reads=(), writes=()):
        reads = [self._key(r) for r in reads]
        writes = [self._key(r) for r in writes]
        deps = self._deps(reads, writes)
        self._emit_waits(stream, deps)
        c = self.cnt.get(stream, 0) + 1
        self.cnt[stream] = c
        self.streams[stream].append(("op", _freeze(fn), stream, 1))
        self._commit(stream, c, reads, writes)
        self.ninst += 1

    def dma(self, out, in_, reads=None, writes=None, q="sp", is_output=False, **kw):
        reads = [self._key(r) for r in (reads if reads is not None else [in_])]
        writes = [self._key(r) for r in (writes if writes is not None else [out])]
        slot = self.slot_next[q]
        self.slot_next[q] = (slot + 1) % NSLOT
        sem = ("dma", q, slot)
        deps = self._deps(reads, writes)
        prev = self.cnt.get(sem, 0)
        if prev:
            deps[sem] = max(deps.get(sem, 0), prev)
        self._emit_waits(q, deps)
        c = prev + 16
        self.cnt[sem] = c
        self.streams[q].append(("dma", out, in_, kw, sem))
        self._commit(sem, c, reads, writes)
        if is_output:
            self.out_deps.append((sem, c))
        self.ninst += 1

    def barrier(self):
        allc = dict(self.cnt)
        for st in self.streams:
            self._emit_waits(st, allc)

    def emit(self):
        nc = self.nc
        fin = {}
        for s, c in self.out_deps:
            fin[s] = max(fin.get(s, 0), c)
        self._emit_waits("sp", fin)
        semkeys = list(self.cnt.keys())
        with contextlib.ExitStack() as es:
            sems = {}
            for i, k in enumerate(semkeys):
                sems[k] = es.enter_context(nc.semaphore("s%d" % i))
            block = es.enter_context(nc.Block())
            engs = {"pe": block.tensor, "act": block.scalar, "dve": block.vector,
                    "pool": block.gpsimd, "sp": block.sync}

            def make(stream):
                items = self.streams[stream]

                def body(eng):
                    for it in items:
                        if it[0] == "wait":
                            eng.wait_ge(sems[it[1]], it[2])
                        elif it[0] == "op":
                            it[1](eng).then_inc(sems[it[2]], 1)
                        else:
                            _, out, in_, kw, sem = it
                            eng.dma_start(out=out, in_=in_, **kw).then_inc(sems[sem], 16)
                return body
            for st in ("sp", "act", "pool", "dve", "pe"):
                if self.streams[st]:
                    engs[st](make(st))


def _constants():
    c = {}
    c["ident"] = np.eye(128, dtype=np.float32).astype(ml_dtypes.bfloat16)
    c["identf"] = np.eye(128, dtype=np.float32)
    sm = np.zeros((128, 128), np.float32)
    sp = np.zeros((128, 128), np.float32)
    for t in range(128):
        if t % 64 != 0:
            sm[t - 1, t] = 1
        if t % 64 != 63:
            sp[t + 1, t] = 1
    c["shm"] = sm.astype(ml_dtypes.bfloat16)
    c["shp"] = sp.astype(ml_dtypes.bfloat16)
    t = np.arange(L, dtype=np.float64) + 0.5
    ang = 2 * np.pi * np.outer(t, t) / NFFT
    c["ctab"] = np.cos(ang).astype(np.float32).astype(ml_dtypes.bfloat16)
    c["stab"] = np.sin(ang).astype(np.float32).astype(ml_dtypes.bfloat16)
    phi = np.pi * (np.arange(L, dtype=np.float64) + 0.5) / NFFT
    c["cphi"] = np.cos(phi).astype(np.float32).reshape(16, 128).T.copy()
    c["sphi"] = np.sin(phi).astype(np.float32).reshape(16, 128).T.copy()
    pos = np.arange(L, dtype=np.float32)
    tt = pos / np.float32(L - 1)
    bands = np.linspace(1e-4, 15, 16, dtype=np.float32)
    a = (np.float32(2.0 * math.pi / L) * pos[:, None]) * bands[None, :]
    feats = np.concatenate([tt[:, None], np.cos(a), -np.sin(a)], axis=-1).astype(np.float32)
    c["featsT"] = feats.T.copy()
    c["tneg"] = (-tt).reshape(16, 128).T.copy()
    j = np.repeat(np.arange(8), 16).astype(np.float32)
    ex = np.stack([7 - j, j, j + 1, 8 - j, j - 7, -j])
    c["s5exp"] = np.broadcast_to(ex[None], (64, 6, 128)).astype(np.float32).copy()
    jj = np.repeat(np.arange(8), 16)
    c["mf"] = (jj[None, :] >= jj[:, None]).astype(np.float32)
    c["mb"] = (jj[:, None] >= jj[None, :]).astype(np.float32)
    m0 = np.ones((128, 1), np.float32)
    m0[0, 0] = 0.0
    c["m0"] = m0
    c["onesf"] = np.ones((128, 128), np.float32)
    c["mix"] = np.broadcast_to(np.arange(256, dtype=np.float32)[None], (64, 256)).copy()
    return c


CONST = None


def build_program(upto=99, debug=False, lite=False):
    nc = bass.Bass("TRN2", target_bir_lowering=False)
    P = Prog(nc)
    dbg = {}

    def din(name, shape, dt=F32):
        return nc.dram_tensor(name, list(shape), dt, kind="ExternalInput").ap()

    def dscr(name, shape, dt=F32):
        return nc.dram_tensor(name, list(shape), dt).ap()

    def dout(name, shape, dt=F32):
        return nc.dram_tensor(name, list(shape), dt, kind="ExternalOutput").ap()

    x_d = din("x", [NS * L, D])
    ctx_d = din("ctx", [NS * LC, D])
    cT_d = din("cT", [128, 8, NS + 1])
    w_ada = din("w_ada", [D, 6 * D])
    b_ada = din("b_ada", [6 * D])
    g1_d = din("g_norm1", [D])
    g2_d = din("g_norm2", [D])
    w_in_d = din("w_in", [D, 2048])
    b_in_d = din("b_in", [2048])
    s5p_d = din("s5p", [64, 3, 64])
    s5b_d = din("s5b", [64, 2, 64, 16])
    s5c_d = din("s5c", [64, 2, 64, 16])
    s5d_d = din("s5d", [128, 32])
    w_glu_d = din("w_glu", [512, 512])
    b_glu_d = din("b_glu", [512])
    cw_d = din("hy_conv_w", [3, 1536])
    cb_d = din("hy_conv_b", [1536])
    hf_w1 = din("hf_w1", [33, 64])
    hf_b1 = din("hf_b1", [64, 1])
    hf_wh = din("hf_wh", [2, 64, 64])
    hf_bh = din("hf_bh", [64, 2])
    hf_fr = din("hf_freq", [64, 1])
    hf_wout = din("hf_wout", [64, 2048])
    hf_dec = din("hf_decay", [2048])
    hyd_d = din("hy_d", [2, 512])
    gs5_d = din("g_out_s5", [512])
    ghy_d = din("g_out_hy", [512])
    w_out_d = din("w_out", [D, D])
    wq_d = din("peer_wq", [D, 2048])
    kT_d = din("peer_kT", [128, 16, 128])
    uT_d = din("peer_uT", [D, 16384 if not lite else 128])
    v_d = din("peer_v", [16384 if not lite else 128, D])
    gf_d = din("g_final", [D])
    cst = {k: din("k_" + k, list(v.shape), BF16 if v.dtype == ml_dtypes.bfloat16 else F32)
           for k, v in CONST.items()}
    out_d = dout("out", [NS * L, D])

    modx_d = dscr("modx_s", [NS + 1, 128, 6 * D])
    q_s = dscr("q_s", [NS * L, 1536])
    y_s = dscr("y_s", [NS * L, 512])
    z_s = dscr("z_s", [NS * L, 512])
    kspec_s = dscr("kspec_s", [2, 2, 16, 128, 512])
    uTb_s = dscr("uTb_s", [32, 128, 8, 512], BF16)
    vb_s = dscr("vb_s", [32, 128, 4, 1024], BF16)

    if debug:
        dbg["modx"] = dout("dbg_modx", [NS + 1, 6 * D])

    top = contextlib.ExitStack()
    uid = [0]

    def SB(es, shape, dt=F32, name=None):
        uid[0] += 1
        return es.enter_context(nc.sbuf_tensor((name or "t") + str(uid[0]), list(shape), dt))

    def PS(es, shape, dt=F32, name=None):
        uid[0] += 1
        return es.enter_context(nc.psum_tensor((name or "p") + str(uid[0]), list(shape), dt))

    def mm(ps_ap, pairs, reads, writes):
        n = len(pairs)
        for i, (l, r) in enumerate(pairs):
            P.op("pe", lambda e, l=l, r=r, i=i: e.matmul(ps_ap, lhsT=l, rhs=r, start=(i == 0), stop=(i == n - 1)),
                 reads=reads, writes=writes)

    def V(eng, fn, reads, writes):
        P.op(eng, fn, reads=reads, writes=writes)

    ident = SB(top, [128, 128], BF16, "ident")
    identf = SB(top, [128, 128], F32, "identf")
    mhalf = SB(top, [128, 1], F32, "mhalf")
    P.dma(ident[:], cst["ident"][:, :])
    P.dma(identf[:], cst["identf"][:, :])
    V("pool", lambda e: e.memset(mhalf[:], -0.5), [], [mhalf])

    def rsqrt_mean(es, ssum, n, out_rstd):
        V("dve", lambda e: e.tensor_scalar(out=out_rstd[:], in0=ssum[:], scalar1=1.0 / n, scalar2=EPS,
                                           op0=ALU.mult, op1=ALU.add), [ssum], [out_rstd])
        V("pool", lambda e: e.tensor_tensor(out=out_rstd[:], in0=out_rstd[:], in1=mhalf[:], op=ALU.pow),
          [out_rstd, mhalf], [out_rstd])

    with contextlib.ExitStack() as es:
        cT = SB(es, [128, 8, NS + 1])
        sil = SB(es, [128, 8, NS + 1])
        rep = SB(es, [128, NS + 1, 8, 128])
        bada = SB(es, [128, 6 * D])
        P.dma(cT[:], cT_d[:, :, :])
        P.dma(bada[:], b_ada.partition_broadcast(128))
        V("act", lambda e: e.activation(out=sil[:], in_=cT[:], func=AF.Silu), [cT], [sil])
        for n in range(NS + 1):
            V("dve", lambda e, n=n: e.tensor_copy(out=rep[:, n], in_=sil[:, :, n:n + 1].to_broadcast([128, 8, 128])),
              [sil], [rep])
        wts = [SB(es, [128, 8, 512]) for _ in range(2)]
        mps = [PS(es, [128, 512]) for _ in range(2)]
        mo = [SB(es, [128, 512]) for _ in range(2)]
        it = 0
        for nt in range(12):
            wt = wts[nt % 2]
            P.dma(wt[:], w_ada[:, nt * 512:(nt + 1) * 512].rearrange("(k p) n -> p k n", p=128))
            for n in range(NS + 1):
                ps = mps[it % 2]
                o = mo[it % 2]
                it += 1
                mm(ps[:], [(rep[:, n, k, :], wt[:, k, :]) for k in range(8)], [rep, wt], [ps])
                V("dve", lambda e, ps=ps, o=o, nt=nt: e.tensor_tensor(out=o[:], in0=ps[:], in1=bada[:, nt * 512:(nt + 1) * 512],
                                                                      op=ALU.add), [ps, bada], [o])
                P.dma(modx_d[n, :, nt * 512:(nt + 1) * 512], o[:], q="act")
                if debug:
                    P.dma(dbg["modx"][n:n + 1, nt * 512:(nt + 1) * 512], o[0:1, :], q="act", is_output=True)
    P.barrier()
    if upto <= 0:
        P.emit()
        top.close()
        return nc


    KMASK = 1.0e5
    NEXP = 16384 if not lite else 128
    NCH = NEXP // 512 if not lite else 0
    TS = 256
    NT = TS // 128
    wq_b = dscr("wq_b", [128, 8, 2048], BF16)
    wout_b = dscr("wout_b", [128, 8, 1024], BF16)
    wglu_b = dscr("wglu_b", [128, 4, 512], BF16)
    kT_b = dscr("kT_b", [128, 16, 128], BF16)
    if debug:
        dbg["x1"] = dout("dbg_x1", [NS * L, D])
        dbg["hx2"] = dout("dbg_hx2", [NS * L, D], BF16)
        dbg["pe"] = dout("dbg_pe", [NS * L, D])
    jobs = []
    for k in range(8):
        jobs.append((wq_b[:, k, :], wq_d[k * 128:(k + 1) * 128, :], 2048))
    for k in range(8):
        jobs.append((wout_b[:, k, :], w_out_d[k * 128:(k + 1) * 128, :], 1024))
    for k in range(4):
        jobs.append((wglu_b[:, k, :], w_glu_d[k * 128:(k + 1) * 128, :], 512))
    jobs.append((kT_b[:].rearrange("p c n -> p (c n)"), kT_d[:].rearrange("p c n -> p (c n)"), 2048))
    for ec in range(NCH):
        jobs.append((uTb_s[ec].rearrange("p k e -> p (k e)"), uT_d[:, ec * 512:(ec + 1) * 512].rearrange("(k p) e -> p k e", p=128), 4096))
        jobs.append((vb_s[ec].rearrange("p b d -> p (b d)"), v_d[ec * 512:(ec + 1) * 512, :].rearrange("(b p) d -> p b d", p=128), 4096))
    job_i = [0]

    def emit_job(stg, stb):
        i = job_i[0]
        if i >= len(jobs):
            return
        job_i[0] += 1
        dst, srcap, nel = jobs[i]
        a = stg[i % 2]
        b = stb[i % 2]
        if len(srcap.shape) == 3:
            P.dma(a[:, 0:nel].rearrange("p (k e) -> p k e", k=srcap.shape[1]), srcap, q="sp")
        else:
            P.dma(a[:, 0:nel], srcap, q="sp")
        V("act", lambda e: e.copy(out=b[:, 0:nel], in_=a[:, 0:nel]), [a], [b])
        P.dma(dst, b[:, 0:nel], q="sp")

    U_s = dscr("U_s", [32, 128, NS * 256 + 128], BF16)
    if debug:
        dbg["U"] = dout("dbg_U", [32, 128, NS * 256 + 128], BF16)
        dbg["q"] = dout("dbg_q", [NS * L, 1536])
    with contextlib.ExitStack() as es:
        w_in = SB(es, [128, 8, 2048], BF16)
        stage = [SB(es, [128, 2048]) for _ in range(2)]
        for k in range(8):
            st = stage[k % 2]
            P.dma(st[:], w_in_d[k * 128:(k + 1) * 128, :])
            V("dve", lambda e, st=st, k=k: e.tensor_copy(out=w_in[:, k, :], in_=st[:]), [st], [w_in])
        b_in = SB(es, [128, 2048])
        g1 = SB(es, [128, D])
        cw = SB(es, [128, 3, 1536])
        cb = SB(es, [128, 1536])
        shm = SB(es, [128, 128], BF16)
        shp = SB(es, [128, 128], BF16)
        P.dma(b_in[:], b_in_d.partition_broadcast(128))
        P.dma(g1[:], g1_d.partition_broadcast(128))
        for j in range(3):
            P.dma(cw[:, j, :], cw_d[j].partition_broadcast(128))
        P.dma(cb[:], cb_d.partition_broadcast(128))
        P.dma(shm[:], cst["shm"][:, :])
        P.dma(shp[:], cst["shp"][:, :])
        hT = SB(es, [128, 8, 1024], BF16)
        um2 = SB(es, [128, 32, 8, 16], BF16)
        ug = [SB(es, [128, 8, 128], BF16) for _ in range(2)]
        xt = [SB(es, [128, D]) for _ in range(2)]
        tmp = SB(es, [128, D])
        hb = [SB(es, [128, D], BF16) for _ in range(2)]
        g1p = SB(es, [128, D])
        sh1 = SB(es, [128, D])
        ssum = [SB(es, [128, 1]) for _ in range(2)]
        rstd = [SB(es, [128, 1]) for _ in range(2)]
        junk = SB(es, [128, D], BF16)
        pbf = SB(es, [128, 1536], BF16)
        t1 = SB(es, [128, 1536])
        t2 = SB(es, [128, 1536])
        pA = PS(es, [128, 1536])
        pB = PS(es, [128, 1536])
        trp = [PS(es, [128, 8, 128], BF16) for _ in range(1)]
        ps5 = PS(es, [128, 512])

        cstg = [SB(es, [128, 4096]) for _ in range(2)]
        cstb = [SB(es, [128, 4096], BF16) for _ in range(2)]
        blocks = [("x", n, half) for n in range(NS) for half in range(2)] + [("c", NS, 0)]
        cur_mod = None
        for bi, (kind, n, half) in enumerate(blocks):
            if cur_mod != n:
                cur_mod = n
                P.dma(sh1[:], modx_d[n, :, 0:D])
                P.dma(g1p[:], modx_d[n, :, D:2 * D])
                V("dve", lambda e: e.scalar_tensor_tensor(out=g1p[:], in0=g1p[:], scalar=1.0, in1=g1[:],
                                                          op0=ALU.add, op1=ALU.mult), [g1p, g1], [g1p])
            src = x_d if kind == "x" else ctx_d
            row0 = (n * L + half * 1024) if kind == "x" else 0
            for ti in range(8):
                X = xt[ti % 2]
                H = hb[ti % 2]
                ss = ssum[ti % 2]
                rs = rstd[ti % 2]
                P.dma(X[:], src[row0 + ti * 128: row0 + (ti + 1) * 128, :], q="sp" if ti % 2 else "act")
                V("act", lambda e, X=X, ss=ss: e.activation(out=junk[:], in_=X[:], func=AF.Square, accum_out=ss[:]),
                  [X], [junk, ss])
                rsqrt_mean(es, ss, D, rs)
                V("dve", lambda e, X=X, rs=rs: e.scalar_tensor_tensor(out=tmp[:], in0=X[:], scalar=rs[:, 0:1], in1=g1p[:],
                                                                      op0=ALU.mult, op1=ALU.mult), [X, rs, g1p], [tmp])
                V("pool", lambda e, H=H: e.tensor_tensor(out=H[:], in0=tmp[:], in1=sh1[:], op=ALU.add), [tmp, sh1], [H])
                tp = trp[0]
                for k in range(8):
                    P.op("pe", lambda e, k=k, H=H, tp=tp: e.transpose(out=tp[:, k, :], in_=H[:, k * 128:(k + 1) * 128], identity=ident[:]),
                         reads=[H, ident], writes=[tp])
                V("act", lambda e, tp=tp, ti=ti: e.copy(out=hT[:, :, ti * 128:(ti + 1) * 128], in_=tp[:]), [tp], [hT])
                emit_job(cstg, cstb)
            if kind == "x":
                for ti in range(8):
                    for nt in range(3):
                        mm(pA[:, nt * 512:(nt + 1) * 512],
                           [(hT[:, k, ti * 128:(ti + 1) * 128], w_in[:, k, 512 + nt * 512: 512 + (nt + 1) * 512]) for k in range(8)],
                           [hT, w_in], [pA])
                    V("dve", lambda e: e.tensor_tensor(out=pbf[:], in0=pA[:], in1=b_in[:, 512:2048], op=ALU.add), [pA, b_in], [pbf])
                    for nt in range(3):
                        mm(pB[:, nt * 512:(nt + 1) * 512], [(shm[:], pbf[:, nt * 512:(nt + 1) * 512])], [shm, pbf], [pB])
                    for nt in range(3):
                        mm(pA[:, nt * 512:(nt + 1) * 512], [(shp[:], pbf[:, nt * 512:(nt + 1) * 512])], [shp, pbf], [pA])
                    V("pool", lambda e: e.tensor_tensor(out=t1[:], in0=pbf[:], in1=cw[:, 1, :], op=ALU.mult), [pbf, cw], [t1])
                    V("pool", lambda e: e.tensor_tensor(out=t1[:], in0=t1[:], in1=cb[:], op=ALU.add), [t1, cb], [t1])
                    V("dve", lambda e: e.tensor_tensor(out=t2[:], in0=pB[:], in1=cw[:, 0, :], op=ALU.mult), [pB, cw], [t2])
                    V("pool", lambda e: e.tensor_tensor(out=t1[:], in0=t1[:], in1=t2[:], op=ALU.add), [t1, t2], [t1])
                    V("dve", lambda e: e.tensor_tensor(out=t2[:], in0=pA[:], in1=cw[:, 2, :], op=ALU.mult), [pA, cw], [t2])
                    V("pool", lambda e: e.tensor_tensor(out=t1[:], in0=t1[:], in1=t2[:], op=ALU.add), [t1, t2], [t1])
                    r0 = row0 + ti * 128
                    P.dma(q_s[r0:r0 + 128, :], t1[:], q="act")
                    if debug:
                        P.dma(dbg["q"][r0:r0 + 128, :], t1[:], q="act", is_output=True)
            for j in range(8):
                mm(ps5[:], [(hT[:, k, j::8], w_in[:, k, 0:512]) for k in range(8)], [hT, w_in], [ps5])
                V("dve", lambda e, j=j: e.tensor_tensor(out=um2[:, :, j, :], in0=ps5[:].rearrange("p (g h) -> p g h", g=32),
                                                        in1=b_in[:, 0:512].rearrange("p (g h) -> p g h", g=32), op=ALU.add),
                  [ps5, b_in], [um2])
            col0 = (n * 256 + half * 128) if kind == "x" else NS * 256
            for gb in range(4):
                tp = trp[0]
                ugt = ug[gb % 2]
                for gi in range(8):
                    g = gb * 8 + gi
                    P.op("pe", lambda e, g=g, gi=gi, tp=tp: e.transpose(out=tp[:, gi, :], in_=um2[:, g].rearrange("p j h -> p (j h)"),
                                                                        identity=ident[:]), reads=[um2, ident], writes=[tp])
                V("act", lambda e, tp=tp, ugt=ugt: e.copy(out=ugt[:], in_=tp[:]), [tp], [ugt])
                P.dma(U_s[gb * 8:(gb + 1) * 8, :, col0:col0 + 128].rearrange("g p c -> p g c"), ugt[:], q="sp")
                if debug:
                    P.dma(dbg["U"][gb * 8:(gb + 1) * 8, :, col0:col0 + 128].rearrange("g p c -> p g c"), ugt[:], q="sp", is_output=True)
        while job_i[0] < len(jobs):
            emit_job(cstg, cstb)
    P.barrier()
    if upto <= 1:
        P.emit()
        top.close()
        return nc


    TWO_PI = 2.0 * math.pi
    PI_LO = 3.1415925

    def trig(ang, n_free_shape, scratch, out_sin=None, out_cos=None):
        ki, kf, r = scratch
        for out, off in ((out_sin, 0.0), (out_cos, math.pi / 2)):
            if out is None:
                continue
            V("dve", lambda e, off=off: e.tensor_scalar(out=kf, in0=ang, scalar1=off, scalar2=1.0 / TWO_PI, op0=ALU.add, op1=ALU.mult),
              [ang], [kf])
            V("dve", lambda e: e.tensor_copy(out=ki, in_=kf), [kf], [ki])
            V("dve", lambda e: e.tensor_copy(out=kf, in_=ki), [ki], [kf])
            V("dve", lambda e: e.scalar_tensor_tensor(out=r, in0=kf, scalar=-TWO_PI, in1=ang, op0=ALU.mult, op1=ALU.add), [kf, ang], [r])
            V("dve", lambda e, off=off: e.tensor_scalar(out=r, in0=r, scalar1=off, scalar2=PI_LO, op0=ALU.add, op1=ALU.min), [r], [r])
            V("dve", lambda e: e.tensor_scalar(out=r, in0=r, scalar1=-PI_LO, scalar2=None, op0=ALU.max), [r], [r])
            V("act", lambda e, out=out: e.activation(out=out, in_=r, func=AF.Sin), [r], [out])

    if debug:
        dbg["y"] = dout("dbg_y", [NS * L, 512])
        dbg["h0"] = dout("dbg_h0", [64, 2, 64, NS])
    s5es = contextlib.ExitStack()
    R8 = SB(s5es, [128, 64, 2, 64], BF16)
    O8 = SB(s5es, [64, 64, 2, 128], BF16)
    T8 = SB(s5es, [128, 32, 128], BF16)
    th8 = SB(s5es, [64, 64])
    r8 = SB(s5es, [64, 64])
    with contextlib.ExitStack() as es:
        s5p = SB(es, [64, 3, 64])
        s5b = SB(es, [64, 2, 64, 16])
        s5c = SB(es, [64, 2, 64, 16])
        s5d = SB(es, [128, 32])
        sexp = SB(es, [64, 6, 128])
        mf = SB(es, [128, 128])
        mb = SB(es, [128, 128])
        P.dma(s5p[:], s5p_d[:, :, :])
        P.dma(s5b[:], s5b_d[:, :, :, :])
        P.dma(s5c[:], s5c_d[:, :, :, :])
        P.dma(s5d[:], s5d_d[:, :])
        P.dma(sexp[:], cst["s5exp"][:, :, :])
        P.dma(mf[:], cst["mf"][:, :])
        P.dma(mb[:], cst["mb"][:, :])
        step = SB(es, [64, 64])
        rho = SB(es, [64, 64])
        th = SB(es, [64, 64])
        ebase = SB(es, [64, 64])
        V("pool", lambda e: e.memset(ebase[:], math.e), [], [ebase])
        V("pool", lambda e: e.tensor_tensor(out=step[:], in0=ebase[:], in1=s5p[:, 2, :], op=ALU.pow), [ebase, s5p], [step])
        V("dve", lambda e: e.tensor_tensor(out=rho[:], in0=s5p[:, 0, :], in1=step[:], op=ALU.mult), [s5p, step], [rho])
        V("dve", lambda e: e.tensor_tensor(out=th[:], in0=s5p[:, 1, :], in1=step[:], op=ALU.mult), [s5p, step], [th])
        ki_s = SB(es, [64, 1024], I32)
        kf_s = SB(es, [64, 1024])
        r_s = SB(es, [64, 1024])
        sn = SB(es, [64, 1024])
        cs = SB(es, [64, 1024])
        mg = SB(es, [64, 1024])
        ang = SB(es, [64, 1024])
        V("dve", lambda e: e.tensor_copy(out=ang[:, 0:64], in_=th[:]), [th], [ang])
        trig(ang[:, 0:64], None, (ki_s[:, 0:64], kf_s[:, 0:64], r_s[:, 0:64]), sn[:, 0:64], cs[:, 0:64])
        V("act", lambda e: e.activation(out=mg[:, 0:64], in_=rho[:], func=AF.Exp), [rho], [mg])
        l1re = SB(es, [64, 64])
        l1im = SB(es, [64, 64])
        V("dve", lambda e: e.tensor_tensor(out=l1re[:], in0=mg[:, 0:64], in1=cs[:, 0:64], op=ALU.mult), [mg, cs], [l1re])
        V("dve", lambda e: e.tensor_tensor(out=l1im[:], in0=mg[:, 0:64], in1=sn[:, 0:64], op=ALU.mult), [mg, sn], [l1im])
        V("dve", lambda e: e.tensor_scalar(out=l1re[:], in0=l1re[:], scalar1=-1.0, scalar2=None, op0=ALU.add), [l1re], [l1re])
        den = SB(es, [64, 64])
        t64 = SB(es, [64, 64])
        cre = SB(es, [64, 64])
        cim = SB(es, [64, 64])
        are = s5p[:, 0, :]
        aim = s5p[:, 1, :]
        V("dve", lambda e: e.tensor_tensor(out=den[:], in0=are, in1=are, op=ALU.mult), [s5p], [den])
        V("dve", lambda e: e.tensor_tensor(out=t64[:], in0=aim, in1=aim, op=ALU.mult), [s5p], [t64])
        V("dve", lambda e: e.tensor_tensor(out=den[:], in0=den[:], in1=t64[:], op=ALU.add), [den, t64], [den])
        V("dve", lambda e: e.reciprocal(out=den[:], in_=den[:]), [den], [den])
        V("dve", lambda e: e.tensor_tensor(out=cre[:], in0=l1re[:], in1=are, op=ALU.mult), [l1re, s5p], [cre])
        V("dve", lambda e: e.tensor_tensor(out=t64[:], in0=l1im[:], in1=aim, op=ALU.mult), [l1im, s5p], [t64])
        V("dve", lambda e: e.tensor_tensor(out=cre[:], in0=cre[:], in1=t64[:], op=ALU.add), [cre, t64], [cre])
        V("dve", lambda e: e.tensor_tensor(out=cre[:], in0=cre[:], in1=den[:], op=ALU.mult), [cre, den], [cre])
        V("dve", lambda e: e.tensor_tensor(out=cim[:], in0=l1im[:], in1=are, op=ALU.mult), [l1im, s5p], [cim])
        V("dve", lambda e: e.tensor_tensor(out=t64[:], in0=l1re[:], in1=aim, op=ALU.mult), [l1re, s5p], [t64])
        V("dve", lambda e: e.tensor_tensor(out=cim[:], in0=cim[:], in1=t64[:], op=ALU.subtract), [cim, t64], [cim])
        V("dve", lambda e: e.tensor_tensor(out=cim[:], in0=cim[:], in1=den[:], op=ALU.mult), [cim, den], [cim])
        bbre = SB(es, [64, 64, 16])
        bbim = SB(es, [64, 64, 16])
        tb = SB(es, [64, 64, 16])
        creb = cre[:].unsqueeze(2).to_broadcast([64, 64, 16])
        cimb = cim[:].unsqueeze(2).to_broadcast([64, 64, 16])
        V("dve", lambda e: e.tensor_tensor(out=bbre[:], in0=s5b[:, 0], in1=creb, op=ALU.mult), [s5b, cre], [bbre])
        V("dve", lambda e: e.tensor_tensor(out=tb[:], in0=s5b[:, 1], in1=cimb, op=ALU.mult), [s5b, cim], [tb])
        V("dve", lambda e: e.tensor_tensor(out=bbre[:], in0=bbre[:], in1=tb[:], op=ALU.subtract), [bbre, tb], [bbre])
        V("dve", lambda e: e.tensor_tensor(out=bbim[:], in0=s5b[:, 1], in1=creb, op=ALU.mult), [s5b, cre], [bbim])
        V("dve", lambda e: e.tensor_tensor(out=tb[:], in0=s5b[:, 0], in1=cimb, op=ALU.mult), [s5b, cim], [tb])
        V("dve", lambda e: e.tensor_tensor(out=bbim[:], in0=bbim[:], in1=tb[:], op=ALU.add), [bbim, tb], [bbim])
        V("dve", lambda e: e.tensor_scalar(out=ang[:, 0:64], in0=th[:], scalar1=8.0, scalar2=None, op0=ALU.mult), [th], [ang])
        V("dve", lambda e: e.tensor_scalar(out=kf_s[:, 0:64], in0=ang[:, 0:64], scalar1=1.0 / TWO_PI, scalar2=None, op0=ALU.mult), [ang], [kf_s])
        V("dve", lambda e: e.tensor_copy(out=ki_s[:, 0:64], in_=kf_s[:, 0:64]), [kf_s], [ki_s])
        V("dve", lambda e: e.tensor_copy(out=kf_s[:, 0:64], in_=ki_s[:, 0:64]), [ki_s], [kf_s])
        V("dve", lambda e: e.scalar_tensor_tensor(out=th8[:], in0=kf_s[:, 0:64], scalar=-TWO_PI, in1=ang[:, 0:64], op0=ALU.mult, op1=ALU.add),
          [kf_s, ang], [th8])
        V("act", lambda e: e.activation(out=r8[:], in_=rho[:], func=AF.Exp, scale=8.0), [rho], [r8])
        Lre = SB(es, [64, 3, 8, 128])
        Lim = SB(es, [64, 3, 8, 128])
        RT = SB(es, [64, 2, 8, 128])
        OP = SB(es, [64, 2, 8, 128])
        OO = SB(es, [64, 2, 8, 128])
        tw = SB(es, [64, 8, 128])
        pT = [PS(es, [128, 128]) for _ in range(2)]
        pR = PS(es, [128, 4, 64])
        tt8 = SB(es, [128, 128])
        tt8b = SB(es, [128, 128])
        for d in range(2):
            for gb in range(4):
                dg0 = d * 32 + gb * 8
                for kk, kind in enumerate((d, 2 + d, 4 + d)):
                    a3 = ang[:].rearrange("p (a c) -> p a c", a=8)
                    exb = sexp[:, kind, :].unsqueeze(1).to_broadcast([64, 8, 128])
                    V("dve", lambda e, exb=exb, dg0=dg0: e.tensor_tensor(out=a3, in0=exb, in1=th[:, dg0:dg0 + 8].unsqueeze(2).to_broadcast([64, 8, 128]),
                                                                         op=ALU.mult), [sexp, th], [ang])
                    trig(ang[:], None, (ki_s[:], kf_s[:], r_s[:]), sn[:], cs[:])
                    m3 = mg[:].rearrange("p (a c) -> p a c", a=8)
                    V("dve", lambda e, exb=exb, dg0=dg0: e.tensor_tensor(out=m3, in0=exb, in1=rho[:, dg0:dg0 + 8].unsqueeze(2).to_broadcast([64, 8, 128]),
                                                                         op=ALU.mult), [sexp, rho], [mg])
                    V("act", lambda e: e.activation(out=mg[:], in_=mg[:], func=AF.Exp), [mg], [mg])
                    V("dve", lambda e, kk=kk: e.tensor_tensor(out=Lre[:, kk].rearrange("p a c -> p (a c)"), in0=mg[:], in1=cs[:], op=ALU.mult), [mg, cs], [Lre])
                    V("dve", lambda e, kk=kk: e.tensor_tensor(out=Lim[:, kk].rearrange("p a c -> p (a c)"), in0=mg[:], in1=sn[:], op=ALU.mult), [mg, sn], [Lim])
                def cmul(out_re, out_im, kind, vre, vim, neg_im):
                    lre = Lre[:, kind].rearrange("p a (j h) -> p a j h", j=8)
                    lim = Lim[:, kind].rearrange("p a (j h) -> p a j h", j=8)
                    vr = vre.unsqueeze(2).to_broadcast([64, 8, 8, 16])
                    vi = vim.unsqueeze(2).to_broadcast([64, 8, 8, 16])
                    o_re = out_re.rearrange("p a (j h) -> p a j h", j=8)
                    o_im = out_im.rearrange("p a (j h) -> p a j h", j=8)
                    t4 = tw[:].rearrange("p a (j h) -> p a j h", j=8)
                    V("dve", lambda e: e.tensor_tensor(out=o_re, in0=lre, in1=vr, op=ALU.mult), [Lre, bbre, s5c], [out_re])
                    V("dve", lambda e: e.tensor_tensor(out=t4, in0=lim, in1=vi, op=ALU.mult), [Lim, bbim, s5c], [tw])
                    V("dve", lambda e: e.tensor_tensor(out=out_re, in0=out_re, in1=tw[:], op=ALU.subtract), [out_re, tw], [out_re])
                    V("dve", lambda e: e.tensor_tensor(out=o_im, in0=lre, in1=vi, op=ALU.mult), [Lre, bbim, s5c], [out_im])
                    V("dve", lambda e: e.tensor_tensor(out=t4, in0=lim, in1=vr, op=ALU.mult), [Lim, bbre, s5c], [tw])
                    if neg_im:
                        V("dve", lambda e: e.scalar_tensor_tensor(out=out_im.rearrange("p a c -> p (a c)"), in0=out_im.rearrange("p a c -> p (a c)"), scalar=-1.0,
                                                                  in1=tw[:].rearrange("p a c -> p (a c)"), op0=ALU.mult, op1=ALU.subtract), [out_im, tw], [out_im])
                    else:
                        V("dve", lambda e: e.tensor_tensor(out=out_im, in0=out_im, in1=tw[:], op=ALU.add), [out_im, tw], [out_im])
                cmul(RT[:, 0], RT[:, 1], 0, bbre[:, dg0:dg0 + 8, :], bbim[:, dg0:dg0 + 8, :], False)
                cmul(OO[:, 0], OO[:, 1], 1, s5c[:, 0, dg0:dg0 + 8, :], s5c[:, 1, dg0:dg0 + 8, :], True)
                cmul(OP[:, 0], OP[:, 1], 2, s5c[:, 0, dg0:dg0 + 8, :], s5c[:, 1, dg0:dg0 + 8, :], True)
                for i in range(8):
                    dg = dg0 + i
                    g = gb * 8 + i
                    V("act", lambda e, dg=dg, i=i: e.copy(out=O8[:, dg], in_=OO[:, :, i, :]), [OO], [O8])
                    for c2 in range(2):
                        P.op("pe", lambda e, c2=c2, i=i: e.transpose(out=pR[:, c2, :], in_=RT[:, c2, i, :], identity=identf[0:64, 0:64]),
                             reads=[RT, identf], writes=[pR])
                    V("act", lambda e, dg=dg: e.copy(out=R8[:, dg], in_=pR[:, 0:2, :]), [pR], [R8])
                    pt = pT[i % 2]
                    mm(pt[:], [(RT[:, 0, i, :], OP[:, 0, i, :]), (RT[:, 1, i, :], OP[:, 1, i, :])], [RT, OP], [pt])
                    if d == 0:
                        V("dve", lambda e, pt=pt: e.tensor_tensor(out=tt8[:], in0=pt[:], in1=mf[:], op=ALU.mult), [pt, mf], [tt8])
                        V("dve", lambda e, g=g: e.scalar_tensor_tensor(out=tt8[:], in0=identf[:], scalar=s5d[:, g:g + 1], in1=tt8[:],
                                                                       op0=ALU.mult, op1=ALU.add), [identf, s5d, tt8], [tt8])
                        V("pool", lambda e, g=g: e.tensor_copy(out=T8[:, g, :], in_=tt8[:]), [tt8], [T8])
                    else:
                        V("dve", lambda e, pt=pt: e.tensor_tensor(out=tt8b[:], in0=pt[:], in1=mb[:], op=ALU.mult), [pt, mb], [tt8b])
                        V("pool", lambda e, g=g: e.tensor_tensor(out=T8[:, g, :], in0=T8[:, g, :], in1=tt8b[:], op=ALU.add), [tt8b, T8], [T8])
    P.barrier()
    if upto <= 2:
        if debug:
            dbg["T8"] = dout("dbg_T8", [128, 32, 128], BF16)
            dbg["O8"] = dout("dbg_O8", [64, 64, 2, 128], BF16)
            dbg["R8"] = dout("dbg_R8", [128, 64, 2, 64], BF16)
            P.dma(dbg["T8"][:, :, :], T8[:], is_output=True)
            P.dma(dbg["O8"][:, :, :, :], O8[:], is_output=True)
            P.dma(dbg["R8"][:, :, :, :], R8[:], is_output=True)
        P.emit()
        s5es.close()
        top.close()
        return nc


    with contextlib.ExitStack() as es:
        mix = SB(es, [64, 256])
        P.dma(mix[:], cst["mix"][:, :])
        Ugs = [SB(es, [128, NS * 256 + 128], BF16) for _ in range(2)]
        cosr = [SB(es, [64, 256]) for _ in range(2)]
        sinr = [SB(es, [64, 256]) for _ in range(2)]
        angr = SB(es, [64, 256])
        kir = SB(es, [64, 256], I32)
        kfr = SB(es, [64, 256])
        rr = SB(es, [64, 256])
        cc = [SB(es, [64, NS, 255]) for _ in range(2)]
        ta = SB(es, [64, NS, 256])
        tb2 = SB(es, [64, NS, 256])
        W = [SB(es, [64, NS, 256]) for _ in range(2)]
        Wc = [SB(es, [64, NS, 32]) for _ in range(2)]
        h0 = SB(es, [64, 2, 2, NS])
        t4 = SB(es, [64, NS])
        Sin = SB(es, [64, 2, 2, NS * 256], BF16)
        Ysb = SB(es, [128, NS * 256])
        Yout = SB(es, [128, 8, 8, 128])
        psS = [PS(es, [64, NS * 256]) for _ in range(2)]
        psY = PS(es, [128, NS * 256])
        pTr = PS(es, [128, 4, 128])
        psC = PS(es, [64, 2, 128])

        def rot(out_re, out_im, s_re, s_im, cs_, sn_, conj, eng2="pool"):
            V("dve", lambda e: e.tensor_tensor(out=ta_v(out_re), in0=s_re, in1=cs_, op=ALU.mult), [psS[0], psC, W[0], Wc[0], cosr[0], cosr[1]], [ta])
            V("dve", lambda e: e.tensor_tensor(out=tb_v(out_re), in0=s_im, in1=sn_, op=ALU.mult), [psS[1], psC, W[1], Wc[1], sinr[0], sinr[1]], [tb2])
            V(eng2, lambda e: e.tensor_tensor(out=out_re, in0=ta_v(out_re), in1=tb_v(out_re), op=(ALU.add if conj else ALU.subtract)),
              [ta, tb2], [cc[0], Sin])
            V("dve", lambda e: e.tensor_tensor(out=ta_v(out_re), in0=s_im, in1=cs_, op=ALU.mult), [psS[1], psC, W[1], Wc[1], cosr[0], cosr[1]], [ta])
            V("dve", lambda e: e.tensor_tensor(out=tb_v(out_re), in0=s_re, in1=sn_, op=ALU.mult), [psS[0], psC, W[0], Wc[0], sinr[0], sinr[1]], [tb2])
            V(eng2, lambda e: e.tensor_tensor(out=out_im, in0=ta_v(out_re), in1=tb_v(out_re), op=(ALU.subtract if conj else ALU.add)),
              [ta, tb2], [cc[1], Sin])

        def ta_v(like):
            return ta[:, :, 0:like.shape[2]]

        def tb_v(like):
            return tb2[:, :, 0:like.shape[2]]

        for g in range(32):
            Ug = Ugs[g % 2]
            P.dma(Ug[:], U_s[g], q="act" if g % 2 else "sp")
            for d in range(2):
                dg = d * 32 + g
                V("dve", lambda e, dg=dg: e.tensor_scalar(out=angr[:], in0=mix[:], scalar1=th8[:, dg:dg + 1], scalar2=None, op0=ALU.mult),
                  [mix, th8], [angr])
                trig(angr[:], None, (kir[:], kfr[:], rr[:]), sinr[d][:], cosr[d][:])
            for d in range(2):
                dg = d * 32 + g
                for c2 in range(2):
                    mm(psC[:, c2, :], [(R8[:, dg, c2, :], Ug[:, NS * 256:NS * 256 + 128])], [R8, Ug], [psC])
                sre = psC[:, 0, :].rearrange("p (n m) -> p n m", n=NS)
                sim = psC[:, 1, :].rearrange("p (n m) -> p n m", n=NS)
                if d == 1:
                    sre = sre[:, :, ::-1]
                    sim = sim[:, :, ::-1]
                csb = cosr[d][:, 1:33].unsqueeze(1).to_broadcast([64, NS, 32])
                snb = sinr[d][:, 1:33].unsqueeze(1).to_broadcast([64, NS, 32])
                rot(cc[0][:, :, 0:32], cc[1][:, :, 0:32], sre, sim, csb, snb, True)
                for c2 in range(2):
                    for n in range(NS):
                        V("dve", lambda e, c2=c2, n=n, dg=dg: e.tensor_tensor_scan(out=Wc[c2][:, n, :], data0=r8[:, dg:dg + 1].to_broadcast([64, 32]),
                                                                                  data1=cc[c2][:, n, 0:32], initial=0.0, op0=ALU.mult, op1=ALU.add),
                          [r8, cc[c2]], [Wc[c2]])
                c32 = cosr[d][:, 32:33]
                s32 = sinr[d][:, 32:33]
                V("dve", lambda e, s32=s32: e.tensor_scalar(out=t4[:], in0=Wc[1][:, :, 31], scalar1=s32, scalar2=None, op0=ALU.mult), [Wc[1], sinr[d]], [t4])
                V("dve", lambda e, c32=c32, d=d: e.scalar_tensor_tensor(out=h0[:, d, 0, :], in0=Wc[0][:, :, 31], scalar=c32, in1=t4[:], op0=ALU.mult, op1=ALU.subtract),
                  [Wc[0], cosr[d], t4], [h0])
                V("dve", lambda e, c32=c32: e.tensor_scalar(out=t4[:], in0=Wc[1][:, :, 31], scalar1=c32, scalar2=None, op0=ALU.mult), [Wc[1], cosr[d]], [t4])
                V("dve", lambda e, s32=s32, d=d: e.scalar_tensor_tensor(out=h0[:, d, 1, :], in0=Wc[0][:, :, 31], scalar=s32, in1=t4[:], op0=ALU.mult, op1=ALU.add),
                  [Wc[0], sinr[d], t4], [h0])
                if debug:
                    P.dma(dbg["h0"][:, d, g, :], h0[:, d, 0, :], is_output=True)
                    if g == 0 and d == 0:
                        dbg["m"] = dout("dbg_m", [64, 8, 256])
                        dm = SB(es, [64, 8, 256])
                        V("dve", lambda e: e.memset(dm[:], 0.0), [], [dm])
                        V("dve", lambda e: e.tensor_copy(out=dm[:, 0, :], in_=psC[:].rearrange("p a b -> p (a b)")), [psC], [dm])
                        V("dve", lambda e: e.tensor_copy(out=dm[:, 1, 0:128].rearrange("p (n m) -> p n m", n=NS), in_=cc[0][:, :, 0:32]), [cc[0]], [dm])
                        V("dve", lambda e: e.tensor_copy(out=dm[:, 2, 0:128].rearrange("p (n m) -> p n m", n=NS), in_=Wc[0][:]), [Wc[0]], [dm])
                        V("dve", lambda e: e.tensor_copy(out=dm[:, 3, :], in_=cosr[0][:]), [cosr[0]], [dm])
                        V("dve", lambda e: e.tensor_copy(out=dm[:, 4, :], in_=sinr[0][:]), [sinr[0]], [dm])
                        V("dve", lambda e: e.tensor_copy(out=dm[:, 5, 0:64], in_=r8[:]), [r8], [dm])
                        V("dve", lambda e: e.tensor_copy(out=dm[:, 6, 0:64], in_=th8[:]), [th8], [dm])
                        V("dve", lambda e: e.tensor_copy(out=dm[:, 7, 0:128].rearrange("p (n m) -> p n m", n=NS), in_=cc[1][:, :, 0:32]), [cc[1]], [dm])
                        P.dma(dbg["m"][:, :, :], dm[:], is_output=True)
            for d in range(2):
                dg = d * 32 + g
                for c2 in range(2):
                    for hf in range(2):
                        mm(psS[c2][:, hf * 512:(hf + 1) * 512], [(R8[:, dg, c2, :], Ug[:, hf * 512:(hf + 1) * 512])], [R8, Ug], [psS[c2]])
                sre = psS[0][:].rearrange("p (n m) -> p n m", n=NS)
                sim = psS[1][:].rearrange("p (n m) -> p n m", n=NS)
                if d == 0:
                    sre = sre[:, :, 0:255]
                    sim = sim[:, :, 0:255]
                else:
                    sre = sre[:, :, 255:0:-1]
                    sim = sim[:, :, 255:0:-1]
                csb = cosr[d][:, 1:256].unsqueeze(1).to_broadcast([64, NS, 255])
                snb = sinr[d][:, 1:256].unsqueeze(1).to_broadcast([64, NS, 255])
                rot(cc[0][:], cc[1][:], sre, sim, csb, snb, True)
                for c2 in range(2):
                    V("pool", lambda e, c2=c2, d=d: e.tensor_copy(out=W[c2][:, :, 0:1], in_=h0[:, d, c2, :].unsqueeze(2)), [h0], [W[c2]])
                    for n in range(NS):
                        V("dve", lambda e, c2=c2, n=n, dg=dg, d=d: e.tensor_tensor_scan(out=W[c2][:, n, 1:256], data0=r8[:, dg:dg + 1].to_broadcast([64, 255]),
                                                                                       data1=cc[c2][:, n, :], initial=h0[:, d, c2, n:n + 1], op0=ALU.mult, op1=ALU.add),
                          [r8, cc[c2], h0], [W[c2]])
                csb = cosr[d][:].unsqueeze(1).to_broadcast([64, NS, 256])
                snb = sinr[d][:].unsqueeze(1).to_broadcast([64, NS, 256])
                o_re = Sin[:, d, 0, :].rearrange("p (n m) -> p n m", n=NS)
                o_im = Sin[:, d, 1, :].rearrange("p (n m) -> p n m", n=NS)
                if d == 1:
                    o_re = o_re[:, :, ::-1]
                    o_im = o_im[:, :, ::-1]
                rot(o_re, o_im, W[0][:], W[1][:], csb, snb, False)
            for hf in range(2):
                cs_ = slice(hf * 512, (hf + 1) * 512)
                mm(psY[:, cs_], [(T8[:, g, :], Ug[:, cs_]), (O8[:, g, 0, :], Sin[:, 0, 0, cs_]), (O8[:, g, 1, :], Sin[:, 0, 1, cs_]),
                                 (O8[:, 32 + g, 0, :], Sin[:, 1, 0, cs_]), (O8[:, 32 + g, 1, :], Sin[:, 1, 1, cs_])],
                   [T8, Ug, O8, Sin], [psY])
            V("act", lambda e: e.copy(out=Ysb[:], in_=psY[:]), [psY], [Ysb])
            gi = g % 8
            for q4 in range(2):
                for b4 in range(4):
                    blk = q4 * 4 + b4
                    P.op("pe", lambda e, blk=blk, b4=b4: e.transpose(out=pTr[:, b4, :], in_=Ysb[:, blk * 128:(blk + 1) * 128], identity=identf[:]),
                         reads=[Ysb, identf], writes=[pTr])
                V("act", lambda e, q4=q4, gi=gi: e.copy(out=Yout[:, q4 * 4:(q4 + 1) * 4, :, gi * 16:(gi + 1) * 16],
                                                        in_=pTr[:].rearrange("p b (j h) -> p b j h", j=8)), [pTr], [Yout])
            if gi == 7:
                gb = g // 8
                for blk in range(8):
                    dst = y_s[blk * 1024:(blk + 1) * 1024, gb * 128:(gb + 1) * 128].rearrange("(m j) c -> m j c", j=8)
                    P.dma(dst, Yout[:, blk], q="sp" if blk % 2 else "act")
                    if debug:
                        P.dma(dbg["y"][blk * 1024:(blk + 1) * 1024, gb * 128:(gb + 1) * 128].rearrange("(m j) c -> m j c", j=8), Yout[:, blk], is_output=True)
    s5es.close()
    P.barrier()
    if upto <= 3:
        P.emit()
        top.close()
        return nc


    h_s = dscr("h_s", [L, 2048])
    if debug:
        dbg["filt"] = dout("dbg_filt", [L, 2048])
        dbg["z"] = dout("dbg_z", [NS * L, 512])
    hyes = contextlib.ExitStack()
    rn = SB(hyes, [128, 2, 512])
    with contextlib.ExitStack() as es:
        fT = SB(es, [33, 2048])
        w1 = SB(es, [33, 64])
        b1 = SB(es, [64, 1])
        wh = SB(es, [64, 2, 64])
        bh = SB(es, [64, 2])
        fr = SB(es, [64, 1])
        wout = SB(es, [64, 2048])
        absd = SB(es, [128, 2048])
        tneg = SB(es, [128, 16])
        onesf = SB(es, [128, 128])
        P.dma(fT[:], cst["featsT"][:, :])
        P.dma(w1[:], hf_w1[:, :])
        P.dma(b1[:], hf_b1[:, :])
        P.dma(wh[:], hf_wh.rearrange("i a b -> a i b"))
        P.dma(bh[:], hf_bh[:, :])
        P.dma(fr[:], hf_fr[:, :])
        P.dma(wout[:], hf_wout[:, :])
        P.dma(absd[:], hf_dec.partition_broadcast(128))
        P.dma(tneg[:], cst["tneg"][:, :])
        P.dma(onesf[:], cst["onesf"][:, :])
        V("act", lambda e: e.activation(out=absd[:], in_=absd[:], func=AF.Abs), [absd], [absd])
        hid = [SB(es, [64, 2048]) for _ in range(2)]
        pre = SB(es, [64, 2048])
        kiH = SB(es, [64, 2048], I32)
        kfH = SB(es, [64, 2048])
        rH = SB(es, [64, 2048])
        psA = PS(es, [128, 2048])
        psB = PS(es, [128, 2048])
        for layer in range(3):
            for nt in range(4):
                cs_ = slice(nt * 512, (nt + 1) * 512)
                if layer == 0:
                    mm(psA[0:64, cs_], [(w1[:], fT[:, cs_])], [w1, fT], [psA])
                else:
                    mm(psA[0:64, cs_], [(wh[:, layer - 1, :], hid[(layer - 1) % 2][:, cs_])], [wh, hid[(layer - 1) % 2]], [psA])
            bias = b1[:, 0:1] if layer == 0 else bh[:, layer - 1:layer]
            V("dve", lambda e, bias=bias: e.tensor_scalar(out=pre[:], in0=psA[0:64, :], scalar1=bias, scalar2=fr[:, 0:1], op0=ALU.add, op1=ALU.mult),
              [psA, b1, bh, fr], [pre])
            trig(pre[:], None, (kiH[:], kfH[:], rH[:]), hid[layer % 2][:], None)
        hidF = hid[0]
        dec = SB(es, [128, 2048])
        hraw = [SB(es, [128, 2048]) for _ in range(2)]
        hsq = SB(es, [128, 2048])
        for tc in range(16):
            hr = hraw[tc % 2]
            for nt in range(4):
                cs_ = slice(nt * 512, (nt + 1) * 512)
                mm(psA[:, cs_], [(hidF[:, tc * 128:(tc + 1) * 128], wout[:, cs_])], [hidF, wout], [psA])
            V("act", lambda e, tc=tc: e.activation(out=dec[:], in_=absd[:], func=AF.Exp, scale=tneg[:, tc:tc + 1]), [absd, tneg], [dec])
            V("dve", lambda e, hr=hr: e.tensor_tensor(out=hr[:], in0=psA[:], in1=dec[:], op=ALU.mult), [psA, dec], [hr])
            V("pool", lambda e, hr=hr: e.tensor_tensor(out=hsq[:], in0=hr[:], in1=hr[:], op=ALU.mult), [hr], [hsq])
            for nt in range(4):
                cs_ = slice(nt * 512, (nt + 1) * 512)
                P.op("pe", lambda e, cs_=cs_, tc=tc: e.matmul(psB[:, cs_], lhsT=onesf[:], rhs=hsq[:, cs_], start=(tc == 0), stop=(tc == 15)),
                     reads=[onesf, hsq], writes=[psB])
            P.dma(h_s[tc * 128:(tc + 1) * 128, :], hr[:], q="act")
        ssb = SB(es, [128, 2048])
        V("act", lambda e: e.copy(out=ssb[:], in_=psB[:]), [psB], [ssb])
        s4 = ssb[:].rearrange("p (o d c) -> p o d c", o=2, d=2)
        V("dve", lambda e: e.tensor_tensor(out=rn[:], in0=s4[:, :, 0, :], in1=s4[:, :, 1, :], op=ALU.add), [ssb], [rn])
        V("dve", lambda e: e.tensor_scalar(out=rn[:], in0=rn[:], scalar1=EPS, scalar2=None, op0=ALU.add), [rn], [rn])
        mh = SB(es, [128, 1024])
        V("pool", lambda e: e.memset(mh[:], -0.5), [], [mh])
        V("pool", lambda e: e.tensor_tensor(out=rn[:].rearrange("p o c -> p (o c)"), in0=rn[:].rearrange("p o c -> p (o c)"), in1=mh[:], op=ALU.pow), [rn, mh], [rn])
    P.barrier()
    Ct = SB(hyes, [128, 16, 2048], BF16)
    St = SB(hyes, [128, 16, 2048], BF16)
    P.dma(Ct[:], cst["ctab"].rearrange("(tc p) f -> p tc f", p=128))
    P.dma(St[:], cst["stab"].rearrange("(tc p) f -> p tc f", p=128), q="act")
    with contextlib.ExitStack() as es:
        hraw = [SB(es, [128, 2048]) for _ in range(2)]
        psA = PS(es, [128, 2048])
        m0 = SB(es, [128, 1])
        cphi = SB(es, [128, 16])
        sphi = SB(es, [128, 16])
        P.dma(m0[:], cst["m0"][:, :])
        P.dma(cphi[:], cst["cphi"][:, :])
        P.dma(sphi[:], cst["sphi"][:, :])
        PM = SB(es, [128, 16, 2, 512], BF16)
        kre = [SB(es, [128, 512]) for _ in range(2)]
        kim = [SB(es, [128, 512]) for _ in range(2)]
        for o in range(2):
            for tc in range(16):
                hr = hraw[tc % 2]
                P.dma(hr[:], h_s[tc * 128:(tc + 1) * 128, :])
                h4 = hr[:].rearrange("p (o d c) -> p o d c", o=2, d=2)
                V("dve", lambda e, h4=h4, o=o, hr=hr: e.tensor_tensor(out=h4[:, o], in0=h4[:, o], in1=rn[:, o:o + 1, :].to_broadcast([128, 2, 512]), op=ALU.mult),
                  [hr, rn], [hr])
                if debug:
                    P.dma(dbg["filt"][tc * 128:(tc + 1) * 128, o * 1024:(o + 1) * 1024], hr[:, o * 1024:(o + 1) * 1024], is_output=True)
                if tc == 0:
                    V("dve", lambda e, h4=h4, o=o, hr=hr: e.tensor_scalar(out=h4[:, o, 1, :], in0=h4[:, o, 1, :], scalar1=m0[:, 0:1], scalar2=None, op0=ALU.mult),
                      [hr, m0], [hr])
                V("dve", lambda e, h4=h4, o=o, tc=tc, hr=hr: e.tensor_tensor(out=PM[:, tc, 0, :], in0=h4[:, o, 0, :], in1=h4[:, o, 1, :], op=ALU.add), [hr], [PM])
                V("pool", lambda e, h4=h4, o=o, tc=tc, hr=hr: e.tensor_tensor(out=PM[:, tc, 1, :], in0=h4[:, o, 0, :], in1=h4[:, o, 1, :], op=ALU.subtract), [hr], [PM])
            for fc in range(16):
                fs = slice(fc * 128, (fc + 1) * 128)
                mm(psA[:, 0:512], [(Ct[:, tc, fs], PM[:, tc, 0, :]) for tc in range(16)], [Ct, PM], [psA])
                mm(psA[:, 512:1024], [(St[:, tc, fs], PM[:, tc, 0, :]) for tc in range(16)], [St, PM], [psA])
                mm(psA[:, 1024:1536], [(Ct[:, tc, fs], PM[:, tc, 1, :]) for tc in range(16)], [Ct, PM], [psA])
                mm(psA[:, 1536:2048], [(St[:, tc, fs], PM[:, tc, 1, :]) for tc in range(16)], [St, PM], [psA])
                kr = kre[fc % 2]
                kq = kim[fc % 2]
                V("dve", lambda e, kr=kr, fc=fc: e.tensor_scalar(out=kr[:], in0=psA[:, 0:512], scalar1=cphi[:, fc:fc + 1], scalar2=None, op0=ALU.mult), [psA, cphi], [kr])
                V("dve", lambda e, kr=kr, fc=fc: e.scalar_tensor_tensor(out=kr[:], in0=psA[:, 512:1024], scalar=sphi[:, fc:fc + 1], in1=kr[:], op0=ALU.mult, op1=ALU.add),
                  [psA, sphi, kr], [kr])
                V("dve", lambda e, kq=kq, fc=fc: e.tensor_scalar(out=kq[:], in0=psA[:, 1536:2048], scalar1=cphi[:, fc:fc + 1], scalar2=None, op0=ALU.mult), [psA, cphi], [kq])
                V("dve", lambda e, kq=kq, fc=fc: e.scalar_tensor_tensor(out=kq[:], in0=psA[:, 1024:1536], scalar=sphi[:, fc:fc + 1], in1=kq[:], op0=ALU.mult, op1=ALU.subtract),
                  [psA, sphi, kq], [kq])
                P.dma(kspec_s[o, 0, fc], kr[:], q="act")
                P.dma(kspec_s[o, 1, fc], kq[:], q="act")
    P.barrier()
    if upto <= 4:
        P.emit()
        hyes.close()
        top.close()
        return nc


    with contextlib.ExitStack() as es:
        hyd = SB(es, [128, 2, 512])
        P.dma(hyd[:].rearrange("p o c -> p (o c)"), hyd_d.rearrange("o c -> (o c)").partition_broadcast(128))
        zin = SB(es, [128, 16, 512], BF16)
        YY = SB(es, [128, 16, 2, 512], BF16)
        kr2 = [SB(es, [128, 2, 512]) for _ in range(2)]
        gt = [SB(es, [128, 512]) for _ in range(2)]
        vst = gt
        u1 = SB(es, [128, 512])
        u2 = SB(es, [128, 512])
        u3 = SB(es, [128, 512])
        ot = [u1, u2]
        psX = [PS(es, [128, 2, 512]) for _ in range(2)]
        psO = [PS(es, [128, 512]) for _ in range(2)]
        for n in range(NS):
            for tc in range(16):
                vt = vst[tc % 2]
                r0 = n * L + tc * 128
                P.dma(vt[:], q_s[r0:r0 + 128, 0:512], q="act" if tc % 2 else "sp")
                V("act" if tc % 2 else "pool", lambda e, vt=vt, tc=tc: (e.copy if hasattr(e, "copy") else e.tensor_copy)(out=zin[:, tc, :], in_=vt[:]), [vt], [zin])
            for o in range(2):
                for fc in range(16):
                    fs = slice(fc * 128, (fc + 1) * 128)
                    px_ = psX[fc % 2]
                    kk = kr2[fc % 2]
                    P.dma(kk[:, 0, :], kspec_s[o, 0, fc], q="sp")
                    P.dma(kk[:, 1, :], kspec_s[o, 1, fc], q="act")
                    mm(px_[:, 0, :], [(Ct[:, tc, fs], zin[:, tc, :]) for tc in range(16)], [Ct, zin], [px_])
                    mm(px_[:, 1, :], [(St[:, tc, fs], zin[:, tc, :]) for tc in range(16)], [St, zin], [px_])
                    V("dve", lambda e, px_=px_, kk=kk: e.tensor_tensor(out=u1[:], in0=px_[:, 0, :], in1=kk[:, 0, :], op=ALU.mult), [px_, kk], [u1])
                    V("dve", lambda e, px_=px_, kk=kk: e.tensor_tensor(out=u2[:], in0=px_[:, 1, :], in1=kk[:, 1, :], op=ALU.mult), [px_, kk], [u2])
                    V("pool", lambda e, fc=fc: e.tensor_tensor(out=YY[:, fc, 0, :], in0=u1[:], in1=u2[:], op=ALU.add), [u1, u2], [YY])
                    V("dve", lambda e, px_=px_, kk=kk: e.tensor_tensor(out=u1[:], in0=px_[:, 1, :], in1=kk[:, 0, :], op=ALU.mult), [px_, kk], [u1])
                    V("dve", lambda e, px_=px_, kk=kk: e.tensor_tensor(out=u2[:], in0=px_[:, 0, :], in1=kk[:, 1, :], op=ALU.mult), [px_, kk], [u2])
                    V("pool", lambda e, fc=fc: e.tensor_tensor(out=YY[:, fc, 1, :], in0=u1[:], in1=u2[:], op=ALU.subtract), [u1, u2], [YY])
                for tc in range(16):
                    ts_ = slice(tc * 128, (tc + 1) * 128)
                    po = psO[tc % 2]
                    g_ = gt[tc % 2]
                    r0 = n * L + tc * 128
                    P.dma(g_[:], q_s[r0:r0 + 128, 512 * (o + 1):512 * (o + 2)], q="sp")
                    pairs = []
                    for fc in range(16):
                        pairs.append((Ct[:, fc, ts_], YY[:, fc, 0, :]))
                        pairs.append((St[:, fc, ts_], YY[:, fc, 1, :]))
                    mm(po[:], pairs, [Ct, St, YY], [po])
                    V("pool", lambda e, tc=tc, o=o: e.tensor_tensor(out=u3[:], in0=zin[:, tc, :], in1=hyd[:, o, :], op=ALU.mult), [zin, hyd], [u3])
                    V("dve", lambda e, po=po: e.scalar_tensor_tensor(out=u3[:], in0=po[:], scalar=2.0 / NFFT, in1=u3[:], op0=ALU.mult, op1=ALU.add), [po, u3], [u3])
                    if o == 0:
                        V("dve", lambda e, tc=tc, g_=g_: e.tensor_tensor(out=zin[:, tc, :], in0=u3[:], in1=g_[:], op=ALU.mult), [u3, g_], [zin])
                    else:
                        o_ = ot[tc % 2]
                        V("dve", lambda e, o_=o_, g_=g_: e.tensor_tensor(out=o_[:], in0=u3[:], in1=g_[:], op=ALU.mult), [u3, g_], [o_])
                        P.dma(z_s[r0:r0 + 128, :], o_[:], q="act")
                        if debug:
                            P.dma(dbg["z"][r0:r0 + 128, :], o_[:], q="act", is_output=True)
    hyes.close()
    P.barrier()
    if upto <= 5:
        P.emit()
        top.close()
        return nc


    NTG = 2
    TSG = NTG * 128
    x1_s = dscr("x1_s", [NS * L, D])
    hx2T_s = dscr("hx2T_s", [NS * L // TS, 128, 8, TS], BF16)
    sc_s = dscr("sc_s", [NS * L // 128, 128, 16 * 128])
    thr_s = dscr("thr_s", [NS * L // 128, 128, 16])
    with contextlib.ExitStack() as es:
        bglu = SB(es, [128, 512])
        gs5 = SB(es, [128, 512])
        ghy = SB(es, [128, 512])
        g2 = SB(es, [128, D])
        gx1 = SB(es, [128, D])
        g2p = SB(es, [128, D])
        sh2 = SB(es, [128, D])
        P.dma(bglu[:], b_glu_d.partition_broadcast(128))
        P.dma(gs5[:], gs5_d.partition_broadcast(128))
        P.dma(ghy[:], ghy_d.partition_broadcast(128))
        P.dma(g2[:], g2_d.partition_broadcast(128))
        hx2T = SB(es, [128, 8, TSG], BF16)
        x1t = [SB(es, [128, D]) for _ in range(NTG)]
        scs = [SB(es, [128, 16, 128]) for _ in range(NTG)]
        c16s = [SB(es, [128, 8, 16]) for _ in range(NTG)]
        thrs = [SB(es, [128, 16]) for _ in range(NTG)]
        nbs = [t_[:, 0:8] for t_ in thrs]
        ntaus = [t_[:, 8:16] for t_ in thrs]
        woutb = SB(es, [128, 8, 1024], BF16)
        wglub = SB(es, [128, 4, 512], BF16)
        wqb = SB(es, [128, 8, 2048], BF16)
        kTb = SB(es, [128, 16, 128], BF16)
        P.dma(woutb[:], wout_b[:, :, :], q="sp")
        P.dma(wglub[:], wglu_b[:, :, :], q="act")
        P.dma(wqb[:], wq_b[:, :, :], q="sp")
        P.dma(kTb[:], kT_b[:, :, :], q="act")
        qT = SB(es, [128, 16, TSG], BF16)
        yts = [SB(es, [128, 512]) for _ in range(NTG)]
        zts = [SB(es, [128, 512]) for _ in range(NTG)]
        xt4s = [SB(es, [128, D]) for _ in range(NTG)]
        gys = [SB(es, [128, 512]) for _ in range(NTG)]
        gybs = [SB(es, [128, 512], BF16) for _ in range(NTG)]
        gyTs = [SB(es, [128, 4, 128], BF16) for _ in range(NTG)]
        gpres = [SB(es, [128, 512]) for _ in range(NTG)]
        s5os = [SB(es, [128, 512]) for _ in range(NTG)]
        catbs = [SB(es, [128, D], BF16) for _ in range(NTG)]
        catTs = [SB(es, [128, 8, 128], BF16) for _ in range(NTG)]
        tms = [SB(es, [128, D]) for _ in range(NTG)]
        hb4s = [SB(es, [128, D], BF16) for _ in range(NTG)]
        jks = [SB(es, [128, D], BF16) for _ in range(NTG)]
        sss = [[SB(es, [128, 1]) for _ in range(6)] for _ in range(NTG)]
        wks = [SB(es, [128, 256]) for _ in range(NTG)]
        m16s = [SB(es, [128, 16, 16]) for _ in range(NTG)]
        cands = [SB(es, [128, 8, 256]) for _ in range(NTG)]
        ews = [SB(es, [128, 8, 16]) for _ in range(NTG)]
        zss = [SB(es, [128, 8]) for _ in range(NTG)]
        ptrs = [PS(es, [128, 8, 128], BF16) for _ in range(2)]
        pAB = [PS(es, [128, 512]) for _ in range(2)]
        pMs = [PS(es, [128, D]) for _ in range(2)]
        for g_i in range(NS * L // TSG):
            n = (g_i * TSG) // L
            row0 = g_i * TSG
            if (g_i * TSG) % L == 0:
                P.dma(gx1[:], modx_d[n, :, 2 * D:3 * D])
                P.dma(sh2[:], modx_d[n, :, 3 * D:4 * D])
                P.dma(g2p[:], modx_d[n, :, 4 * D:5 * D])
                V("dve", lambda e: e.scalar_tensor_tensor(out=g2p[:], in0=g2p[:], scalar=1.0, in1=g2[:], op0=ALU.add, op1=ALU.mult), [g2p, g2], [g2p])
            TI = list(range(NTG))

            def ctx(ti):
                return dict(r0=row0 + ti * 128, x1=x1t[ti], yt=yts[ti], zt=zts[ti], xt4=xt4s[ti], gy=gys[ti], gyb=gybs[ti], gyT=gyTs[ti], gpre=gpres[ti],
                            s5o=s5os[ti], catb=catbs[ti], catT=catTs[ti], tm=tms[ti], hb4=hb4s[ti], jk=jks[ti], ss=sss[ti], ptr=ptrs[ti % 2], pG=pAB[ti % 2], pM=pMs[ti % 2])

            for ti in TI:
                c_ = ctx(ti); r0, yt, zt, xt4, gy, gyb = c_["r0"], c_["yt"], c_["zt"], c_["xt4"], c_["gy"], c_["gyb"]
                P.dma(yt[:], y_s[r0:r0 + 128, :], q="sp")
                P.dma(zt[:], z_s[r0:r0 + 128, :], q="act")
                P.dma(xt4[:], x_d[r0:r0 + 128, :], q="sp")
                V("act", lambda e: e.activation(out=gy[:], in_=yt[:], func=AF.Gelu_apprx_tanh), [yt], [gy])
                V("pool", lambda e: e.tensor_copy(out=gyb[:], in_=gy[:]), [gy], [gyb])
            for ti in TI:
                c_ = ctx(ti); gyb, ptr, gyT, pG = c_["gyb"], c_["ptr"], c_["gyT"], c_["pG"]
                for k in range(4):
                    P.op("pe", lambda e, k=k: e.transpose(out=ptr[:, k, :], in_=gyb[:, k * 128:(k + 1) * 128], identity=ident[:]), reads=[gyb, ident], writes=[ptr])
                V("act", lambda e: e.copy(out=gyT[:], in_=ptr[:, 0:4, :]), [ptr], [gyT])
            for ti in TI:
                c_ = ctx(ti); gyT, pG = c_["gyT"], c_["pG"]
                mm(pG[:], [(gyT[:, k, :], wglub[:, k, :]) for k in range(4)], [gyT, wglub], [pG])
            for ti in TI:
                c_ = ctx(ti); pG, gpre = c_["pG"], c_["gpre"]
                V("dve", lambda e: e.tensor_tensor(out=gpre[:], in0=pG[:], in1=bglu[:], op=ALU.add), [pG, bglu], [gpre])
                V("act", lambda e: e.activation(out=gpre[:], in_=gpre[:], func=AF.Sigmoid), [gpre], [gpre])
            for ti in TI:
                c_ = ctx(ti); gy, gpre, s5o, jk, zt = c_["gy"], c_["gpre"], c_["s5o"], c_["jk"], c_["zt"]
                ss1, ss2, ss3, rs1, rs2, rs3 = c_["ss"]
                V("dve", lambda e: e.tensor_tensor(out=s5o[:], in0=gy[:], in1=gpre[:], op=ALU.mult), [gy, gpre], [s5o])
                V("act", lambda e: e.activation(out=jk[:, 0:512], in_=s5o[:], func=AF.Square, accum_out=ss1[:]), [s5o], [jk, ss1])
                V("act", lambda e: e.activation(out=jk[:, 512:1024], in_=zt[:], func=AF.Square, accum_out=ss2[:]), [zt], [jk, ss2])
            for ti in TI:
                c_ = ctx(ti)
                ss1, ss2, ss3, rs1, rs2, rs3 = c_["ss"]
                rsqrt_mean(es, ss1, 512, rs1)
                rsqrt_mean(es, ss2, 512, rs2)
            for ti in TI:
                c_ = ctx(ti); s5o, zt, catb = c_["s5o"], c_["zt"], c_["catb"]
                ss1, ss2, ss3, rs1, rs2, rs3 = c_["ss"]
                V("dve", lambda e: e.scalar_tensor_tensor(out=catb[:, 0:512], in0=s5o[:], scalar=rs1[:, 0:1], in1=gs5[:], op0=ALU.mult, op1=ALU.mult),
                  [s5o, rs1, gs5], [catb])
                V("dve", lambda e: e.scalar_tensor_tensor(out=catb[:, 512:1024], in0=zt[:], scalar=rs2[:, 0:1], in1=ghy[:], op0=ALU.mult, op1=ALU.mult),
                  [zt, rs2, ghy], [catb])
            for ti in TI:
                c_ = ctx(ti); catb, ptr, catT = c_["catb"], c_["ptr"], c_["catT"]
                for k in range(8):
                    P.op("pe", lambda e, k=k: e.transpose(out=ptr[:, k, :], in_=catb[:, k * 128:(k + 1) * 128], identity=ident[:]), reads=[catb, ident], writes=[ptr])
                V("act", lambda e: e.copy(out=catT[:], in_=ptr[:]), [ptr], [catT])
            for ti in TI:
                c_ = ctx(ti); catT, pM = c_["catT"], c_["pM"]
                for hf in range(2):
                    mm(pM[:, hf * 512:(hf + 1) * 512], [(catT[:, k, :], woutb[:, k, hf * 512:(hf + 1) * 512]) for k in range(8)], [catT, woutb], [pM])
            for ti in TI:
                c_ = ctx(ti); pM, tm, x1, xt4, jk, r0 = c_["pM"], c_["tm"], c_["x1"], c_["xt4"], c_["jk"], c_["r0"]
                ss1, ss2, ss3, rs1, rs2, rs3 = c_["ss"]
                V("dve", lambda e: e.tensor_tensor(out=tm[:], in0=pM[:], in1=gx1[:], op=ALU.mult), [pM, gx1], [tm])
                V("pool", lambda e: e.tensor_tensor(out=x1[:], in0=tm[:], in1=xt4[:], op=ALU.add), [tm, xt4], [x1])
                if debug:
                    P.dma(dbg["x1"][r0:r0 + 128, :], x1[:], is_output=True)
                V("act", lambda e: e.activation(out=jk[:], in_=x1[:], func=AF.Square, accum_out=ss3[:]), [x1], [jk, ss3])
            for ti in TI:
                c_ = ctx(ti)
                ss1, ss2, ss3, rs1, rs2, rs3 = c_["ss"]
                rsqrt_mean(es, ss3, D, rs3)
            for ti in TI:
                c_ = ctx(ti); tm, x1, hb4, r0 = c_["tm"], c_["x1"], c_["hb4"], c_["r0"]
                ss1, ss2, ss3, rs1, rs2, rs3 = c_["ss"]
                V("dve", lambda e: e.scalar_tensor_tensor(out=tm[:], in0=x1[:], scalar=rs3[:, 0:1], in1=g2p[:], op0=ALU.mult, op1=ALU.mult),
                  [x1, rs3, g2p], [tm])
                V("pool", lambda e: e.tensor_tensor(out=hb4[:], in0=tm[:], in1=sh2[:], op=ALU.add), [tm, sh2], [hb4])
                if debug:
                    P.dma(dbg["hx2"][r0:r0 + 128, :], hb4[:], is_output=True)
            for ti in TI:
                c_ = ctx(ti); hb4, ptr = c_["hb4"], c_["ptr"]
                for k in range(8):
                    P.op("pe", lambda e, k=k: e.transpose(out=ptr[:, k, :], in_=hb4[:, k * 128:(k + 1) * 128], identity=ident[:]), reads=[hb4, ident], writes=[ptr])
                V("act", lambda e: e.copy(out=hx2T[:, :, ti * 128:(ti + 1) * 128], in_=ptr[:]), [ptr], [hx2T])
            for c in range(16):
                pp = pAB[c % 2]
                mm(pp[:, 0:TSG], [(wqb[:, k, c * 128:(c + 1) * 128], hx2T[:, k, :]) for k in range(8)], [wqb, hx2T], [pp])
                V("act", lambda e: e.copy(out=qT[:, c, :], in_=pp[:, 0:TSG]), [pp], [qT])
            if not lite:
                for hh in range(4):
                    for ti in TI:
                        sc = scs[ti]
                        ps_ = pAB[ti % 2]
                        for c4 in range(4):
                            c = hh * 4 + c4
                            mm(ps_[:, c4 * 128:(c4 + 1) * 128], [(qT[:, c, ti * 128:(ti + 1) * 128], kTb[:, c, :])], [qT, kTb], [ps_])
                        V("act", lambda e: e.copy(out=sc[:, hh * 4:(hh + 1) * 4, :], in_=ps_[:].rearrange("p (a b) -> p a b", a=4)), [ps_], [sc])
                for c in range(16):
                    for ti in TI:
                        sc, m16 = scs[ti], m16s[ti]
                        V("dve", lambda e: e.max(out=m16[:, c, 0:8], in_=sc[:, c, :]), [sc], [m16])
                    for ti in TI:
                        sc, m16, wk = scs[ti], m16s[ti], wks[ti]
                        V("dve", lambda e: e.match_replace(out=wk[:, 0:128], in_to_replace=m16[:, c, 0:8], in_values=sc[:, c, :], imm_value=-1e30), [sc, m16], [wk])
                    for ti in TI:
                        m16, wk = m16s[ti], wks[ti]
                        V("dve", lambda e: e.max(out=m16[:, c, 8:16], in_=wk[:, 0:128]), [wk], [m16])
                for ti in TI:
                    m16, cand = m16s[ti], cands[ti]
                    m4 = m16[:].rearrange("p (h two) k -> p h two k", two=2)
                    V("dve", lambda e: e.tensor_tensor(out=cand[:].rearrange("p h (a b) -> p h a b", a=16),
                                                       in0=m4[:, :, 0, :].unsqueeze(3).to_broadcast([128, 8, 16, 16]),
                                                       in1=m4[:, :, 1, :].unsqueeze(2).to_broadcast([128, 8, 16, 16]), op=ALU.add), [m16], [cand])
                for h in range(8):
                    for ti in TI:
                        cand, c16 = cands[ti], c16s[ti]
                        V("dve", lambda e: e.max(out=c16[:, h, 0:8], in_=cand[:, h, :]), [cand], [c16])
                    for ti in TI:
                        cand, c16, wk = cands[ti], c16s[ti], wks[ti]
                        V("dve", lambda e: e.match_replace(out=wk[:], in_to_replace=c16[:, h, 0:8], in_values=cand[:, h, :], imm_value=-1e30), [cand, c16], [wk])
                    for ti in TI:
                        c16, wk = c16s[ti], wks[ti]
                        V("dve", lambda e: e.max(out=c16[:, h, 8:16], in_=wk[:]), [wk], [c16])
                for ti in TI:
                    c16, ew, zs, nb = c16s[ti], ews[ti], zss[ti], nbs[ti]
                    V("dve", lambda e: e.tensor_tensor(out=ew[:], in0=c16[:], in1=c16[:, :, 0:1].to_broadcast([128, 8, 16]), op=ALU.subtract), [c16], [ew])
                    V("act", lambda e: e.activation(out=ew[:], in_=ew[:], func=AF.Exp), [ew], [ew])
                for ti in TI:
                    c16, ew, zs, nb = c16s[ti], ews[ti], zss[ti], nbs[ti]
                    V("dve", lambda e: e.tensor_reduce(out=zs[:], in_=ew[:], axis=AX.X, op=ALU.add), [ew], [zs])
                    V("act", lambda e: e.activation(out=zs[:], in_=zs[:], func=AF.Ln), [zs], [zs])
                for ti in TI:
                    c16, ew, zs, nb = c16s[ti], ews[ti], zss[ti], nbs[ti]
                    V("dve", lambda e: e.tensor_tensor(out=nb[:], in0=zs[:], in1=c16[:, :, 0], op=ALU.add), [zs, c16], [nb])
                    ntau_ = ntaus[ti]
                    V("dve", lambda e: e.tensor_scalar(out=ntau_[:], in0=c16[:, :, 15], scalar1=-1e-5, scalar2=None, op0=ALU.add), [c16], [ntau_])
                    V("dve", lambda e: e.tensor_tensor(out=nb[:], in0=ntau_[:], in1=nb[:], op=ALU.subtract), [nb, ntau_], [nb])
                    sc = scs[ti]
                    for h in range(8):
                        V("dve", lambda e: e.tensor_scalar(out=sc[:, 2 * h, :], in0=sc[:, 2 * h, :], scalar1=ntau_[:, h:h + 1], scalar2=None, op0=ALU.subtract),
                          [sc, ntau_], [sc])
            for ti in TI:
                r0 = row0 + ti * 128
                P.dma(x1_s[r0:r0 + 128, :], x1t[ti][:], q="sp")
                if not lite:
                    P.dma(sc_s[r0 // 128], scs[ti][:].rearrange("p a b -> p (a b)"), q="act")
                    P.dma(thr_s[r0 // 128], thrs[ti][:], q="sp")
            for hf2 in range(TSG // TS):
                P.dma(hx2T_s[row0 // TS + hf2], hx2T[:, :, hf2 * TS:(hf2 + 1) * TS], q="act")
    P.barrier()
    if not lite:
      with contextlib.ExitStack() as es:
        gx2 = SB(es, [128, D])
        gfin = SB(es, [128, D])
        P.dma(gfin[:], gf_d.partition_broadcast(128))
        hx2Tb = [SB(es, [128, 8, TS], BF16) for _ in range(2)]
        scsb = [[SB(es, [128, 16, 128]) for _ in range(NT)] for _ in range(2)]
        thrb = [[SB(es, [128, 16]) for _ in range(NT)] for _ in range(2)]
        x1t = [SB(es, [128, D]) for _ in range(NT)]
        NB_G = 4
        Sb = [SB(es, [128, 16, 128]) for _ in range(NB_G)]
        Eb = [SB(es, [128, 16, 128], BF16) for _ in range(NB_G)]
        Mb = [SB(es, [128, 16, 128], BF16) for _ in range(NB_G)]
        Gh = Eb
        Gc = [[SB(es, [128, 2048], BF16) for _ in range(2)] for _ in range(NT)]
        uTc = [SB(es, [128, 8, 512], BF16) for _ in range(2)]
        vc = [SB(es, [128, 4, 1024], BF16) for _ in range(3)]
        actb = [SB(es, [128, 512], BF16) for _ in range(2)]
        wab = [SB(es, [128, 512], BF16) for _ in range(2)]
        waT = [SB(es, [128, 4, 128], BF16) for _ in range(2)]
        tm2 = SB(es, [128, D])
        x2 = SB(es, [128, D])
        jk2 = SB(es, [128, D], BF16)
        ss4 = SB(es, [128, 1])
        rs4 = SB(es, [128, 1])
        ob = [SB(es, [128, D]) for _ in range(1)]
        pO = [PS(es, [128, D]) for _ in range(NT)]
        pA = [PS(es, [128, 512]) for _ in range(2)]
        pW = [PS(es, [128, 4, 128], BF16) for _ in range(2)]
        cnt = {"g": 0, "d": 0}
        for st_i in range(NS * L // TS):
            n = (st_i * TS) // L
            row0 = st_i * TS
            par = st_i % 2
            if (st_i * TS) % L == 0:
                P.dma(gx2[:], modx_d[n, :, 5 * D:6 * D])
            def srcs_of(p_):
                return (scsb[p_], [t_[:, 0:8] for t_ in thrb[p_]], [t_[:, 8:16] for t_ in thrb[p_]])

            def preload(si):
                p_ = si % 2
                P.dma(hx2Tb[p_][:], hx2T_s[si], q="sp")
                for ti in range(NT):
                    r0_ = si * TS + ti * 128
                    P.dma(scsb[p_][ti][:].rearrange("p a b -> p (a b)"), sc_s[r0_ // 128], q="act" if ti else "sp")
                    P.dma(thrb[p_][ti][:], thr_s[r0_ // 128], q="sp")

            if st_i == 0:
                preload(0)
            if st_i + 1 < NS * L // TS:
                preload(st_i + 1)
            hx2T = hx2Tb[par]
            for ti in range(NT):
                r0 = row0 + ti * 128
                P.dma(x1t[ti][:], x1_s[r0:r0 + 128, :], q="act")
            cnt_unused = None

            def g_pair(ic, ulist, srcs):
                scs_, nbs_, ntaus_ = srcs
                st = []
                for (ti, h) in ulist:
                    i3 = cnt["g"] % NB_G
                    cnt["g"] += 1
                    st.append((ti, h, Sb[i3], Eb[i3], Mb[i3], Gh[i3]))
                for (ti, h, S_, E_, M_, G_) in st:
                    sc = scs_[ti]
                    V("pool", lambda e: e.tensor_tensor(out=S_[:], in0=sc[:, 2 * h, ic * 16:(ic + 1) * 16].unsqueeze(2).to_broadcast([128, 16, 128]),
                                                        in1=sc[:, 2 * h + 1, :].unsqueeze(1).to_broadcast([128, 16, 128]), op=ALU.add), [sc], [S_])
                for (ti, h, S_, E_, M_, G_) in st:
                    V("act", lambda e: e.activation(out=S_[:], in_=S_[:], func=AF.Prelu, alpha=KMASK), [S_], [S_])
                for (ti, h, S_, E_, M_, G_) in st:
                    nb = nbs_[ti]
                    gdst = Gc[ti][ic % 2]
                    if h == 0:
                        V("act", lambda e: e.activation(out=gdst[:], in_=S_[:].rearrange("p a b -> p (a b)"), func=AF.Exp, bias=nb[:, h:h + 1]), [S_, nb], [gdst])
                    else:
                        V("act", lambda e: e.activation(out=E_[:], in_=S_[:], func=AF.Exp, bias=nb[:, h:h + 1]), [S_, nb], [E_])
                for (ti, h, S_, E_, M_, G_) in st:
                    gdst = Gc[ti][ic % 2]
                    if h != 0:
                        V("dve", lambda e: e.tensor_tensor(out=gdst[:], in0=gdst[:], in1=E_[:].rearrange("p a b -> p (a b)"), op=ALU.add), [E_, gdst], [gdst])

            def item_ctx(j):
                ic, r = divmod(j, 2 * 4)
                e4, ti = divmod(r, NT)
                ec = ic * 4 + e4
                return dict(ic=ic, e4=e4, ti=ti, ec=ec, u_=uTc[ec % 2], v_=vc[ec % 3], pa=pA[j % 2], a_=actb[j % 2], w_=wab[j % 2], wT=waT[j % 2], pw=pW[j % 2],
                            gsrc=Gc[ti][ic % 2])

            def D1(j):
                c_ = item_ctx(j); ec, ti, u_, v_, pa = c_["ec"], c_["ti"], c_["u_"], c_["v_"], c_["pa"]
                if ti == 0:
                    P.dma(u_[:], uTb_s[ec], q="sp")
                    P.dma(v_[:], vb_s[ec], q="act")
                mm(pa[:], [(hx2T[:, k, ti * 128:(ti + 1) * 128], u_[:, k, :]) for k in range(8)], [hx2T, u_], [pa])

            def D2(j):
                c_ = item_ctx(j); pa, a_ = c_["pa"], c_["a_"]
                V("act", lambda e: e.activation(out=a_[:], in_=pa[:], func=AF.Gelu_apprx_tanh), [pa], [a_])

            def D3(j):
                c_ = item_ctx(j); a_, w_, gsrc, e4 = c_["a_"], c_["w_"], c_["gsrc"], c_["e4"]
                V("dve", lambda e: e.tensor_tensor(out=w_[:], in0=a_[:], in1=gsrc[:, e4 * 512:(e4 + 1) * 512], op=ALU.mult), [a_, gsrc], [w_])

            def D4(j):
                c_ = item_ctx(j); w_, pw = c_["w_"], c_["pw"]
                for eb in range(4):
                    P.op("pe", lambda e, eb=eb: e.transpose(out=pw[:, eb, :], in_=w_[:, eb * 128:(eb + 1) * 128], identity=ident[:]), reads=[w_, ident], writes=[pw])

            def D5(j):
                c_ = item_ctx(j); pw, wT = c_["pw"], c_["wT"]
                if j % 2 == 1:
                    V("dve", lambda e: e.tensor_copy(out=wT[:], in_=pw[:]), [pw], [wT])
                else:
                    V("act", lambda e: e.copy(out=wT[:], in_=pw[:]), [pw], [wT])

            def D6(j):
                c_ = item_ctx(j); ec, ti, wT, v_ = c_["ec"], c_["ti"], c_["wT"], c_["v_"]
                for hf in range(2):
                    for eb in range(4):
                        first = (ec == 0 and eb == 0)
                        last = (ec == NCH - 1 and eb == 3)
                        P.op("pe", lambda e, hf=hf, eb=eb, first=first, last=last:
                             e.matmul(pO[ti][:, hf * 512:(hf + 1) * 512], lhsT=wT[:, eb, :], rhs=v_[:, eb, hf * 512:(hf + 1) * 512], start=first, stop=last),
                             reads=[wT, v_], writes=[pO[ti]])

            units = [(ti, h) for h in range(8) for ti in range(NT)]
            NJ = 64
            upi = 2
            if st_i == 0:
                for j in range(8):
                    g_pair(0, units[j * upi:(j + 1) * upi], srcs_of(0))
            def ok(j):
                return 0 <= j < NJ

            for s in range(NJ + 3):
                if ok(s - 2):
                    D4(s - 2)
                if s % 2 == 0 and ok(s - 1):
                    D2(s - 1)
                    D3(s - 1)
                if s < NJ:
                    ic, r = divmod(s, 8)
                    if ic + 1 < 8:
                        g_pair(ic + 1, units[r * upi:(r + 1) * upi], srcs_of(par))
                    elif st_i + 1 < NS * L // TS:
                        g_pair(0, units[r * upi:(r + 1) * upi], srcs_of(1 - par))
                if s % 2 == 1 and ok(s - 1):
                    D2(s - 1)
                    D3(s - 1)
                if ok(s - 2):
                    D5(s - 2)
                if ok(s - 3):
                    D6(s - 3)
                if s < NJ:
                    D1(s)
            for ti in range(NT):
                r0 = row0 + ti * 128
                x1 = x1t[ti]
                o_ = ob[0]
                if debug:
                    V("act", lambda e: e.copy(out=tm2[:], in_=pO[ti][:]), [pO[ti]], [tm2])
                    P.dma(dbg["pe"][r0:r0 + 128, :], tm2[:], is_output=True)
                V("dve", lambda e: e.tensor_tensor(out=tm2[:], in0=pO[ti][:], in1=gx2[:], op=ALU.mult), [pO[ti], gx2], [tm2])
                V("pool", lambda e: e.tensor_tensor(out=x2[:], in0=tm2[:], in1=x1[:], op=ALU.add), [tm2, x1], [x2])
                V("act", lambda e: e.activation(out=jk2[:], in_=x2[:], func=AF.Square, accum_out=ss4[:]), [x2], [jk2, ss4])
                rsqrt_mean(es, ss4, D, rs4)
                V("dve", lambda e: e.scalar_tensor_tensor(out=o_[:], in0=x2[:], scalar=rs4[:, 0:1], in1=gfin[:], op0=ALU.mult, op1=ALU.mult),
                  [x2, rs4, gfin], [o_])
                P.dma(out_d[r0:r0 + 128, :], o_[:], q="sp", is_output=True)

    P.emit()
    top.close()
    return nc


def _prep_inputs(inp):
    global CONST
    if CONST is None:
        CONST = _constants()
    f = lambda a: np.ascontiguousarray(np.asarray(a, dtype=np.float32))
    shared = {}
    shared["w_ada"] = f(inp["w_ada"][0])
    shared["b_ada"] = f(inp["b_ada"][0])
    shared["g_norm1"] = f(inp["g_norm1"][0])
    shared["g_norm2"] = f(inp["g_norm2"][0])
    shared["w_in"] = f(inp["w_in"][0])
    shared["b_in"] = f(inp["b_in"][0])
    a_re = np.asarray(inp["s5_a_re"][0]).reshape(64, 64).T
    a_im = np.asarray(inp["s5_a_im"][0]).reshape(64, 64).T
    ls = np.broadcast_to(np.asarray(inp["s5_log_step"][0]).reshape(1, 64), (64, 64))
    shared["s5p"] = f(np.stack([a_re, a_im, ls], axis=1))
    bre = np.asarray(inp["s5_b_re"][0]).reshape(64, 64, 16).transpose(1, 0, 2)
    bim = np.asarray(inp["s5_b_im"][0]).reshape(64, 64, 16).transpose(1, 0, 2)
    shared["s5b"] = f(np.stack([bre, bim], axis=1))
    cre = np.asarray(inp["s5_c_re"][0]).reshape(64, 16, 64).transpose(2, 0, 1)
    cim = np.asarray(inp["s5_c_im"][0]).reshape(64, 16, 64).transpose(2, 0, 1)
    shared["s5c"] = f(np.stack([cre, cim], axis=1))
    d5 = np.asarray(inp["s5_d"][0]).reshape(32, 16)
    shared["s5d"] = f(np.broadcast_to(d5.T[None], (8, 16, 32)).reshape(128, 32))
    shared["w_glu"] = f(inp["w_glu"][0])
    shared["b_glu"] = f(inp["b_glu"][0])
    shared["hy_conv_w"] = f(inp["hy_conv_w"][0])
    shared["hy_conv_b"] = f(inp["hy_conv_b"][0])
    shared["hf_w1"] = f(inp["hf_w1"][0])
    shared["hf_b1"] = f(np.asarray(inp["hf_b1"][0]).reshape(64, 1))
    shared["hf_wh"] = f(inp["hf_wh"][0])
    shared["hf_bh"] = f(np.asarray(inp["hf_bh"][0]).T)
    shared["hf_freq"] = f(np.asarray(inp["hf_freq"][0]).reshape(64, 1))
    shared["hf_wout"] = f(inp["hf_wout"][0])
    shared["hf_decay"] = f(inp["hf_decay"][0])
    shared["hy_d"] = f(inp["hy_d"][0])
    shared["g_out_s5"] = f(inp["g_out_s5"][0])
    shared["g_out_hy"] = f(inp["g_out_hy"][0])
    shared["w_out"] = f(inp["w_out"][0])
    shared["peer_wq"] = f(inp["peer_wq"][0])
    k1 = np.asarray(inp["peer_k1"][0])
    k2 = np.asarray(inp["peer_k2"][0])
    kk = np.stack([k1, k2], axis=1).reshape(16, 128, 128)
    shared["peer_kT"] = f(kk.transpose(2, 0, 1))
    shared["peer_uT"] = f(np.asarray(inp["peer_u"][0]).T)
    shared["peer_v"] = f(inp["peer_v"][0])
    shared["g_final"] = f(inp["g_final"])
    for k, v in CONST.items():
        shared["k_" + k] = v
    maps = []
    x = np.asarray(inp["x"])
    ctx = np.asarray(inp["ctx"])
    c = np.asarray(inp["c"])
    cc = np.asarray(inp["c_ctx"])
    for i in range(NCORES):
        m = dict(shared)
        m["x"] = f(x[i * NS:(i + 1) * NS].reshape(NS * L, D))
        m["ctx"] = f(ctx[i * NS:(i + 1) * NS].reshape(NS * LC, D))
        c5 = np.concatenate([c[i * NS:(i + 1) * NS], cc[None]], axis=0)
        m["cT"] = f(c5.reshape(NS + 1, 8, 128).transpose(2, 1, 0))
        maps.append(m)
    return maps


def kernel(**inputs):
    maps = _prep_inputs(inputs)
    nc = build_program()
    res = run_bass_kernel_spmd(nc, maps, core_ids=list(range(NCORES)))
    out = np.concatenate([np.asarray(r["out"]).reshape(NS, L, D) for r in res.results], axis=0)
    return out.astype(np.float32)
```
